# Optimizing a Trainium2 kernel written in Bass

```python
import jax, jax.numpy as jnp
from jax import lax
import numpy as np

D_MODEL = 1024
BATCH = 16
SEQ = 2048
DEPTH = 1

HEAD_DIM = 64
N_ATTN_HEADS = 8
ATTN_WIDTH = N_ATTN_HEADS * HEAD_DIM
N_SGU_GROUPS = 8
SGU_GROUP_DIM = 64
SGU_WIDTH = N_SGU_GROUPS * SGU_GROUP_DIM
MIX_WIDTH = ATTN_WIDTH + SGU_WIDTH
IN_WIDTH = 3 * ATTN_WIDTH + 2 * SGU_WIDTH
DILATION_PATTERNS = ((128, 1), (512, 4), (2048, 16))
SGU_CHUNK = 128
ROPE_THETA = 500000.0
ROT_DIM = HEAD_DIM // 4
N_GROUPS = 4
EXPERTS_PER_GROUP = 8
TOP_K = 2
EXPERT_FF = 256
LN_EPS = 1e-5
NEG_INF = -1e30
DEEPNORM_ALPHA = (2 * DEPTH) ** 0.25
DEEPNORM_BETA = (8 * DEPTH) ** -0.25

kernel_name = "hybrid_dilated_attn_sgu_hmoe_deepnorm"


def layer_norm(x, g, b):
    xf = x.astype(jnp.float32)
    mu = jnp.mean(xf, -1, keepdims=True)
    xc = xf - mu
    var = jnp.mean(xc * xc, -1, keepdims=True)
    return xc * lax.rsqrt(var + LN_EPS) * g.astype(jnp.float32) + b.astype(jnp.float32)


def rope_tables(positions):
    inv = ROPE_THETA ** (-jnp.arange(0, ROT_DIM, 2, dtype=jnp.float32) / ROT_DIM)
    ang = positions.astype(jnp.float32)[..., None] * inv
    return jnp.cos(ang)[:, :, None, :], jnp.sin(ang)[:, :, None, :]


def apply_partial_rope(t, cos, sin):
    half = ROT_DIM // 2
    t1, t2 = t[..., :half], t[..., half:ROT_DIM]
    return jnp.concatenate([t1 * cos - t2 * sin, t2 * cos + t1 * sin, t[..., ROT_DIM:]], -1)


def dilated_window_attention(q, k, v, window, dilation):
    B, S, H, Dh = q.shape
    d = dilation
    L = S // d
    w_sub = window // d
    blk = min(w_sub, L)
    nb = -(-L // blk)
    Lp = nb * blk

    def to_blocks(t):
        t = t.reshape(B, L, d, H, Dh)
        t = jnp.pad(t, ((0, 0), (0, Lp - L), (0, 0), (0, 0), (0, 0)))
        return t.reshape(B, nb, blk, d, H, Dh)

    qb, kb, vb = to_blocks(q), to_blocks(k), to_blocks(v)

    def with_prev(t):
        prev = jnp.pad(t, ((0, 0), (1, 0), (0, 0), (0, 0), (0, 0), (0, 0)))[:, :nb]
        return jnp.concatenate([prev, t], axis=2)

    kk, vv = with_prev(kb), with_prev(vb)
    s = jnp.einsum('bnqrhe,bnkrhe->bnrhqk', qb, kk) * (Dh ** -0.5)
    qi = jnp.arange(blk)[:, None]
    kj = jnp.arange(2 * blk)[None, :]
    dist = qi + blk - kj
    key_pos = jnp.arange(nb)[:, None, None] * blk - blk + kj[None]
    valid = (dist >= 0) & (dist <= w_sub) & (key_pos >= 0)
    valid = valid[None, :, None, None]
    s = jnp.where(valid, s, NEG_INF)
    m = jnp.max(s, -1)
    p = jnp.where(valid, jnp.exp(s - m[..., None]), 0.0)
    l = jnp.sum(p, -1)
    m_t = jnp.moveaxis(m, -1, 2)
    l_t = jnp.moveaxis(l, -1, 2)
    o = jnp.einsum('bnrhqk,bnkrhe->bnqrhe', p, vv) / l_t[..., None]
    o = o.reshape(B, Lp, d, H, Dh)[:, :L].reshape(B, S, H, Dh)
    m_t = m_t.reshape(B, Lp, d, H)[:, :L].reshape(B, S, H)
    l_t = l_t.reshape(B, Lp, d, H)[:, :L].reshape(B, S, H)
    return o, m_t, l_t


def dilated_mixture_attention(q, k, v):
    res = [dilated_window_attention(q, k, v, w, d) for (w, d) in DILATION_PATTERNS]
    m_all = jnp.stack([r[1] for r in res], 0)
    l_all = jnp.stack([r[2] for r in res], 0)
    o_all = jnp.stack([r[0] for r in res], 0)
    wts = l_all * jnp.exp(m_all - jnp.max(m_all, 0, keepdims=True))
    o = jnp.sum(wts[..., None] * o_all, 0) / jnp.sum(wts, 0)[..., None]
    B, S, H, Dh = q.shape
    return o.reshape(B, S, H * Dh)


def spatial_gating(u, vs, ln_g, ln_b, w_spatial, b_spatial):
    B, S, _ = u.shape
    u = jax.nn.gelu(u.astype(jnp.float32), approximate=False)
    vs = layer_norm(jax.nn.gelu(vs.astype(jnp.float32), approximate=False), ln_g, ln_b)
    vc = vs.reshape(B, S // SGU_CHUNK, SGU_CHUNK, N_SGU_GROUPS, SGU_GROUP_DIM)
    causal = jnp.tril(jnp.ones((SGU_CHUNK, SGU_CHUNK), jnp.float32))
    w = w_spatial.astype(jnp.float32) * causal
    z = jnp.einsum('gts,bcsge->bctge', w, vc)
    z = z + b_spatial.astype(jnp.float32).T[None, None, :, :, None]
    return u * z.reshape(B, S, SGU_WIDTH)


def hierarchical_moe(h, w_group, b_group, w_expert, b_expert, w_gate_up, w_down):
    B, S, D = h.shape
    t = h.reshape(B * S, D)
    g_logits = (t @ w_group + b_group).astype(jnp.float32)
    p_group = jax.nn.softmax(g_logits, -1)
    g_onehot = jax.nn.one_hot(jnp.argmax(g_logits, -1), N_GROUPS, dtype=jnp.float32)
    p_sel = jnp.sum(p_group * g_onehot, -1)
    e_logits = (jnp.einsum('td,gde->tge', t, w_expert) + b_expert).astype(jnp.float32)
    e_sel = jnp.einsum('tge,tg->te', e_logits, g_onehot)
    top_vals, top_idx = lax.top_k(e_sel, TOP_K)
    top_w = jax.nn.softmax(top_vals, -1)
    e_gate = jnp.einsum('tk,tke->te', top_w,
                        jax.nn.one_hot(top_idx, EXPERTS_PER_GROUP, dtype=jnp.float32))
    gate = g_onehot[:, :, None] * e_gate[:, None, :] * p_sel[:, None, None]
    y = jnp.zeros((B * S, D), jnp.float32)
    for g in range(N_GROUPS):
        gu = jnp.einsum('td,edf->tef', t, w_gate_up[g]).astype(jnp.float32)
        a, b = gu[..., :EXPERT_FF], gu[..., EXPERT_FF:]
        act = jax.nn.silu(a) * b * gate[:, g, :, None]
        y = y + jnp.einsum('tef,efd->td', act, w_down[g].astype(jnp.float32))
    return y.reshape(B, S, D)


def setup_inputs(seed: int = 0) -> dict:
    key = jax.random.key(seed)
    ks = jax.random.split(key, 18)
    f32 = jnp.float32
    x = jax.random.normal(ks[0], (BATCH, SEQ, D_MODEL), f32)
    positions = (jax.random.randint(ks[1], (BATCH, 1), 0, 4096, dtype=jnp.int32)
                 + jnp.arange(SEQ, dtype=jnp.int32)[None, :])
    col_scale = jnp.concatenate([jnp.ones((2 * ATTN_WIDTH,), f32),
                                 jnp.full((ATTN_WIDTH,), DEEPNORM_BETA, f32),
                                 jnp.ones((2 * SGU_WIDTH,), f32)])
    w_in = jax.random.normal(ks[2], (DEPTH, D_MODEL, IN_WIDTH), f32) * (D_MODEL ** -0.5) * col_scale
    sgu_ln_g = 1.0 + 0.1 * jax.random.normal(ks[3], (DEPTH, SGU_WIDTH), f32)
    sgu_ln_b = 0.02 * jax.random.normal(ks[4], (DEPTH, SGU_WIDTH), f32)
    w_spatial = jax.random.normal(ks[5], (DEPTH, N_SGU_GROUPS, SGU_CHUNK, SGU_CHUNK), f32) * (SGU_CHUNK ** -0.5)
    b_spatial = 1.0 + 0.1 * jax.random.normal(ks[6], (DEPTH, N_SGU_GROUPS, SGU_CHUNK), f32)
    w_out = jax.random.normal(ks[7], (DEPTH, MIX_WIDTH, D_MODEL), f32) * (MIX_WIDTH ** -0.5) * DEEPNORM_BETA
    ln1_g = 1.0 + 0.1 * jax.random.normal(ks[8], (DEPTH, D_MODEL), f32)
    ln1_b = 0.02 * jax.random.normal(ks[9], (DEPTH, D_MODEL), f32)
    w_group = jax.random.normal(ks[10], (DEPTH, D_MODEL, N_GROUPS), f32) * (D_MODEL ** -0.5)
    b_group = 0.01 * jax.random.normal(ks[11], (DEPTH, N_GROUPS), f32)
    w_expert = jax.random.normal(ks[12], (DEPTH, N_GROUPS, D_MODEL, EXPERTS_PER_GROUP), f32) * (D_MODEL ** -0.5)
    b_expert = 0.01 * jax.random.normal(ks[13], (DEPTH, N_GROUPS, EXPERTS_PER_GROUP), f32)
    w_gate_up = jax.random.normal(ks[14], (DEPTH, N_GROUPS, EXPERTS_PER_GROUP, D_MODEL, 2 * EXPERT_FF), f32) * (D_MODEL ** -0.5)
    w_down = jax.random.normal(ks[15], (DEPTH, N_GROUPS, EXPERTS_PER_GROUP, EXPERT_FF, D_MODEL), f32) * (EXPERT_FF ** -0.5) * DEEPNORM_BETA
    ln2_g = 1.0 + 0.1 * jax.random.normal(ks[16], (DEPTH, D_MODEL), f32)
    ln2_b = 0.02 * jax.random.normal(ks[17], (DEPTH, D_MODEL), f32)
    return {"x": x, "positions": positions, "w_in": w_in, "sgu_ln_g": sgu_ln_g,
            "sgu_ln_b": sgu_ln_b, "w_spatial": w_spatial, "b_spatial": b_spatial,
            "w_out": w_out, "ln1_g": ln1_g, "ln1_b": ln1_b, "w_group": w_group,
            "b_group": b_group, "w_expert": w_expert, "b_expert": b_expert,
            "w_gate_up": w_gate_up, "w_down": w_down, "ln2_g": ln2_g, "ln2_b": ln2_b}


def reference(x, positions, w_in, sgu_ln_g, sgu_ln_b, w_spatial, b_spatial, w_out,
              ln1_g, ln1_b, w_group, b_group, w_expert, b_expert, w_gate_up, w_down,
              ln2_g, ln2_b):
    B, S, _ = x.shape
    cos, sin = rope_tables(positions)
    h = x.astype(jnp.float32)
    for layer in range(DEPTH):
        proj = h @ w_in[layer]
        a0 = ATTN_WIDTH
        q = proj[..., 0:a0].reshape(B, S, N_ATTN_HEADS, HEAD_DIM).astype(jnp.float32)
        k = proj[..., a0:2 * a0].reshape(B, S, N_ATTN_HEADS, HEAD_DIM).astype(jnp.float32)
        v = proj[..., 2 * a0:3 * a0].reshape(B, S, N_ATTN_HEADS, HEAD_DIM).astype(jnp.float32)
        u = proj[..., 3 * a0:3 * a0 + SGU_WIDTH]
        vs = proj[..., 3 * a0 + SGU_WIDTH:]
        q = apply_partial_rope(q, cos, sin)
        k = apply_partial_rope(k, cos, sin)
        attn_out = dilated_mixture_attention(q, k, v)
        sgu_out = spatial_gating(u, vs, sgu_ln_g[layer], sgu_ln_b[layer],
                                 w_spatial[layer], b_spatial[layer])
        mix = jnp.concatenate([attn_out, sgu_out], -1) @ w_out[layer].astype(jnp.float32)
        h = layer_norm(DEEPNORM_ALPHA * h + mix, ln1_g[layer], ln1_b[layer])
        moe = hierarchical_moe(h, w_group[layer], b_group[layer], w_expert[layer],
                               b_expert[layer], w_gate_up[layer], w_down[layer])
        h = layer_norm(DEEPNORM_ALPHA * h + moe, ln2_g[layer], ln2_b[layer])
    return h.astype(x.dtype)
```

```python
import contextlib
import numpy as np
import concourse.bass as bass
import concourse.mybir as mybir
from concourse.bass_utils import run_bass_kernel_spmd

F32, BF16, I32 = mybir.dt.float32, mybir.dt.bfloat16, mybir.dt.int32
AF = mybir.ActivationFunctionType
ALU = mybir.AluOpType
AX = mybir.AxisListType

NCORES = 8
S = 2048
D = 1024
NB = 2
NT = S // 128
ALPHA = float(2.0 ** 0.25)
EPS = 1e-5
NEG = -30000.0
NE = 32
TWO_PI = float(2 * np.pi)
DEBUG = False
DEBUG_B = 0
STOP_AFTER = None


class _Stop(Exception):
    pass


class Sem:
    _serial = 0

    def __init__(self, h):
        self.h = h
        self.n = 0
        Sem._serial += 1
        self.uid = Sem._serial


class KB:
    def __init__(self, nc, es, waited=None, prog=None):
        self.nc = nc
        self.es = es
        self.waited = {} if waited is None else waited
        self.prog = {} if prog is None else prog

    sem_es = None

    def sem(self, name):
        return Sem(KB.sem_es.enter_context(self.nc.semaphore(name)))

    def sb(self, name, shape, dt):
        return self.es.enter_context(self.nc.sbuf_tensor(name, shape, dt))

    def ps(self, name, shape, dt):
        return self.es.enter_context(self.nc.psum_tensor(name, shape, dt))

    def inc(self, instr, sem, k=1):
        instr.then_inc(sem.h, k)
        sem.n += k
        return (sem, sem.n)

    def mark(self, eng, instr):
        if isinstance(instr, Tok):
            return instr.tok
        return self.inc(instr, self.prog[id(getattr(eng, "raw", eng))])

    def wait(self, eng, tok):
        if tok is None:
            return
        sem, val = tok
        if val <= 0:
            return
        raw = getattr(eng, "raw", eng)
        key = (id(raw), sem.uid)
        if self.waited.get(key, 0) >= val:
            return
        self.waited[key] = val
        raw.wait_ge(sem.h, val)


class Tok:
    def __init__(self, instr, tok):
        self.instr = instr
        self.tok = tok


_COMPUTE = {"activation", "tensor_tensor", "tensor_scalar", "scalar_tensor_tensor", "tensor_copy", "tensor_reduce",
            "reciprocal", "bn_stats", "bn_aggr", "memset", "affine_select", "iota"}


class EngProxy:
    def __init__(self, raw, kb):
        self.raw = raw
        self.kb = kb
        self.last = None

    def __getattr__(self, name):
        attr = getattr(self.raw, name)
        if name not in _COMPUTE:
            return attr

        def wrapper(*a, **k):
            if self.last is not None:
                self.kb.wait(self, self.last)
            instr = attr(*a, **k)
            tok = self.kb.inc(instr, self.kb.prog[id(self.raw)])
            self.last = tok
            return Tok(instr, tok)
        return wrapper


class Chan:
    def __init__(self, kb, name, depth, ncons=1):
        self.kb = kb
        self.depth = depth
        self.ready = []
        self.free = []
        self.ci = [0] * ncons

    def pslot(self, *engs):
        i = len(self.ready)
        if i >= self.depth:
            for tok in self.free[i - self.depth]:
                for e in engs:
                    self.kb.wait(e, tok)
        return i % self.depth

    def produced(self, *toks):
        self.ready.append(list(toks))

    def cslot(self, eng, c=0):
        i = self.ci[c]
        for tok in self.ready[i]:
            self.kb.wait(eng, tok)
        return i % self.depth

    def consumed(self, tok, c=0):
        i = self.ci[c]
        while len(self.free) <= i:
            self.free.append([])
        self.free[i].append(tok)
        self.ci[c] += 1


class DmaRing:
    def __init__(self, kb, name, depth):
        self.kb = kb
        self.sems = [kb.sem(f"{name}{i}") for i in range(depth)]

    def start(self, instr, slot):
        return self.kb.inc(instr, self.sems[slot], 16)


def build_nc():
    nc = bass.Bass("TRN2", target_bir_lowering=False)
    dr = lambda name, shape, dt=F32: nc.dram_tensor(name, shape, dt, kind="ExternalInput").ap()
    xT = dr("xT", [NB, D, S])
    xtm = dr("xtm", [NB, S, D])
    posT = dr("posT", [NB, 128, NT], I32)
    w_in = dr("w_in", [D, 2560])
    w_out = dr("w_out", [D, D])
    ws_tgs = dr("ws_tgs", [128, 8, 128])
    ws_sgt = dr("ws_sgt", [128, 8, 128])
    bspT = dr("bspT", [128, 8])
    sgu_g = dr("sgu_g", [1, 512])
    sgu_b = dr("sgu_b", [1, 512])
    ln1_g = dr("ln1_g", [1, D])
    ln1_b = dr("ln1_b", [1, D])
    ln2_g = dr("ln2_g", [1, D])
    ln2_b = dr("ln2_b", [1, D])
    w_r = dr("w_r", [D, 36])
    b_r = dr("b_r", [1, 36])
    w_gu = dr("w_gu", [NE, D, 512])
    w_dn = dr("w_dn", [NE, 256, D])
    out = nc.dram_tensor("out", [NB, S, D], F32, kind="ExternalOutput").ap()
    if DEBUG:
        dbg_cat = nc.dram_tensor("dbg_cat", [128, 8, S], F32, kind="ExternalOutput").ap()
        dbg_y = nc.dram_tensor("dbg_y", [128, NT, D], F32, kind="ExternalOutput").ap()
        dbg_gate = nc.dram_tensor("dbg_gate", [128, NT, 32], F32, kind="ExternalOutput").ap()

    PE, ACT, DVE, POOL, SP = nc.tensor, nc.scalar, nc.vector, nc.gpsimd, nc.sync
    engines = [PE, ACT, DVE, POOL, SP]

    with contextlib.suppress(_Stop), contextlib.ExitStack() as es:
        KB.sem_es = es
        kb = KB(nc, es)
        progs = [{id(getattr(e, "raw", e)): kb.sem(f"prog{bb}_{n}") for e, n in zip(engines, "pe act dve pool sp".split())} for bb in range(NB + 1)]
        kb.prog = progs[NB]
        M = kb.mark
        ACT, DVE, POOL = EngProxy(nc.scalar, kb), EngProxy(nc.vector, kb), EngProxy(nc.gpsimd, kb)
        engines = [PE, ACT, DVE, POOL, SP]
        banks = [kb.ps(f"bank{i}", [128, 512], F32) for i in range(8)]

        ident = kb.sb("ident", [128, 128], BF16)
        ones_bf = kb.sb("ones_bf", [128, 128], BF16)
        ones_z = kb.sb("ones_z", [128, 2, 128], BF16)
        zer = kb.sb("zer", [128, 512], F32)
        mhalf = kb.sb("mhalf", [128, 1], F32)
        m_cur = kb.sb("m_cur", [128, 512], BF16)
        m_prev = kb.sb("m_prev", [128, 512], BF16)
        m3 = kb.sb("m3", [128, 4, 512], BF16)
        wmT = kb.sb("wmT", [128, 8, 128], BF16)
        rs = kb.sb("rs", [128, 8], F32)
        bsp_sb = kb.sb("bsp_sb", [128, 8], F32)
        sg_bc = kb.sb("sg_bc", [128, 512], F32)
        sb_bc = kb.sb("sb_bc", [128, 512], F32)
        Bp = kb.sb("Bp", [128, 512], F32)
        br_bc = kb.sb("br_bc", [128, 36], F32)
        wr_hl = kb.sb("wr_hl", [128, 8, 72], BF16)
        invf = kb.sb("invf", [128, NT, 8], F32)
        catT = kb.sb("catT", [128, 8, S], BF16)

        s_c = kb.sem("const_dma")
        s_bar = kb.sem("barrier")

        def barrier():
            base = s_bar.n
            for e in engines:
                if isinstance(e, EngProxy) and e.last is not None:
                    kb.wait(e, e.last)
                kb.inc(e.nop(), s_bar)
            for e in engines:
                kb.wait(e, (s_bar, base + len(engines)))

        def stop_if(tag):
            if STOP_AFTER == tag:
                barrier()
                raise _Stop()

        def wait_all(tok):
            for e in engines:
                kb.wait(e, tok)

        with contextlib.ExitStack() as es0:
            k0 = KB(nc, es0, kb.waited, kb.prog)
            wtmp = k0.sb("wtmp", [128, 8, 128], F32)
            wtmp2 = k0.sb("wtmp2", [128, 8, 128], F32)
            wr_f = k0.sb("wr_f", [128, 8, 36], F32)
            wr_t = k0.sb("wr_t", [128, 8, 36], F32)

            def dma_c(o, i):
                kb.inc(SP.dma_start(out=o, in_=i), s_c, 16)

            dma_c(wtmp[:], ws_sgt)
            dma_c(wtmp2[:], ws_tgs)
            dma_c(bsp_sb[:], bspT)
            dma_c(sg_bc[:], sgu_g.partition_broadcast(128))
            dma_c(sb_bc[:], sgu_b.partition_broadcast(128))
            dma_c(br_bc[:], b_r.partition_broadcast(128))
            dma_c(wr_f[:], w_r.rearrange("(k p) c -> p k c", p=128))
            tok_cdma = (s_c, s_c.n)

            POOL.memset(zer[:], 0.0)
            POOL.memset(ones_bf[:], 1.0)
            POOL.memset(ones_z[:], 0.0)
            POOL.memset(ones_z[:, 0, 0:64], 1.0)
            POOL.memset(ones_z[:, 1, 64:128], 1.0)
            POOL.memset(mhalf[:], -0.5)
            POOL.affine_select(out=ident[:], in_=ones_bf[:], pattern=[[1, 128]], compare_op=ALU.is_equal,
                               fill=0.0, base=0, channel_multiplier=-1)
            POOL.affine_select(out=m_cur[:], in_=zer[:], pattern=[[0, 4], [1, 128]], compare_op=ALU.is_ge,
                               fill=NEG, base=0, channel_multiplier=-1)
            POOL.affine_select(out=m_prev[:], in_=zer[:], pattern=[[0, 4], [-1, 128]], compare_op=ALU.is_ge,
                               fill=NEG, base=0, channel_multiplier=1)
            for s in range(4):
                POOL.affine_select(out=m3[:, s, :], in_=zer[:], pattern=[[0, 16], [1, 32]], compare_op=ALU.is_ge,
                                   fill=NEG, base=32 * s, channel_multiplier=-1)
            inv = (np.float32(500000.0) ** (-(np.arange(0, 16, 2, dtype=np.float32)) / np.float32(16))).astype(np.float32)
            for i in range(8):
                POOL.memset(invf[:, :, i:i + 1], float(inv[i]))
            kb.wait(POOL, tok_cdma)
            POOL.affine_select(out=wmT[:], in_=wtmp[:], pattern=[[0, 8], [1, 128]], compare_op=ALU.is_ge,
                               fill=0.0, base=0, channel_multiplier=-1)
            i_last = POOL.affine_select(out=wtmp[:], in_=wtmp2[:], pattern=[[0, 8], [-1, 128]], compare_op=ALU.is_ge,
                                        fill=0.0, base=0, channel_multiplier=1)
            tok_cpool = M(POOL, i_last)

            kb.wait(DVE, tok_cdma)
            kb.wait(DVE, tok_cpool)
            DVE.tensor_reduce(out=rs[:], in_=wtmp[:], axis=AX.X, op=ALU.add)
            for g in range(8):
                DVE.tensor_scalar(out=Bp[:, g * 64:(g + 1) * 64], in0=sb_bc[:, g * 64:(g + 1) * 64],
                                  scalar1=rs[:, g:g + 1], scalar2=bsp_sb[:, g:g + 1], op0=ALU.mult, op1=ALU.add)
            DVE.tensor_copy(out=wr_hl[:, :, 0:36], in_=wr_f[:])
            DVE.tensor_copy(out=wr_t[:], in_=wr_hl[:, :, 0:36])
            DVE.tensor_tensor(out=wr_t[:], in0=wr_f[:], in1=wr_t[:], op=ALU.subtract)
            i_last = DVE.tensor_copy(out=wr_hl[:, :, 36:72], in_=wr_t[:])
            tok_cdve = M(DVE, i_last)
            wait_all(tok_cdma)
            wait_all(tok_cpool)
            wait_all(tok_cdve)
            barrier()
        stop_if("const")

        def rstd_via_pool(mv, dst, last_dve_instr):
            t = M(DVE, last_dve_instr)
            kb.wait(POOL, t)
            POOL.tensor_scalar(out=dst, in0=mv[:, 1:2], scalar1=EPS, scalar2=None, op0=ALU.add)
            i = POOL.tensor_tensor(out=dst, in0=dst, in1=mhalf[:], op=ALU.pow)
            kb.wait(DVE, M(POOL, i))

        ring_x = DmaRing(kb, "ring_x", 2)
        ring_w = DmaRing(kb, "ring_w", 2)
        ring_o = DmaRing(kb, "ring_o", 2)
        ring_m = DmaRing(kb, "ring_m", 4)

        for b in range(NB):
            kb.prog = progs[b]
            ch_u = Chan(kb, "ch_u", 2)
            ch_v = Chan(kb, "ch_v", 2)
            ch_z = Chan(kb, "ch_z", 2)
            ch_tp = Chan(kb, "ch_tp", 2)
            ch_n = Chan(kb, "ch_n", 2)
            ch_gu = Chan(kb, "ch_gu", 2)
            ch_gv = Chan(kb, "ch_gv", 2)
            ch_sg = Chan(kb, "ch_sg", 2)
            ch_qk = Chan(kb, "ch_qk", 2)
            ch_rot = Chan(kb, "ch_rot", 2)
            ch_qs = Chan(kb, "ch_qs", 2)
            ch_vp = Chan(kb, "ch_vp", 2)
            ch_s = Chan(kb, "ch_s", 4)
            ch_p = Chan(kb, "ch_p", 6)
            ch_o = Chan(kb, "ch_o", 2)
            ch_op = Chan(kb, "ch_op", 2)
            ch_x = Chan(kb, "ch_x", 2)
            ch_hb = Chan(kb, "ch_hb", 2)
            ch_lo = Chan(kb, "ch_lo", 2)
            ch_r = Chan(kb, "ch_r", 1)
            ch_g2 = Chan(kb, "ch_g2", 2, ncons=2)
            ch_sl = Chan(kb, "ch_sl", 2)
            ch_at = Chan(kb, "ch_at", 2)
            ch_dn = Chan(kb, "ch_dn", 2)
            ch_wt = Chan(kb, "ch_wt", 2)
            ch_ot = Chan(kb, "ch_ot", 2)

            with contextlib.ExitStack() as es1:
                k1 = KB(nc, es1, kb.waited, kb.prog)
                xT_sb = k1.sb(f"xT_sb{b}", [128, 8, S], BF16)
                wsec = k1.sb(f"wsec{b}", [128, 8, 1024], BF16)
                pos_i = k1.sb(f"pos_i{b}", [128, NT], I32)
                pos_f = k1.sb(f"pos_f{b}", [128, NT], F32)
                ang = k1.sb(f"ang{b}", [128, 2, NT, 8], F32)
                kk_i = k1.sb(f"kk_i{b}", [128, 2, NT, 8], I32)
                kk_f = k1.sb(f"kk_f{b}", [128, 2, NT, 8], F32)
                rr = k1.sb(f"rr{b}", [128, 2, NT, 8], F32)
                mm = k1.sb(f"mm{b}", [128, 2, NT, 8], F32)
                cs_t = k1.sb(f"cs_t{b}", [128, 2, NT, 8], F32)
                s_ld = k1.sem(f"a1_ld{b}")
                s_w = k1.sem(f"a1_w{b}")

                for k in range(8):
                    k1.inc(POOL.dma_start(out=xT_sb[:, k, :], in_=xT[b, k * 128:(k + 1) * 128, :]), s_ld, 16)
                k1.inc(SP.dma_start(out=pos_i[:], in_=posT[b]), s_ld, 16)
                tok_x = (s_ld, s_ld.n)
                k1.inc(POOL.dma_start(out=wsec[:], in_=w_in[:, 1536:2560].rearrange("(k p) c -> p k c", p=128)), s_w, 16)
                tok_w = (s_w, s_w.n)

                k1.wait(DVE, tok_x)
                DVE.tensor_copy(out=pos_f[:], in_=pos_i[:])
                DVE.tensor_tensor(out=ang[:, 1], in0=invf[:], in1=pos_f[:].unsqueeze(2).broadcast_to([128, NT, 8]), op=ALU.mult)
                DVE.tensor_scalar(out=ang[:, 0], in0=ang[:, 1], scalar1=float(np.pi / 2), scalar2=None, op0=ALU.add)
                DVE.tensor_scalar(out=kk_f[:], in0=ang[:], scalar1=float(1.0 / TWO_PI), scalar2=None, op0=ALU.mult)
                DVE.tensor_copy(out=kk_i[:], in_=kk_f[:])
                DVE.tensor_copy(out=kk_f[:], in_=kk_i[:])
                DVE.scalar_tensor_tensor(out=rr[:], in0=kk_f[:], scalar=-TWO_PI, in1=ang[:], op0=ALU.mult, op1=ALU.add)
                DVE.tensor_scalar(out=mm[:], in0=rr[:], scalar1=float(np.pi), scalar2=None, op0=ALU.is_gt)
                DVE.scalar_tensor_tensor(out=rr[:], in0=mm[:], scalar=-TWO_PI, in1=rr[:], op0=ALU.mult, op1=ALU.add)
                DVE.tensor_scalar(out=mm[:], in0=rr[:], scalar1=float(-np.pi), scalar2=None, op0=ALU.is_lt)
                i_l = DVE.scalar_tensor_tensor(out=rr[:], in0=mm[:], scalar=TWO_PI, in1=rr[:], op0=ALU.mult, op1=ALU.add)
                k1.wait(ACT, M(DVE, i_l))
                i_l = ACT.activation(out=cs_t[:], in_=rr[:], func=AF.Sin)
                tok_cs = M(ACT, i_l)
                stop_if("rope")

                with contextlib.ExitStack() as es2:
                    k2 = KB(nc, es2, kb.waited, kb.prog)
                    gu_sb = [k2.sb(f"gu_sb{b}_{i}", [128, 512], F32) for i in range(2)]
                    gv_sb = [k2.sb(f"gv_sb{b}_{i}", [128, 512], F32) for i in range(2)]
                    n_sb = [k2.sb(f"n_sb{b}_{i}", [128, 512], BF16) for i in range(2)]
                    t1_sb = k2.sb(f"t1_sb{b}", [128, 512], F32)
                    sg_sb = [k2.sb(f"sg_sb{b}_{i}", [128, 512], BF16) for i in range(2)]
                    st_sb = k2.sb(f"st_sb{b}", [128, 6], F32)
                    mv_sb = k2.sb(f"mv_sb{b}", [128, 2], F32)
                    rstd_sb = k2.sb(f"rstd_sb{b}", [128, 1], F32)

                    k2.wait(PE, tok_x)
                    k2.wait(PE, tok_w)

                    def sgu_pe_front(n):
                        su = ch_u.pslot(PE)
                        for k in range(8):
                            i = PE.matmul(banks[0 + su][:], xT_sb[:, k, n * 128:(n + 1) * 128], wsec[:, k, 0:512],
                                          start=(k == 0), stop=(k == 7))
                        ch_u.produced(M(PE, i))
                        sv = ch_v.pslot(PE)
                        for k in range(8):
                            i = PE.matmul(banks[2 + sv][:], xT_sb[:, k, n * 128:(n + 1) * 128], wsec[:, k, 512:1024],
                                          start=(k == 0), stop=(k == 7))
                        ch_v.produced(M(PE, i))

                    def sgu_act(n):
                        su = ch_u.cslot(ACT)
                        sg = ch_gu.pslot(ACT)
                        t = M(ACT, ACT.activation(out=gu_sb[sg][:], in_=banks[0 + su][:], func=AF.Gelu))
                        ch_u.consumed(t)
                        ch_gu.produced(t)
                        sv = ch_v.cslot(ACT)
                        sg = ch_gv.pslot(ACT)
                        t = M(ACT, ACT.activation(out=gv_sb[sg][:], in_=banks[2 + sv][:], func=AF.Gelu))
                        ch_v.consumed(t)
                        ch_gv.produced(t)

                    def sgu_dve_norm(n):
                        sg = ch_gv.cslot(DVE)
                        DVE.bn_stats(out=st_sb[:], in_=gv_sb[sg][:])
                        i = DVE.bn_aggr(out=mv_sb[:], in_=st_sb[:])
                        rstd_via_pool(mv_sb, rstd_sb[:], i)
                        sn = ch_n.pslot(DVE)
                        t = M(DVE, DVE.tensor_scalar(out=n_sb[sn][:], in0=gv_sb[sg][:], scalar1=mv_sb[:, 0:1], scalar2=rstd_sb[:, 0:1],
                                                     op0=ALU.subtract, op1=ALU.mult))
                        ch_gv.consumed(t)
                        ch_n.produced(t)

                    def sgu_pe_z(n):
                        sn = ch_n.cslot(PE)
                        sz = ch_z.pslot(PE)
                        for g in range(8):
                            i = PE.matmul(banks[4 + sz][:, g * 64:(g + 1) * 64], wmT[:, g, :], n_sb[sn][:, g * 64:(g + 1) * 64],
                                          start=True, stop=True, skip_group_check=True)
                        t = M(PE, i)
                        ch_n.consumed(t)
                        ch_z.produced(t)

                    def sgu_dve_out(n):
                        sz = ch_z.cslot(DVE)
                        t = M(DVE, DVE.tensor_tensor(out=t1_sb[:], in0=banks[4 + sz][:], in1=sg_bc[:], op=ALU.mult))
                        ch_z.consumed(t)
                        DVE.tensor_tensor(out=t1_sb[:], in0=t1_sb[:], in1=Bp[:], op=ALU.add)
                        sgu_ = ch_gu.cslot(DVE)
                        so = ch_sg.pslot(DVE)
                        t = M(DVE, DVE.tensor_tensor(out=sg_sb[so][:], in0=t1_sb[:], in1=gu_sb[sgu_][:], op=ALU.mult))
                        ch_gu.consumed(t)
                        ch_sg.produced(t)

                    def sgu_pe_tp(n):
                        so = ch_sg.cslot(PE)
                        st = ch_tp.pslot(PE)
                        tpv = banks[6 + st][:].bitcast(BF16)
                        for j in range(4):
                            i = PE.transpose(tpv[:, j * 128:(j + 1) * 128], sg_sb[so][:, j * 128:(j + 1) * 128], ident[:])
                        t = M(PE, i)
                        ch_sg.consumed(t)
                        ch_tp.produced(t)

                    def sgu_act_tp(n):
                        st = ch_tp.cslot(ACT)
                        tpv = banks[6 + st][:].bitcast(BF16)
                        i = ACT.activation(out=catT[:, 4:8, n * 128:(n + 1) * 128],
                                           in_=tpv[:, 0:512].rearrange("p (j t) -> p j t", j=4), func=AF.Copy)
                        ch_tp.consumed(M(ACT, i))

                    for n in range(NT + 2):
                        if n < NT:
                            sgu_pe_front(n)
                            sgu_act(n)
                            sgu_dve_norm(n)
                        if 1 <= n <= NT:
                            sgu_pe_z(n - 1)
                            sgu_dve_out(n - 1)
                        if 2 <= n:
                            sgu_pe_tp(n - 2)
                            sgu_act_tp(n - 2)
                    barrier()
                stop_if("sgu")

                with contextlib.ExitStack() as es3:
                    k3 = KB(nc, es3, kb.waited, kb.prog)
                    QTz = k3.sb(f"QTz{b}", [128, 2, S], BF16)
                    KT = k3.sb(f"KT{b}", [128, S], BF16)
                    Vz = k3.sb(f"Vz{b}", [128, 48, 2, 128], BF16)
                    qs_sb = [k3.sb(f"qs_sb{b}_{i}", [128, 4, 256], BF16) for i in range(2)]
                    ra = k3.sb(f"ra{b}", [128, 4, 4, 8], F32)
                    rb = k3.sb(f"rb{b}", [128, 4, 4, 8], F32)
                    rot_sb = [k3.sb(f"rot_sb{b}_{i}", [128, 4, 2, 2, 16], F32) for i in range(2)]
                    p_sb = [k3.sb(f"p_sb{b}_{i}", [128, 512], BF16) for i in range(6)]
                    rl_sb = k3.sb(f"rl_sb{b}", [128, 512], F32)
                    s_ws = k3.sem(f"a1_ws{b}")

                    POOL.memset(QTz[:], 0.0)
                    tok_zero = M(POOL, POOL.memset(Vz[:], 0.0))
                    for e in (ACT, DVE, PE):
                        k3.wait(e, tok_zero)
                    stop_if("att_ms")

                    def tv_blk(T, j):
                        return T[:, j * 128:(j + 1) * 128]

                    def tv_p2(T, s, r4):
                        return T[:, 512 * s:512 * (s + 1)].rearrange("p (i r) -> p r i", r=4)[:, r4, :]

                    def tv_p3k(T, r):
                        return T.rearrange("p (l r) -> p r l", r=16)[:, r, :]

                    def tv_p3q(T, s, r):
                        return T.rearrange("p (l r) -> p r l", r=16)[:, r, 32 * s:32 * (s + 1)]

                    vdefs = []
                    for j in range(16):
                        vdefs.append(lambda T, j=j: tv_blk(T, j))
                    for s in range(4):
                        for r4 in range(4):
                            vdefs.append(lambda T, s=s, r4=r4: tv_p2(T, s, r4))
                    for r in range(16):
                        vdefs.append(lambda T, r=r: tv_p3k(T, r))

                    for c in range(4):
                        for j, c0 in enumerate((c * 128, 512 + c * 128, 1024 + c * 128)):
                            k3.inc(POOL.dma_start(out=wsec[:, :, j * 128:(j + 1) * 128],
                                                  in_=w_in[:, c0:c0 + 128].rearrange("(k p) c -> p k c", p=128)), s_ws, 16)
                        k3.wait(PE, (s_ws, s_ws.n))
                        k3.wait(DVE, tok_cs)
                        stop_if("att_dma")

                        def qk_pe(gi):
                            sq = ch_qk.pslot(PE)
                            for w in range(2):
                                for tt in range(4):
                                    n = gi * 4 + tt
                                    for k in range(8):
                                        i = PE.matmul(banks[2 * sq + w][:, tt * 128:(tt + 1) * 128],
                                                      xT_sb[:, k, n * 128:(n + 1) * 128], wsec[:, k, w * 128:(w + 1) * 128],
                                                      start=(tt == 0 and k == 0), stop=(k == 7), skip_group_check=True)
                            ch_qk.produced(M(PE, i))

                        def qk_evac(gi):
                            sq = ch_qk.cslot(ACT, 0)
                            so = ch_qs.pslot(ACT, DVE)
                            sr = ch_rot.pslot(ACT)
                            qv = banks[2 * sq + 0][:].rearrange("p (t h e) -> p t h e", t=4, h=2)
                            kv = banks[2 * sq + 1][:].rearrange("p (t h e) -> p t h e", t=4, h=2)
                            ov = qs_sb[so][:].rearrange("p t (w h e) -> p t w h e", w=2, h=2)
                            ACT.activation(out=ov[:, :, 0, :, 16:64], in_=qv[:, :, :, 16:64], func=AF.Copy)
                            ACT.activation(out=ov[:, :, 1, :, 16:64], in_=kv[:, :, :, 16:64], func=AF.Copy)
                            ACT.activation(out=rot_sb[sr][:, :, 0, :, :], in_=qv[:, :, :, 0:16], func=AF.Copy)
                            ta = M(ACT, ACT.activation(out=rot_sb[sr][:, :, 1, :, :], in_=kv[:, :, :, 0:16], func=AF.Copy))
                            ch_qk.consumed(ta, 0)
                            ch_rot.produced(ta)
                            sr = ch_rot.cslot(DVE)
                            rv = rot_sb[sr][:].rearrange("p t w h e -> p t (w h) e")
                            t1 = rv[:, :, :, 0:8]
                            t2 = rv[:, :, :, 8:16]
                            og = qs_sb[so][:].rearrange("p t (g e) -> p t g e", g=4)
                            cosb = cs_t[:, 0, gi * 4:(gi + 1) * 4, :].unsqueeze(2).broadcast_to([128, 4, 4, 8])
                            sinb = cs_t[:, 1, gi * 4:(gi + 1) * 4, :].unsqueeze(2).broadcast_to([128, 4, 4, 8])
                            DVE.tensor_tensor(out=ra[:], in0=t1, in1=cosb, op=ALU.mult)
                            DVE.tensor_tensor(out=rb[:], in0=t2, in1=sinb, op=ALU.mult)
                            DVE.tensor_tensor(out=og[:, :, :, 0:8], in0=ra[:], in1=rb[:], op=ALU.subtract)
                            DVE.tensor_tensor(out=ra[:], in0=t2, in1=cosb, op=ALU.mult)
                            DVE.tensor_tensor(out=rb[:], in0=t1, in1=sinb, op=ALU.mult)
                            td = M(DVE, DVE.tensor_tensor(out=og[:, :, :, 8:16], in0=ra[:], in1=rb[:], op=ALU.add))
                            ch_rot.consumed(td)
                            ch_qs.produced(ta, td)

                        def qk_tp(gi):
                            so = ch_qs.cslot(PE)
                            st = ch_tp.pslot(PE)
                            tpv = banks[6 + st][:].bitcast(BF16).rearrange("p (t w e) -> p t w e", t=4, w=2)
                            for tt in range(4):
                                for w in range(2):
                                    i = PE.transpose(tpv[:, tt, w, :], qs_sb[so][:, tt, w * 128:(w + 1) * 128], ident[:])
                            t = M(PE, i)
                            ch_qs.consumed(t)
                            ch_tp.produced(t)

                        def qk_tp_evac(gi):
                            st = ch_tp.cslot(ACT)
                            tpv = banks[6 + st][:].bitcast(BF16).rearrange("p (t w e) -> p t w e", t=4, w=2)
                            cols = slice(gi * 512, (gi + 1) * 512)
                            ACT.activation(out=QTz[0:64, 0, cols].rearrange("p (t e) -> p t e", t=4), in_=tpv[0:64, :, 0, :], func=AF.Copy)
                            ACT.activation(out=QTz[64:128, 1, cols].rearrange("p (t e) -> p t e", t=4), in_=tpv[64:128, :, 0, :], func=AF.Copy)
                            i = ACT.activation(out=KT[:, cols].rearrange("p (t e) -> p t e", t=4), in_=tpv[:, :, 1, :], func=AF.Copy)
                            ch_tp.consumed(M(ACT, i))

                        for gi in range(4 + 2):
                            if gi < 4:
                                qk_pe(gi)
                                stop_if("qk_pe0")
                                qk_evac(gi)
                                stop_if("qk_ev0")
                            if 1 <= gi <= 4:
                                qk_tp(gi - 1)
                                stop_if("qk_tp0")
                            if 2 <= gi:
                                qk_tp_evac(gi - 2)
                                stop_if("qk_te0")
                        stop_if("qk")

                        for gi in range(12):
                            sv = ch_vp.pslot(PE)
                            for tt in range(4):
                                vd = vdefs[gi * 4 + tt]
                                for k in range(8):
                                    i = PE.matmul(banks[4 + sv][:, tt * 128:(tt + 1) * 128], vd(xT_sb[:, k, :]), wsec[:, k, 256:384],
                                                  start=(tt == 0 and k == 0), stop=(k == 7), skip_group_check=True)
                            ch_vp.produced(M(PE, i))
                            eng = ACT if gi % 2 == 0 else DVE
                            sv = ch_vp.cslot(eng)
                            src = banks[4 + sv][:].rearrange("p (t h e) -> p t h e", t=4, h=2)
                            dst = Vz[:, gi * 4:(gi + 1) * 4, :, :]
                            if eng is ACT:
                                ACT.activation(out=dst[:, :, 0, 0:64], in_=src[:, :, 0, :], func=AF.Copy)
                                i = ACT.activation(out=dst[:, :, 1, 64:128], in_=src[:, :, 1, :], func=AF.Copy)
                            else:
                                DVE.tensor_copy(out=dst[:, :, 0, 0:64], in_=src[:, :, 0, :])
                                i = DVE.tensor_copy(out=dst[:, :, 1, 64:128], in_=src[:, :, 1, :])
                            ch_vp.consumed(M(eng, i))
                        barrier()
                        stop_if("v")

                        V1 = lambda j, hh: Vz[:, j, hh, :]
                        V2 = lambda s, r4, hh: Vz[:, 16 + 4 * s + r4, hh, :]
                        V3 = lambda r, hh: Vz[:, 32 + r, hh, :]

                        def emit_s(maskt, mms, lo=0, hi=512):
                            ss = ch_s.pslot(PE)
                            PE.matmul(banks[ss][:], ident[:], maskt, start=True, stop=False, skip_group_check=True)
                            for (oc, lhsT, rhs) in mms:
                                i = PE.matmul(banks[ss][:, oc[0]:oc[1]], lhsT, rhs, start=False, stop=True, skip_group_check=True)
                            ch_s.produced(M(PE, i))
                            ss2 = ch_s.cslot(ACT)
                            sp = ch_p.pslot(ACT)
                            t = M(ACT, ACT.activation(out=p_sb[sp][:, lo:hi], in_=banks[ss2][:, lo:hi], func=AF.Exp, scale=0.125))
                            ch_s.consumed(t)
                            ch_p.produced(t)
                            return sp

                        def oview(Bk, oc):
                            if oc[0] == "blk":
                                return Bk[:, oc[1] * 128:(oc[1] + 1) * 128]
                            if oc[0] == "p2":
                                return Bk[:].rearrange("p (i r) -> p r i", r=4)[:, oc[1], :]
                            return Bk[:].rearrange("p (i r) -> p r i", r=16)[:, oc[1], :]

                        for s in range(4):
                            so_ = ch_o.pslot(PE)
                            Ob = banks[4 + 2 * so_]
                            Lb = banks[5 + 2 * so_]
                            first_pv = True
                            for hh in range(2):
                                Q = QTz[:, hh, :]
                                pend = []
                                mms = [((j * 128, (j + 1) * 128), tv_blk(KT, 4 * s + j), tv_blk(Q, 4 * s + j)) for j in range(4)]
                                sp = emit_s(m_cur[:], mms)
                                pend.append((sp, [(V1(4 * s + j, hh), (j * 128, (j + 1) * 128), ("blk", j)) for j in range(4)]))
                                js = [j for j in range(4) if 4 * s + j >= 1]
                                mms = [((j * 128, (j + 1) * 128), tv_blk(KT, 4 * s + j - 1), tv_blk(Q, 4 * s + j)) for j in js]
                                sp = emit_s(m_prev[:], mms, lo=js[0] * 128)
                                pend.append((sp, [(V1(4 * s + j - 1, hh), (j * 128, (j + 1) * 128), ("blk", j)) for j in js]))
                                mms = [((r4 * 128, (r4 + 1) * 128), tv_p2(KT, s, r4), tv_p2(Q, s, r4)) for r4 in range(4)]
                                sp = emit_s(m_cur[:], mms)
                                pend.append((sp, [(V2(s, r4, hh), (r4 * 128, (r4 + 1) * 128), ("p2", r4)) for r4 in range(4)]))
                                if s >= 1:
                                    mms = [((r4 * 128, (r4 + 1) * 128), tv_p2(KT, s - 1, r4), tv_p2(Q, s, r4)) for r4 in range(4)]
                                    sp = emit_s(m_prev[:], mms)
                                    pend.append((sp, [(V2(s - 1, r4, hh), (r4 * 128, (r4 + 1) * 128), ("p2", r4)) for r4 in range(4)]))
                                mms = [((r * 32, (r + 1) * 32), tv_p3k(KT, r), tv_p3q(Q, s, r)) for r in range(16)]
                                sp = emit_s(m3[:, s, :], mms)
                                pend.append((sp, [(V3(r, hh), (r * 32, (r + 1) * 32), ("p3", r)) for r in range(16)]))

                                for (sp, pvs) in pend:
                                    sp2 = ch_p.cslot(PE)
                                    assert sp2 == sp
                                    for (vt, pc, oc) in pvs:
                                        PE.matmul(oview(Ob, oc), vt, p_sb[sp][:, pc[0]:pc[1]], start=first_pv, stop=False, skip_group_check=True)
                                        i = PE.matmul(oview(Lb, oc), ones_z[:, hh, :], p_sb[sp][:, pc[0]:pc[1]], start=first_pv, stop=False,
                                                      skip_group_check=True)
                                        first_pv = False
                                    t_last = M(PE, i)
                                    ch_p.consumed(t_last)
                            ch_o.produced(t_last)
                            so2 = ch_o.cslot(DVE)
                            DVE.reciprocal(out=rl_sb[:], in_=banks[5 + 2 * so2][:])
                            i = DVE.tensor_tensor(out=catT[:, c, 512 * s:512 * (s + 1)], in0=banks[4 + 2 * so2][:], in1=rl_sb[:], op=ALU.mult)
                            ch_o.consumed(M(DVE, i))
                            stop_if("attn_s0")
                        barrier()
                        if DEBUG and b == DEBUG_B and STOP_AFTER == "attn":
                            with contextlib.ExitStack() as esd:
                                kd = KB(nc, esd, kb.waited, kb.prog)
                                dtmp = kd.sb("dtmpa", [128, 4, S], F32)
                                sd = kd.sem("dbga")
                                kd.wait(SP, M(DVE, DVE.tensor_copy(out=dtmp[:], in_=catT[:, 0:4, :])))
                                kd.wait(SP, kd.inc(SP.dma_start(out=dbg_cat[:, 0:4, :], in_=dtmp[:]), sd, 16))
                            stop_if("attn")

                if DEBUG and b == DEBUG_B:
                    with contextlib.ExitStack() as esd:
                        kd = KB(nc, esd, kb.waited, kb.prog)
                        dtmp = kd.sb("dtmp", [128, 8, S], F32)
                        sd = kd.sem("dbg1")
                        kd.wait(SP, M(DVE, DVE.tensor_copy(out=dtmp[:], in_=catT[:])))
                        kd.wait(SP, kd.inc(SP.dma_start(out=dbg_cat, in_=dtmp[:]), sd, 16))
                        barrier()

            esp = contextlib.ExitStack()
            kp = KB(nc, esp, kb.waited, kb.prog)
            h1T = kp.sb(f"h1T{b}", [128, 8, S], BF16)
            ybuf = kp.sb(f"ybuf{b}", [128, NT, D], F32)
            gate_all = kp.sb(f"gate_all{b}", [128, NT, 32], F32)
            with contextlib.ExitStack() as es4:
                k4 = KB(nc, es4, kb.waited, kb.prog)
                wo_sb = k4.sb(f"wo_sb{b}", [128, 8, D], BF16)
                x_sb = [k4.sb(f"x_sb{b}_{i}", [128, D], F32) for i in range(2)]
                s_sb = k4.sb(f"s_sb{b}", [128, D], F32)
                hb_sb = [k4.sb(f"hb_sb{b}_{i}", [128, D], BF16) for i in range(2)]
                hf_sb = k4.sb(f"hf_sb{b}", [128, D], F32)
                lo_sb = [k4.sb(f"lo_sb{b}_{i}", [128, D], BF16) for i in range(2)]
                loT_sb = [k4.sb(f"loT_sb{b}_{i}", [128, 8, 128], BF16) for i in range(2)]
                st2 = k4.sb(f"st2_{b}", [128, 2, 6], F32)
                mv2 = k4.sb(f"mv2_{b}", [128, 2], F32)
                rstd2 = k4.sb(f"rstd2_{b}", [128, 1], F32)
                lg72 = k4.sb(f"lg72_{b}", [128, 72], F32)
                lg = k4.sb(f"lg_{b}", [128, 36], F32)
                rt = k4.sb(f"rt_{b}", [128, 64], F32)
                s_wo = k4.sem(f"a2_wo{b}")
                g1_bc = k4.sb(f"g1_bc{b}", [128, D], F32)
                b1_bc = k4.sb(f"b1_bc{b}", [128, D], F32)
                k4.inc(SP.dma_start(out=g1_bc[:], in_=ln1_g.partition_broadcast(128)), s_wo, 16)
                k4.inc(SP.dma_start(out=b1_bc[:], in_=ln1_b.partition_broadcast(128)), s_wo, 16)

                t_wo = k4.inc(POOL.dma_start(out=wo_sb[:], in_=w_out.rearrange("(k p) c -> p k c", p=128)), s_wo, 16)
                k4.wait(PE, t_wo)
                k4.wait(DVE, t_wo)

                def a2_load(n):
                    sx = ch_x.pslot(SP)
                    i = SP.dma_start(out=x_sb[sx][:], in_=xtm[b, n * 128:(n + 1) * 128, :])
                    ch_x.produced(ring_x.start(i, sx))

                def a2_pe_op(n):
                    so = ch_op.pslot(PE)
                    for hf in range(2):
                        for k in range(8):
                            i = PE.matmul(banks[2 * so + hf][:], catT[:, k, n * 128:(n + 1) * 128], wo_sb[:, k, hf * 512:(hf + 1) * 512],
                                          start=(k == 0), stop=(k == 7))
                    ch_op.produced(M(PE, i))

                def a2_dve_ln(n):
                    so = ch_op.cslot(DVE)
                    sx = ch_x.cslot(DVE)
                    for hf in range(2):
                        i = DVE.scalar_tensor_tensor(out=s_sb[:, hf * 512:(hf + 1) * 512], in0=x_sb[sx][:, hf * 512:(hf + 1) * 512],
                                                     scalar=ALPHA, in1=banks[2 * so + hf][:], op0=ALU.mult, op1=ALU.add)
                    t = M(DVE, i)
                    ch_op.consumed(t)
                    ch_x.consumed(t)
                    for hf in range(2):
                        DVE.bn_stats(out=st2[:, hf, :], in_=s_sb[:, hf * 512:(hf + 1) * 512])
                    i = DVE.bn_aggr(out=mv2[:], in_=st2[:].rearrange("p a c -> p (a c)"))
                    rstd_via_pool(mv2, rstd2[:], i)
                    DVE.tensor_scalar(out=hf_sb[:], in0=s_sb[:], scalar1=mv2[:, 0:1], scalar2=rstd2[:, 0:1],
                                      op0=ALU.subtract, op1=ALU.mult)
                    DVE.tensor_tensor(out=hf_sb[:], in0=hf_sb[:], in1=g1_bc[:], op=ALU.mult)
                    DVE.tensor_tensor(out=hf_sb[:], in0=hf_sb[:], in1=b1_bc[:], op=ALU.add)
                    sh = ch_hb.pslot(DVE)
                    DVE.tensor_copy(out=hb_sb[sh][:], in_=hf_sb[:])
                    DVE.tensor_tensor(out=lo_sb[sh][:], in0=hf_sb[:], in1=hb_sb[sh][:], op=ALU.subtract)
                    i = DVE.tensor_scalar(out=ybuf[:, n, :], in0=hf_sb[:], scalar1=ALPHA, scalar2=None, op0=ALU.mult)
                    ch_hb.produced(M(DVE, i))

                def a2_pe_tp(n):
                    sh = ch_hb.cslot(PE)
                    st = ch_tp.pslot(PE)
                    tpv = banks[6 + st][:].bitcast(BF16)
                    for k in range(8):
                        i = PE.transpose(tpv[:, k * 128:(k + 1) * 128], hb_sb[sh][:, k * 128:(k + 1) * 128], ident[:])
                    ch_tp.produced(M(PE, i))
                    st = ch_tp.pslot(PE)
                    tpv = banks[6 + st][:].bitcast(BF16)
                    for k in range(8):
                        i = PE.transpose(tpv[:, k * 128:(k + 1) * 128], lo_sb[sh][:, k * 128:(k + 1) * 128], ident[:])
                    t = M(PE, i)
                    ch_hb.consumed(t)
                    ch_tp.produced(t)

                def a2_act_tp(n):
                    st = ch_tp.cslot(ACT)
                    tpv = banks[6 + st][:].bitcast(BF16)
                    i = ACT.activation(out=h1T[:, :, n * 128:(n + 1) * 128], in_=tpv.rearrange("p (k t) -> p k t", k=8), func=AF.Copy)
                    t_h = M(ACT, i)
                    ch_tp.consumed(t_h)
                    st = ch_tp.cslot(ACT)
                    tpv = banks[6 + st][:].bitcast(BF16)
                    sl = ch_lo.pslot(ACT)
                    i = ACT.activation(out=loT_sb[sl][:], in_=tpv.rearrange("p (k t) -> p k t", k=8), func=AF.Copy)
                    t = M(ACT, i)
                    ch_tp.consumed(t)
                    ch_lo.produced(t)
                    return t_h

                def a2_pe_route(n, t_h):
                    sl = ch_lo.cslot(PE)
                    kb.wait(PE, t_h)
                    ch_r.pslot(PE)
                    rp = banks[4]
                    for k in range(8):
                        PE.matmul(rp[:, 0:72], h1T[:, k, n * 128:(n + 1) * 128], wr_hl[:, k, 0:72], start=(k == 0), stop=False,
                                  skip_group_check=True)
                    for k in range(8):
                        i = PE.matmul(rp[:, 0:36], loT_sb[sl][:, k, :], wr_hl[:, k, 0:36], start=False, stop=(k == 7),
                                      skip_group_check=True)
                    t = M(PE, i)
                    ch_lo.consumed(t)
                    ch_r.produced(t)

                def a2_route_math(n):
                    ch_r.cslot(ACT)
                    t = M(ACT, ACT.activation(out=lg72[:], in_=banks[4][:, 0:72], func=AF.Copy))
                    ch_r.consumed(t)
                    kb.wait(DVE, t)
                    DVE.tensor_tensor(out=lg[:], in0=lg72[:, 0:36], in1=lg72[:, 36:72], op=ALU.add)
                    DVE.tensor_tensor(out=lg[:], in0=lg[:], in1=br_bc[:], op=ALU.add)
                    gmax, oh, ge, sume, psel = rt[:, 0:1], rt[:, 1:5], rt[:, 5:9], rt[:, 9:10], rt[:, 10:11]
                    esel, m1, eq1, e2, m2, eq2 = rt[:, 11:19], rt[:, 19:20], rt[:, 20:28], rt[:, 28:36], rt[:, 36:37], rt[:, 37:45]
                    dd, w1, w2, eg = rt[:, 45:46], rt[:, 46:47], rt[:, 47:48], rt[:, 48:56]
                    DVE.tensor_reduce(out=gmax, in_=lg[:, 0:4], axis=AX.X, op=ALU.max)
                    DVE.tensor_scalar(out=oh, in0=lg[:, 0:4], scalar1=gmax, scalar2=None, op0=ALU.is_equal)
                    DVE.tensor_scalar(out=ge, in0=lg[:, 0:4], scalar1=gmax, scalar2=None, op0=ALU.subtract)
                    DVE.tensor_scalar(out=esel, in0=lg[:, 4:12], scalar1=oh[:, 0:1], scalar2=None, op0=ALU.mult)
                    for g in range(1, 4):
                        DVE.scalar_tensor_tensor(out=esel, in0=lg[:, 4 + 8 * g:12 + 8 * g], scalar=oh[:, g:g + 1], in1=esel,
                                                 op0=ALU.mult, op1=ALU.add)
                    DVE.tensor_reduce(out=m1, in_=esel, axis=AX.X, op=ALU.max)
                    DVE.tensor_scalar(out=eq1, in0=esel, scalar1=m1, scalar2=None, op0=ALU.is_equal)
                    DVE.scalar_tensor_tensor(out=e2, in0=eq1, scalar=-1e30, in1=esel, op0=ALU.mult, op1=ALU.add)
                    DVE.tensor_reduce(out=m2, in_=e2, axis=AX.X, op=ALU.max)
                    DVE.tensor_scalar(out=eq2, in0=e2, scalar1=m2, scalar2=None, op0=ALU.is_equal)
                    i = DVE.tensor_tensor(out=dd, in0=m2, in1=m1, op=ALU.subtract)
                    kb.wait(ACT, M(DVE, i))
                    ACT.activation(out=ge, in_=ge, func=AF.Exp)
                    i = ACT.activation(out=dd, in_=dd, func=AF.Exp)
                    kb.wait(DVE, M(ACT, i))
                    DVE.tensor_reduce(out=sume, in_=ge, axis=AX.X, op=ALU.add)
                    DVE.reciprocal(out=psel, in_=sume)
                    DVE.tensor_scalar(out=w1, in0=dd, scalar1=1.0, scalar2=None, op0=ALU.add)
                    DVE.reciprocal(out=w1, in_=w1)
                    DVE.tensor_tensor(out=w2, in0=dd, in1=w1, op=ALU.mult)
                    DVE.tensor_tensor(out=w1, in0=w1, in1=psel, op=ALU.mult)
                    DVE.tensor_tensor(out=w2, in0=w2, in1=psel, op=ALU.mult)
                    DVE.tensor_scalar(out=eg, in0=eq1, scalar1=w1, scalar2=None, op0=ALU.mult)
                    DVE.scalar_tensor_tensor(out=eg, in0=eq2, scalar=w2, in1=eg, op0=ALU.mult, op1=ALU.add)
                    for g in range(4):
                        DVE.tensor_scalar(out=gate_all[:, n, g * 8:(g + 1) * 8], in0=eg, scalar1=oh[:, g:g + 1], scalar2=None, op0=ALU.mult)

                a2_load(0)
                t_hs = {}
                for n in range(NT + 2):
                    if n + 1 < NT:
                        a2_load(n + 1)
                    if n < NT:
                        a2_pe_op(n)
                        a2_dve_ln(n)
                    if 1 <= n <= NT:
                        a2_pe_tp(n - 1)
                        t_hs[n - 1] = a2_act_tp(n - 1)
                    if 2 <= n:
                        a2_pe_route(n - 2, t_hs[n - 2])
                        a2_route_math(n - 2)
                barrier()

            if DEBUG and b == DEBUG_B:
                with contextlib.ExitStack() as esd:
                    kd = KB(nc, esd, kb.waited, kb.prog)
                    sd = kd.sem("dbg2")
                    kd.inc(SP.dma_start(out=dbg_y, in_=ybuf[:]), sd, 16)
                    kd.wait(SP, kd.inc(SP.dma_start(out=dbg_gate, in_=gate_all[:]), sd, 16))
                    barrier()
            if not DEBUG or b == DEBUG_B:
                stop_if("A2")

            with contextlib.ExitStack() as es5:
                k5 = KB(nc, es5, kb.waited, kb.prog)
                wgu_sb = [k5.sb(f"wgu_sb{b}_{i}", [128, 8, 512], BF16) for i in range(2)]
                wdn_sb = [k5.sb(f"wdn_sb{b}_{i}", [128, 2, D], BF16) for i in range(2)]
                sl_sb = [k5.sb(f"sl_sb{b}_{i}", [128, 512], F32) for i in range(2)]
                at_sb = [k5.sb(f"at_sb{b}_{i}", [128, 2, 512], BF16) for i in range(2)]

                def m_load(e):
                    sw = ch_wt.pslot(POOL)
                    i = POOL.dma_start(out=wgu_sb[sw][:], in_=w_gu[e].rearrange("(k p) f -> p k f", p=128))
                    t1 = ring_m.start(i, 2 * sw)
                    i = POOL.dma_start(out=wdn_sb[sw][:], in_=w_dn[e].rearrange("(k p) f -> p k f", p=128))
                    t2 = ring_m.start(i, 2 * sw + 1)
                    ch_wt.produced(t1, t2)

                units = [(e, sp) for e in range(NE) for sp in range(4)]

                def m_pe_gu(u):
                    e, sp = units[u]
                    sw = e % 2
                    if sp == 0:
                        for tok in ch_wt.ready[e]:
                            kb.wait(PE, tok)
                    for hf in range(2):
                        sg = ch_g2.pslot(PE)
                        for part in range(2):
                            fc = part * 256 + hf * 128
                            for k in range(8):
                                i = PE.matmul(banks[2 * sg + part][:], wgu_sb[sw][:, k, fc:fc + 128], h1T[:, k, sp * 512:(sp + 1) * 512],
                                              start=(k == 0), stop=(k == 7))
                        ch_g2.produced(M(PE, i))

                def m_act(u):
                    for hf in range(2):
                        sg = ch_g2.cslot(ACT, 0)
                        ss = ch_sl.pslot(ACT)
                        t = M(ACT, ACT.activation(out=sl_sb[ss][:], in_=banks[2 * sg][:], func=AF.Silu))
                        ch_g2.consumed(t, 0)
                        ch_sl.produced(t)

                def m_dve_act(u):
                    sa = ch_at.pslot(DVE)
                    for hf in range(2):
                        sg = ch_g2.cslot(DVE, 1)
                        ss = ch_sl.cslot(DVE)
                        t = M(DVE, DVE.tensor_tensor(out=at_sb[sa][:, hf, :], in0=banks[2 * sg + 1][:], in1=sl_sb[ss][:], op=ALU.mult))
                        ch_g2.consumed(t, 1)
                        ch_sl.consumed(t)
                    ch_at.produced(t)

                def m_dn(u):
                    e, sp = units[u]
                    sw = e % 2
                    sa = ch_at.cslot(PE)
                    for tt in range(4):
                        n = sp * 4 + tt
                        sd = ch_dn.pslot(PE)
                        for hf in range(2):
                            for fc in range(2):
                                i = PE.matmul(banks[4 + 2 * sd + hf][:], at_sb[sa][:, fc, tt * 128:(tt + 1) * 128],
                                              wdn_sb[sw][:, fc, hf * 512:(hf + 1) * 512], start=(fc == 0), stop=(fc == 1))
                        t = M(PE, i)
                        ch_dn.produced(t)
                        sd2 = ch_dn.cslot(DVE)
                        for hf in range(2):
                            i = DVE.scalar_tensor_tensor(out=ybuf[:, n, hf * 512:(hf + 1) * 512], in0=banks[4 + 2 * sd2 + hf][:],
                                                         scalar=gate_all[:, n, e:e + 1], in1=ybuf[:, n, hf * 512:(hf + 1) * 512],
                                                         op0=ALU.mult, op1=ALU.add)
                        ch_dn.consumed(M(DVE, i))
                    ch_at.consumed(t)
                    if sp == 3:
                        ch_wt.consumed(t)

                m_load(0)
                NU = len(units)
                for u in range(NU + 1):
                    if u < NU:
                        e, sp = units[u]
                        if sp == 1 and e + 1 < NE:
                            m_load(e + 1)
                        m_pe_gu(u)
                        m_act(u)
                        m_dve_act(u)
                    if u >= 1:
                        m_dn(u - 1)
                barrier()
                stop_if("M")

            with contextlib.ExitStack() as es6:
                k6 = KB(nc, es6, kb.waited, kb.prog)
                o_sb = [k6.sb(f"o_sb{b}_{i}", [128, D], F32) for i in range(2)]
                st3 = k6.sb(f"st3_{b}", [128, 2, 6], F32)
                mv3 = k6.sb(f"mv3_{b}", [128, 2], F32)
                rstd3 = k6.sb(f"rstd3_{b}", [128, 1], F32)
                g2_bc = k6.sb(f"g2_bc{b}", [128, D], F32)
                b2_bc = k6.sb(f"b2_bc{b}", [128, D], F32)
                s_g2 = k6.sem(f"f_g2{b}")
                k6.inc(SP.dma_start(out=g2_bc[:], in_=ln2_g.partition_broadcast(128)), s_g2, 16)
                k6.wait(DVE, k6.inc(SP.dma_start(out=b2_bc[:], in_=ln2_b.partition_broadcast(128)), s_g2, 16))
                last_store = None
                for n in range(NT):
                    for hf in range(2):
                        DVE.bn_stats(out=st3[:, hf, :], in_=ybuf[:, n, hf * 512:(hf + 1) * 512])
                    i = DVE.bn_aggr(out=mv3[:], in_=st3[:].rearrange("p a c -> p (a c)"))
                    rstd_via_pool(mv3, rstd3[:], i)
                    so = ch_ot.pslot(DVE)
                    DVE.tensor_scalar(out=o_sb[so][:], in0=ybuf[:, n, :], scalar1=mv3[:, 0:1], scalar2=rstd3[:, 0:1],
                                      op0=ALU.subtract, op1=ALU.mult)
                    DVE.tensor_tensor(out=o_sb[so][:], in0=o_sb[so][:], in1=g2_bc[:], op=ALU.mult)
                    i = DVE.tensor_tensor(out=o_sb[so][:], in0=o_sb[so][:], in1=b2_bc[:], op=ALU.add)
                    ch_ot.produced(M(DVE, i))
                    so = ch_ot.cslot(SP)
                    i = SP.dma_start(out=out[b, n * 128:(n + 1) * 128, :], in_=o_sb[so][:])
                    t = ring_o.start(i, so)
                    ch_ot.consumed(t)
                    if n >= NT - 2:
                        kb.wait(SP, t) if n == NT - 2 else None
                        last_store = t
                        if n == NT - 2:
                            prev_store = t
                kb.wait(SP, prev_store)
                kb.wait(SP, last_store)
                barrier()
            esp.close()
    return nc


def _prep_inputs(inputs):
    x = np.ascontiguousarray(inputs["x"], dtype=np.float32)
    pos = np.ascontiguousarray(inputs["positions"], dtype=np.int32)
    w_r = np.concatenate([inputs["w_group"][0], np.transpose(inputs["w_expert"][0], (1, 0, 2)).reshape(D, 32)], axis=1)
    b_r = np.concatenate([inputs["b_group"][0], inputs["b_expert"][0].reshape(32)])[None, :]
    ws = inputs["w_spatial"][0]
    shared = {
        "w_in": np.ascontiguousarray(inputs["w_in"][0]),
        "w_out": np.ascontiguousarray(inputs["w_out"][0]),
        "ws_tgs": np.ascontiguousarray(np.transpose(ws, (1, 0, 2))),
        "ws_sgt": np.ascontiguousarray(np.transpose(ws, (2, 0, 1))),
        "bspT": np.ascontiguousarray(inputs["b_spatial"][0].T),
        "sgu_g": np.ascontiguousarray(inputs["sgu_ln_g"]),
        "sgu_b": np.ascontiguousarray(inputs["sgu_ln_b"]),
        "ln1_g": np.ascontiguousarray(inputs["ln1_g"]),
        "ln1_b": np.ascontiguousarray(inputs["ln1_b"]),
        "ln2_g": np.ascontiguousarray(inputs["ln2_g"]),
        "ln2_b": np.ascontiguousarray(inputs["ln2_b"]),
        "w_r": np.ascontiguousarray(w_r, dtype=np.float32),
        "b_r": np.ascontiguousarray(b_r, dtype=np.float32),
        "w_gu": np.ascontiguousarray(inputs["w_gate_up"][0].reshape(NE, D, 512)),
        "w_dn": np.ascontiguousarray(inputs["w_down"][0].reshape(NE, 256, D)),
    }
    in_maps = []
    for c in range(NCORES):
        xs = x[c * NB:(c + 1) * NB]
        m = dict(shared)
        m["xT"] = np.ascontiguousarray(np.transpose(xs, (0, 2, 1)))
        m["xtm"] = xs
        m["posT"] = np.ascontiguousarray(np.transpose(pos[c * NB:(c + 1) * NB].reshape(NB, NT, 128), (0, 2, 1)))
        in_maps.append(m)
    return in_maps


def kernel(**inputs):
    in_maps = _prep_inputs(inputs)
    nc = build_nc()
    res = run_bass_kernel_spmd(nc, in_maps, core_ids=list(range(NCORES)))
    return np.concatenate([r["out"] for r in res.results], axis=0).astype(np.float32)
```

```python
import contextlib
import numpy as np
import concourse.bass as bass
import concourse.mybir as mybir
from concourse.bass_utils import run_bass_kernel_spmd

F32, BF16, I32 = mybir.dt.float32, mybir.dt.bfloat16, mybir.dt.int32
AF = mybir.ActivationFunctionType
ALU = mybir.AluOpType
AX = mybir.AxisListType

NCORES = 8
S = 2048
D = 1024
NB = 2
NT = S // 128
ALPHA = float(2.0 ** 0.25)
EPS = 1e-5
NEG = -30000.0
NE = 32
TWO_PI = float(2 * np.pi)
DEBUG = False
DEBUG_B = 0
STOP_AFTER = None


class _Stop(Exception):
    pass


class Sem:
    _serial = 0

    def __init__(self, h):
        self.h = h
        self.n = 0
        Sem._serial += 1
        self.uid = Sem._serial


class KB:
    def __init__(self, nc, es, waited=None, prog=None):
        self.nc = nc
        self.es = es
        self.waited = {} if waited is None else waited
        self.prog = {} if prog is None else prog

    sem_es = None

    def sem(self, name):
        return Sem(KB.sem_es.enter_context(self.nc.semaphore(name)))

    def sb(self, name, shape, dt):
        return self.es.enter_context(self.nc.sbuf_tensor(name, shape, dt))

    def ps(self, name, shape, dt):
        return self.es.enter_context(self.nc.psum_tensor(name, shape, dt))

    def inc(self, instr, sem, k=1):
        instr.then_inc(sem.h, k)
        sem.n += k
        return (sem, sem.n)

    def mark(self, eng, instr):
        if isinstance(instr, Tok):
            return instr.tok
        return self.inc(instr, self.prog[id(getattr(eng, "raw", eng))])

    def wait(self, eng, tok):
        if tok is None:
            return
        sem, val = tok
        if val <= 0:
            return
        raw = getattr(eng, "raw", eng)
        key = (id(raw), sem.uid)
        if self.waited.get(key, 0) >= val:
            return
        self.waited[key] = val
        raw.wait_ge(sem.h, val)


class Tok:
    def __init__(self, instr, tok):
        self.instr = instr
        self.tok = tok


_COMPUTE = {"activation", "tensor_tensor", "tensor_scalar", "scalar_tensor_tensor", "tensor_copy", "tensor_reduce",
            "reciprocal", "bn_stats", "bn_aggr", "memset", "affine_select", "iota"}


class EngProxy:
    def __init__(self, raw, kb):
        self.raw = raw
        self.kb = kb
        self.last = None

    def __getattr__(self, name):
        attr = getattr(self.raw, name)
        if name not in _COMPUTE:
            return attr

        def wrapper(*a, **k):
            if self.last is not None:
                self.kb.wait(self, self.last)
            instr = attr(*a, **k)
            tok = self.kb.inc(instr, self.kb.prog[id(self.raw)])
            self.last = tok
            return Tok(instr, tok)
        return wrapper


class Chan:
    def __init__(self, kb, name, depth, ncons=1):
        self.kb = kb
        self.depth = depth
        self.ready = []
        self.free = []
        self.ci = [0] * ncons

    def pslot(self, *engs):
        i = len(self.ready)
        if i >= self.depth:
            for tok in self.free[i - self.depth]:
                for e in engs:
                    self.kb.wait(e, tok)
        return i % self.depth

    def produced(self, *toks):
        self.ready.append(list(toks))

    def cslot(self, eng, c=0):
        i = self.ci[c]
        for tok in self.ready[i]:
            self.kb.wait(eng, tok)
        return i % self.depth

    def consumed(self, tok, c=0):
        i = self.ci[c]
        while len(self.free) <= i:
            self.free.append([])
        self.free[i].append(tok)
        self.ci[c] += 1


class DmaRing:
    def __init__(self, kb, name, depth):
        self.kb = kb
        self.sems = [kb.sem(f"{name}{i}") for i in range(depth)]

    def start(self, instr, slot):
        return self.kb.inc(instr, self.sems[slot], 16)


def build_nc():
    nc = bass.Bass("TRN2", target_bir_lowering=False)
    dr = lambda name, shape, dt=F32: nc.dram_tensor(name, shape, dt, kind="ExternalInput").ap()
    xT = dr("xT", [NB, D, S])
    xtm = dr("xtm", [NB, S, D])
    posT = dr("posT", [NB, 128, NT], I32)
    w_in = dr("w_in", [D, 2560])
    w_out = dr("w_out", [D, D])
    ws_tgs = dr("ws_tgs", [128, 8, 128])
    ws_sgt = dr("ws_sgt", [128, 8, 128])
    bspT = dr("bspT", [128, 8])
    sgu_g = dr("sgu_g", [1, 512])
    sgu_b = dr("sgu_b", [1, 512])
    ln1_g = dr("ln1_g", [1, D])
    ln1_b = dr("ln1_b", [1, D])
    ln2_g = dr("ln2_g", [1, D])
    ln2_b = dr("ln2_b", [1, D])
    w_r = dr("w_r", [D, 36])
    b_r = dr("b_r", [1, 36])
    w_gu = dr("w_gu", [NE, D, 512])
    w_dn = dr("w_dn", [NE, 256, D])
    out = nc.dram_tensor("out", [NB, S, D], F32, kind="ExternalOutput").ap()
    if DEBUG:
        dbg_cat = nc.dram_tensor("dbg_cat", [128, 8, S], F32, kind="ExternalOutput").ap()
        dbg_y = nc.dram_tensor("dbg_y", [128, NT, D], F32, kind="ExternalOutput").ap()
        dbg_gate = nc.dram_tensor("dbg_gate", [128, NT, 32], F32, kind="ExternalOutput").ap()

    PE, ACT, DVE, POOL, SP = nc.tensor, nc.scalar, nc.vector, nc.gpsimd, nc.sync
    engines = [PE, ACT, DVE, POOL, SP]

    with contextlib.suppress(_Stop), contextlib.ExitStack() as es:
        KB.sem_es = es
        kb = KB(nc, es)
        progs = [{id(getattr(e, "raw", e)): kb.sem(f"prog{bb}_{n}") for e, n in zip(engines, "pe act dve pool sp".split())} for bb in range(NB + 1)]
        kb.prog = progs[NB]
        M = kb.mark
        ACT, DVE, POOL = EngProxy(nc.scalar, kb), EngProxy(nc.vector, kb), EngProxy(nc.gpsimd, kb)
        engines = [PE, ACT, DVE, POOL, SP]
        banks = [kb.ps(f"bank{i}", [128, 512], F32) for i in range(8)]

        ident = kb.sb("ident", [128, 128], BF16)
        ones_bf = kb.sb("ones_bf", [128, 128], BF16)
        ones_z = kb.sb("ones_z", [128, 2, 128], BF16)
        zer = kb.sb("zer", [128, 512], F32)
        mhalf = kb.sb("mhalf", [128, 1], F32)
        m_cur = kb.sb("m_cur", [128, 512], BF16)
        m_prev = kb.sb("m_prev", [128, 512], BF16)
        m3 = kb.sb("m3", [128, 4, 512], BF16)
        wmT = kb.sb("wmT", [128, 8, 128], BF16)
        rs = kb.sb("rs", [128, 8], F32)
        bsp_sb = kb.sb("bsp_sb", [128, 8], F32)
        sg_bc = kb.sb("sg_bc", [128, 512], F32)
        sb_bc = kb.sb("sb_bc", [128, 512], F32)
        Bp = kb.sb("Bp", [128, 512], F32)
        br_bc = kb.sb("br_bc", [128, 36], F32)
        wr_hl = kb.sb("wr_hl", [128, 8, 72], BF16)
        invf = kb.sb("invf", [128, NT, 8], F32)
        catT = kb.sb("catT", [128, 8, S], BF16)

        s_c = kb.sem("const_dma")
        s_bar = kb.sem("barrier")

        def barrier():
            base = s_bar.n
            for e in engines:
                if isinstance(e, EngProxy) and e.last is not None:
                    kb.wait(e, e.last)
                kb.inc(e.nop(), s_bar)
            for e in engines:
                kb.wait(e, (s_bar, base + len(engines)))

        def stop_if(tag):
            if STOP_AFTER == tag:
                barrier()
                raise _Stop()

        def wait_all(tok):
            for e in engines:
                kb.wait(e, tok)

        with contextlib.ExitStack() as es0:
            k0 = KB(nc, es0, kb.waited, kb.prog)
            wtmp = k0.sb("wtmp", [128, 8, 128], F32)
            wtmp2 = k0.sb("wtmp2", [128, 8, 128], F32)
            wr_f = k0.sb("wr_f", [128, 8, 36], F32)
            wr_t = k0.sb("wr_t", [128, 8, 36], F32)

            def dma_c(o, i):
                kb.inc(SP.dma_start(out=o, in_=i), s_c, 16)

            dma_c(wtmp[:], ws_sgt)
            dma_c(wtmp2[:], ws_tgs)
            dma_c(bsp_sb[:], bspT)
            dma_c(sg_bc[:], sgu_g.partition_broadcast(128))
            dma_c(sb_bc[:], sgu_b.partition_broadcast(128))
            dma_c(br_bc[:], b_r.partition_broadcast(128))
            dma_c(wr_f[:], w_r.rearrange("(k p) c -> p k c", p=128))
            tok_cdma = (s_c, s_c.n)

            POOL.memset(zer[:], 0.0)
            POOL.memset(ones_bf[:], 1.0)
            POOL.memset(ones_z[:], 0.0)
            POOL.memset(ones_z[:, 0, 0:64], 1.0)
            POOL.memset(ones_z[:, 1, 64:128], 1.0)
            POOL.memset(mhalf[:], -0.5)
            POOL.affine_select(out=ident[:], in_=ones_bf[:], pattern=[[1, 128]], compare_op=ALU.is_equal,
                               fill=0.0, base=0, channel_multiplier=-1)
            POOL.affine_select(out=m_cur[:], in_=zer[:], pattern=[[0, 4], [1, 128]], compare_op=ALU.is_ge,
                               fill=NEG, base=0, channel_multiplier=-1)
            POOL.affine_select(out=m_prev[:], in_=zer[:], pattern=[[0, 4], [-1, 128]], compare_op=ALU.is_ge,
                               fill=NEG, base=0, channel_multiplier=1)
            for s in range(4):
                POOL.affine_select(out=m3[:, s, :], in_=zer[:], pattern=[[0, 16], [1, 32]], compare_op=ALU.is_ge,
                                   fill=NEG, base=32 * s, channel_multiplier=-1)
            inv = (np.float32(500000.0) ** (-(np.arange(0, 16, 2, dtype=np.float32)) / np.float32(16))).astype(np.float32)
            for i in range(8):
                POOL.memset(invf[:, :, i:i + 1], float(inv[i]))
            kb.wait(POOL, tok_cdma)
            POOL.affine_select(out=wmT[:], in_=wtmp[:], pattern=[[0, 8], [1, 128]], compare_op=ALU.is_ge,
                               fill=0.0, base=0, channel_multiplier=-1)
            i_last = POOL.affine_select(out=wtmp[:], in_=wtmp2[:], pattern=[[0, 8], [-1, 128]], compare_op=ALU.is_ge,
                                        fill=0.0, base=0, channel_multiplier=1)
            tok_cpool = M(POOL, i_last)

            kb.wait(DVE, tok_cdma)
            kb.wait(DVE, tok_cpool)
            DVE.tensor_reduce(out=rs[:], in_=wtmp[:], axis=AX.X, op=ALU.add)
            for g in range(8):
                DVE.tensor_scalar(out=Bp[:, g * 64:(g + 1) * 64], in0=sb_bc[:, g * 64:(g + 1) * 64],
                                  scalar1=rs[:, g:g + 1], scalar2=bsp_sb[:, g:g + 1], op0=ALU.mult, op1=ALU.add)
            DVE.tensor_copy(out=wr_hl[:, :, 0:36], in_=wr_f[:])
            DVE.tensor_copy(out=wr_t[:], in_=wr_hl[:, :, 0:36])
            DVE.tensor_tensor(out=wr_t[:], in0=wr_f[:], in1=wr_t[:], op=ALU.subtract)
            i_last = DVE.tensor_copy(out=wr_hl[:, :, 36:72], in_=wr_t[:])
            tok_cdve = M(DVE, i_last)
            wait_all(tok_cdma)
            wait_all(tok_cpool)
            wait_all(tok_cdve)
            barrier()
        stop_if("const")

        def rstd_via_pool(mv, dst, last_dve_instr):
            t = M(DVE, last_dve_instr)
            kb.wait(POOL, t)
            POOL.tensor_scalar(out=dst, in0=mv[:, 1:2], scalar1=EPS, scalar2=None, op0=ALU.add)
            i = POOL.tensor_tensor(out=dst, in0=dst, in1=mhalf[:], op=ALU.pow)
            kb.wait(DVE, M(POOL, i))

        ring_x = DmaRing(kb, "ring_x", 2)
        ring_w = DmaRing(kb, "ring_w", 2)
        ring_o = DmaRing(kb, "ring_o", 2)
        ring_m = DmaRing(kb, "ring_m", 4)
        ring_xs = DmaRing(kb, "ring_xs", 2)
        ring_ys = DmaRing(kb, "ring_ys", 2)
        ring_g = DmaRing(kb, "ring_g", 2)
        s_sc = kb.sem("scatter")
        NJ = 48
        xs_d = nc.dram_tensor("xs_scratch", [NJ * 256, D], BF16, kind="Internal").ap()
        ys_d = nc.dram_tensor("ys_scratch", [NJ * 256, D], F32, kind="Internal").ap()

        for b in range(NB):
            kb.prog = progs[b]
            ch_u = Chan(kb, "ch_u", 2)
            ch_v = Chan(kb, "ch_v", 2)
            ch_z = Chan(kb, "ch_z", 2)
            ch_tp = Chan(kb, "ch_tp", 2)
            ch_n = Chan(kb, "ch_n", 2)
            ch_gu = Chan(kb, "ch_gu", 2)
            ch_gv = Chan(kb, "ch_gv", 2)
            ch_sg = Chan(kb, "ch_sg", 2)
            ch_qk = Chan(kb, "ch_qk", 2)
            ch_rot = Chan(kb, "ch_rot", 2)
            ch_qs = Chan(kb, "ch_qs", 2)
            ch_vp = Chan(kb, "ch_vp", 2)
            ch_s = Chan(kb, "ch_s", 4)
            ch_p = Chan(kb, "ch_p", 6)
            ch_o = Chan(kb, "ch_o", 2)
            ch_op = Chan(kb, "ch_op", 2)
            ch_x = Chan(kb, "ch_x", 2)
            ch_hb = Chan(kb, "ch_hb", 2)
            ch_lo = Chan(kb, "ch_lo", 2)
            ch_ht = Chan(kb, "ch_ht", 2)
            ch_r = Chan(kb, "ch_r", 1)
            ch_g2 = Chan(kb, "ch_g2", 2, ncons=2)
            ch_sl = Chan(kb, "ch_sl", 2)
            ch_at = Chan(kb, "ch_at", 2)
            ch_dn = Chan(kb, "ch_dn", 1, ncons=2)
            ch_wt = Chan(kb, "ch_wt", 2)
            ch_ot = Chan(kb, "ch_ot", 2)
            ch_xs = Chan(kb, "ch_xs", 2)
            ch_xT = Chan(kb, "ch_xT", 2)
            ch_yo = Chan(kb, "ch_yo", 2)
            ch_rg = Chan(kb, "ch_rg", 2)

            with contextlib.ExitStack() as es1:
                k1 = KB(nc, es1, kb.waited, kb.prog)
                xT_sb = k1.sb(f"xT_sb{b}", [128, 8, S], BF16)
                wsec = k1.sb(f"wsec{b}", [128, 8, 1024], BF16)
                pos_i = k1.sb(f"pos_i{b}", [128, NT], I32)
                pos_f = k1.sb(f"pos_f{b}", [128, NT], F32)
                ang = k1.sb(f"ang{b}", [128, 2, NT, 8], F32)
                kk_i = k1.sb(f"kk_i{b}", [128, 2, NT, 8], I32)
                kk_f = k1.sb(f"kk_f{b}", [128, 2, NT, 8], F32)
                rr = k1.sb(f"rr{b}", [128, 2, NT, 8], F32)
                mm = k1.sb(f"mm{b}", [128, 2, NT, 8], F32)
                cs_t = k1.sb(f"cs_t{b}", [128, 2, NT, 8], F32)
                s_ld = k1.sem(f"a1_ld{b}")
                s_w = k1.sem(f"a1_w{b}")

                for k in range(8):
                    k1.inc(POOL.dma_start(out=xT_sb[:, k, :], in_=xT[b, k * 128:(k + 1) * 128, :]), s_ld, 16)
                tok_x = (s_ld, s_ld.n)
                s_pos = k1.sem(f"a1_pos{b}")
                tok_pos = k1.inc(SP.dma_start(out=pos_i[:], in_=posT[b]), s_pos, 16)
                k1.inc(POOL.dma_start(out=wsec[:], in_=w_in[:, 1536:2560].rearrange("(k p) c -> p k c", p=128)), s_w, 16)
                tok_w = (s_w, s_w.n)

                k1.wait(DVE, tok_pos)
                DVE.tensor_copy(out=pos_f[:], in_=pos_i[:])
                DVE.tensor_tensor(out=ang[:, 1], in0=invf[:], in1=pos_f[:].unsqueeze(2).broadcast_to([128, NT, 8]), op=ALU.mult)
                DVE.tensor_scalar(out=ang[:, 0], in0=ang[:, 1], scalar1=float(np.pi / 2), scalar2=None, op0=ALU.add)
                DVE.tensor_scalar(out=kk_f[:], in0=ang[:], scalar1=float(1.0 / TWO_PI), scalar2=None, op0=ALU.mult)
                DVE.tensor_copy(out=kk_i[:], in_=kk_f[:])
                DVE.tensor_copy(out=kk_f[:], in_=kk_i[:])
                DVE.scalar_tensor_tensor(out=rr[:], in0=kk_f[:], scalar=-TWO_PI, in1=ang[:], op0=ALU.mult, op1=ALU.add)
                DVE.tensor_scalar(out=mm[:], in0=rr[:], scalar1=float(np.pi), scalar2=None, op0=ALU.is_gt)
                DVE.scalar_tensor_tensor(out=rr[:], in0=mm[:], scalar=-TWO_PI, in1=rr[:], op0=ALU.mult, op1=ALU.add)
                DVE.tensor_scalar(out=mm[:], in0=rr[:], scalar1=float(-np.pi), scalar2=None, op0=ALU.is_lt)
                i_l = DVE.scalar_tensor_tensor(out=rr[:], in0=mm[:], scalar=TWO_PI, in1=rr[:], op0=ALU.mult, op1=ALU.add)
                k1.wait(ACT, M(DVE, i_l))
                i_l = ACT.activation(out=cs_t[:], in_=rr[:], func=AF.Sin)
                tok_cs = M(ACT, i_l)
                stop_if("rope")

                with contextlib.ExitStack() as es2:
                    k2 = KB(nc, es2, kb.waited, kb.prog)
                    gu_sb = [k2.sb(f"gu_sb{b}_{i}", [128, 512], F32) for i in range(2)]
                    gv_sb = [k2.sb(f"gv_sb{b}_{i}", [128, 512], F32) for i in range(2)]
                    n_sb = [k2.sb(f"n_sb{b}_{i}", [128, 512], BF16) for i in range(2)]
                    t1_sb = k2.sb(f"t1_sb{b}", [128, 512], F32)
                    sg_sb = [k2.sb(f"sg_sb{b}_{i}", [128, 512], BF16) for i in range(2)]
                    st_sb = k2.sb(f"st_sb{b}", [128, 6], F32)
                    mv_sb = k2.sb(f"mv_sb{b}", [128, 2], F32)
                    rstd_sb = k2.sb(f"rstd_sb{b}", [128, 1], F32)

                    k2.wait(PE, tok_x)
                    k2.wait(PE, tok_w)

                    def sgu_pe_front(n):
                        su = ch_u.pslot(PE)
                        for k in range(8):
                            i = PE.matmul(banks[0 + su][:], xT_sb[:, k, n * 128:(n + 1) * 128], wsec[:, k, 0:512],
                                          start=(k == 0), stop=(k == 7))
                        ch_u.produced(M(PE, i))
                        sv = ch_v.pslot(PE)
                        for k in range(8):
                            i = PE.matmul(banks[2 + sv][:], xT_sb[:, k, n * 128:(n + 1) * 128], wsec[:, k, 512:1024],
                                          start=(k == 0), stop=(k == 7))
                        ch_v.produced(M(PE, i))

                    def sgu_act(n):
                        su = ch_u.cslot(ACT)
                        sg = ch_gu.pslot(ACT)
                        t = M(ACT, ACT.activation(out=gu_sb[sg][:], in_=banks[0 + su][:], func=AF.Gelu))
                        ch_u.consumed(t)
                        ch_gu.produced(t)
                        sv = ch_v.cslot(ACT)
                        sg = ch_gv.pslot(ACT)
                        t = M(ACT, ACT.activation(out=gv_sb[sg][:], in_=banks[2 + sv][:], func=AF.Gelu))
                        ch_v.consumed(t)
                        ch_gv.produced(t)

                    def sgu_dve_norm(n):
                        sg = ch_gv.cslot(DVE)
                        DVE.bn_stats(out=st_sb[:], in_=gv_sb[sg][:])
                        i = DVE.bn_aggr(out=mv_sb[:], in_=st_sb[:])
                        rstd_via_pool(mv_sb, rstd_sb[:], i)
                        sn = ch_n.pslot(DVE)
                        t = M(DVE, DVE.tensor_scalar(out=n_sb[sn][:], in0=gv_sb[sg][:], scalar1=mv_sb[:, 0:1], scalar2=rstd_sb[:, 0:1],
                                                     op0=ALU.subtract, op1=ALU.mult))
                        ch_gv.consumed(t)
                        ch_n.produced(t)

                    def sgu_pe_z(n):
                        sn = ch_n.cslot(PE)
                        sz = ch_z.pslot(PE)
                        for g in range(8):
                            i = PE.matmul(banks[4 + sz][:, g * 64:(g + 1) * 64], wmT[:, g, :], n_sb[sn][:, g * 64:(g + 1) * 64],
                                          start=True, stop=True, skip_group_check=True)
                        t = M(PE, i)
                        ch_n.consumed(t)
                        ch_z.produced(t)

                    def sgu_dve_out(n):
                        sz = ch_z.cslot(DVE)
                        t = M(DVE, DVE.tensor_tensor(out=t1_sb[:], in0=banks[4 + sz][:], in1=sg_bc[:], op=ALU.mult))
                        ch_z.consumed(t)
                        DVE.tensor_tensor(out=t1_sb[:], in0=t1_sb[:], in1=Bp[:], op=ALU.add)
                        sgu_ = ch_gu.cslot(DVE)
                        so = ch_sg.pslot(DVE)
                        t = M(DVE, DVE.tensor_tensor(out=sg_sb[so][:], in0=t1_sb[:], in1=gu_sb[sgu_][:], op=ALU.mult))
                        ch_gu.consumed(t)
                        ch_sg.produced(t)

                    def sgu_pe_tp(n):
                        so = ch_sg.cslot(PE)
                        st = ch_tp.pslot(PE)
                        tpv = banks[6 + st][:].bitcast(BF16)
                        for j in range(4):
                            i = PE.transpose(tpv[:, j * 128:(j + 1) * 128], sg_sb[so][:, j * 128:(j + 1) * 128], ident[:])
                        t = M(PE, i)
                        ch_sg.consumed(t)
                        ch_tp.produced(t)

                    def sgu_act_tp(n):
                        st = ch_tp.cslot(ACT)
                        tpv = banks[6 + st][:].bitcast(BF16)
                        i = ACT.activation(out=catT[:, 4:8, n * 128:(n + 1) * 128],
                                           in_=tpv[:, 0:512].rearrange("p (j t) -> p j t", j=4), func=AF.Copy)
                        ch_tp.consumed(M(ACT, i))

                    for n in range(NT + 2):
                        if n < NT:
                            sgu_pe_front(n)
                            sgu_act(n)
                            sgu_dve_norm(n)
                        if 1 <= n <= NT:
                            sgu_pe_z(n - 1)
                            sgu_dve_out(n - 1)
                        if 2 <= n:
                            sgu_pe_tp(n - 2)
                            sgu_act_tp(n - 2)
                    barrier()
                stop_if("sgu")

                with contextlib.ExitStack() as es3:
                    k3 = KB(nc, es3, kb.waited, kb.prog)
                    QTz = k3.sb(f"QTz{b}", [128, 2, S], BF16)
                    KT = k3.sb(f"KT{b}", [128, S], BF16)
                    Vz = k3.sb(f"Vz{b}", [128, 48, 2, 128], BF16)
                    qs_sb = [k3.sb(f"qs_sb{b}_{i}", [128, 4, 256], BF16) for i in range(2)]
                    ra = k3.sb(f"ra{b}", [128, 4, 4, 8], F32)
                    rb = k3.sb(f"rb{b}", [128, 4, 4, 8], F32)
                    rot_sb = [k3.sb(f"rot_sb{b}_{i}", [128, 4, 2, 2, 16], F32) for i in range(2)]
                    p_sb = [k3.sb(f"p_sb{b}_{i}", [128, 512], BF16) for i in range(6)]
                    rl_sb = k3.sb(f"rl_sb{b}", [128, 512], F32)
                    s_ws = k3.sem(f"a1_ws{b}")

                    POOL.memset(QTz[:], 0.0)
                    tok_zero = M(POOL, POOL.memset(Vz[:], 0.0))
                    for e in (ACT, DVE, PE):
                        k3.wait(e, tok_zero)
                    stop_if("att_ms")

                    def tv_blk(T, j):
                        return T[:, j * 128:(j + 1) * 128]

                    def tv_p2(T, s, r4):
                        return T[:, 512 * s:512 * (s + 1)].rearrange("p (i r) -> p r i", r=4)[:, r4, :]

                    def tv_p3k(T, r):
                        return T.rearrange("p (l r) -> p r l", r=16)[:, r, :]

                    def tv_p3q(T, s, r):
                        return T.rearrange("p (l r) -> p r l", r=16)[:, r, 32 * s:32 * (s + 1)]

                    vdefs = []
                    for j in range(16):
                        vdefs.append(lambda T, j=j: tv_blk(T, j))
                    for s in range(4):
                        for r4 in range(4):
                            vdefs.append(lambda T, s=s, r4=r4: tv_p2(T, s, r4))
                    for r in range(16):
                        vdefs.append(lambda T, r=r: tv_p3k(T, r))

                    for c in range(4):
                        for j, c0 in enumerate((c * 128, 512 + c * 128, 1024 + c * 128)):
                            k3.inc(POOL.dma_start(out=wsec[:, :, j * 128:(j + 1) * 128],
                                                  in_=w_in[:, c0:c0 + 128].rearrange("(k p) c -> p k c", p=128)), s_ws, 16)
                        k3.wait(PE, (s_ws, s_ws.n))
                        k3.wait(DVE, tok_cs)
                        stop_if("att_dma")

                        def qk_pe(gi):
                            sq = ch_qk.pslot(PE)
                            for w in range(2):
                                for tt in range(4):
                                    n = gi * 4 + tt
                                    for k in range(8):
                                        i = PE.matmul(banks[2 * sq + w][:, tt * 128:(tt + 1) * 128],
                                                      xT_sb[:, k, n * 128:(n + 1) * 128], wsec[:, k, w * 128:(w + 1) * 128],
                                                      start=(tt == 0 and k == 0), stop=(k == 7), skip_group_check=True)
                            ch_qk.produced(M(PE, i))

                        def qk_evac(gi):
                            sq = ch_qk.cslot(ACT, 0)
                            so = ch_qs.pslot(ACT, DVE)
                            sr = ch_rot.pslot(ACT)
                            qv = banks[2 * sq + 0][:].rearrange("p (t h e) -> p t h e", t=4, h=2)
                            kv = banks[2 * sq + 1][:].rearrange("p (t h e) -> p t h e", t=4, h=2)
                            ov = qs_sb[so][:].rearrange("p t (w h e) -> p t w h e", w=2, h=2)
                            ACT.activation(out=ov[:, :, 0, :, 16:64], in_=qv[:, :, :, 16:64], func=AF.Copy)
                            ACT.activation(out=ov[:, :, 1, :, 16:64], in_=kv[:, :, :, 16:64], func=AF.Copy)
                            ACT.activation(out=rot_sb[sr][:, :, 0, :, :], in_=qv[:, :, :, 0:16], func=AF.Copy)
                            ta = M(ACT, ACT.activation(out=rot_sb[sr][:, :, 1, :, :], in_=kv[:, :, :, 0:16], func=AF.Copy))
                            ch_qk.consumed(ta, 0)
                            ch_rot.produced(ta)
                            sr = ch_rot.cslot(DVE)
                            rv = rot_sb[sr][:].rearrange("p t w h e -> p t (w h) e")
                            t1 = rv[:, :, :, 0:8]
                            t2 = rv[:, :, :, 8:16]
                            og = qs_sb[so][:].rearrange("p t (g e) -> p t g e", g=4)
                            cosb = cs_t[:, 0, gi * 4:(gi + 1) * 4, :].unsqueeze(2).broadcast_to([128, 4, 4, 8])
                            sinb = cs_t[:, 1, gi * 4:(gi + 1) * 4, :].unsqueeze(2).broadcast_to([128, 4, 4, 8])
                            DVE.tensor_tensor(out=ra[:], in0=t1, in1=cosb, op=ALU.mult)
                            DVE.tensor_tensor(out=rb[:], in0=t2, in1=sinb, op=ALU.mult)
                            DVE.tensor_tensor(out=og[:, :, :, 0:8], in0=ra[:], in1=rb[:], op=ALU.subtract)
                            DVE.tensor_tensor(out=ra[:], in0=t2, in1=cosb, op=ALU.mult)
                            DVE.tensor_tensor(out=rb[:], in0=t1, in1=sinb, op=ALU.mult)
                            td = M(DVE, DVE.tensor_tensor(out=og[:, :, :, 8:16], in0=ra[:], in1=rb[:], op=ALU.add))
                            ch_rot.consumed(td)
                            ch_qs.produced(ta, td)

                        def qk_tp(gi):
                            so = ch_qs.cslot(PE)
                            st = ch_tp.pslot(PE)
                            tpv = banks[6 + st][:].bitcast(BF16).rearrange("p (t w e) -> p t w e", t=4, w=2)
                            for tt in range(4):
                                for w in range(2):
                                    i = PE.transpose(tpv[:, tt, w, :], qs_sb[so][:, tt, w * 128:(w + 1) * 128], ident[:])
                            t = M(PE, i)
                            ch_qs.consumed(t)
                            ch_tp.produced(t)

                        def qk_tp_evac(gi):
                            st = ch_tp.cslot(ACT)
                            tpv = banks[6 + st][:].bitcast(BF16).rearrange("p (t w e) -> p t w e", t=4, w=2)
                            cols = slice(gi * 512, (gi + 1) * 512)
                            ACT.activation(out=QTz[0:64, 0, cols].rearrange("p (t e) -> p t e", t=4), in_=tpv[0:64, :, 0, :], func=AF.Copy)
                            ACT.activation(out=QTz[64:128, 1, cols].rearrange("p (t e) -> p t e", t=4), in_=tpv[64:128, :, 0, :], func=AF.Copy)
                            i = ACT.activation(out=KT[:, cols].rearrange("p (t e) -> p t e", t=4), in_=tpv[:, :, 1, :], func=AF.Copy)
                            ch_tp.consumed(M(ACT, i))

                        for gi in range(4 + 2):
                            if gi < 4:
                                qk_pe(gi)
                                stop_if("qk_pe0")
                                qk_evac(gi)
                                stop_if("qk_ev0")
                            if 1 <= gi <= 4:
                                qk_tp(gi - 1)
                                stop_if("qk_tp0")
                            if 2 <= gi:
                                qk_tp_evac(gi - 2)
                                stop_if("qk_te0")
                        stop_if("qk")

                        for gi in range(12):
                            sv = ch_vp.pslot(PE)
                            for tt in range(4):
                                vd = vdefs[gi * 4 + tt]
                                for k in range(8):
                                    i = PE.matmul(banks[4 + sv][:, tt * 128:(tt + 1) * 128], vd(xT_sb[:, k, :]), wsec[:, k, 256:384],
                                                  start=(tt == 0 and k == 0), stop=(k == 7), skip_group_check=True)
                            ch_vp.produced(M(PE, i))
                            eng = ACT if gi % 2 == 0 else DVE
                            sv = ch_vp.cslot(eng)
                            src = banks[4 + sv][:].rearrange("p (t h e) -> p t h e", t=4, h=2)
                            dst = Vz[:, gi * 4:(gi + 1) * 4, :, :]
                            if eng is ACT:
                                ACT.activation(out=dst[:, :, 0, 0:64], in_=src[:, :, 0, :], func=AF.Copy)
                                i = ACT.activation(out=dst[:, :, 1, 64:128], in_=src[:, :, 1, :], func=AF.Copy)
                            else:
                                DVE.tensor_copy(out=dst[:, :, 0, 0:64], in_=src[:, :, 0, :])
                                i = DVE.tensor_copy(out=dst[:, :, 1, 64:128], in_=src[:, :, 1, :])
                            ch_vp.consumed(M(eng, i))
                        barrier()
                        stop_if("v")

                        V1 = lambda j, hh: Vz[:, j, hh, :]
                        V2 = lambda s, r4, hh: Vz[:, 16 + 4 * s + r4, hh, :]
                        V3 = lambda r, hh: Vz[:, 32 + r, hh, :]

                        def emit_s(maskt, mms, lo=0, hi=512):
                            ss = ch_s.pslot(PE)
                            PE.matmul(banks[ss][:], ident[:], maskt, start=True, stop=False, skip_group_check=True)
                            for (oc, lhsT, rhs) in mms:
                                i = PE.matmul(banks[ss][:, oc[0]:oc[1]], lhsT, rhs, start=False, stop=True, skip_group_check=True)
                            ch_s.produced(M(PE, i))
                            ss2 = ch_s.cslot(ACT)
                            sp = ch_p.pslot(ACT)
                            t = M(ACT, ACT.activation(out=p_sb[sp][:, lo:hi], in_=banks[ss2][:, lo:hi], func=AF.Exp, scale=0.125))
                            ch_s.consumed(t)
                            ch_p.produced(t)
                            return sp

                        def oview(Bk, oc):
                            if oc[0] == "blk":
                                return Bk[:, oc[1] * 128:(oc[1] + 1) * 128]
                            if oc[0] == "p2":
                                return Bk[:].rearrange("p (i r) -> p r i", r=4)[:, oc[1], :]
                            return Bk[:].rearrange("p (i r) -> p r i", r=16)[:, oc[1], :]

                        for s in range(4):
                            so_ = ch_o.pslot(PE)
                            Ob = banks[4 + 2 * so_]
                            Lb = banks[5 + 2 * so_]
                            first_pv = True
                            for hh in range(2):
                                Q = QTz[:, hh, :]
                                pend = []
                                mms = [((j * 128, (j + 1) * 128), tv_blk(KT, 4 * s + j), tv_blk(Q, 4 * s + j)) for j in range(4)]
                                sp = emit_s(m_cur[:], mms)
                                pend.append((sp, [(V1(4 * s + j, hh), (j * 128, (j + 1) * 128), ("blk", j)) for j in range(4)]))
                                js = [j for j in range(4) if 4 * s + j >= 1]
                                mms = [((j * 128, (j + 1) * 128), tv_blk(KT, 4 * s + j - 1), tv_blk(Q, 4 * s + j)) for j in js]
                                sp = emit_s(m_prev[:], mms, lo=js[0] * 128)
                                pend.append((sp, [(V1(4 * s + j - 1, hh), (j * 128, (j + 1) * 128), ("blk", j)) for j in js]))
                                mms = [((r4 * 128, (r4 + 1) * 128), tv_p2(KT, s, r4), tv_p2(Q, s, r4)) for r4 in range(4)]
                                sp = emit_s(m_cur[:], mms)
                                pend.append((sp, [(V2(s, r4, hh), (r4 * 128, (r4 + 1) * 128), ("p2", r4)) for r4 in range(4)]))
                                if s >= 1:
                                    mms = [((r4 * 128, (r4 + 1) * 128), tv_p2(KT, s - 1, r4), tv_p2(Q, s, r4)) for r4 in range(4)]
                                    sp = emit_s(m_prev[:], mms)
                                    pend.append((sp, [(V2(s - 1, r4, hh), (r4 * 128, (r4 + 1) * 128), ("p2", r4)) for r4 in range(4)]))
                                mms = [((r * 32, (r + 1) * 32), tv_p3k(KT, r), tv_p3q(Q, s, r)) for r in range(16)]
                                sp = emit_s(m3[:, s, :], mms)
                                pend.append((sp, [(V3(r, hh), (r * 32, (r + 1) * 32), ("p3", r)) for r in range(16)]))

                                for (sp, pvs) in pend:
                                    sp2 = ch_p.cslot(PE)
                                    assert sp2 == sp
                                    for (vt, pc, oc) in pvs:
                                        PE.matmul(oview(Ob, oc), vt, p_sb[sp][:, pc[0]:pc[1]], start=first_pv, stop=False, skip_group_check=True)
                                        i = PE.matmul(oview(Lb, oc), ones_z[:, hh, :], p_sb[sp][:, pc[0]:pc[1]], start=first_pv, stop=False,
                                                      skip_group_check=True)
                                        first_pv = False
                                    t_last = M(PE, i)
                                    ch_p.consumed(t_last)
                            ch_o.produced(t_last)
                            so2 = ch_o.cslot(DVE)
                            DVE.reciprocal(out=rl_sb[:], in_=banks[5 + 2 * so2][:])
                            i = DVE.tensor_tensor(out=catT[:, c, 512 * s:512 * (s + 1)], in0=banks[4 + 2 * so2][:], in1=rl_sb[:], op=ALU.mult)
                            ch_o.consumed(M(DVE, i))
                            stop_if("attn_s0")
                        barrier()
                        if DEBUG and b == DEBUG_B and STOP_AFTER == "attn":
                            with contextlib.ExitStack() as esd:
                                kd = KB(nc, esd, kb.waited, kb.prog)
                                dtmp = kd.sb("dtmpa", [128, 4, S], F32)
                                sd = kd.sem("dbga")
                                kd.wait(SP, M(DVE, DVE.tensor_copy(out=dtmp[:], in_=catT[:, 0:4, :])))
                                kd.wait(SP, kd.inc(SP.dma_start(out=dbg_cat[:, 0:4, :], in_=dtmp[:]), sd, 16))
                            stop_if("attn")

                if DEBUG and b == DEBUG_B:
                    with contextlib.ExitStack() as esd:
                        kd = KB(nc, esd, kb.waited, kb.prog)
                        dtmp = kd.sb("dtmp", [128, 8, S], F32)
                        sd = kd.sem("dbg1")
                        kd.wait(SP, M(DVE, DVE.tensor_copy(out=dtmp[:], in_=catT[:])))
                        kd.wait(SP, kd.inc(SP.dma_start(out=dbg_cat, in_=dtmp[:]), sd, 16))
                        barrier()

            esp = contextlib.ExitStack()
            kp = KB(nc, esp, kb.waited, kb.prog)
            hb_all = kp.sb(f"hb_all{b}", [128, NT, D], BF16)
            ybuf = kp.sb(f"ybuf{b}", [128, NT, D], F32)
            M1a = kp.sb(f"M1a{b}", [128, NT, 32], F32)
            M2a = kp.sb(f"M2a{b}", [128, NT, 32], F32)
            g12 = kp.sb(f"g12{b}", [128, 2, NT], F32)
            with contextlib.ExitStack() as es4:
                k4 = KB(nc, es4, kb.waited, kb.prog)
                wo_sb = k4.sb(f"wo_sb{b}", [128, 8, D], BF16)
                x_sb = [k4.sb(f"x_sb{b}_{i}", [128, D], F32) for i in range(2)]
                s_sb = k4.sb(f"s_sb{b}", [128, D], F32)
                hT_sb = [k4.sb(f"hT_sb{b}_{i}", [128, 8, 128], BF16) for i in range(2)]
                hf_sb = k4.sb(f"hf_sb{b}", [128, D], F32)
                lo_sb = [k4.sb(f"lo_sb{b}_{i}", [128, D], BF16) for i in range(2)]
                loT_sb = [k4.sb(f"loT_sb{b}_{i}", [128, 8, 128], BF16) for i in range(2)]
                st2 = k4.sb(f"st2_{b}", [128, 2, 6], F32)
                mv2 = k4.sb(f"mv2_{b}", [128, 2], F32)
                rstd2 = k4.sb(f"rstd2_{b}", [128, 1], F32)
                lg72 = k4.sb(f"lg72_{b}", [128, 72], F32)
                lg = k4.sb(f"lg_{b}", [128, 36], F32)
                rt = k4.sb(f"rt_{b}", [128, 64], F32)
                s_wo = k4.sem(f"a2_wo{b}")
                g1_bc = k4.sb(f"g1_bc{b}", [128, D], F32)
                b1_bc = k4.sb(f"b1_bc{b}", [128, D], F32)
                s_g1 = k4.sem(f"a2_g1{b}")
                k4.inc(SP.dma_start(out=g1_bc[:], in_=ln1_g.partition_broadcast(128)), s_g1, 16)
                k4.wait(DVE, k4.inc(SP.dma_start(out=b1_bc[:], in_=ln1_b.partition_broadcast(128)), s_g1, 16))

                t_wo = k4.inc(POOL.dma_start(out=wo_sb[:], in_=w_out.rearrange("(k p) c -> p k c", p=128)), s_wo, 16)
                k4.wait(PE, t_wo)
                k4.wait(DVE, t_wo)

                def a2_load(n):
                    sx = ch_x.pslot(SP)
                    i = SP.dma_start(out=x_sb[sx][:], in_=xtm[b, n * 128:(n + 1) * 128, :])
                    ch_x.produced(ring_x.start(i, sx))

                def a2_pe_op(n):
                    so = ch_op.pslot(PE)
                    for hf in range(2):
                        for k in range(8):
                            i = PE.matmul(banks[2 * so + hf][:], catT[:, k, n * 128:(n + 1) * 128], wo_sb[:, k, hf * 512:(hf + 1) * 512],
                                          start=(k == 0), stop=(k == 7))
                    ch_op.produced(M(PE, i))

                def a2_dve_ln(n):
                    so = ch_op.cslot(DVE)
                    sx = ch_x.cslot(DVE)
                    for hf in range(2):
                        i = DVE.scalar_tensor_tensor(out=s_sb[:, hf * 512:(hf + 1) * 512], in0=x_sb[sx][:, hf * 512:(hf + 1) * 512],
                                                     scalar=ALPHA, in1=banks[2 * so + hf][:], op0=ALU.mult, op1=ALU.add)
                    t = M(DVE, i)
                    ch_op.consumed(t)
                    ch_x.consumed(t)
                    for hf in range(2):
                        DVE.bn_stats(out=st2[:, hf, :], in_=s_sb[:, hf * 512:(hf + 1) * 512])
                    i = DVE.bn_aggr(out=mv2[:], in_=st2[:].rearrange("p a c -> p (a c)"))
                    rstd_via_pool(mv2, rstd2[:], i)
                    DVE.tensor_scalar(out=hf_sb[:], in0=s_sb[:], scalar1=mv2[:, 0:1], scalar2=rstd2[:, 0:1],
                                      op0=ALU.subtract, op1=ALU.mult)
                    DVE.tensor_tensor(out=hf_sb[:], in0=hf_sb[:], in1=g1_bc[:], op=ALU.mult)
                    DVE.tensor_tensor(out=hf_sb[:], in0=hf_sb[:], in1=b1_bc[:], op=ALU.add)
                    sh = ch_hb.pslot(DVE)
                    DVE.tensor_copy(out=hb_all[:, n, :], in_=hf_sb[:])
                    DVE.tensor_tensor(out=lo_sb[sh][:], in0=hf_sb[:], in1=hb_all[:, n, :], op=ALU.subtract)
                    i = DVE.tensor_scalar(out=ybuf[:, n, :], in0=hf_sb[:], scalar1=ALPHA, scalar2=None, op0=ALU.mult)
                    ch_hb.produced(M(DVE, i))

                def a2_pe_tp(n):
                    sh = ch_hb.cslot(PE)
                    st = ch_tp.pslot(PE)
                    tpv = banks[6 + st][:].bitcast(BF16)
                    for k in range(8):
                        i = PE.transpose(tpv[:, k * 128:(k + 1) * 128], hb_all[:, n, k * 128:(k + 1) * 128], ident[:])
                    ch_tp.produced(M(PE, i))
                    st = ch_tp.pslot(PE)
                    tpv = banks[6 + st][:].bitcast(BF16)
                    for k in range(8):
                        i = PE.transpose(tpv[:, k * 128:(k + 1) * 128], lo_sb[sh][:, k * 128:(k + 1) * 128], ident[:])
                    t = M(PE, i)
                    ch_hb.consumed(t)
                    ch_tp.produced(t)

                def a2_act_tp(n):
                    st = ch_tp.cslot(ACT)
                    tpv = banks[6 + st][:].bitcast(BF16)
                    sht = ch_ht.pslot(ACT)
                    i = ACT.activation(out=hT_sb[sht][:], in_=tpv.rearrange("p (k t) -> p k t", k=8), func=AF.Copy)
                    t_h = M(ACT, i)
                    ch_tp.consumed(t_h)
                    ch_ht.produced(t_h)
                    st = ch_tp.cslot(ACT)
                    tpv = banks[6 + st][:].bitcast(BF16)
                    sl = ch_lo.pslot(ACT)
                    i = ACT.activation(out=loT_sb[sl][:], in_=tpv.rearrange("p (k t) -> p k t", k=8), func=AF.Copy)
                    t = M(ACT, i)
                    ch_tp.consumed(t)
                    ch_lo.produced(t)
                    return t_h

                def a2_pe_route(n, t_h):
                    sl = ch_lo.cslot(PE)
                    sht = ch_ht.cslot(PE)
                    ch_r.pslot(PE)
                    rp = banks[4]
                    for k in range(8):
                        PE.matmul(rp[:, 0:72], hT_sb[sht][:, k, :], wr_hl[:, k, 0:72], start=(k == 0), stop=False,
                                  skip_group_check=True)
                    for k in range(8):
                        i = PE.matmul(rp[:, 0:36], loT_sb[sl][:, k, :], wr_hl[:, k, 0:36], start=False, stop=(k == 7),
                                      skip_group_check=True)
                    t = M(PE, i)
                    ch_lo.consumed(t)
                    ch_ht.consumed(t)
                    ch_r.produced(t)

                def a2_route_math(n):
                    ch_r.cslot(ACT)
                    t = M(ACT, ACT.activation(out=lg72[:], in_=banks[4][:, 0:72], func=AF.Copy))
                    ch_r.consumed(t)
                    kb.wait(DVE, t)
                    DVE.tensor_tensor(out=lg[:], in0=lg72[:, 0:36], in1=lg72[:, 36:72], op=ALU.add)
                    DVE.tensor_tensor(out=lg[:], in0=lg[:], in1=br_bc[:], op=ALU.add)
                    gmax, oh, ge, sume, psel = rt[:, 0:1], rt[:, 1:5], rt[:, 5:9], rt[:, 9:10], rt[:, 10:11]
                    esel, m1, eq1, e2, m2, eq2 = rt[:, 11:19], rt[:, 19:20], rt[:, 20:28], rt[:, 28:36], rt[:, 36:37], rt[:, 37:45]
                    dd, w1, w2, eg = rt[:, 45:46], rt[:, 46:47], rt[:, 47:48], rt[:, 48:56]
                    DVE.tensor_reduce(out=gmax, in_=lg[:, 0:4], axis=AX.X, op=ALU.max)
                    DVE.tensor_scalar(out=oh, in0=lg[:, 0:4], scalar1=gmax, scalar2=None, op0=ALU.is_equal)
                    DVE.tensor_scalar(out=ge, in0=lg[:, 0:4], scalar1=gmax, scalar2=None, op0=ALU.subtract)
                    DVE.tensor_scalar(out=esel, in0=lg[:, 4:12], scalar1=oh[:, 0:1], scalar2=None, op0=ALU.mult)
                    for g in range(1, 4):
                        DVE.scalar_tensor_tensor(out=esel, in0=lg[:, 4 + 8 * g:12 + 8 * g], scalar=oh[:, g:g + 1], in1=esel,
                                                 op0=ALU.mult, op1=ALU.add)
                    DVE.tensor_reduce(out=m1, in_=esel, axis=AX.X, op=ALU.max)
                    DVE.tensor_scalar(out=eq1, in0=esel, scalar1=m1, scalar2=None, op0=ALU.is_equal)
                    DVE.scalar_tensor_tensor(out=e2, in0=eq1, scalar=-1e30, in1=esel, op0=ALU.mult, op1=ALU.add)
                    DVE.tensor_reduce(out=m2, in_=e2, axis=AX.X, op=ALU.max)
                    DVE.tensor_scalar(out=eq2, in0=e2, scalar1=m2, scalar2=None, op0=ALU.is_equal)
                    i = DVE.tensor_tensor(out=dd, in0=m2, in1=m1, op=ALU.subtract)
                    kb.wait(ACT, M(DVE, i))
                    ACT.activation(out=ge, in_=ge, func=AF.Exp)
                    i = ACT.activation(out=dd, in_=dd, func=AF.Exp)
                    kb.wait(DVE, M(ACT, i))
                    DVE.tensor_reduce(out=sume, in_=ge, axis=AX.X, op=ALU.add)
                    DVE.reciprocal(out=psel, in_=sume)
                    DVE.tensor_scalar(out=w1, in0=dd, scalar1=1.0, scalar2=None, op0=ALU.add)
                    DVE.reciprocal(out=w1, in_=w1)
                    DVE.tensor_tensor(out=w2, in0=dd, in1=w1, op=ALU.mult)
                    DVE.tensor_tensor(out=w1, in0=w1, in1=psel, op=ALU.mult)
                    DVE.tensor_tensor(out=w2, in0=w2, in1=psel, op=ALU.mult)
                    for g in range(4):
                        DVE.tensor_scalar(out=M1a[:, n, g * 8:(g + 1) * 8], in0=eq1, scalar1=oh[:, g:g + 1], scalar2=None, op0=ALU.mult)
                        DVE.tensor_scalar(out=M2a[:, n, g * 8:(g + 1) * 8], in0=eq2, scalar1=oh[:, g:g + 1], scalar2=None, op0=ALU.mult)
                    DVE.tensor_copy(out=g12[:, 0, n:n + 1], in_=w1)
                    DVE.tensor_copy(out=g12[:, 1, n:n + 1], in_=w2)

                a2_load(0)
                t_hs = {}
                for n in range(NT + 2):
                    if n + 1 < NT:
                        a2_load(n + 1)
                    if n < NT:
                        a2_pe_op(n)
                        a2_dve_ln(n)
                    if 1 <= n <= NT:
                        a2_pe_tp(n - 1)
                        t_hs[n - 1] = a2_act_tp(n - 1)
                    if 2 <= n:
                        a2_pe_route(n - 2, t_hs[n - 2])
                        a2_route_math(n - 2)
                barrier()

            if DEBUG and b == DEBUG_B:
                with contextlib.ExitStack() as esd:
                    kd = KB(nc, esd, kb.waited, kb.prog)
                    sd = kd.sem("dbg2")
                    kd.inc(SP.dma_start(out=dbg_y, in_=ybuf[:]), sd, 16)
                    kd.wait(SP, kd.inc(SP.dma_start(out=dbg_gate, in_=M1a[:]), sd, 16))
                    barrier()
            if not DEBUG or b == DEBUG_B:
                stop_if("A2")

            with contextlib.ExitStack() as es5:
                k5 = KB(nc, es5, kb.waited, kb.prog)
                wgu_sb = [catT[:, 2 * i:2 * i + 2, :].rearrange("p a (k f) -> p (a k) f", f=512) for i in range(2)]
                wdn_t = [k5.sb(f"wdn_t{b}_{i}", [128, 2, D], BF16) for i in range(2)]
                wdn_sb = [t_[:] for t_ in wdn_t]
                thr = k5.sb(f"thr{b}", [128, NJ, 32], F32)
                Mb = k5.sb(f"Mb{b}", [128, NT * 32], BF16)
                Ms = k5.sb(f"Ms{b}", [128, NT, 32], F32)
                cs = k5.sb(f"cs{b}", [128, NT, 32], F32)
                off = k5.sb(f"off{b}", [128, NT, 32], F32)
                Sf = k5.sb(f"Sf{b}", [128, NT, 32], F32)
                tS = Ms
                sc_a = k5.sb(f"sc_a{b}", [128, 32], F32)
                sc_b = k5.sb(f"sc_b{b}", [128, 32], F32)
                pt = k5.sb(f"pt{b}", [128, 32], F32)
                base = k5.sb(f"base{b}", [128, 32], F32)
                qi = k5.sb(f"qi{b}", [128, 32], I32)
                sl_f = k5.sb(f"sl_f{b}", [128, 2, NT], F32)
                sl_i = k5.sb(f"sl_i{b}", [128, 2, NT], I32)
                ej_f = k5.sb(f"ej_f{b}", [128, NJ], F32)
                wi_f = thr[:].rearrange("p j e -> p (j e)")[:, 0:NJ * 10].rearrange("p (j c) -> p j c", c=10)
                wi_i = k5.sb(f"wi_i{b}", [128, NJ, 10], I32)
                pidx = k5.sb(f"pidx{b}", [128, 1], F32)
                ko = k5.sb(f"ko{b}", [128, 8], F32)
                Lst = k5.sb(f"Lst{b}", [128, 128], BF16)
                xt_sb = [k5.sb(f"xt_sb{b}_{i}", [128, 2, D], BF16) for i in range(2)]
                xsT = [k5.sb(f"xsT{b}_{i}", [128, 8, 256], BF16) for i in range(2)]
                sl_sb = [k5.sb(f"sl_sb{b}_{i}", [128, 512], F32) for i in range(2)]
                at_sb = [k5.sb(f"at_sb{b}_{i}", [128, 2, 256], BF16) for i in range(2)]
                yo_sb = [k5.sb(f"yo_sb{b}_{i}", [128, D], F32) for i in range(2)]
                r_sb = yo_sb

                POOL.affine_select(out=Lst[:], in_=ones_bf[:], pattern=[[1, 128]], compare_op=ALU.is_gt,
                                   fill=0.0, base=0, channel_multiplier=-1)
                t_thr = M(POOL, POOL.iota(thr[:], pattern=[[256, NJ], [0, 32]], base=0, channel_multiplier=0,
                                          allow_small_or_imprecise_dtypes=True))
                POOL.iota(pidx[:], pattern=[[0, 1]], base=0, channel_multiplier=1, allow_small_or_imprecise_dtypes=True)
                t_io = M(POOL, POOL.iota(ko[:], pattern=[[128, 8]], base=0, channel_multiplier=0, allow_small_or_imprecise_dtypes=True))
                DVE.tensor_tensor(out=Ms[:], in0=M1a[:], in1=M2a[:], op=ALU.add)
                t = M(DVE, DVE.tensor_copy(out=Mb[:], in_=Ms[:].rearrange("p n e -> p (n e)")))
                kb.wait(PE, t)
                kb.wait(PE, t_thr)
                PE.matmul(banks[0][:], Lst[:], Mb[:], start=True, stop=True)
                t = M(PE, PE.matmul(banks[1][:], ones_bf[:], Mb[:], start=True, stop=True))
                kb.wait(DVE, t)
                DVE.tensor_copy(out=cs[:], in_=banks[1][:].rearrange("p (n e) -> p n e", e=32))
                DVE.memset(off[:, 0, :], 0.0)
                for n in range(1, NT):
                    DVE.tensor_tensor(out=off[:, n, :], in0=off[:, n - 1, :], in1=cs[:, n - 1, :], op=ALU.add)
                DVE.tensor_tensor(out=sc_a[:], in0=off[:, NT - 1, :], in1=cs[:, NT - 1, :], op=ALU.add)
                DVE.tensor_scalar(out=sc_b[:], in0=sc_a[:], scalar1=127.5, scalar2=1.0 / 256.0, op0=ALU.add, op1=ALU.mult)
                DVE.tensor_copy(out=qi[:], in_=sc_b[:])
                DVE.tensor_scalar(out=pt[:], in0=qi[:], scalar1=256.0, scalar2=None, op0=ALU.mult)
                DVE.tensor_copy(out=sc_a[:], in_=pt[:])
                pa, pb = sc_a, sc_b
                for sh in (1, 2, 4, 8, 16):
                    DVE.tensor_copy(out=pb[:, 0:sh], in_=pa[:, 0:sh])
                    DVE.tensor_tensor(out=pb[:, sh:32], in0=pa[:, sh:32], in1=pa[:, 0:32 - sh], op=ALU.add)
                    pa, pb = pb, pa
                incl = pa
                DVE.tensor_tensor(out=base[:], in0=incl[:], in1=pt[:], op=ALU.subtract)
                DVE.tensor_tensor(out=Sf[:], in0=banks[0][:].rearrange("p (n e) -> p n e", e=32), in1=off[:], op=ALU.add)
                DVE.tensor_tensor(out=Sf[:], in0=Sf[:], in1=base[:].unsqueeze(1).broadcast_to([128, NT, 32]), op=ALU.add)
                DVE.tensor_tensor(out=tS[:], in0=Sf[:], in1=M1a[:], op=ALU.mult)
                DVE.tensor_reduce(out=sl_f[:, 0, :], in_=tS[:], axis=AX.X, op=ALU.add)
                DVE.tensor_tensor(out=tS[:], in0=Sf[:], in1=M2a[:], op=ALU.mult)
                DVE.tensor_reduce(out=sl_f[:, 1, :], in_=tS[:], axis=AX.X, op=ALU.add)
                DVE.tensor_copy(out=sl_i[:], in_=sl_f[:])
                kb.wait(DVE, t_thr)
                DVE.tensor_tensor(out=thr[:], in0=thr[:], in1=incl[:].unsqueeze(1).broadcast_to([128, NJ, 32]), op=ALU.is_ge)
                DVE.tensor_reduce(out=ej_f[:], in_=thr[:], axis=AX.X, op=ALU.add)
                DVE.tensor_scalar(out=ej_f[:], in0=ej_f[:], scalar1=31.0, scalar2=None, op0=ALU.min)
                kb.wait(DVE, t_io)
                DVE.tensor_scalar(out=ej_f[:], in0=ej_f[:], scalar1=1024.0, scalar2=pidx[:, 0:1], op0=ALU.mult, op1=ALU.add)
                DVE.tensor_tensor(out=wi_f[:, :, 0:8], in0=ej_f[:].unsqueeze(2).broadcast_to([128, NJ, 8]),
                                  in1=ko[:, 0:8].unsqueeze(1).broadcast_to([128, NJ, 8]), op=ALU.add)
                DVE.tensor_scalar(out=ej_f[:], in0=ej_f[:], scalar1=pidx[:, 0:1], scalar2=0.25, op0=ALU.subtract, op1=ALU.mult)
                DVE.tensor_scalar(out=ej_f[:], in0=ej_f[:], scalar1=pidx[:, 0:1], scalar2=None, op0=ALU.add)
                DVE.tensor_tensor(out=wi_f[:, :, 8:10], in0=ej_f[:].unsqueeze(2).broadcast_to([128, NJ, 2]),
                                  in1=ko[:, 0:2].unsqueeze(1).broadcast_to([128, NJ, 2]), op=ALU.add)
                t_disp = M(DVE, DVE.tensor_copy(out=wi_i[:], in_=wi_f))

                kb.wait(POOL, t_disp)
                for n in range(NT):
                    for kk in range(2):
                        i = POOL.indirect_dma_start(out=xs_d, out_offset=bass.IndirectOffsetOnAxis(ap=sl_i[:, kk, n:n + 1], axis=0),
                                                    in_=hb_all[:, n, :], in_offset=None)
                        kb.inc(i, s_sc, 16)
                tok_sc = (s_sc, s_sc.n)
                kb.wait(SP, tok_sc)
                kb.wait(POOL, tok_sc)

                wgu_rows = w_gu.rearrange("e d f -> (e d) f")
                wdn_rows = w_dn.rearrange("e f d -> (e f) d")

                def m_load_w(j):
                    sw = ch_wt.pslot(POOL)
                    for k in range(8):
                        i = POOL.indirect_dma_start(out=wgu_sb[sw][:, k, :], out_offset=None, in_=wgu_rows,
                                                    in_offset=bass.IndirectOffsetOnAxis(ap=wi_i[:, j, k:k + 1], axis=0))
                        t1 = ring_m.start(i, sw)
                    for k in range(2):
                        i = POOL.indirect_dma_start(out=wdn_sb[sw][:, k, :], out_offset=None, in_=wdn_rows,
                                                    in_offset=bass.IndirectOffsetOnAxis(ap=wi_i[:, j, 8 + k:9 + k], axis=0))
                        t1 = ring_m.start(i, sw)
                    ch_wt.produced(t1)

                def m_load_x(j):
                    sx = ch_xs.pslot(SP)
                    i = SP.dma_start(out=xt_sb[sx][:], in_=xs_d[256 * j:256 * (j + 1), :].rearrange("(a p) d -> p a d", p=128))
                    ch_xs.produced(ring_xs.start(i, sx))

                def m_tile(j):
                    sw = j % 2
                    sx = ch_xs.cslot(PE)
                    ch_tp.pslot(PE)
                    ch_tp.pslot(PE)
                    for half in range(2):
                        tpv = banks[6 + half][:].bitcast(BF16).rearrange("p (k a t) -> p k a t", k=4, a=2)
                        for k4 in range(4):
                            for a_ in range(2):
                                k = half * 4 + k4
                                i = PE.transpose(tpv[:, k4, a_, :], xt_sb[sx][:, a_, k * 128:(k + 1) * 128], ident[:])
                    t = M(PE, i)
                    ch_xs.consumed(t)
                    ch_tp.produced(t)
                    ch_tp.produced(t)
                    sT = ch_xT.pslot(ACT, DVE)
                    ch_tp.cslot(ACT)
                    ta = M(ACT, ACT.activation(out=xsT[sT][:, 0:4, :], in_=banks[6][:].bitcast(BF16).rearrange("p (k t) -> p k t", k=4), func=AF.Copy))
                    ch_tp.consumed(ta)
                    ch_tp.cslot(DVE)
                    td = M(DVE, DVE.tensor_copy(out=xsT[sT][:, 4:8, :], in_=banks[7][:].bitcast(BF16).rearrange("p (k t) -> p k t", k=4)))
                    ch_tp.consumed(td)
                    ch_xT.produced(ta, td)
                    sT = ch_xT.cslot(PE)
                    for tok in ch_wt.ready[j]:
                        kb.wait(PE, tok)
                    sg = ch_g2.pslot(PE)
                    for part in range(2):
                        for fcp in range(2):
                            fc = part * 256 + fcp * 128
                            for k in range(8):
                                i = PE.matmul(banks[2 * sg + part][:, fcp * 256:(fcp + 1) * 256], wgu_sb[sw][:, k, fc:fc + 128], xsT[sT][:, k, :],
                                              start=(fcp == 0 and k == 0), stop=(k == 7), skip_group_check=True)
                    t = M(PE, i)
                    ch_xT.consumed(t)
                    ch_g2.produced(t)
                    sg = ch_g2.cslot(ACT, 0)
                    ss = ch_sl.pslot(ACT)
                    t = M(ACT, ACT.activation(out=sl_sb[ss][:], in_=banks[2 * sg][:], func=AF.Silu))
                    ch_g2.consumed(t, 0)
                    ch_sl.produced(t)
                    sg = ch_g2.cslot(DVE, 1)
                    ss = ch_sl.cslot(DVE)
                    sa = ch_at.pslot(DVE)
                    t = M(DVE, DVE.tensor_tensor(out=at_sb[sa][:].rearrange("p c t -> p (c t)"), in0=banks[2 * sg + 1][:], in1=sl_sb[ss][:], op=ALU.mult))
                    ch_g2.consumed(t, 1)
                    ch_sl.consumed(t)
                    ch_at.produced(t)
                    sa = ch_at.cslot(PE)
                    for a_ in range(2):
                        ch_dn.pslot(PE)
                        for hf in range(2):
                            for fc in range(2):
                                i = PE.matmul(banks[4 + hf][:], at_sb[sa][:, fc, a_ * 128:(a_ + 1) * 128],
                                              wdn_sb[sw][:, fc, hf * 512:(hf + 1) * 512], start=(fc == 0), stop=(fc == 1))
                        t = M(PE, i)
                        ch_dn.produced(t)
                        so = ch_yo.pslot(ACT, DVE)
                        ch_dn.cslot(ACT, 0)
                        ta = M(ACT, ACT.activation(out=yo_sb[so][:, 0:512], in_=banks[4][:], func=AF.Copy))
                        ch_dn.consumed(ta, 0)
                        ch_dn.cslot(DVE, 1)
                        td = M(DVE, DVE.tensor_copy(out=yo_sb[so][:, 512:1024], in_=banks[5][:]))
                        ch_dn.consumed(td, 1)
                        ch_yo.produced(ta, td)
                        so = ch_yo.cslot(SP)
                        i = SP.dma_start(out=ys_d[256 * j + 128 * a_:256 * j + 128 * (a_ + 1), :], in_=yo_sb[so][:])
                        ch_yo.consumed(ring_ys.start(i, so))
                        last_ys[so] = (ring_ys.sems[so], ring_ys.sems[so].n)
                    ch_at.consumed(t)
                    ch_wt.consumed(t)

                last_ys = {}
                m_load_w(0)
                m_load_x(0)
                for j in range(NJ):
                    if j + 1 < NJ:
                        m_load_w(j + 1)
                        m_load_x(j + 1)
                    m_tile(j)
                for so, tk in last_ys.items():
                    kb.wait(POOL, tk)
                for n in range(NT):
                    for kk in range(2):
                        sr = ch_rg.pslot(POOL)
                        i = POOL.indirect_dma_start(out=r_sb[sr][:], out_offset=None, in_=ys_d,
                                                    in_offset=bass.IndirectOffsetOnAxis(ap=sl_i[:, kk, n:n + 1], axis=0))
                        ch_rg.produced(ring_g.start(i, sr))
                        sr = ch_rg.cslot(DVE)
                        t = M(DVE, DVE.scalar_tensor_tensor(out=ybuf[:, n, :], in0=r_sb[sr][:], scalar=g12[:, kk, n:n + 1], in1=ybuf[:, n, :],
                                                            op0=ALU.mult, op1=ALU.add))
                        ch_rg.consumed(t)
                barrier()
                stop_if("M")

            with contextlib.ExitStack() as es6:
                k6 = KB(nc, es6, kb.waited, kb.prog)
                o_sb = [k6.sb(f"o_sb{b}_{i}", [128, D], F32) for i in range(2)]
                st3 = k6.sb(f"st3_{b}", [128, 2, 6], F32)
                mv3 = k6.sb(f"mv3_{b}", [128, 2], F32)
                rstd3 = k6.sb(f"rstd3_{b}", [128, 1], F32)
                g2_bc = k6.sb(f"g2_bc{b}", [128, D], F32)
                b2_bc = k6.sb(f"b2_bc{b}", [128, D], F32)
                s_g2 = k6.sem(f"f_g2{b}")
                k6.inc(SP.dma_start(out=g2_bc[:], in_=ln2_g.partition_broadcast(128)), s_g2, 16)
                k6.wait(DVE, k6.inc(SP.dma_start(out=b2_bc[:], in_=ln2_b.partition_broadcast(128)), s_g2, 16))
                last_store = None
                for n in range(NT):
                    for hf in range(2):
                        DVE.bn_stats(out=st3[:, hf, :], in_=ybuf[:, n, hf * 512:(hf + 1) * 512])
                    i = DVE.bn_aggr(out=mv3[:], in_=st3[:].rearrange("p a c -> p (a c)"))
                    rstd_via_pool(mv3, rstd3[:], i)
                    so = ch_ot.pslot(DVE)
                    DVE.tensor_scalar(out=o_sb[so][:], in0=ybuf[:, n, :], scalar1=mv3[:, 0:1], scalar2=rstd3[:, 0:1],
                                      op0=ALU.subtract, op1=ALU.mult)
                    DVE.tensor_tensor(out=o_sb[so][:], in0=o_sb[so][:], in1=g2_bc[:], op=ALU.mult)
                    i = DVE.tensor_tensor(out=o_sb[so][:], in0=o_sb[so][:], in1=b2_bc[:], op=ALU.add)
                    ch_ot.produced(M(DVE, i))
                    so = ch_ot.cslot(SP)
                    i = SP.dma_start(out=out[b, n * 128:(n + 1) * 128, :], in_=o_sb[so][:])
                    t = ring_o.start(i, so)
                    ch_ot.consumed(t)
                    if n >= NT - 2:
                        kb.wait(SP, t) if n == NT - 2 else None
                        last_store = t
                        if n == NT - 2:
                            prev_store = t
                kb.wait(SP, prev_store)
                kb.wait(SP, last_store)
                barrier()
            esp.close()
    return nc


def _prep_inputs(inputs):
    x = np.ascontiguousarray(inputs["x"], dtype=np.float32)
    pos = np.ascontiguousarray(inputs["positions"], dtype=np.int32)
    w_r = np.concatenate([inputs["w_group"][0], np.transpose(inputs["w_expert"][0], (1, 0, 2)).reshape(D, 32)], axis=1)
    b_r = np.concatenate([inputs["b_group"][0], inputs["b_expert"][0].reshape(32)])[None, :]
    ws = inputs["w_spatial"][0]
    shared = {
        "w_in": np.ascontiguousarray(inputs["w_in"][0]),
        "w_out": np.ascontiguousarray(inputs["w_out"][0]),
        "ws_tgs": np.ascontiguousarray(np.transpose(ws, (1, 0, 2))),
        "ws_sgt": np.ascontiguousarray(np.transpose(ws, (2, 0, 1))),
        "bspT": np.ascontiguousarray(inputs["b_spatial"][0].T),
        "sgu_g": np.ascontiguousarray(inputs["sgu_ln_g"]),
        "sgu_b": np.ascontiguousarray(inputs["sgu_ln_b"]),
        "ln1_g": np.ascontiguousarray(inputs["ln1_g"]),
        "ln1_b": np.ascontiguousarray(inputs["ln1_b"]),
        "ln2_g": np.ascontiguousarray(inputs["ln2_g"]),
        "ln2_b": np.ascontiguousarray(inputs["ln2_b"]),
        "w_r": np.ascontiguousarray(w_r, dtype=np.float32),
        "b_r": np.ascontiguousarray(b_r, dtype=np.float32),
        "w_gu": np.ascontiguousarray(inputs["w_gate_up"][0].reshape(NE, D, 512)),
        "w_dn": np.ascontiguousarray(inputs["w_down"][0].reshape(NE, 256, D)),
    }
    in_maps = []
    for c in range(NCORES):
        xs = x[c * NB:(c + 1) * NB]
        m = dict(shared)
        m["xT"] = np.ascontiguousarray(np.transpose(xs, (0, 2, 1)))
        m["xtm"] = xs
        m["posT"] = np.ascontiguousarray(np.transpose(pos[c * NB:(c + 1) * NB].reshape(NB, NT, 128), (0, 2, 1)))
        in_maps.append(m)
    return in_maps


def kernel(**inputs):
    in_maps = _prep_inputs(inputs)
    nc = build_nc()
    res = run_bass_kernel_spmd(nc, in_maps, core_ids=list(range(NCORES)))
    return np.concatenate([r["out"] for r in res.results], axis=0).astype(np.float32)
```

```python
import contextlib
import numpy as np
import concourse.bass as bass
import concourse.mybir as mybir
from concourse.bass_utils import run_bass_kernel_spmd

F32, BF16, I32 = mybir.dt.float32, mybir.dt.bfloat16, mybir.dt.int32
AF = mybir.ActivationFunctionType
ALU = mybir.AluOpType
AX = mybir.AxisListType

NCORES = 8
S = 2048
D = 1024
NB = 2
NT = S // 128
ALPHA = float(2.0 ** 0.25)
EPS = 1e-5
NEG = -30000.0
NE = 32
TWO_PI = float(2 * np.pi)
DEBUG = False
DEBUG_B = 0
STOP_AFTER = None


class _Stop(Exception):
    pass


class Sem:
    _serial = 0

    def __init__(self, h):
        self.h = h
        self.n = 0
        Sem._serial += 1
        self.uid = Sem._serial


class KB:
    def __init__(self, nc, es, waited=None, prog=None):
        self.nc = nc
        self.es = es
        self.waited = {} if waited is None else waited
        self.prog = {} if prog is None else prog

    sem_es = None

    def sem(self, name):
        return Sem(KB.sem_es.enter_context(self.nc.semaphore(name)))

    def sb(self, name, shape, dt):
        return self.es.enter_context(self.nc.sbuf_tensor(name, shape, dt))

    def ps(self, name, shape, dt):
        return self.es.enter_context(self.nc.psum_tensor(name, shape, dt))

    def inc(self, instr, sem, k=1):
        instr.then_inc(sem.h, k)
        sem.n += k
        return (sem, sem.n)

    def mark(self, eng, instr):
        if isinstance(instr, Tok):
            return instr.tok
        return self.inc(instr, self.prog[id(getattr(eng, "raw", eng))])

    def wait(self, eng, tok):
        if tok is None:
            return
        sem, val = tok
        if val <= 0:
            return
        raw = getattr(eng, "raw", eng)
        key = (id(raw), sem.uid)
        if self.waited.get(key, 0) >= val:
            return
        self.waited[key] = val
        raw.wait_ge(sem.h, val)


class Tok:
    def __init__(self, instr, tok):
        self.instr = instr
        self.tok = tok


_COMPUTE = {"activation", "tensor_tensor", "tensor_scalar", "scalar_tensor_tensor", "tensor_copy", "tensor_reduce",
            "reciprocal", "bn_stats", "bn_aggr", "memset", "affine_select", "iota"}


class EngProxy:
    def __init__(self, raw, kb):
        self.raw = raw
        self.kb = kb
        self.last = None

    def __getattr__(self, name):
        attr = getattr(self.raw, name)
        if name not in _COMPUTE:
            return attr

        def wrapper(*a, **k):
            if self.last is not None:
                self.kb.wait(self, self.last)
            instr = attr(*a, **k)
            tok = self.kb.inc(instr, self.kb.prog[id(self.raw)])
            self.last = tok
            return Tok(instr, tok)
        return wrapper


class Chan:
    def __init__(self, kb, name, depth, ncons=1):
        self.kb = kb
        self.depth = depth
        self.ready = []
        self.free = []
        self.ci = [0] * ncons

    def pslot(self, *engs):
        i = len(self.ready)
        if i >= self.depth:
            for tok in self.free[i - self.depth]:
                for e in engs:
                    self.kb.wait(e, tok)
        return i % self.depth

    def produced(self, *toks):
        self.ready.append(list(toks))

    def cslot(self, eng, c=0):
        i = self.ci[c]
        for tok in self.ready[i]:
            self.kb.wait(eng, tok)
        return i % self.depth

    def consumed(self, tok, c=0):
        i = self.ci[c]
        while len(self.free) <= i:
            self.free.append([])
        self.free[i].append(tok)
        self.ci[c] += 1


class DmaRing:
    def __init__(self, kb, name, depth):
        self.kb = kb
        self.sems = [kb.sem(f"{name}{i}") for i in range(depth)]

    def start(self, instr, slot):
        return self.kb.inc(instr, self.sems[slot], 16)


def build_nc():
    nc = bass.Bass("TRN2", target_bir_lowering=False)
    dr = lambda name, shape, dt=F32: nc.dram_tensor(name, shape, dt, kind="ExternalInput").ap()
    xT = dr("xT", [NB, D, S])
    xtm = dr("xtm", [NB, S, D])
    posT = dr("posT", [NB, 128, NT], I32)
    w_in = dr("w_in", [D, 2560])
    w_out = dr("w_out", [D, D])
    ws_tgs = dr("ws_tgs", [128, 8, 128])
    ws_sgt = dr("ws_sgt", [128, 8, 128])
    bspT = dr("bspT", [128, 8])
    sgu_g = dr("sgu_g", [1, 512])
    sgu_b = dr("sgu_b", [1, 512])
    ln1_g = dr("ln1_g", [1, D])
    ln1_b = dr("ln1_b", [1, D])
    ln2_g = dr("ln2_g", [1, D])
    ln2_b = dr("ln2_b", [1, D])
    w_r = dr("w_r", [D, 36])
    b_r = dr("b_r", [1, 36])
    w_gu = dr("w_gu", [NE, D, 512])
    w_dn = dr("w_dn", [NE, 256, D])
    out = nc.dram_tensor("out", [NB, S, D], F32, kind="ExternalOutput").ap()
    if DEBUG:
        dbg_cat = nc.dram_tensor("dbg_cat", [128, 8, S], F32, kind="ExternalOutput").ap()
        dbg_y = nc.dram_tensor("dbg_y", [128, NT, D], F32, kind="ExternalOutput").ap()
        dbg_gate = nc.dram_tensor("dbg_gate", [128, NT, 32], F32, kind="ExternalOutput").ap()

    PE, ACT, DVE, POOL, SP = nc.tensor, nc.scalar, nc.vector, nc.gpsimd, nc.sync
    engines = [PE, ACT, DVE, POOL, SP]

    with contextlib.suppress(_Stop), contextlib.ExitStack() as es:
        KB.sem_es = es
        kb = KB(nc, es)
        progs = [{id(getattr(e, "raw", e)): kb.sem(f"prog{bb}_{n}") for e, n in zip(engines, "pe act dve pool sp".split())} for bb in range(NB + 1)]
        kb.prog = progs[NB]
        M = kb.mark
        ACT, DVE, POOL = EngProxy(nc.scalar, kb), EngProxy(nc.vector, kb), EngProxy(nc.gpsimd, kb)
        engines = [PE, ACT, DVE, POOL, SP]
        banks = [kb.ps(f"bank{i}", [128, 512], F32) for i in range(8)]

        ident = kb.sb("ident", [128, 128], BF16)
        ones_bf = kb.sb("ones_bf", [128, 128], BF16)
        ones_z = kb.sb("ones_z", [128, 2, 128], BF16)
        zer = kb.sb("zer", [128, 512], F32)
        mhalf = kb.sb("mhalf", [128, 1], F32)
        m_cur = kb.sb("m_cur", [128, 512], BF16)
        m_prev = kb.sb("m_prev", [128, 512], BF16)
        m3 = kb.sb("m3", [128, 4, 512], BF16)
        wmT = kb.sb("wmT", [128, 8, 128], BF16)
        rs = kb.sb("rs", [128, 8], F32)
        bsp_sb = kb.sb("bsp_sb", [128, 8], F32)
        sg_bc = kb.sb("sg_bc", [128, 512], F32)
        sb_bc = kb.sb("sb_bc", [128, 512], F32)
        Bp = kb.sb("Bp", [128, 512], F32)
        br_bc = kb.sb("br_bc", [128, 36], F32)
        wr_hl = kb.sb("wr_hl", [128, 8, 72], BF16)
        invf = kb.sb("invf", [128, NT, 8], F32)
        catT = kb.sb("catT", [128, 8, S], BF16)

        s_c = kb.sem("const_dma")
        s_bar = kb.sem("barrier")

        def barrier():
            base = s_bar.n
            for e in engines:
                if isinstance(e, EngProxy) and e.last is not None:
                    kb.wait(e, e.last)
                kb.inc(e.nop(), s_bar)
            for e in engines:
                kb.wait(e, (s_bar, base + len(engines)))

        def stop_if(tag):
            if STOP_AFTER == tag:
                barrier()
                raise _Stop()

        def wait_all(tok):
            for e in engines:
                kb.wait(e, tok)

        with contextlib.ExitStack() as es0:
            k0 = KB(nc, es0, kb.waited, kb.prog)
            wtmp = k0.sb("wtmp", [128, 8, 128], F32)
            wtmp2 = k0.sb("wtmp2", [128, 8, 128], F32)
            wr_f = k0.sb("wr_f", [128, 8, 36], F32)
            wr_t = k0.sb("wr_t", [128, 8, 36], F32)

            def dma_c(o, i):
                kb.inc(SP.dma_start(out=o, in_=i), s_c, 16)

            dma_c(wtmp[:], ws_sgt)
            dma_c(wtmp2[:], ws_tgs)
            dma_c(bsp_sb[:], bspT)
            dma_c(sg_bc[:], sgu_g.partition_broadcast(128))
            dma_c(sb_bc[:], sgu_b.partition_broadcast(128))
            dma_c(br_bc[:], b_r.partition_broadcast(128))
            dma_c(wr_f[:], w_r.rearrange("(k p) c -> p k c", p=128))
            tok_cdma = (s_c, s_c.n)

            POOL.memset(zer[:], 0.0)
            POOL.memset(ones_bf[:], 1.0)
            POOL.memset(ones_z[:], 0.0)
            POOL.memset(ones_z[:, 0, 0:64], 1.0)
            POOL.memset(ones_z[:, 1, 64:128], 1.0)
            POOL.memset(mhalf[:], -0.5)
            POOL.affine_select(out=ident[:], in_=ones_bf[:], pattern=[[1, 128]], compare_op=ALU.is_equal,
                               fill=0.0, base=0, channel_multiplier=-1)
            POOL.affine_select(out=m_cur[:], in_=zer[:], pattern=[[0, 4], [1, 128]], compare_op=ALU.is_ge,
                               fill=NEG, base=0, channel_multiplier=-1)
            POOL.affine_select(out=m_prev[:], in_=zer[:], pattern=[[0, 4], [-1, 128]], compare_op=ALU.is_ge,
                               fill=NEG, base=0, channel_multiplier=1)
            for s in range(4):
                POOL.affine_select(out=m3[:, s, :], in_=zer[:], pattern=[[0, 16], [1, 32]], compare_op=ALU.is_ge,
                                   fill=NEG, base=32 * s, channel_multiplier=-1)
            inv = (np.float32(500000.0) ** (-(np.arange(0, 16, 2, dtype=np.float32)) / np.float32(16))).astype(np.float32)
            for i in range(8):
                POOL.memset(invf[:, :, i:i + 1], float(inv[i]))
            kb.wait(POOL, tok_cdma)
            POOL.affine_select(out=wmT[:], in_=wtmp[:], pattern=[[0, 8], [1, 128]], compare_op=ALU.is_ge,
                               fill=0.0, base=0, channel_multiplier=-1)
            i_last = POOL.affine_select(out=wtmp[:], in_=wtmp2[:], pattern=[[0, 8], [-1, 128]], compare_op=ALU.is_ge,
                                        fill=0.0, base=0, channel_multiplier=1)
            tok_cpool = M(POOL, i_last)

            kb.wait(DVE, tok_cdma)
            kb.wait(DVE, tok_cpool)
            DVE.tensor_reduce(out=rs[:], in_=wtmp[:], axis=AX.X, op=ALU.add)
            for g in range(8):
                DVE.tensor_scalar(out=Bp[:, g * 64:(g + 1) * 64], in0=sb_bc[:, g * 64:(g + 1) * 64],
                                  scalar1=rs[:, g:g + 1], scalar2=bsp_sb[:, g:g + 1], op0=ALU.mult, op1=ALU.add)
            DVE.tensor_copy(out=wr_hl[:, :, 0:36], in_=wr_f[:])
            DVE.tensor_copy(out=wr_t[:], in_=wr_hl[:, :, 0:36])
            DVE.tensor_tensor(out=wr_t[:], in0=wr_f[:], in1=wr_t[:], op=ALU.subtract)
            i_last = DVE.tensor_copy(out=wr_hl[:, :, 36:72], in_=wr_t[:])
            tok_cdve = M(DVE, i_last)
            wait_all(tok_cdma)
            wait_all(tok_cpool)
            wait_all(tok_cdve)
            barrier()
        stop_if("const")

        def rstd_via_pool(mv, dst, last_dve_instr):
            t = M(DVE, last_dve_instr)
            kb.wait(POOL, t)
            POOL.tensor_scalar(out=dst, in0=mv[:, 1:2], scalar1=EPS, scalar2=None, op0=ALU.add)
            i = POOL.tensor_tensor(out=dst, in0=dst, in1=mhalf[:], op=ALU.pow)
            kb.wait(DVE, M(POOL, i))

        ring_x = DmaRing(kb, "ring_x", 2)
        ring_w = DmaRing(kb, "ring_w", 2)
        ring_o = DmaRing(kb, "ring_o", 2)
        ring_m = DmaRing(kb, "ring_m", 4)
        ring_xs = DmaRing(kb, "ring_xs", 2)
        ring_ys = DmaRing(kb, "ring_ys", 2)
        ring_g = DmaRing(kb, "ring_g", 2)
        s_sc = kb.sem("scatter")
        NJ = 48
        xs_d = nc.dram_tensor("xs_scratch", [NJ * 256, D], BF16, kind="Internal").ap()
        ys_d = nc.dram_tensor("ys_scratch", [NJ * 256, D], F32, kind="Internal").ap()

        for b in range(NB):
            kb.prog = progs[b]
            ch_u = Chan(kb, "ch_u", 2)
            ch_v = Chan(kb, "ch_v", 2)
            ch_z = Chan(kb, "ch_z", 2)
            ch_tp = Chan(kb, "ch_tp", 2)
            ch_n = Chan(kb, "ch_n", 2)
            ch_gu = Chan(kb, "ch_gu", 2)
            ch_gv = Chan(kb, "ch_gv", 2)
            ch_sg = Chan(kb, "ch_sg", 2)
            ch_qk = Chan(kb, "ch_qk", 2)
            ch_rot = Chan(kb, "ch_rot", 2)
            ch_qs = Chan(kb, "ch_qs", 2)
            ch_vp = Chan(kb, "ch_vp", 2)
            ch_s = Chan(kb, "ch_s", 4)
            ch_p = Chan(kb, "ch_p", 6)
            ch_o = Chan(kb, "ch_o", 2)
            ch_op = Chan(kb, "ch_op", 2)
            ch_x = Chan(kb, "ch_x", 2)
            ch_hb = Chan(kb, "ch_hb", 2)
            ch_lo = Chan(kb, "ch_lo", 2)
            ch_ht = Chan(kb, "ch_ht", 2)
            ch_r = Chan(kb, "ch_r", 1)
            ch_g2 = Chan(kb, "ch_g2", 2, ncons=2)
            ch_sl = Chan(kb, "ch_sl", 2)
            ch_at = Chan(kb, "ch_at", 2)
            ch_dn = Chan(kb, "ch_dn", 1, ncons=2)
            ch_wt = Chan(kb, "ch_wt", 2)
            ch_ot = Chan(kb, "ch_ot", 2)
            ch_xs = Chan(kb, "ch_xs", 2)
            ch_xT = Chan(kb, "ch_xT", 2)
            ch_yo = Chan(kb, "ch_yo", 2)
            ch_rg = Chan(kb, "ch_rg", 2)

            with contextlib.ExitStack() as es1:
                k1 = KB(nc, es1, kb.waited, kb.prog)
                xT_sb = k1.sb(f"xT_sb{b}", [128, 8, S], BF16)
                wsec = k1.sb(f"wsec{b}", [128, 8, 1024], BF16)
                pos_i = k1.sb(f"pos_i{b}", [128, NT], I32)
                pos_f = k1.sb(f"pos_f{b}", [128, NT], F32)
                ang = k1.sb(f"ang{b}", [128, 2, NT, 8], F32)
                kk_i = k1.sb(f"kk_i{b}", [128, 2, NT, 8], I32)
                kk_f = k1.sb(f"kk_f{b}", [128, 2, NT, 8], F32)
                rr = k1.sb(f"rr{b}", [128, 2, NT, 8], F32)
                mm = k1.sb(f"mm{b}", [128, 2, NT, 8], F32)
                cs_t = k1.sb(f"cs_t{b}", [128, 2, NT, 8], F32)
                s_ld = k1.sem(f"a1_ld{b}")
                s_w = k1.sem(f"a1_w{b}")

                for k in range(8):
                    k1.inc(POOL.dma_start(out=xT_sb[:, k, :], in_=xT[b, k * 128:(k + 1) * 128, :]), s_ld, 16)
                tok_x = (s_ld, s_ld.n)
                s_pos = k1.sem(f"a1_pos{b}")
                tok_pos = k1.inc(SP.dma_start(out=pos_i[:], in_=posT[b]), s_pos, 16)
                k1.inc(POOL.dma_start(out=wsec[:], in_=w_in[:, 1536:2560].rearrange("(k p) c -> p k c", p=128)), s_w, 16)
                tok_w = (s_w, s_w.n)

                k1.wait(DVE, tok_pos)
                DVE.tensor_copy(out=pos_f[:], in_=pos_i[:])
                DVE.tensor_tensor(out=ang[:, 1], in0=invf[:], in1=pos_f[:].unsqueeze(2).broadcast_to([128, NT, 8]), op=ALU.mult)
                DVE.tensor_scalar(out=ang[:, 0], in0=ang[:, 1], scalar1=float(np.pi / 2), scalar2=None, op0=ALU.add)
                DVE.tensor_scalar(out=kk_f[:], in0=ang[:], scalar1=float(1.0 / TWO_PI), scalar2=None, op0=ALU.mult)
                DVE.tensor_copy(out=kk_i[:], in_=kk_f[:])
                DVE.tensor_copy(out=kk_f[:], in_=kk_i[:])
                DVE.scalar_tensor_tensor(out=rr[:], in0=kk_f[:], scalar=-TWO_PI, in1=ang[:], op0=ALU.mult, op1=ALU.add)
                DVE.tensor_scalar(out=mm[:], in0=rr[:], scalar1=float(np.pi), scalar2=None, op0=ALU.is_gt)
                DVE.scalar_tensor_tensor(out=rr[:], in0=mm[:], scalar=-TWO_PI, in1=rr[:], op0=ALU.mult, op1=ALU.add)
                DVE.tensor_scalar(out=mm[:], in0=rr[:], scalar1=float(-np.pi), scalar2=None, op0=ALU.is_lt)
                i_l = DVE.scalar_tensor_tensor(out=rr[:], in0=mm[:], scalar=TWO_PI, in1=rr[:], op0=ALU.mult, op1=ALU.add)
                k1.wait(ACT, M(DVE, i_l))
                i_l = ACT.activation(out=cs_t[:], in_=rr[:], func=AF.Sin)
                tok_cs = M(ACT, i_l)
                stop_if("rope")

                with contextlib.ExitStack() as es2:
                    k2 = KB(nc, es2, kb.waited, kb.prog)
                    gu_sb = [k2.sb(f"gu_sb{b}_{i}", [128, 512], F32) for i in range(2)]
                    gv_sb = [k2.sb(f"gv_sb{b}_{i}", [128, 512], F32) for i in range(2)]
                    n_sb = [k2.sb(f"n_sb{b}_{i}", [128, 512], BF16) for i in range(2)]
                    t1_sb = k2.sb(f"t1_sb{b}", [128, 512], F32)
                    sg_sb = [k2.sb(f"sg_sb{b}_{i}", [128, 512], BF16) for i in range(2)]
                    st_sb = k2.sb(f"st_sb{b}", [128, 6], F32)
                    mv_sb = k2.sb(f"mv_sb{b}", [128, 2], F32)
                    rstd_sb = k2.sb(f"rstd_sb{b}", [128, 1], F32)

                    k2.wait(PE, tok_x)
                    k2.wait(PE, tok_w)

                    def sgu_pe_front(n):
                        su = ch_u.pslot(PE)
                        for k in range(8):
                            i = PE.matmul(banks[0 + su][:], xT_sb[:, k, n * 128:(n + 1) * 128], wsec[:, k, 0:512],
                                          start=(k == 0), stop=(k == 7))
                        ch_u.produced(M(PE, i))
                        sv = ch_v.pslot(PE)
                        for k in range(8):
                            i = PE.matmul(banks[2 + sv][:], xT_sb[:, k, n * 128:(n + 1) * 128], wsec[:, k, 512:1024],
                                          start=(k == 0), stop=(k == 7))
                        ch_v.produced(M(PE, i))

                    def sgu_act(n):
                        su = ch_u.cslot(ACT)
                        sg = ch_gu.pslot(ACT)
                        t = M(ACT, ACT.activation(out=gu_sb[sg][:], in_=banks[0 + su][:], func=AF.Gelu))
                        ch_u.consumed(t)
                        ch_gu.produced(t)
                        sv = ch_v.cslot(ACT)
                        sg = ch_gv.pslot(ACT)
                        t = M(ACT, ACT.activation(out=gv_sb[sg][:], in_=banks[2 + sv][:], func=AF.Gelu))
                        ch_v.consumed(t)
                        ch_gv.produced(t)

                    def sgu_dve_norm(n):
                        sg = ch_gv.cslot(DVE)
                        DVE.bn_stats(out=st_sb[:], in_=gv_sb[sg][:])
                        i = DVE.bn_aggr(out=mv_sb[:], in_=st_sb[:])
                        rstd_via_pool(mv_sb, rstd_sb[:], i)
                        sn = ch_n.pslot(DVE)
                        t = M(DVE, DVE.tensor_scalar(out=n_sb[sn][:], in0=gv_sb[sg][:], scalar1=mv_sb[:, 0:1], scalar2=rstd_sb[:, 0:1],
                                                     op0=ALU.subtract, op1=ALU.mult))
                        ch_gv.consumed(t)
                        ch_n.produced(t)

                    def sgu_pe_z(n):
                        sn = ch_n.cslot(PE)
                        sz = ch_z.pslot(PE)
                        for g in range(8):
                            i = PE.matmul(banks[4 + sz][:, g * 64:(g + 1) * 64], wmT[:, g, :], n_sb[sn][:, g * 64:(g + 1) * 64],
                                          start=True, stop=True, skip_group_check=True)
                        t = M(PE, i)
                        ch_n.consumed(t)
                        ch_z.produced(t)

                    def sgu_dve_out(n):
                        sz = ch_z.cslot(DVE)
                        t = M(DVE, DVE.tensor_tensor(out=t1_sb[:], in0=banks[4 + sz][:], in1=sg_bc[:], op=ALU.mult))
                        ch_z.consumed(t)
                        DVE.tensor_tensor(out=t1_sb[:], in0=t1_sb[:], in1=Bp[:], op=ALU.add)
                        sgu_ = ch_gu.cslot(DVE)
                        so = ch_sg.pslot(DVE)
                        t = M(DVE, DVE.tensor_tensor(out=sg_sb[so][:], in0=t1_sb[:], in1=gu_sb[sgu_][:], op=ALU.mult))
                        ch_gu.consumed(t)
                        ch_sg.produced(t)

                    def sgu_pe_tp(n):
                        so = ch_sg.cslot(PE)
                        st = ch_tp.pslot(PE)
                        tpv = banks[6 + st][:].bitcast(BF16)
                        for j in range(4):
                            i = PE.transpose(tpv[:, j * 128:(j + 1) * 128], sg_sb[so][:, j * 128:(j + 1) * 128], ident[:])
                        t = M(PE, i)
                        ch_sg.consumed(t)
                        ch_tp.produced(t)

                    def sgu_act_tp(n):
                        st = ch_tp.cslot(ACT)
                        tpv = banks[6 + st][:].bitcast(BF16)
                        i = ACT.activation(out=catT[:, 4:8, n * 128:(n + 1) * 128],
                                           in_=tpv[:, 0:512].rearrange("p (j t) -> p j t", j=4), func=AF.Copy)
                        ch_tp.consumed(M(ACT, i))

                    for n in range(NT + 2):
                        if n < NT:
                            sgu_pe_front(n)
                            sgu_act(n)
                            sgu_dve_norm(n)
                        if 1 <= n <= NT:
                            sgu_pe_z(n - 1)
                            sgu_dve_out(n - 1)
                        if 2 <= n:
                            sgu_pe_tp(n - 2)
                            sgu_act_tp(n - 2)
                    barrier()
                stop_if("sgu")

                with contextlib.ExitStack() as es3:
                    k3 = KB(nc, es3, kb.waited, kb.prog)
                    QTz = k3.sb(f"QTz{b}", [128, 2, S], BF16)
                    KT = k3.sb(f"KT{b}", [128, S], BF16)
                    Vz = k3.sb(f"Vz{b}", [128, 48, 2, 128], BF16)
                    qs_sb = [k3.sb(f"qs_sb{b}_{i}", [128, 4, 256], BF16) for i in range(2)]
                    ra = k3.sb(f"ra{b}", [128, 4, 4, 8], F32)
                    rb = k3.sb(f"rb{b}", [128, 4, 4, 8], F32)
                    rot_sb = [k3.sb(f"rot_sb{b}_{i}", [128, 4, 2, 2, 16], F32) for i in range(2)]
                    p_sb = [k3.sb(f"p_sb{b}_{i}", [128, 512], BF16) for i in range(6)]
                    rl_sb = k3.sb(f"rl_sb{b}", [128, 512], F32)
                    s_ws = k3.sem(f"a1_ws{b}")

                    POOL.memset(QTz[:], 0.0)
                    tok_zero = M(POOL, POOL.memset(Vz[:], 0.0))
                    for e in (ACT, DVE, PE):
                        k3.wait(e, tok_zero)
                    stop_if("att_ms")

                    def tv_blk(T, j):
                        return T[:, j * 128:(j + 1) * 128]

                    def tv_p2(T, s, r4):
                        return T[:, 512 * s:512 * (s + 1)].rearrange("p (i r) -> p r i", r=4)[:, r4, :]

                    def tv_p3k(T, r):
                        return T.rearrange("p (l r) -> p r l", r=16)[:, r, :]

                    def tv_p3q(T, s, r):
                        return T.rearrange("p (l r) -> p r l", r=16)[:, r, 32 * s:32 * (s + 1)]

                    vdefs = []
                    for j in range(16):
                        vdefs.append(lambda T, j=j: tv_blk(T, j))
                    for s in range(4):
                        for r4 in range(4):
                            vdefs.append(lambda T, s=s, r4=r4: tv_p2(T, s, r4))
                    for r in range(16):
                        vdefs.append(lambda T, r=r: tv_p3k(T, r))

                    for c in range(4):
                        for j, c0 in enumerate((c * 128, 512 + c * 128, 1024 + c * 128)):
                            k3.inc(POOL.dma_start(out=wsec[:, :, j * 128:(j + 1) * 128],
                                                  in_=w_in[:, c0:c0 + 128].rearrange("(k p) c -> p k c", p=128)), s_ws, 16)
                        k3.wait(PE, (s_ws, s_ws.n))
                        k3.wait(DVE, tok_cs)
                        stop_if("att_dma")

                        def qk_pe(gi):
                            sq = ch_qk.pslot(PE)
                            for w in range(2):
                                for tt in range(4):
                                    n = gi * 4 + tt
                                    for k in range(8):
                                        i = PE.matmul(banks[2 * sq + w][:, tt * 128:(tt + 1) * 128],
                                                      xT_sb[:, k, n * 128:(n + 1) * 128], wsec[:, k, w * 128:(w + 1) * 128],
                                                      start=(tt == 0 and k == 0), stop=(k == 7), skip_group_check=True)
                            ch_qk.produced(M(PE, i))

                        def qk_evac(gi):
                            sq = ch_qk.cslot(ACT, 0)
                            so = ch_qs.pslot(ACT, DVE)
                            sr = ch_rot.pslot(ACT)
                            qv = banks[2 * sq + 0][:].rearrange("p (t h e) -> p t h e", t=4, h=2)
                            kv = banks[2 * sq + 1][:].rearrange("p (t h e) -> p t h e", t=4, h=2)
                            ov = qs_sb[so][:].rearrange("p t (w h e) -> p t w h e", w=2, h=2)
                            ACT.activation(out=ov[:, :, 0, :, 16:64], in_=qv[:, :, :, 16:64], func=AF.Copy)
                            ACT.activation(out=ov[:, :, 1, :, 16:64], in_=kv[:, :, :, 16:64], func=AF.Copy)
                            ACT.activation(out=rot_sb[sr][:, :, 0, :, :], in_=qv[:, :, :, 0:16], func=AF.Copy)
                            ta = M(ACT, ACT.activation(out=rot_sb[sr][:, :, 1, :, :], in_=kv[:, :, :, 0:16], func=AF.Copy))
                            ch_qk.consumed(ta, 0)
                            ch_rot.produced(ta)
                            sr = ch_rot.cslot(DVE)
                            rv = rot_sb[sr][:].rearrange("p t w h e -> p t (w h) e")
                            t1 = rv[:, :, :, 0:8]
                            t2 = rv[:, :, :, 8:16]
                            og = qs_sb[so][:].rearrange("p t (g e) -> p t g e", g=4)
                            cosb = cs_t[:, 0, gi * 4:(gi + 1) * 4, :].unsqueeze(2).broadcast_to([128, 4, 4, 8])
                            sinb = cs_t[:, 1, gi * 4:(gi + 1) * 4, :].unsqueeze(2).broadcast_to([128, 4, 4, 8])
                            DVE.tensor_tensor(out=ra[:], in0=t1, in1=cosb, op=ALU.mult)
                            DVE.tensor_tensor(out=rb[:], in0=t2, in1=sinb, op=ALU.mult)
                            DVE.tensor_tensor(out=og[:, :, :, 0:8], in0=ra[:], in1=rb[:], op=ALU.subtract)
                            DVE.tensor_tensor(out=ra[:], in0=t2, in1=cosb, op=ALU.mult)
                            DVE.tensor_tensor(out=rb[:], in0=t1, in1=sinb, op=ALU.mult)
                            td = M(DVE, DVE.tensor_tensor(out=og[:, :, :, 8:16], in0=ra[:], in1=rb[:], op=ALU.add))
                            ch_rot.consumed(td)
                            ch_qs.produced(ta, td)

                        def qk_tp(gi):
                            so = ch_qs.cslot(PE)
                            st = ch_tp.pslot(PE)
                            tpv = banks[6 + st][:].bitcast(BF16).rearrange("p (t w e) -> p t w e", t=4, w=2)
                            for tt in range(4):
                                for w in range(2):
                                    i = PE.transpose(tpv[:, tt, w, :], qs_sb[so][:, tt, w * 128:(w + 1) * 128], ident[:])
                            t = M(PE, i)
                            ch_qs.consumed(t)
                            ch_tp.produced(t)

                        def qk_tp_evac(gi):
                            st = ch_tp.cslot(ACT)
                            tpv = banks[6 + st][:].bitcast(BF16).rearrange("p (t w e) -> p t w e", t=4, w=2)
                            cols = slice(gi * 512, (gi + 1) * 512)
                            ACT.activation(out=QTz[0:64, 0, cols].rearrange("p (t e) -> p t e", t=4), in_=tpv[0:64, :, 0, :], func=AF.Copy)
                            ACT.activation(out=QTz[64:128, 1, cols].rearrange("p (t e) -> p t e", t=4), in_=tpv[64:128, :, 0, :], func=AF.Copy)
                            i = ACT.activation(out=KT[:, cols].rearrange("p (t e) -> p t e", t=4), in_=tpv[:, :, 1, :], func=AF.Copy)
                            ch_tp.consumed(M(ACT, i))

                        for gi in range(4 + 2):
                            if gi < 4:
                                qk_pe(gi)
                                stop_if("qk_pe0")
                                qk_evac(gi)
                                stop_if("qk_ev0")
                            if 1 <= gi <= 4:
                                qk_tp(gi - 1)
                                stop_if("qk_tp0")
                            if 2 <= gi:
                                qk_tp_evac(gi - 2)
                                stop_if("qk_te0")
                        stop_if("qk")

                        for gi in range(12):
                            sv = ch_vp.pslot(PE)
                            for tt in range(4):
                                vd = vdefs[gi * 4 + tt]
                                for k in range(8):
                                    i = PE.matmul(banks[4 + sv][:, tt * 128:(tt + 1) * 128], vd(xT_sb[:, k, :]), wsec[:, k, 256:384],
                                                  start=(tt == 0 and k == 0), stop=(k == 7), skip_group_check=True)
                            ch_vp.produced(M(PE, i))
                            eng = ACT if gi % 2 == 0 else DVE
                            sv = ch_vp.cslot(eng)
                            src = banks[4 + sv][:].rearrange("p (t h e) -> p t h e", t=4, h=2)
                            dst = Vz[:, gi * 4:(gi + 1) * 4, :, :]
                            if eng is ACT:
                                ACT.activation(out=dst[:, :, 0, 0:64], in_=src[:, :, 0, :], func=AF.Copy)
                                i = ACT.activation(out=dst[:, :, 1, 64:128], in_=src[:, :, 1, :], func=AF.Copy)
                            else:
                                DVE.tensor_copy(out=dst[:, :, 0, 0:64], in_=src[:, :, 0, :])
                                i = DVE.tensor_copy(out=dst[:, :, 1, 64:128], in_=src[:, :, 1, :])
                            ch_vp.consumed(M(eng, i))
                        barrier()
                        stop_if("v")

                        V1 = lambda j, hh: Vz[:, j, hh, :]
                        V2 = lambda s, r4, hh: Vz[:, 16 + 4 * s + r4, hh, :]
                        V3 = lambda r, hh: Vz[:, 32 + r, hh, :]

                        def emit_s(maskt, mms, lo=0, hi=512):
                            ss = ch_s.pslot(PE)
                            PE.matmul(banks[ss][:], ident[:], maskt, start=True, stop=False, skip_group_check=True)
                            for (oc, lhsT, rhs) in mms:
                                i = PE.matmul(banks[ss][:, oc[0]:oc[1]], lhsT, rhs, start=False, stop=True, skip_group_check=True)
                            ch_s.produced(M(PE, i))
                            ss2 = ch_s.cslot(ACT)
                            sp = ch_p.pslot(ACT)
                            t = M(ACT, ACT.activation(out=p_sb[sp][:, lo:hi], in_=banks[ss2][:, lo:hi], func=AF.Exp, scale=0.125))
                            ch_s.consumed(t)
                            ch_p.produced(t)
                            return sp

                        def oview(Bk, oc):
                            if oc[0] == "blk":
                                return Bk[:, oc[1] * 128:(oc[1] + 1) * 128]
                            if oc[0] == "p2":
                                return Bk[:].rearrange("p (i r) -> p r i", r=4)[:, oc[1], :]
                            return Bk[:].rearrange("p (i r) -> p r i", r=16)[:, oc[1], :]

                        for s in range(4):
                            so_ = ch_o.pslot(PE)
                            Ob = banks[4 + 2 * so_]
                            Lb = banks[5 + 2 * so_]
                            first_pv = True
                            for hh in range(2):
                                Q = QTz[:, hh, :]
                                pend = []
                                mms = [((j * 128, (j + 1) * 128), tv_blk(KT, 4 * s + j), tv_blk(Q, 4 * s + j)) for j in range(4)]
                                sp = emit_s(m_cur[:], mms)
                                pend.append((sp, [(V1(4 * s + j, hh), (j * 128, (j + 1) * 128), ("blk", j)) for j in range(4)]))
                                js = [j for j in range(4) if 4 * s + j >= 1]
                                mms = [((j * 128, (j + 1) * 128), tv_blk(KT, 4 * s + j - 1), tv_blk(Q, 4 * s + j)) for j in js]
                                sp = emit_s(m_prev[:], mms, lo=js[0] * 128)
                                pend.append((sp, [(V1(4 * s + j - 1, hh), (j * 128, (j + 1) * 128), ("blk", j)) for j in js]))
                                mms = [((r4 * 128, (r4 + 1) * 128), tv_p2(KT, s, r4), tv_p2(Q, s, r4)) for r4 in range(4)]
                                sp = emit_s(m_cur[:], mms)
                                pend.append((sp, [(V2(s, r4, hh), (r4 * 128, (r4 + 1) * 128), ("p2", r4)) for r4 in range(4)]))
                                if s >= 1:
                                    mms = [((r4 * 128, (r4 + 1) * 128), tv_p2(KT, s - 1, r4), tv_p2(Q, s, r4)) for r4 in range(4)]
                                    sp = emit_s(m_prev[:], mms)
                                    pend.append((sp, [(V2(s - 1, r4, hh), (r4 * 128, (r4 + 1) * 128), ("p2", r4)) for r4 in range(4)]))
                                mms = [((r * 32, (r + 1) * 32), tv_p3k(KT, r), tv_p3q(Q, s, r)) for r in range(16)]
                                sp = emit_s(m3[:, s, :], mms)
                                pend.append((sp, [(V3(r, hh), (r * 32, (r + 1) * 32), ("p3", r)) for r in range(16)]))

                                for (sp, pvs) in pend:
                                    sp2 = ch_p.cslot(PE)
                                    assert sp2 == sp
                                    for (vt, pc, oc) in pvs:
                                        PE.matmul(oview(Ob, oc), vt, p_sb[sp][:, pc[0]:pc[1]], start=first_pv, stop=False, skip_group_check=True)
                                        i = PE.matmul(oview(Lb, oc), ones_z[:, hh, :], p_sb[sp][:, pc[0]:pc[1]], start=first_pv, stop=False,
                                                      skip_group_check=True)
                                        first_pv = False
                                    t_last = M(PE, i)
                                    ch_p.consumed(t_last)
                            ch_o.produced(t_last)
                            so2 = ch_o.cslot(DVE)
                            DVE.reciprocal(out=rl_sb[:], in_=banks[5 + 2 * so2][:])
                            i = DVE.tensor_tensor(out=catT[:, c, 512 * s:512 * (s + 1)], in0=banks[4 + 2 * so2][:], in1=rl_sb[:], op=ALU.mult)
                            ch_o.consumed(M(DVE, i))
                            stop_if("attn_s0")
                        barrier()
                        if DEBUG and b == DEBUG_B and STOP_AFTER == "attn":
                            with contextlib.ExitStack() as esd:
                                kd = KB(nc, esd, kb.waited, kb.prog)
                                dtmp = kd.sb("dtmpa", [128, 4, S], F32)
                                sd = kd.sem("dbga")
                                kd.wait(SP, M(DVE, DVE.tensor_copy(out=dtmp[:], in_=catT[:, 0:4, :])))
                                kd.wait(SP, kd.inc(SP.dma_start(out=dbg_cat[:, 0:4, :], in_=dtmp[:]), sd, 16))
                            stop_if("attn")

                if DEBUG and b == DEBUG_B:
                    with contextlib.ExitStack() as esd:
                        kd = KB(nc, esd, kb.waited, kb.prog)
                        dtmp = kd.sb("dtmp", [128, 8, S], F32)
                        sd = kd.sem("dbg1")
                        kd.wait(SP, M(DVE, DVE.tensor_copy(out=dtmp[:], in_=catT[:])))
                        kd.wait(SP, kd.inc(SP.dma_start(out=dbg_cat, in_=dtmp[:]), sd, 16))
                        barrier()

            esp = contextlib.ExitStack()
            kp = KB(nc, esp, kb.waited, kb.prog)
            hb_all = kp.sb(f"hb_all{b}", [128, NT, D], BF16)
            ybuf = kp.sb(f"ybuf{b}", [128, NT, D], F32)
            M1a = kp.sb(f"M1a{b}", [128, NT, 32], F32)
            M2a = kp.sb(f"M2a{b}", [128, NT, 32], F32)
            g12 = kp.sb(f"g12{b}", [128, 2, NT], F32)
            with contextlib.ExitStack() as es4:
                k4 = KB(nc, es4, kb.waited, kb.prog)
                wo_sb = k4.sb(f"wo_sb{b}", [128, 8, D], BF16)
                x_sb = [k4.sb(f"x_sb{b}_{i}", [128, D], F32) for i in range(2)]
                s_sb = k4.sb(f"s_sb{b}", [128, D], F32)
                hT_sb = [k4.sb(f"hT_sb{b}_{i}", [128, 8, 128], BF16) for i in range(2)]
                hf_sb = k4.sb(f"hf_sb{b}", [128, D], F32)
                lo_sb = [k4.sb(f"lo_sb{b}_{i}", [128, D], BF16) for i in range(2)]
                loT_sb = [k4.sb(f"loT_sb{b}_{i}", [128, 8, 128], BF16) for i in range(2)]
                st2 = k4.sb(f"st2_{b}", [128, 2, 6], F32)
                mv2 = k4.sb(f"mv2_{b}", [128, 2], F32)
                rstd2 = k4.sb(f"rstd2_{b}", [128, 1], F32)
                lg72 = k4.sb(f"lg72_{b}", [128, 72], F32)
                lg = k4.sb(f"lg_{b}", [128, 36], F32)
                rt = k4.sb(f"rt_{b}", [128, 64], F32)
                s_wo = k4.sem(f"a2_wo{b}")
                g1_bc = k4.sb(f"g1_bc{b}", [128, D], F32)
                b1_bc = k4.sb(f"b1_bc{b}", [128, D], F32)
                s_g1 = k4.sem(f"a2_g1{b}")
                k4.inc(SP.dma_start(out=g1_bc[:], in_=ln1_g.partition_broadcast(128)), s_g1, 16)
                k4.wait(DVE, k4.inc(SP.dma_start(out=b1_bc[:], in_=ln1_b.partition_broadcast(128)), s_g1, 16))

                t_wo = k4.inc(POOL.dma_start(out=wo_sb[:], in_=w_out.rearrange("(k p) c -> p k c", p=128)), s_wo, 16)
                k4.wait(PE, t_wo)
                k4.wait(DVE, t_wo)

                def a2_load(n):
                    sx = ch_x.pslot(SP)
                    i = SP.dma_start(out=x_sb[sx][:], in_=xtm[b, n * 128:(n + 1) * 128, :])
                    ch_x.produced(ring_x.start(i, sx))

                def a2_pe_op(n):
                    so = ch_op.pslot(PE)
                    for hf in range(2):
                        for k in range(8):
                            i = PE.matmul(banks[2 * so + hf][:], catT[:, k, n * 128:(n + 1) * 128], wo_sb[:, k, hf * 512:(hf + 1) * 512],
                                          start=(k == 0), stop=(k == 7))
                    ch_op.produced(M(PE, i))

                def a2_dve_ln(n):
                    so = ch_op.cslot(DVE)
                    sx = ch_x.cslot(DVE)
                    for hf in range(2):
                        i = DVE.scalar_tensor_tensor(out=s_sb[:, hf * 512:(hf + 1) * 512], in0=x_sb[sx][:, hf * 512:(hf + 1) * 512],
                                                     scalar=ALPHA, in1=banks[2 * so + hf][:], op0=ALU.mult, op1=ALU.add)
                    t = M(DVE, i)
                    ch_op.consumed(t)
                    ch_x.consumed(t)
                    for hf in range(2):
                        DVE.bn_stats(out=st2[:, hf, :], in_=s_sb[:, hf * 512:(hf + 1) * 512])
                    i = DVE.bn_aggr(out=mv2[:], in_=st2[:].rearrange("p a c -> p (a c)"))
                    rstd_via_pool(mv2, rstd2[:], i)
                    DVE.tensor_scalar(out=hf_sb[:], in0=s_sb[:], scalar1=mv2[:, 0:1], scalar2=rstd2[:, 0:1],
                                      op0=ALU.subtract, op1=ALU.mult)
                    DVE.tensor_tensor(out=hf_sb[:], in0=hf_sb[:], in1=g1_bc[:], op=ALU.mult)
                    DVE.tensor_tensor(out=hf_sb[:], in0=hf_sb[:], in1=b1_bc[:], op=ALU.add)
                    sh = ch_hb.pslot(DVE)
                    DVE.tensor_copy(out=hb_all[:, n, :], in_=hf_sb[:])
                    DVE.tensor_tensor(out=lo_sb[sh][:], in0=hf_sb[:], in1=hb_all[:, n, :], op=ALU.subtract)
                    i = DVE.tensor_scalar(out=ybuf[:, n, :], in0=hf_sb[:], scalar1=ALPHA, scalar2=None, op0=ALU.mult)
                    ch_hb.produced(M(DVE, i))

                def a2_pe_tp(n):
                    sh = ch_hb.cslot(PE)
                    st = ch_tp.pslot(PE)
                    tpv = banks[6 + st][:].bitcast(BF16)
                    for k in range(8):
                        i = PE.transpose(tpv[:, k * 128:(k + 1) * 128], hb_all[:, n, k * 128:(k + 1) * 128], ident[:])
                    ch_tp.produced(M(PE, i))
                    st = ch_tp.pslot(PE)
                    tpv = banks[6 + st][:].bitcast(BF16)
                    for k in range(8):
                        i = PE.transpose(tpv[:, k * 128:(k + 1) * 128], lo_sb[sh][:, k * 128:(k + 1) * 128], ident[:])
                    t = M(PE, i)
                    ch_hb.consumed(t)
                    ch_tp.produced(t)

                def a2_act_tp(n):
                    st = ch_tp.cslot(ACT)
                    tpv = banks[6 + st][:].bitcast(BF16)
                    sht = ch_ht.pslot(ACT)
                    i = ACT.activation(out=hT_sb[sht][:], in_=tpv.rearrange("p (k t) -> p k t", k=8), func=AF.Copy)
                    t_h = M(ACT, i)
                    ch_tp.consumed(t_h)
                    ch_ht.produced(t_h)
                    st = ch_tp.cslot(ACT)
                    tpv = banks[6 + st][:].bitcast(BF16)
                    sl = ch_lo.pslot(ACT)
                    i = ACT.activation(out=loT_sb[sl][:], in_=tpv.rearrange("p (k t) -> p k t", k=8), func=AF.Copy)
                    t = M(ACT, i)
                    ch_tp.consumed(t)
                    ch_lo.produced(t)
                    return t_h

                def a2_pe_route(n, t_h):
                    sl = ch_lo.cslot(PE)
                    sht = ch_ht.cslot(PE)
                    ch_r.pslot(PE)
                    rp = banks[4]
                    for k in range(8):
                        PE.matmul(rp[:, 0:72], hT_sb[sht][:, k, :], wr_hl[:, k, 0:72], start=(k == 0), stop=False,
                                  skip_group_check=True)
                    for k in range(8):
                        i = PE.matmul(rp[:, 0:36], loT_sb[sl][:, k, :], wr_hl[:, k, 0:36], start=False, stop=(k == 7),
                                      skip_group_check=True)
                    t = M(PE, i)
                    ch_lo.consumed(t)
                    ch_ht.consumed(t)
                    ch_r.produced(t)

                def a2_route_math(n):
                    ch_r.cslot(ACT)
                    t = M(ACT, ACT.activation(out=lg72[:], in_=banks[4][:, 0:72], func=AF.Copy))
                    ch_r.consumed(t)
                    kb.wait(DVE, t)
                    DVE.tensor_tensor(out=lg[:], in0=lg72[:, 0:36], in1=lg72[:, 36:72], op=ALU.add)
                    DVE.tensor_tensor(out=lg[:], in0=lg[:], in1=br_bc[:], op=ALU.add)
                    gmax, oh, ge, sume, psel = rt[:, 0:1], rt[:, 1:5], rt[:, 5:9], rt[:, 9:10], rt[:, 10:11]
                    esel, m1, eq1, e2, m2, eq2 = rt[:, 11:19], rt[:, 19:20], rt[:, 20:28], rt[:, 28:36], rt[:, 36:37], rt[:, 37:45]
                    dd, w1, w2, eg = rt[:, 45:46], rt[:, 46:47], rt[:, 47:48], rt[:, 48:56]
                    DVE.tensor_reduce(out=gmax, in_=lg[:, 0:4], axis=AX.X, op=ALU.max)
                    DVE.tensor_scalar(out=oh, in0=lg[:, 0:4], scalar1=gmax, scalar2=None, op0=ALU.is_equal)
                    DVE.tensor_scalar(out=ge, in0=lg[:, 0:4], scalar1=gmax, scalar2=None, op0=ALU.subtract)
                    DVE.tensor_scalar(out=esel, in0=lg[:, 4:12], scalar1=oh[:, 0:1], scalar2=None, op0=ALU.mult)
                    for g in range(1, 4):
                        DVE.scalar_tensor_tensor(out=esel, in0=lg[:, 4 + 8 * g:12 + 8 * g], scalar=oh[:, g:g + 1], in1=esel,
                                                 op0=ALU.mult, op1=ALU.add)
                    DVE.tensor_reduce(out=m1, in_=esel, axis=AX.X, op=ALU.max)
                    DVE.tensor_scalar(out=eq1, in0=esel, scalar1=m1, scalar2=None, op0=ALU.is_equal)
                    DVE.scalar_tensor_tensor(out=e2, in0=eq1, scalar=-1e30, in1=esel, op0=ALU.mult, op1=ALU.add)
                    DVE.tensor_reduce(out=m2, in_=e2, axis=AX.X, op=ALU.max)
                    DVE.tensor_scalar(out=eq2, in0=e2, scalar1=m2, scalar2=None, op0=ALU.is_equal)
                    i = DVE.tensor_tensor(out=dd, in0=m2, in1=m1, op=ALU.subtract)
                    kb.wait(ACT, M(DVE, i))
                    ACT.activation(out=ge, in_=ge, func=AF.Exp)
                    i = ACT.activation(out=dd, in_=dd, func=AF.Exp)
                    kb.wait(DVE, M(ACT, i))
                    DVE.tensor_reduce(out=sume, in_=ge, axis=AX.X, op=ALU.add)
                    DVE.reciprocal(out=psel, in_=sume)
                    DVE.tensor_scalar(out=w1, in0=dd, scalar1=1.0, scalar2=None, op0=ALU.add)
                    DVE.reciprocal(out=w1, in_=w1)
                    DVE.tensor_tensor(out=w2, in0=dd, in1=w1, op=ALU.mult)
                    DVE.tensor_tensor(out=w1, in0=w1, in1=psel, op=ALU.mult)
                    DVE.tensor_tensor(out=w2, in0=w2, in1=psel, op=ALU.mult)
                    for g in range(4):
                        DVE.tensor_scalar(out=M1a[:, n, g * 8:(g + 1) * 8], in0=eq1, scalar1=oh[:, g:g + 1], scalar2=None, op0=ALU.mult)
                        DVE.tensor_scalar(out=M2a[:, n, g * 8:(g + 1) * 8], in0=eq2, scalar1=oh[:, g:g + 1], scalar2=None, op0=ALU.mult)
                    DVE.tensor_copy(out=g12[:, 0, n:n + 1], in_=w1)
                    DVE.tensor_copy(out=g12[:, 1, n:n + 1], in_=w2)

                a2_load(0)
                t_hs = {}
                for n in range(NT + 2):
                    if n + 1 < NT:
                        a2_load(n + 1)
                    if n < NT:
                        a2_pe_op(n)
                        a2_dve_ln(n)
                    if 1 <= n <= NT:
                        a2_pe_tp(n - 1)
                        t_hs[n - 1] = a2_act_tp(n - 1)
                    if 2 <= n:
                        a2_pe_route(n - 2, t_hs[n - 2])
                        a2_route_math(n - 2)
                barrier()

            if DEBUG and b == DEBUG_B:
                with contextlib.ExitStack() as esd:
                    kd = KB(nc, esd, kb.waited, kb.prog)
                    sd = kd.sem("dbg2")
                    kd.inc(SP.dma_start(out=dbg_y, in_=ybuf[:]), sd, 16)
                    kd.wait(SP, kd.inc(SP.dma_start(out=dbg_gate, in_=M1a[:]), sd, 16))
                    barrier()
            if not DEBUG or b == DEBUG_B:
                stop_if("A2")

            with contextlib.ExitStack() as es5:
                k5 = KB(nc, es5, kb.waited, kb.prog)
                wgu_sb = [catT[:, 2 * i:2 * i + 2, :].rearrange("p a (k f) -> p (a k) f", f=512) for i in range(2)]
                wdn_t = [k5.sb(f"wdn_t{b}_{i}", [128, 2, D], BF16) for i in range(2)]
                wdn_sb = [t_[:] for t_ in wdn_t]
                thr = k5.sb(f"thr{b}", [128, NJ, 32], F32)
                Mb = k5.sb(f"Mb{b}", [128, NT * 32], BF16)
                Ms = k5.sb(f"Ms{b}", [128, NT, 32], F32)
                cs = k5.sb(f"cs{b}", [128, NT, 32], F32)
                off = k5.sb(f"off{b}", [128, NT, 32], F32)
                Sf = k5.sb(f"Sf{b}", [128, NT, 32], F32)
                tS = Ms
                sc_a = k5.sb(f"sc_a{b}", [128, 32], F32)
                sc_b = k5.sb(f"sc_b{b}", [128, 32], F32)
                pt = k5.sb(f"pt{b}", [128, 32], F32)
                base = k5.sb(f"base{b}", [128, 32], F32)
                qi = k5.sb(f"qi{b}", [128, 32], I32)
                sl_f = k5.sb(f"sl_f{b}", [128, 2, NT], F32)
                sl_i = k5.sb(f"sl_i{b}", [128, 2, NT], I32)
                ej_f = k5.sb(f"ej_f{b}", [128, NJ], F32)
                wi_f = thr[:].rearrange("p j e -> p (j e)")[:, 0:NJ * 5].rearrange("p (j c) -> p j c", c=5)
                wi_i = k5.sb(f"wi_i{b}", [128, NJ, 5], I32)
                pidx = k5.sb(f"pidx{b}", [128, 1], F32)
                ko = k5.sb(f"ko{b}", [128, 8], F32)
                Lst = k5.sb(f"Lst{b}", [128, 128], BF16)
                xt_sb = [k5.sb(f"xt_sb{b}_{i}", [128, 2, D], BF16) for i in range(2)]
                xsT = [k5.sb(f"xsT{b}_{i}", [128, 8, 256], BF16) for i in range(2)]
                sl_sb = [k5.sb(f"sl_sb{b}_{i}", [128, 512], F32) for i in range(2)]
                at_sb = [k5.sb(f"at_sb{b}_{i}", [128, 2, 256], BF16) for i in range(2)]
                yo_sb = [k5.sb(f"yo_sb{b}_{i}", [128, D], F32) for i in range(2)]
                r_sb = yo_sb

                POOL.affine_select(out=Lst[:], in_=ones_bf[:], pattern=[[1, 128]], compare_op=ALU.is_gt,
                                   fill=0.0, base=0, channel_multiplier=-1)
                t_thr = M(POOL, POOL.iota(thr[:], pattern=[[256, NJ], [0, 32]], base=0, channel_multiplier=0,
                                          allow_small_or_imprecise_dtypes=True))
                POOL.iota(pidx[:], pattern=[[0, 1]], base=0, channel_multiplier=1, allow_small_or_imprecise_dtypes=True)
                t_io = M(POOL, POOL.iota(ko[:], pattern=[[128, 8]], base=0, channel_multiplier=0, allow_small_or_imprecise_dtypes=True))
                DVE.tensor_tensor(out=Ms[:], in0=M1a[:], in1=M2a[:], op=ALU.add)
                t = M(DVE, DVE.tensor_copy(out=Mb[:], in_=Ms[:].rearrange("p n e -> p (n e)")))
                kb.wait(PE, t)
                kb.wait(PE, t_thr)
                PE.matmul(banks[0][:], Lst[:], Mb[:], start=True, stop=True)
                t = M(PE, PE.matmul(banks[1][:], ones_bf[:], Mb[:], start=True, stop=True))
                kb.wait(DVE, t)
                DVE.tensor_copy(out=cs[:], in_=banks[1][:].rearrange("p (n e) -> p n e", e=32))
                DVE.memset(off[:, 0, :], 0.0)
                for n in range(1, NT):
                    DVE.tensor_tensor(out=off[:, n, :], in0=off[:, n - 1, :], in1=cs[:, n - 1, :], op=ALU.add)
                DVE.tensor_tensor(out=sc_a[:], in0=off[:, NT - 1, :], in1=cs[:, NT - 1, :], op=ALU.add)
                DVE.tensor_scalar(out=sc_b[:], in0=sc_a[:], scalar1=127.5, scalar2=1.0 / 256.0, op0=ALU.add, op1=ALU.mult)
                DVE.tensor_copy(out=qi[:], in_=sc_b[:])
                DVE.tensor_scalar(out=pt[:], in0=qi[:], scalar1=256.0, scalar2=None, op0=ALU.mult)
                DVE.tensor_copy(out=sc_a[:], in_=pt[:])
                pa, pb = sc_a, sc_b
                for sh in (1, 2, 4, 8, 16):
                    DVE.tensor_copy(out=pb[:, 0:sh], in_=pa[:, 0:sh])
                    DVE.tensor_tensor(out=pb[:, sh:32], in0=pa[:, sh:32], in1=pa[:, 0:32 - sh], op=ALU.add)
                    pa, pb = pb, pa
                incl = pa
                DVE.tensor_tensor(out=base[:], in0=incl[:], in1=pt[:], op=ALU.subtract)
                DVE.tensor_tensor(out=Sf[:], in0=banks[0][:].rearrange("p (n e) -> p n e", e=32), in1=off[:], op=ALU.add)
                DVE.tensor_tensor(out=Sf[:], in0=Sf[:], in1=base[:].unsqueeze(1).broadcast_to([128, NT, 32]), op=ALU.add)
                DVE.tensor_tensor(out=tS[:], in0=Sf[:], in1=M1a[:], op=ALU.mult)
                DVE.tensor_reduce(out=sl_f[:, 0, :], in_=tS[:], axis=AX.X, op=ALU.add)
                DVE.tensor_tensor(out=tS[:], in0=Sf[:], in1=M2a[:], op=ALU.mult)
                DVE.tensor_reduce(out=sl_f[:, 1, :], in_=tS[:], axis=AX.X, op=ALU.add)
                DVE.tensor_copy(out=sl_i[:], in_=sl_f[:])
                kb.wait(DVE, t_thr)
                DVE.tensor_tensor(out=thr[:], in0=thr[:], in1=incl[:].unsqueeze(1).broadcast_to([128, NJ, 32]), op=ALU.is_ge)
                DVE.tensor_reduce(out=ej_f[:], in_=thr[:], axis=AX.X, op=ALU.add)
                DVE.tensor_scalar(out=ej_f[:], in0=ej_f[:], scalar1=31.0, scalar2=None, op0=ALU.min)
                kb.wait(DVE, t_io)
                DVE.tensor_scalar(out=ej_f[:], in0=ej_f[:], scalar1=512.0, scalar2=pidx[:, 0:1], op0=ALU.mult, op1=ALU.add)
                DVE.tensor_tensor(out=wi_f[:, :, 0:4], in0=ej_f[:].unsqueeze(2).broadcast_to([128, NJ, 4]),
                                  in1=ko[:, 0:4].unsqueeze(1).broadcast_to([128, NJ, 4]), op=ALU.add)
                DVE.tensor_scalar(out=ej_f[:], in0=ej_f[:], scalar1=pidx[:, 0:1], scalar2=0.25, op0=ALU.subtract, op1=ALU.mult)
                DVE.tensor_scalar(out=wi_f[:, :, 4:5], in0=ej_f[:].unsqueeze(2), scalar1=pidx[:, 0:1], scalar2=None, op0=ALU.add)
                t_disp = M(DVE, DVE.tensor_copy(out=wi_i[:], in_=wi_f))

                kb.wait(POOL, t_disp)
                for n in range(NT):
                    for kk in range(2):
                        i = POOL.indirect_dma_start(out=xs_d, out_offset=bass.IndirectOffsetOnAxis(ap=sl_i[:, kk, n:n + 1], axis=0),
                                                    in_=hb_all[:, n, :], in_offset=None)
                        kb.inc(i, s_sc, 16)
                tok_sc = (s_sc, s_sc.n)
                kb.wait(SP, tok_sc)
                kb.wait(POOL, tok_sc)

                wgu_rows = w_gu.rearrange("e (d2 i) f -> (e d2) (i f)", i=2)
                wdn_rows = w_dn.rearrange("e (f2 i) d -> (e f2) (i d)", i=2)

                def m_load_w(j):
                    sw = ch_wt.pslot(POOL)
                    gv = wgu_sb[sw].rearrange("p (k2 i) f -> p k2 (i f)", i=2)
                    for k2 in range(4):
                        i = POOL.indirect_dma_start(out=gv[:, k2, :], out_offset=None, in_=wgu_rows,
                                                    in_offset=bass.IndirectOffsetOnAxis(ap=wi_i[:, j, k2:k2 + 1], axis=0))
                        t1 = ring_m.start(i, sw)
                    i = POOL.indirect_dma_start(out=wdn_sb[sw].rearrange("p i d -> p (i d)"), out_offset=None, in_=wdn_rows,
                                                in_offset=bass.IndirectOffsetOnAxis(ap=wi_i[:, j, 4:5], axis=0))
                    t1 = ring_m.start(i, sw)
                    ch_wt.produced(t1)

                def m_load_x(j):
                    sx = ch_xs.pslot(SP)
                    i = SP.dma_start(out=xt_sb[sx][:], in_=xs_d[256 * j:256 * (j + 1), :].rearrange("(a p) d -> p a d", p=128))
                    ch_xs.produced(ring_xs.start(i, sx))

                def m_tile(j):
                    sw = j % 2
                    sx = ch_xs.cslot(PE)
                    ch_tp.pslot(PE)
                    ch_tp.pslot(PE)
                    for half in range(2):
                        tpv = banks[6 + half][:].bitcast(BF16).rearrange("p (k a t) -> p k a t", k=4, a=2)
                        for k4 in range(4):
                            for a_ in range(2):
                                k = half * 4 + k4
                                c0 = 256 * (k // 2) + (k % 2)
                                i = PE.transpose(tpv[:, k4, a_, :], xt_sb[sx][:, a_, c0:c0 + 255:2], ident[:])
                    t = M(PE, i)
                    ch_xs.consumed(t)
                    ch_tp.produced(t)
                    ch_tp.produced(t)
                    sT = ch_xT.pslot(ACT, DVE)
                    ch_tp.cslot(ACT)
                    ta = M(ACT, ACT.activation(out=xsT[sT][:, 0:4, :], in_=banks[6][:].bitcast(BF16).rearrange("p (k t) -> p k t", k=4), func=AF.Copy))
                    ch_tp.consumed(ta)
                    ch_tp.cslot(DVE)
                    td = M(DVE, DVE.tensor_copy(out=xsT[sT][:, 4:8, :], in_=banks[7][:].bitcast(BF16).rearrange("p (k t) -> p k t", k=4)))
                    ch_tp.consumed(td)
                    ch_xT.produced(ta, td)
                    sT = ch_xT.cslot(PE)
                    for tok in ch_wt.ready[j]:
                        kb.wait(PE, tok)
                    sg = ch_g2.pslot(PE)
                    for part in range(2):
                        for fcp in range(2):
                            fc = part * 256 + fcp
                            for k in range(8):
                                i = PE.matmul(banks[2 * sg + part][:, fcp * 256:(fcp + 1) * 256], wgu_sb[sw][:, k, fc:fc + 255:2], xsT[sT][:, k, :],
                                              start=(fcp == 0 and k == 0), stop=(k == 7), skip_group_check=True)
                    t = M(PE, i)
                    ch_xT.consumed(t)
                    ch_g2.produced(t)
                    sg = ch_g2.cslot(ACT, 0)
                    ss = ch_sl.pslot(ACT)
                    t = M(ACT, ACT.activation(out=sl_sb[ss][:], in_=banks[2 * sg][:], func=AF.Silu))
                    ch_g2.consumed(t, 0)
                    ch_sl.produced(t)
                    sg = ch_g2.cslot(DVE, 1)
                    ss = ch_sl.cslot(DVE)
                    sa = ch_at.pslot(DVE)
                    t = M(DVE, DVE.tensor_tensor(out=at_sb[sa][:].rearrange("p c t -> p (c t)"), in0=banks[2 * sg + 1][:], in1=sl_sb[ss][:], op=ALU.mult))
                    ch_g2.consumed(t, 1)
                    ch_sl.consumed(t)
                    ch_at.produced(t)
                    sa = ch_at.cslot(PE)
                    for a_ in range(2):
                        ch_dn.pslot(PE)
                        for hf in range(2):
                            for fc in range(2):
                                i = PE.matmul(banks[4 + hf][:], at_sb[sa][:, fc, a_ * 128:(a_ + 1) * 128],
                                              wdn_sb[sw][:, fc, hf * 512:(hf + 1) * 512], start=(fc == 0), stop=(fc == 1))
                        t = M(PE, i)
                        ch_dn.produced(t)
                        so = ch_yo.pslot(ACT, DVE)
                        ch_dn.cslot(ACT, 0)
                        ta = M(ACT, ACT.activation(out=yo_sb[so][:, 0:512], in_=banks[4][:], func=AF.Copy))
                        ch_dn.consumed(ta, 0)
                        ch_dn.cslot(DVE, 1)
                        td = M(DVE, DVE.tensor_copy(out=yo_sb[so][:, 512:1024], in_=banks[5][:]))
                        ch_dn.consumed(td, 1)
                        ch_yo.produced(ta, td)
                        so = ch_yo.cslot(SP)
                        i = SP.dma_start(out=ys_d[256 * j + 128 * a_:256 * j + 128 * (a_ + 1), :], in_=yo_sb[so][:])
                        ch_yo.consumed(ring_ys.start(i, so))
                        last_ys[so] = (ring_ys.sems[so], ring_ys.sems[so].n)
                    ch_at.consumed(t)
                    ch_wt.consumed(t)

                last_ys = {}
                m_load_w(0)
                m_load_x(0)
                for j in range(NJ):
                    if j + 1 < NJ:
                        m_load_w(j + 1)
                        m_load_x(j + 1)
                    m_tile(j)
                for so, tk in last_ys.items():
                    kb.wait(POOL, tk)
                for n in range(NT):
                    for kk in range(2):
                        sr = ch_rg.pslot(POOL)
                        i = POOL.indirect_dma_start(out=r_sb[sr][:], out_offset=None, in_=ys_d,
                                                    in_offset=bass.IndirectOffsetOnAxis(ap=sl_i[:, kk, n:n + 1], axis=0))
                        ch_rg.produced(ring_g.start(i, sr))
                        sr = ch_rg.cslot(DVE)
                        t = M(DVE, DVE.scalar_tensor_tensor(out=ybuf[:, n, :], in0=r_sb[sr][:], scalar=g12[:, kk, n:n + 1], in1=ybuf[:, n, :],
                                                            op0=ALU.mult, op1=ALU.add))
                        ch_rg.consumed(t)
                barrier()
                stop_if("M")

            with contextlib.ExitStack() as es6:
                k6 = KB(nc, es6, kb.waited, kb.prog)
                o_sb = [k6.sb(f"o_sb{b}_{i}", [128, D], F32) for i in range(2)]
                st3 = k6.sb(f"st3_{b}", [128, 2, 6], F32)
                mv3 = k6.sb(f"mv3_{b}", [128, 2], F32)
                rstd3 = k6.sb(f"rstd3_{b}", [128, 1], F32)
                g2_bc = k6.sb(f"g2_bc{b}", [128, D], F32)
                b2_bc = k6.sb(f"b2_bc{b}", [128, D], F32)
                s_g2 = k6.sem(f"f_g2{b}")
                k6.inc(SP.dma_start(out=g2_bc[:], in_=ln2_g.partition_broadcast(128)), s_g2, 16)
                k6.wait(DVE, k6.inc(SP.dma_start(out=b2_bc[:], in_=ln2_b.partition_broadcast(128)), s_g2, 16))
                last_store = None
                for n in range(NT):
                    for hf in range(2):
                        DVE.bn_stats(out=st3[:, hf, :], in_=ybuf[:, n, hf * 512:(hf + 1) * 512])
                    i = DVE.bn_aggr(out=mv3[:], in_=st3[:].rearrange("p a c -> p (a c)"))
                    rstd_via_pool(mv3, rstd3[:], i)
                    so = ch_ot.pslot(DVE)
                    DVE.tensor_scalar(out=o_sb[so][:], in0=ybuf[:, n, :], scalar1=mv3[:, 0:1], scalar2=rstd3[:, 0:1],
                                      op0=ALU.subtract, op1=ALU.mult)
                    DVE.tensor_tensor(out=o_sb[so][:], in0=o_sb[so][:], in1=g2_bc[:], op=ALU.mult)
                    i = DVE.tensor_tensor(out=o_sb[so][:], in0=o_sb[so][:], in1=b2_bc[:], op=ALU.add)
                    ch_ot.produced(M(DVE, i))
                    so = ch_ot.cslot(SP)
                    i = SP.dma_start(out=out[b, n * 128:(n + 1) * 128, :], in_=o_sb[so][:])
                    t = ring_o.start(i, so)
                    ch_ot.consumed(t)
                    if n >= NT - 2:
                        kb.wait(SP, t) if n == NT - 2 else None
                        last_store = t
                        if n == NT - 2:
                            prev_store = t
                kb.wait(SP, prev_store)
                kb.wait(SP, last_store)
                barrier()
            esp.close()
    return nc


def _prep_inputs(inputs):
    x = np.ascontiguousarray(inputs["x"], dtype=np.float32)
    pos = np.ascontiguousarray(inputs["positions"], dtype=np.int32)
    w_r = np.concatenate([inputs["w_group"][0], np.transpose(inputs["w_expert"][0], (1, 0, 2)).reshape(D, 32)], axis=1)
    b_r = np.concatenate([inputs["b_group"][0], inputs["b_expert"][0].reshape(32)])[None, :]
    ws = inputs["w_spatial"][0]
    shared = {
        "w_in": np.ascontiguousarray(inputs["w_in"][0]),
        "w_out": np.ascontiguousarray(inputs["w_out"][0]),
        "ws_tgs": np.ascontiguousarray(np.transpose(ws, (1, 0, 2))),
        "ws_sgt": np.ascontiguousarray(np.transpose(ws, (2, 0, 1))),
        "bspT": np.ascontiguousarray(inputs["b_spatial"][0].T),
        "sgu_g": np.ascontiguousarray(inputs["sgu_ln_g"]),
        "sgu_b": np.ascontiguousarray(inputs["sgu_ln_b"]),
        "ln1_g": np.ascontiguousarray(inputs["ln1_g"]),
        "ln1_b": np.ascontiguousarray(inputs["ln1_b"]),
        "ln2_g": np.ascontiguousarray(inputs["ln2_g"]),
        "ln2_b": np.ascontiguousarray(inputs["ln2_b"]),
        "w_r": np.ascontiguousarray(w_r, dtype=np.float32),
        "b_r": np.ascontiguousarray(b_r, dtype=np.float32),
        "w_gu": np.ascontiguousarray(inputs["w_gate_up"][0].reshape(NE, D, 512)),
        "w_dn": np.ascontiguousarray(inputs["w_down"][0].reshape(NE, 256, D)),
    }
    in_maps = []
    for c in range(NCORES):
        xs = x[c * NB:(c + 1) * NB]
        m = dict(shared)
        m["xT"] = np.ascontiguousarray(np.transpose(xs, (0, 2, 1)))
        m["xtm"] = xs
        m["posT"] = np.ascontiguousarray(np.transpose(pos[c * NB:(c + 1) * NB].reshape(NB, NT, 128), (0, 2, 1)))
        in_maps.append(m)
    return in_maps


def kernel(**inputs):
    in_maps = _prep_inputs(inputs)
    nc = build_nc()
    res = run_bass_kernel_spmd(nc, in_maps, core_ids=list(range(NCORES)))
    return np.concatenate([r["out"] for r in res.results], axis=0).astype(np.float32)
```

```python
import contextlib
import numpy as np
import concourse.bass as bass
import concourse.mybir as mybir
from concourse.bass_utils import run_bass_kernel_spmd

F32, BF16, I32 = mybir.dt.float32, mybir.dt.bfloat16, mybir.dt.int32
AF = mybir.ActivationFunctionType
ALU = mybir.AluOpType
AX = mybir.AxisListType

NCORES = 8
S = 2048
D = 1024
NB = 2
NT = S // 128
ALPHA = float(2.0 ** 0.25)
EPS = 1e-5
NEG = -30000.0
NE = 32
TWO_PI = float(2 * np.pi)
DEBUG = False
DEBUG_B = 0
STOP_AFTER = None


class _Stop(Exception):
    pass


class Sem:
    _serial = 0

    def __init__(self, h):
        self.h = h
        self.n = 0
        Sem._serial += 1
        self.uid = Sem._serial


class KB:
    def __init__(self, nc, es, waited=None, prog=None):
        self.nc = nc
        self.es = es
        self.waited = {} if waited is None else waited
        self.prog = {} if prog is None else prog

    sem_es = None

    def sem(self, name):
        return Sem(KB.sem_es.enter_context(self.nc.semaphore(name)))

    def sb(self, name, shape, dt):
        return self.es.enter_context(self.nc.sbuf_tensor(name, shape, dt))

    def ps(self, name, shape, dt):
        return self.es.enter_context(self.nc.psum_tensor(name, shape, dt))

    def inc(self, instr, sem, k=1):
        instr.then_inc(sem.h, k)
        sem.n += k
        return (sem, sem.n)

    def mark(self, eng, instr):
        if isinstance(instr, Tok):
            return instr.tok
        return self.inc(instr, self.prog[id(getattr(eng, "raw", eng))])

    def wait(self, eng, tok):
        if tok is None:
            return
        sem, val = tok
        if val <= 0:
            return
        raw = getattr(eng, "raw", eng)
        key = (id(raw), sem.uid)
        if self.waited.get(key, 0) >= val:
            return
        self.waited[key] = val
        raw.wait_ge(sem.h, val)


class Tok:
    def __init__(self, instr, tok):
        self.instr = instr
        self.tok = tok


_COMPUTE = {"activation", "tensor_tensor", "tensor_scalar", "scalar_tensor_tensor", "tensor_copy", "tensor_reduce",
            "reciprocal", "bn_stats", "bn_aggr", "memset", "affine_select", "iota"}


class EngProxy:
    def __init__(self, raw, kb):
        self.raw = raw
        self.kb = kb
        self.last = None

    def __getattr__(self, name):
        attr = getattr(self.raw, name)
        if name not in _COMPUTE:
            return attr

        def wrapper(*a, **k):
            if self.last is not None:
                self.kb.wait(self, self.last)
            instr = attr(*a, **k)
            tok = self.kb.inc(instr, self.kb.prog[id(self.raw)])
            self.last = tok
            return Tok(instr, tok)
        return wrapper


class Chan:
    def __init__(self, kb, name, depth, ncons=1):
        self.kb = kb
        self.depth = depth
        self.ready = []
        self.free = []
        self.ci = [0] * ncons

    def pslot(self, *engs):
        i = len(self.ready)
        if i >= self.depth:
            for tok in self.free[i - self.depth]:
                for e in engs:
                    self.kb.wait(e, tok)
        return i % self.depth

    def produced(self, *toks):
        self.ready.append(list(toks))

    def cslot(self, eng, c=0):
        i = self.ci[c]
        for tok in self.ready[i]:
            self.kb.wait(eng, tok)
        return i % self.depth

    def consumed(self, tok, c=0):
        i = self.ci[c]
        while len(self.free) <= i:
            self.free.append([])
        self.free[i].append(tok)
        self.ci[c] += 1


class DmaRing:
    def __init__(self, kb, name, depth):
        self.kb = kb
        self.sems = [kb.sem(f"{name}{i}") for i in range(depth)]

    def start(self, instr, slot):
        return self.kb.inc(instr, self.sems[slot], 16)


def build_nc():
    nc = bass.Bass("TRN2", target_bir_lowering=False)
    dr = lambda name, shape, dt=F32: nc.dram_tensor(name, shape, dt, kind="ExternalInput").ap()
    xT = dr("xT", [NB, D, S])
    xtm = dr("xtm", [NB, S, D])
    posT = dr("posT", [NB, 128, NT], I32)
    w_in = dr("w_in", [D, 2560])
    w_out = dr("w_out", [D, D])
    ws_tgs = dr("ws_tgs", [128, 8, 128])
    ws_sgt = dr("ws_sgt", [128, 8, 128])
    bspT = dr("bspT", [128, 8])
    sgu_g = dr("sgu_g", [1, 512])
    sgu_b = dr("sgu_b", [1, 512])
    ln1_g = dr("ln1_g", [1, D])
    ln1_b = dr("ln1_b", [1, D])
    ln2_g = dr("ln2_g", [1, D])
    ln2_b = dr("ln2_b", [1, D])
    w_r = dr("w_r", [D, 36])
    b_r = dr("b_r", [1, 36])
    w_gu = dr("w_gu", [NE, D, 512])
    w_dn = dr("w_dn", [NE, 256, D])
    out = nc.dram_tensor("out", [NB, S, D], F32, kind="ExternalOutput").ap()
    if DEBUG:
        dbg_cat = nc.dram_tensor("dbg_cat", [128, 8, S], F32, kind="ExternalOutput").ap()
        dbg_y = nc.dram_tensor("dbg_y", [128, NT, D], F32, kind="ExternalOutput").ap()
        dbg_gate = nc.dram_tensor("dbg_gate", [128, NT, 32], F32, kind="ExternalOutput").ap()

    PE, ACT, DVE, POOL, SP = nc.tensor, nc.scalar, nc.vector, nc.gpsimd, nc.sync
    engines = [PE, ACT, DVE, POOL, SP]

    with contextlib.suppress(_Stop), contextlib.ExitStack() as es:
        KB.sem_es = es
        kb = KB(nc, es)
        progs = [{id(getattr(e, "raw", e)): kb.sem(f"prog{bb}_{n}") for e, n in zip(engines, "pe act dve pool sp".split())} for bb in range(NB + 1)]
        kb.prog = progs[NB]
        M = kb.mark
        ACT, DVE, POOL = EngProxy(nc.scalar, kb), EngProxy(nc.vector, kb), EngProxy(nc.gpsimd, kb)
        engines = [PE, ACT, DVE, POOL, SP]
        banks = [kb.ps(f"bank{i}", [128, 512], F32) for i in range(8)]

        ident = kb.sb("ident", [128, 128], BF16)
        ones_bf = kb.sb("ones_bf", [128, 128], BF16)
        ones_z = kb.sb("ones_z", [128, 2, 128], BF16)
        zer = kb.sb("zer", [128, 512], F32)
        mhalf = kb.sb("mhalf", [128, 1], F32)
        m_cur = kb.sb("m_cur", [128, 512], BF16)
        m_prev = kb.sb("m_prev", [128, 512], BF16)
        m3 = kb.sb("m3", [128, 4, 512], BF16)
        wmT = kb.sb("wmT", [128, 8, 128], BF16)
        rs = kb.sb("rs", [128, 8], F32)
        bsp_sb = kb.sb("bsp_sb", [128, 8], F32)
        sg_bc = kb.sb("sg_bc", [128, 512], F32)
        sb_bc = kb.sb("sb_bc", [128, 512], F32)
        Bp = kb.sb("Bp", [128, 512], F32)
        br_bc = kb.sb("br_bc", [128, 36], F32)
        wr_hl = kb.sb("wr_hl", [128, 8, 72], BF16)
        invf = kb.sb("invf", [128, NT, 8], F32)
        catT = kb.sb("catT", [128, 8, S], BF16)

        s_c = kb.sem("const_dma")
        s_bar = kb.sem("barrier")

        def barrier():
            base = s_bar.n
            for e in engines:
                if isinstance(e, EngProxy) and e.last is not None:
                    kb.wait(e, e.last)
                kb.inc(e.nop(), s_bar)
            for e in engines:
                kb.wait(e, (s_bar, base + len(engines)))

        def stop_if(tag):
            if STOP_AFTER == tag:
                barrier()
                raise _Stop()

        def wait_all(tok):
            for e in engines:
                kb.wait(e, tok)

        with contextlib.ExitStack() as es0:
            k0 = KB(nc, es0, kb.waited, kb.prog)
            wtmp = k0.sb("wtmp", [128, 8, 128], F32)
            wtmp2 = k0.sb("wtmp2", [128, 8, 128], F32)
            wr_f = k0.sb("wr_f", [128, 8, 36], F32)
            wr_t = k0.sb("wr_t", [128, 8, 36], F32)

            def dma_c(o, i):
                kb.inc(SP.dma_start(out=o, in_=i), s_c, 16)

            dma_c(wtmp[:], ws_sgt)
            dma_c(wtmp2[:], ws_tgs)
            dma_c(bsp_sb[:], bspT)
            dma_c(sg_bc[:], sgu_g.partition_broadcast(128))
            dma_c(sb_bc[:], sgu_b.partition_broadcast(128))
            dma_c(br_bc[:], b_r.partition_broadcast(128))
            dma_c(wr_f[:], w_r.rearrange("(k p) c -> p k c", p=128))
            tok_cdma = (s_c, s_c.n)

            POOL.memset(zer[:], 0.0)
            POOL.memset(ones_bf[:], 1.0)
            POOL.memset(ones_z[:], 0.0)
            POOL.memset(ones_z[:, 0, 0:64], 1.0)
            POOL.memset(ones_z[:, 1, 64:128], 1.0)
            POOL.memset(mhalf[:], -0.5)
            POOL.affine_select(out=ident[:], in_=ones_bf[:], pattern=[[1, 128]], compare_op=ALU.is_equal,
                               fill=0.0, base=0, channel_multiplier=-1)
            POOL.affine_select(out=m_cur[:], in_=zer[:], pattern=[[0, 4], [1, 128]], compare_op=ALU.is_ge,
                               fill=NEG, base=0, channel_multiplier=-1)
            POOL.affine_select(out=m_prev[:], in_=zer[:], pattern=[[0, 4], [-1, 128]], compare_op=ALU.is_ge,
                               fill=NEG, base=0, channel_multiplier=1)
            for s in range(4):
                POOL.affine_select(out=m3[:, s, :], in_=zer[:], pattern=[[0, 16], [1, 32]], compare_op=ALU.is_ge,
                                   fill=NEG, base=32 * s, channel_multiplier=-1)
            inv = (np.float32(500000.0) ** (-(np.arange(0, 16, 2, dtype=np.float32)) / np.float32(16))).astype(np.float32)
            for i in range(8):
                POOL.memset(invf[:, :, i:i + 1], float(inv[i]))
            kb.wait(POOL, tok_cdma)
            POOL.affine_select(out=wmT[:], in_=wtmp[:], pattern=[[0, 8], [1, 128]], compare_op=ALU.is_ge,
                               fill=0.0, base=0, channel_multiplier=-1)
            i_last = POOL.affine_select(out=wtmp[:], in_=wtmp2[:], pattern=[[0, 8], [-1, 128]], compare_op=ALU.is_ge,
                                        fill=0.0, base=0, channel_multiplier=1)
            tok_cpool = M(POOL, i_last)

            kb.wait(DVE, tok_cdma)
            kb.wait(DVE, tok_cpool)
            DVE.tensor_reduce(out=rs[:], in_=wtmp[:], axis=AX.X, op=ALU.add)
            for g in range(8):
                DVE.tensor_scalar(out=Bp[:, g * 64:(g + 1) * 64], in0=sb_bc[:, g * 64:(g + 1) * 64],
                                  scalar1=rs[:, g:g + 1], scalar2=bsp_sb[:, g:g + 1], op0=ALU.mult, op1=ALU.add)
            DVE.tensor_copy(out=wr_hl[:, :, 0:36], in_=wr_f[:])
            DVE.tensor_copy(out=wr_t[:], in_=wr_hl[:, :, 0:36])
            DVE.tensor_tensor(out=wr_t[:], in0=wr_f[:], in1=wr_t[:], op=ALU.subtract)
            i_last = DVE.tensor_copy(out=wr_hl[:, :, 36:72], in_=wr_t[:])
            tok_cdve = M(DVE, i_last)
            wait_all(tok_cdma)
            wait_all(tok_cpool)
            wait_all(tok_cdve)
            barrier()
        stop_if("const")

        def rstd_via_pool(mv, dst, last_dve_instr):
            t = M(DVE, last_dve_instr)
            kb.wait(POOL, t)
            POOL.tensor_scalar(out=dst, in0=mv[:, 1:2], scalar1=EPS, scalar2=None, op0=ALU.add)
            i = POOL.tensor_tensor(out=dst, in0=dst, in1=mhalf[:], op=ALU.pow)
            kb.wait(DVE, M(POOL, i))

        ring_x = DmaRing(kb, "ring_x", 2)
        ring_w = DmaRing(kb, "ring_w", 2)
        ring_o = DmaRing(kb, "ring_o", 2)
        ring_m = DmaRing(kb, "ring_m", 4)
        ring_xs3 = DmaRing(kb, "ring_xs3", 3)
        ring_xs = DmaRing(kb, "ring_xs", 2)
        ring_ys = DmaRing(kb, "ring_ys", 2)
        ring_g = DmaRing(kb, "ring_g", 2)
        s_sc = kb.sem("scatter")
        rb_gu = es.enter_context(nc.gpsimd.register("rb_gu"))
        rb_dn = es.enter_context(nc.gpsimd.register("rb_dn"))
        nc.gpsimd.reg_mov(rb_gu, NE * 512 - 1)
        nc.gpsimd.reg_mov(rb_dn, NE * 128 - 1)
        NJ = 48
        xs_d = nc.dram_tensor("xs_scratch", [NJ * 256, D], BF16, kind="Internal").ap()
        ys_d = nc.dram_tensor("ys_scratch", [NJ * 256, D], F32, kind="Internal").ap()

        for b in range(NB):
            kb.prog = progs[b]
            ch_u = Chan(kb, "ch_u", 2)
            ch_v = Chan(kb, "ch_v", 2)
            ch_z = Chan(kb, "ch_z", 2)
            ch_tp = Chan(kb, "ch_tp", 2)
            ch_n = Chan(kb, "ch_n", 2)
            ch_gu = Chan(kb, "ch_gu", 2)
            ch_gv = Chan(kb, "ch_gv", 2)
            ch_sg = Chan(kb, "ch_sg", 2)
            ch_qk = Chan(kb, "ch_qk", 2)
            ch_rot = Chan(kb, "ch_rot", 2)
            ch_qs = Chan(kb, "ch_qs", 2)
            ch_vp = Chan(kb, "ch_vp", 2)
            ch_s = Chan(kb, "ch_s", 4)
            ch_p = Chan(kb, "ch_p", 6)
            ch_o = Chan(kb, "ch_o", 2)
            ch_op = Chan(kb, "ch_op", 2)
            ch_x = Chan(kb, "ch_x", 2)
            ch_hb = Chan(kb, "ch_hb", 2)
            ch_lo = Chan(kb, "ch_lo", 2)
            ch_ht = Chan(kb, "ch_ht", 2)
            ch_r = Chan(kb, "ch_r", 1)
            ch_g2 = Chan(kb, "ch_g2", 2, ncons=2)
            ch_sl = Chan(kb, "ch_sl", 2)
            ch_at = Chan(kb, "ch_at", 2)
            ch_dn = Chan(kb, "ch_dn", 1, ncons=2)
            ch_wt = Chan(kb, "ch_wt", 3)
            ch_ot = Chan(kb, "ch_ot", 2)
            ch_xs = Chan(kb, "ch_xs", 2)
            ch_xT = Chan(kb, "ch_xT", 2)
            ch_yo = Chan(kb, "ch_yo", 2)
            ch_rg = Chan(kb, "ch_rg", 2)

            with contextlib.ExitStack() as es1:
                k1 = KB(nc, es1, kb.waited, kb.prog)
                xT_sb = k1.sb(f"xT_sb{b}", [128, 8, S], BF16)
                wsec = k1.sb(f"wsec{b}", [128, 8, 1024], BF16)
                pos_i = k1.sb(f"pos_i{b}", [128, NT], I32)
                pos_f = k1.sb(f"pos_f{b}", [128, NT], F32)
                ang = k1.sb(f"ang{b}", [128, 2, NT, 8], F32)
                kk_i = k1.sb(f"kk_i{b}", [128, 2, NT, 8], I32)
                kk_f = k1.sb(f"kk_f{b}", [128, 2, NT, 8], F32)
                rr = k1.sb(f"rr{b}", [128, 2, NT, 8], F32)
                mm = k1.sb(f"mm{b}", [128, 2, NT, 8], F32)
                cs_t = k1.sb(f"cs_t{b}", [128, 2, NT, 8], F32)
                s_ld = k1.sem(f"a1_ld{b}")
                s_w = k1.sem(f"a1_w{b}")

                for k in range(8):
                    k1.inc(POOL.dma_start(out=xT_sb[:, k, :], in_=xT[b, k * 128:(k + 1) * 128, :]), s_ld, 16)
                tok_x = (s_ld, s_ld.n)
                s_pos = k1.sem(f"a1_pos{b}")
                tok_pos = k1.inc(SP.dma_start(out=pos_i[:], in_=posT[b]), s_pos, 16)
                k1.inc(POOL.dma_start(out=wsec[:], in_=w_in[:, 1536:2560].rearrange("(k p) c -> p k c", p=128)), s_w, 16)
                tok_w = (s_w, s_w.n)

                k1.wait(DVE, tok_pos)
                DVE.tensor_copy(out=pos_f[:], in_=pos_i[:])
                DVE.tensor_tensor(out=ang[:, 1], in0=invf[:], in1=pos_f[:].unsqueeze(2).broadcast_to([128, NT, 8]), op=ALU.mult)
                DVE.tensor_scalar(out=ang[:, 0], in0=ang[:, 1], scalar1=float(np.pi / 2), scalar2=None, op0=ALU.add)
                DVE.tensor_scalar(out=kk_f[:], in0=ang[:], scalar1=float(1.0 / TWO_PI), scalar2=None, op0=ALU.mult)
                DVE.tensor_copy(out=kk_i[:], in_=kk_f[:])
                DVE.tensor_copy(out=kk_f[:], in_=kk_i[:])
                DVE.scalar_tensor_tensor(out=rr[:], in0=kk_f[:], scalar=-TWO_PI, in1=ang[:], op0=ALU.mult, op1=ALU.add)
                DVE.tensor_scalar(out=mm[:], in0=rr[:], scalar1=float(np.pi), scalar2=None, op0=ALU.is_gt)
                DVE.scalar_tensor_tensor(out=rr[:], in0=mm[:], scalar=-TWO_PI, in1=rr[:], op0=ALU.mult, op1=ALU.add)
                DVE.tensor_scalar(out=mm[:], in0=rr[:], scalar1=float(-np.pi), scalar2=None, op0=ALU.is_lt)
                i_l = DVE.scalar_tensor_tensor(out=rr[:], in0=mm[:], scalar=TWO_PI, in1=rr[:], op0=ALU.mult, op1=ALU.add)
                k1.wait(ACT, M(DVE, i_l))
                i_l = ACT.activation(out=cs_t[:], in_=rr[:], func=AF.Sin)
                tok_cs = M(ACT, i_l)
                stop_if("rope")

                with contextlib.ExitStack() as es2:
                    k2 = KB(nc, es2, kb.waited, kb.prog)
                    gu_sb = [k2.sb(f"gu_sb{b}_{i}", [128, 512], F32) for i in range(2)]
                    gv_sb = [k2.sb(f"gv_sb{b}_{i}", [128, 512], F32) for i in range(2)]
                    n_sb = [k2.sb(f"n_sb{b}_{i}", [128, 512], BF16) for i in range(2)]
                    t1_sb = k2.sb(f"t1_sb{b}", [128, 512], F32)
                    sg_sb = [k2.sb(f"sg_sb{b}_{i}", [128, 512], BF16) for i in range(2)]
                    st_sb = k2.sb(f"st_sb{b}", [128, 6], F32)
                    mv_sb = k2.sb(f"mv_sb{b}", [128, 2], F32)
                    rstd_sb = k2.sb(f"rstd_sb{b}", [128, 1], F32)

                    k2.wait(PE, tok_x)
                    k2.wait(PE, tok_w)

                    def sgu_pe_front(n):
                        su = ch_u.pslot(PE)
                        for k in range(8):
                            i = PE.matmul(banks[0 + su][:], xT_sb[:, k, n * 128:(n + 1) * 128], wsec[:, k, 0:512],
                                          start=(k == 0), stop=(k == 7))
                        ch_u.produced(M(PE, i))
                        sv = ch_v.pslot(PE)
                        for k in range(8):
                            i = PE.matmul(banks[2 + sv][:], xT_sb[:, k, n * 128:(n + 1) * 128], wsec[:, k, 512:1024],
                                          start=(k == 0), stop=(k == 7))
                        ch_v.produced(M(PE, i))

                    def sgu_act(n):
                        su = ch_u.cslot(ACT)
                        sg = ch_gu.pslot(ACT)
                        t = M(ACT, ACT.activation(out=gu_sb[sg][:], in_=banks[0 + su][:], func=AF.Gelu))
                        ch_u.consumed(t)
                        ch_gu.produced(t)
                        sv = ch_v.cslot(ACT)
                        sg = ch_gv.pslot(ACT)
                        t = M(ACT, ACT.activation(out=gv_sb[sg][:], in_=banks[2 + sv][:], func=AF.Gelu))
                        ch_v.consumed(t)
                        ch_gv.produced(t)

                    def sgu_dve_norm(n):
                        sg = ch_gv.cslot(DVE)
                        DVE.bn_stats(out=st_sb[:], in_=gv_sb[sg][:])
                        i = DVE.bn_aggr(out=mv_sb[:], in_=st_sb[:])
                        rstd_via_pool(mv_sb, rstd_sb[:], i)
                        sn = ch_n.pslot(DVE)
                        t = M(DVE, DVE.tensor_scalar(out=n_sb[sn][:], in0=gv_sb[sg][:], scalar1=mv_sb[:, 0:1], scalar2=rstd_sb[:, 0:1],
                                                     op0=ALU.subtract, op1=ALU.mult))
                        ch_gv.consumed(t)
                        ch_n.produced(t)

                    def sgu_pe_z(n):
                        sn = ch_n.cslot(PE)
                        sz = ch_z.pslot(PE)
                        for g in range(8):
                            i = PE.matmul(banks[4 + sz][:, g * 64:(g + 1) * 64], wmT[:, g, :], n_sb[sn][:, g * 64:(g + 1) * 64],
                                          start=True, stop=True, skip_group_check=True)
                        t = M(PE, i)
                        ch_n.consumed(t)
                        ch_z.produced(t)

                    def sgu_dve_out(n):
                        sz = ch_z.cslot(DVE)
                        t = M(DVE, DVE.tensor_tensor(out=t1_sb[:], in0=banks[4 + sz][:], in1=sg_bc[:], op=ALU.mult))
                        ch_z.consumed(t)
                        DVE.tensor_tensor(out=t1_sb[:], in0=t1_sb[:], in1=Bp[:], op=ALU.add)
                        sgu_ = ch_gu.cslot(DVE)
                        so = ch_sg.pslot(DVE)
                        t = M(DVE, DVE.tensor_tensor(out=sg_sb[so][:], in0=t1_sb[:], in1=gu_sb[sgu_][:], op=ALU.mult))
                        ch_gu.consumed(t)
                        ch_sg.produced(t)

                    def sgu_pe_tp(n):
                        so = ch_sg.cslot(PE)
                        st = ch_tp.pslot(PE)
                        tpv = banks[6 + st][:].bitcast(BF16)
                        for j in range(4):
                            i = PE.transpose(tpv[:, j * 128:(j + 1) * 128], sg_sb[so][:, j * 128:(j + 1) * 128], ident[:])
                        t = M(PE, i)
                        ch_sg.consumed(t)
                        ch_tp.produced(t)

                    def sgu_act_tp(n):
                        st = ch_tp.cslot(ACT)
                        tpv = banks[6 + st][:].bitcast(BF16)
                        i = ACT.activation(out=catT[:, 4:8, n * 128:(n + 1) * 128],
                                           in_=tpv[:, 0:512].rearrange("p (j t) -> p j t", j=4), func=AF.Copy)
                        ch_tp.consumed(M(ACT, i))

                    for n in range(NT + 2):
                        if n < NT:
                            sgu_pe_front(n)
                            sgu_act(n)
                            sgu_dve_norm(n)
                        if 1 <= n <= NT:
                            sgu_pe_z(n - 1)
                            sgu_dve_out(n - 1)
                        if 2 <= n:
                            sgu_pe_tp(n - 2)
                            sgu_act_tp(n - 2)
                    barrier()
                stop_if("sgu")

                with contextlib.ExitStack() as es3:
                    k3 = KB(nc, es3, kb.waited, kb.prog)
                    QTz = k3.sb(f"QTz{b}", [128, 2, S], BF16)
                    KT = k3.sb(f"KT{b}", [128, S], BF16)
                    Vz = k3.sb(f"Vz{b}", [128, 48, 2, 128], BF16)
                    qs_sb = [k3.sb(f"qs_sb{b}_{i}", [128, 4, 256], BF16) for i in range(2)]
                    ra = k3.sb(f"ra{b}", [128, 4, 4, 8], F32)
                    rb = k3.sb(f"rb{b}", [128, 4, 4, 8], F32)
                    rot_sb = [k3.sb(f"rot_sb{b}_{i}", [128, 4, 2, 2, 16], F32) for i in range(2)]
                    p_sb = [k3.sb(f"p_sb{b}_{i}", [128, 512], BF16) for i in range(6)]
                    rl_sb = k3.sb(f"rl_sb{b}", [128, 512], F32)
                    s_ws = k3.sem(f"a1_ws{b}")

                    POOL.memset(QTz[:], 0.0)
                    tok_zero = M(POOL, POOL.memset(Vz[:], 0.0))
                    for e in (ACT, DVE, PE):
                        k3.wait(e, tok_zero)
                    stop_if("att_ms")

                    def tv_blk(T, j):
                        return T[:, j * 128:(j + 1) * 128]

                    def tv_p2(T, s, r4):
                        return T[:, 512 * s:512 * (s + 1)].rearrange("p (i r) -> p r i", r=4)[:, r4, :]

                    def tv_p3k(T, r):
                        return T.rearrange("p (l r) -> p r l", r=16)[:, r, :]

                    def tv_p3q(T, s, r):
                        return T.rearrange("p (l r) -> p r l", r=16)[:, r, 32 * s:32 * (s + 1)]

                    vdefs = []
                    for j in range(16):
                        vdefs.append(lambda T, j=j: tv_blk(T, j))
                    for s in range(4):
                        for r4 in range(4):
                            vdefs.append(lambda T, s=s, r4=r4: tv_p2(T, s, r4))
                    for r in range(16):
                        vdefs.append(lambda T, r=r: tv_p3k(T, r))

                    for c in range(4):
                        for j, c0 in enumerate((c * 128, 512 + c * 128, 1024 + c * 128)):
                            k3.inc(POOL.dma_start(out=wsec[:, :, j * 128:(j + 1) * 128],
                                                  in_=w_in[:, c0:c0 + 128].rearrange("(k p) c -> p k c", p=128)), s_ws, 16)
                        k3.wait(PE, (s_ws, s_ws.n))
                        k3.wait(DVE, tok_cs)
                        stop_if("att_dma")

                        def qk_pe(gi):
                            sq = ch_qk.pslot(PE)
                            for w in range(2):
                                for tt in range(4):
                                    n = gi * 4 + tt
                                    for k in range(8):
                                        i = PE.matmul(banks[2 * sq + w][:, tt * 128:(tt + 1) * 128],
                                                      xT_sb[:, k, n * 128:(n + 1) * 128], wsec[:, k, w * 128:(w + 1) * 128],
                                                      start=(tt == 0 and k == 0), stop=(k == 7), skip_group_check=True)
                            ch_qk.produced(M(PE, i))

                        def qk_evac(gi):
                            sq = ch_qk.cslot(ACT, 0)
                            so = ch_qs.pslot(ACT, DVE)
                            sr = ch_rot.pslot(ACT)
                            qv = banks[2 * sq + 0][:].rearrange("p (t h e) -> p t h e", t=4, h=2)
                            kv = banks[2 * sq + 1][:].rearrange("p (t h e) -> p t h e", t=4, h=2)
                            ov = qs_sb[so][:].rearrange("p t (w h e) -> p t w h e", w=2, h=2)
                            ACT.activation(out=ov[:, :, 0, :, 16:64], in_=qv[:, :, :, 16:64], func=AF.Copy)
                            ACT.activation(out=ov[:, :, 1, :, 16:64], in_=kv[:, :, :, 16:64], func=AF.Copy)
                            ACT.activation(out=rot_sb[sr][:, :, 0, :, :], in_=qv[:, :, :, 0:16], func=AF.Copy)
                            ta = M(ACT, ACT.activation(out=rot_sb[sr][:, :, 1, :, :], in_=kv[:, :, :, 0:16], func=AF.Copy))
                            ch_qk.consumed(ta, 0)
                            ch_rot.produced(ta)
                            sr = ch_rot.cslot(DVE)
                            rv = rot_sb[sr][:].rearrange("p t w h e -> p t (w h) e")
                            t1 = rv[:, :, :, 0:8]
                            t2 = rv[:, :, :, 8:16]
                            og = qs_sb[so][:].rearrange("p t (g e) -> p t g e", g=4)
                            cosb = cs_t[:, 0, gi * 4:(gi + 1) * 4, :].unsqueeze(2).broadcast_to([128, 4, 4, 8])
                            sinb = cs_t[:, 1, gi * 4:(gi + 1) * 4, :].unsqueeze(2).broadcast_to([128, 4, 4, 8])
                            DVE.tensor_tensor(out=ra[:], in0=t1, in1=cosb, op=ALU.mult)
                            DVE.tensor_tensor(out=rb[:], in0=t2, in1=sinb, op=ALU.mult)
                            DVE.tensor_tensor(out=og[:, :, :, 0:8], in0=ra[:], in1=rb[:], op=ALU.subtract)
                            DVE.tensor_tensor(out=ra[:], in0=t2, in1=cosb, op=ALU.mult)
                            DVE.tensor_tensor(out=rb[:], in0=t1, in1=sinb, op=ALU.mult)
                            td = M(DVE, DVE.tensor_tensor(out=og[:, :, :, 8:16], in0=ra[:], in1=rb[:], op=ALU.add))
                            ch_rot.consumed(td)
                            ch_qs.produced(ta, td)

                        def qk_tp(gi):
                            so = ch_qs.cslot(PE)
                            st = ch_tp.pslot(PE)
                            tpv = banks[6 + st][:].bitcast(BF16).rearrange("p (t w e) -> p t w e", t=4, w=2)
                            for tt in range(4):
                                for w in range(2):
                                    i = PE.transpose(tpv[:, tt, w, :], qs_sb[so][:, tt, w * 128:(w + 1) * 128], ident[:])
                            t = M(PE, i)
                            ch_qs.consumed(t)
                            ch_tp.produced(t)

                        def qk_tp_evac(gi):
                            st = ch_tp.cslot(ACT)
                            tpv = banks[6 + st][:].bitcast(BF16).rearrange("p (t w e) -> p t w e", t=4, w=2)
                            cols = slice(gi * 512, (gi + 1) * 512)
                            ACT.activation(out=QTz[0:64, 0, cols].rearrange("p (t e) -> p t e", t=4), in_=tpv[0:64, :, 0, :], func=AF.Copy)
                            ACT.activation(out=QTz[64:128, 1, cols].rearrange("p (t e) -> p t e", t=4), in_=tpv[64:128, :, 0, :], func=AF.Copy)
                            i = ACT.activation(out=KT[:, cols].rearrange("p (t e) -> p t e", t=4), in_=tpv[:, :, 1, :], func=AF.Copy)
                            ch_tp.consumed(M(ACT, i))

                        for gi in range(4 + 2):
                            if gi < 4:
                                qk_pe(gi)
                                stop_if("qk_pe0")
                                qk_evac(gi)
                                stop_if("qk_ev0")
                            if 1 <= gi <= 4:
                                qk_tp(gi - 1)
                                stop_if("qk_tp0")
                            if 2 <= gi:
                                qk_tp_evac(gi - 2)
                                stop_if("qk_te0")
                        stop_if("qk")

                        for gi in range(12):
                            sv = ch_vp.pslot(PE)
                            for tt in range(4):
                                vd = vdefs[gi * 4 + tt]
                                for k in range(8):
                                    i = PE.matmul(banks[4 + sv][:, tt * 128:(tt + 1) * 128], vd(xT_sb[:, k, :]), wsec[:, k, 256:384],
                                                  start=(tt == 0 and k == 0), stop=(k == 7), skip_group_check=True)
                            ch_vp.produced(M(PE, i))
                            eng = ACT if gi % 2 == 0 else DVE
                            sv = ch_vp.cslot(eng)
                            src = banks[4 + sv][:].rearrange("p (t h e) -> p t h e", t=4, h=2)
                            dst = Vz[:, gi * 4:(gi + 1) * 4, :, :]
                            if eng is ACT:
                                ACT.activation(out=dst[:, :, 0, 0:64], in_=src[:, :, 0, :], func=AF.Copy)
                                i = ACT.activation(out=dst[:, :, 1, 64:128], in_=src[:, :, 1, :], func=AF.Copy)
                            else:
                                DVE.tensor_copy(out=dst[:, :, 0, 0:64], in_=src[:, :, 0, :])
                                i = DVE.tensor_copy(out=dst[:, :, 1, 64:128], in_=src[:, :, 1, :])
                            ch_vp.consumed(M(eng, i))
                        barrier()
                        stop_if("v")

                        V1 = lambda j, hh: Vz[:, j, hh, :]
                        V2 = lambda s, r4, hh: Vz[:, 16 + 4 * s + r4, hh, :]
                        V3 = lambda r, hh: Vz[:, 32 + r, hh, :]

                        def emit_s(maskt, mms, lo=0, hi=512):
                            ss = ch_s.pslot(PE)
                            PE.matmul(banks[ss][:], ident[:], maskt, start=True, stop=False, skip_group_check=True)
                            for (oc, lhsT, rhs) in mms:
                                i = PE.matmul(banks[ss][:, oc[0]:oc[1]], lhsT, rhs, start=False, stop=True, skip_group_check=True)
                            ch_s.produced(M(PE, i))
                            ss2 = ch_s.cslot(ACT)
                            sp = ch_p.pslot(ACT)
                            t = M(ACT, ACT.activation(out=p_sb[sp][:, lo:hi], in_=banks[ss2][:, lo:hi], func=AF.Exp, scale=0.125))
                            ch_s.consumed(t)
                            ch_p.produced(t)
                            return sp

                        def oview(Bk, oc):
                            if oc[0] == "blk":
                                return Bk[:, oc[1] * 128:(oc[1] + 1) * 128]
                            if oc[0] == "p2":
                                return Bk[:].rearrange("p (i r) -> p r i", r=4)[:, oc[1], :]
                            return Bk[:].rearrange("p (i r) -> p r i", r=16)[:, oc[1], :]

                        for s in range(4):
                            so_ = ch_o.pslot(PE)
                            Ob = banks[4 + 2 * so_]
                            Lb = banks[5 + 2 * so_]
                            first_pv = True
                            for hh in range(2):
                                Q = QTz[:, hh, :]
                                pend = []
                                mms = [((j * 128, (j + 1) * 128), tv_blk(KT, 4 * s + j), tv_blk(Q, 4 * s + j)) for j in range(4)]
                                sp = emit_s(m_cur[:], mms)
                                pend.append((sp, [(V1(4 * s + j, hh), (j * 128, (j + 1) * 128), ("blk", j)) for j in range(4)]))
                                js = [j for j in range(4) if 4 * s + j >= 1]
                                mms = [((j * 128, (j + 1) * 128), tv_blk(KT, 4 * s + j - 1), tv_blk(Q, 4 * s + j)) for j in js]
                                sp = emit_s(m_prev[:], mms, lo=js[0] * 128)
                                pend.append((sp, [(V1(4 * s + j - 1, hh), (j * 128, (j + 1) * 128), ("blk", j)) for j in js]))
                                mms = [((r4 * 128, (r4 + 1) * 128), tv_p2(KT, s, r4), tv_p2(Q, s, r4)) for r4 in range(4)]
                                sp = emit_s(m_cur[:], mms)
                                pend.append((sp, [(V2(s, r4, hh), (r4 * 128, (r4 + 1) * 128), ("p2", r4)) for r4 in range(4)]))
                                if s >= 1:
                                    mms = [((r4 * 128, (r4 + 1) * 128), tv_p2(KT, s - 1, r4), tv_p2(Q, s, r4)) for r4 in range(4)]
                                    sp = emit_s(m_prev[:], mms)
                                    pend.append((sp, [(V2(s - 1, r4, hh), (r4 * 128, (r4 + 1) * 128), ("p2", r4)) for r4 in range(4)]))
                                mms = [((r * 32, (r + 1) * 32), tv_p3k(KT, r), tv_p3q(Q, s, r)) for r in range(16)]
                                sp = emit_s(m3[:, s, :], mms)
                                pend.append((sp, [(V3(r, hh), (r * 32, (r + 1) * 32), ("p3", r)) for r in range(16)]))

                                for (sp, pvs) in pend:
                                    sp2 = ch_p.cslot(PE)
                                    assert sp2 == sp
                                    for (vt, pc, oc) in pvs:
                                        PE.matmul(oview(Ob, oc), vt, p_sb[sp][:, pc[0]:pc[1]], start=first_pv, stop=False, skip_group_check=True)
                                        i = PE.matmul(oview(Lb, oc), ones_z[:, hh, :], p_sb[sp][:, pc[0]:pc[1]], start=first_pv, stop=False,
                                                      skip_group_check=True)
                                        first_pv = False
                                    t_last = M(PE, i)
                                    ch_p.consumed(t_last)
                            ch_o.produced(t_last)
                            so2 = ch_o.cslot(DVE)
                            DVE.reciprocal(out=rl_sb[:], in_=banks[5 + 2 * so2][:])
                            i = DVE.tensor_tensor(out=catT[:, c, 512 * s:512 * (s + 1)], in0=banks[4 + 2 * so2][:], in1=rl_sb[:], op=ALU.mult)
                            ch_o.consumed(M(DVE, i))
                            stop_if("attn_s0")
                        barrier()
                        if DEBUG and b == DEBUG_B and STOP_AFTER == "attn":
                            with contextlib.ExitStack() as esd:
                                kd = KB(nc, esd, kb.waited, kb.prog)
                                dtmp = kd.sb("dtmpa", [128, 4, S], F32)
                                sd = kd.sem("dbga")
                                kd.wait(SP, M(DVE, DVE.tensor_copy(out=dtmp[:], in_=catT[:, 0:4, :])))
                                kd.wait(SP, kd.inc(SP.dma_start(out=dbg_cat[:, 0:4, :], in_=dtmp[:]), sd, 16))
                            stop_if("attn")

                if DEBUG and b == DEBUG_B:
                    with contextlib.ExitStack() as esd:
                        kd = KB(nc, esd, kb.waited, kb.prog)
                        dtmp = kd.sb("dtmp", [128, 8, S], F32)
                        sd = kd.sem("dbg1")
                        kd.wait(SP, M(DVE, DVE.tensor_copy(out=dtmp[:], in_=catT[:])))
                        kd.wait(SP, kd.inc(SP.dma_start(out=dbg_cat, in_=dtmp[:]), sd, 16))
                        barrier()

            esp = contextlib.ExitStack()
            kp = KB(nc, esp, kb.waited, kb.prog)
            hb_all = kp.sb(f"hb_all{b}", [128, NT, D], BF16)
            ybuf = kp.sb(f"ybuf{b}", [128, NT, D], F32)
            M1a = kp.sb(f"M1a{b}", [128, NT, 32], F32)
            M2a = kp.sb(f"M2a{b}", [128, NT, 32], F32)
            g12 = kp.sb(f"g12{b}", [128, 2, NT], F32)
            with contextlib.ExitStack() as es4:
                k4 = KB(nc, es4, kb.waited, kb.prog)
                wo_sb = k4.sb(f"wo_sb{b}", [128, 8, D], BF16)
                x_sb = [k4.sb(f"x_sb{b}_{i}", [128, D], F32) for i in range(2)]
                s_sb = k4.sb(f"s_sb{b}", [128, D], F32)
                hT_sb = [k4.sb(f"hT_sb{b}_{i}", [128, 8, 128], BF16) for i in range(2)]
                hf_sb = k4.sb(f"hf_sb{b}", [128, D], F32)
                lo_sb = [k4.sb(f"lo_sb{b}_{i}", [128, D], BF16) for i in range(2)]
                loT_sb = [k4.sb(f"loT_sb{b}_{i}", [128, 8, 128], BF16) for i in range(2)]
                st2 = k4.sb(f"st2_{b}", [128, 2, 6], F32)
                mv2 = k4.sb(f"mv2_{b}", [128, 2], F32)
                rstd2 = k4.sb(f"rstd2_{b}", [128, 1], F32)
                lg72 = k4.sb(f"lg72_{b}", [128, 72], F32)
                lg = k4.sb(f"lg_{b}", [128, 36], F32)
                rt = k4.sb(f"rt_{b}", [128, 64], F32)
                s_wo = k4.sem(f"a2_wo{b}")
                g1_bc = k4.sb(f"g1_bc{b}", [128, D], F32)
                b1_bc = k4.sb(f"b1_bc{b}", [128, D], F32)
                s_g1 = k4.sem(f"a2_g1{b}")
                k4.inc(SP.dma_start(out=g1_bc[:], in_=ln1_g.partition_broadcast(128)), s_g1, 16)
                k4.wait(DVE, k4.inc(SP.dma_start(out=b1_bc[:], in_=ln1_b.partition_broadcast(128)), s_g1, 16))

                t_wo = k4.inc(POOL.dma_start(out=wo_sb[:], in_=w_out.rearrange("(k p) c -> p k c", p=128)), s_wo, 16)
                k4.wait(PE, t_wo)
                k4.wait(DVE, t_wo)

                def a2_load(n):
                    sx = ch_x.pslot(SP)
                    i = SP.dma_start(out=x_sb[sx][:], in_=xtm[b, n * 128:(n + 1) * 128, :])
                    ch_x.produced(ring_x.start(i, sx))

                def a2_pe_op(n):
                    so = ch_op.pslot(PE)
                    for hf in range(2):
                        for k in range(8):
                            i = PE.matmul(banks[2 * so + hf][:], catT[:, k, n * 128:(n + 1) * 128], wo_sb[:, k, hf * 512:(hf + 1) * 512],
                                          start=(k == 0), stop=(k == 7))
                    ch_op.produced(M(PE, i))

                def a2_dve_ln(n):
                    so = ch_op.cslot(DVE)
                    sx = ch_x.cslot(DVE)
                    for hf in range(2):
                        i = DVE.scalar_tensor_tensor(out=s_sb[:, hf * 512:(hf + 1) * 512], in0=x_sb[sx][:, hf * 512:(hf + 1) * 512],
                                                     scalar=ALPHA, in1=banks[2 * so + hf][:], op0=ALU.mult, op1=ALU.add)
                    t = M(DVE, i)
                    ch_op.consumed(t)
                    ch_x.consumed(t)
                    for hf in range(2):
                        DVE.bn_stats(out=st2[:, hf, :], in_=s_sb[:, hf * 512:(hf + 1) * 512])
                    i = DVE.bn_aggr(out=mv2[:], in_=st2[:].rearrange("p a c -> p (a c)"))
                    rstd_via_pool(mv2, rstd2[:], i)
                    DVE.tensor_scalar(out=hf_sb[:], in0=s_sb[:], scalar1=mv2[:, 0:1], scalar2=rstd2[:, 0:1],
                                      op0=ALU.subtract, op1=ALU.mult)
                    DVE.tensor_tensor(out=hf_sb[:], in0=hf_sb[:], in1=g1_bc[:], op=ALU.mult)
                    DVE.tensor_tensor(out=hf_sb[:], in0=hf_sb[:], in1=b1_bc[:], op=ALU.add)
                    sh = ch_hb.pslot(DVE)
                    DVE.tensor_copy(out=hb_all[:, n, :], in_=hf_sb[:])
                    DVE.tensor_tensor(out=lo_sb[sh][:], in0=hf_sb[:], in1=hb_all[:, n, :], op=ALU.subtract)
                    i = DVE.tensor_scalar(out=ybuf[:, n, :], in0=hf_sb[:], scalar1=ALPHA, scalar2=None, op0=ALU.mult)
                    ch_hb.produced(M(DVE, i))

                def a2_pe_tp(n):
                    sh = ch_hb.cslot(PE)
                    st = ch_tp.pslot(PE)
                    tpv = banks[6 + st][:].bitcast(BF16)
                    for k in range(8):
                        i = PE.transpose(tpv[:, k * 128:(k + 1) * 128], hb_all[:, n, k * 128:(k + 1) * 128], ident[:])
                    ch_tp.produced(M(PE, i))
                    st = ch_tp.pslot(PE)
                    tpv = banks[6 + st][:].bitcast(BF16)
                    for k in range(8):
                        i = PE.transpose(tpv[:, k * 128:(k + 1) * 128], lo_sb[sh][:, k * 128:(k + 1) * 128], ident[:])
                    t = M(PE, i)
                    ch_hb.consumed(t)
                    ch_tp.produced(t)

                def a2_act_tp(n):
                    st = ch_tp.cslot(ACT)
                    tpv = banks[6 + st][:].bitcast(BF16)
                    sht = ch_ht.pslot(ACT)
                    i = ACT.activation(out=hT_sb[sht][:], in_=tpv.rearrange("p (k t) -> p k t", k=8), func=AF.Copy)
                    t_h = M(ACT, i)
                    ch_tp.consumed(t_h)
                    ch_ht.produced(t_h)
                    st = ch_tp.cslot(ACT)
                    tpv = banks[6 + st][:].bitcast(BF16)
                    sl = ch_lo.pslot(ACT)
                    i = ACT.activation(out=loT_sb[sl][:], in_=tpv.rearrange("p (k t) -> p k t", k=8), func=AF.Copy)
                    t = M(ACT, i)
                    ch_tp.consumed(t)
                    ch_lo.produced(t)
                    return t_h

                def a2_pe_route(n, t_h):
                    sl = ch_lo.cslot(PE)
                    sht = ch_ht.cslot(PE)
                    ch_r.pslot(PE)
                    rp = banks[4]
                    for k in range(8):
                        PE.matmul(rp[:, 0:72], hT_sb[sht][:, k, :], wr_hl[:, k, 0:72], start=(k == 0), stop=False,
                                  skip_group_check=True)
                    for k in range(8):
                        i = PE.matmul(rp[:, 0:36], loT_sb[sl][:, k, :], wr_hl[:, k, 0:36], start=False, stop=(k == 7),
                                      skip_group_check=True)
                    t = M(PE, i)
                    ch_lo.consumed(t)
                    ch_ht.consumed(t)
                    ch_r.produced(t)

                def a2_route_math(n):
                    ch_r.cslot(ACT)
                    t = M(ACT, ACT.activation(out=lg72[:], in_=banks[4][:, 0:72], func=AF.Copy))
                    ch_r.consumed(t)
                    kb.wait(DVE, t)
                    DVE.tensor_tensor(out=lg[:], in0=lg72[:, 0:36], in1=lg72[:, 36:72], op=ALU.add)
                    DVE.tensor_tensor(out=lg[:], in0=lg[:], in1=br_bc[:], op=ALU.add)
                    gmax, oh, ge, sume, psel = rt[:, 0:1], rt[:, 1:5], rt[:, 5:9], rt[:, 9:10], rt[:, 10:11]
                    esel, m1, eq1, e2, m2, eq2 = rt[:, 11:19], rt[:, 19:20], rt[:, 20:28], rt[:, 28:36], rt[:, 36:37], rt[:, 37:45]
                    dd, w1, w2, eg = rt[:, 45:46], rt[:, 46:47], rt[:, 47:48], rt[:, 48:56]
                    DVE.tensor_reduce(out=gmax, in_=lg[:, 0:4], axis=AX.X, op=ALU.max)
                    DVE.tensor_scalar(out=oh, in0=lg[:, 0:4], scalar1=gmax, scalar2=None, op0=ALU.is_equal)
                    DVE.tensor_scalar(out=ge, in0=lg[:, 0:4], scalar1=gmax, scalar2=None, op0=ALU.subtract)
                    DVE.tensor_scalar(out=esel, in0=lg[:, 4:12], scalar1=oh[:, 0:1], scalar2=None, op0=ALU.mult)
                    for g in range(1, 4):
                        DVE.scalar_tensor_tensor(out=esel, in0=lg[:, 4 + 8 * g:12 + 8 * g], scalar=oh[:, g:g + 1], in1=esel,
                                                 op0=ALU.mult, op1=ALU.add)
                    DVE.tensor_reduce(out=m1, in_=esel, axis=AX.X, op=ALU.max)
                    DVE.tensor_scalar(out=eq1, in0=esel, scalar1=m1, scalar2=None, op0=ALU.is_equal)
                    DVE.scalar_tensor_tensor(out=e2, in0=eq1, scalar=-1e30, in1=esel, op0=ALU.mult, op1=ALU.add)
                    DVE.tensor_reduce(out=m2, in_=e2, axis=AX.X, op=ALU.max)
                    DVE.tensor_scalar(out=eq2, in0=e2, scalar1=m2, scalar2=None, op0=ALU.is_equal)
                    i = DVE.tensor_tensor(out=dd, in0=m2, in1=m1, op=ALU.subtract)
                    kb.wait(ACT, M(DVE, i))
                    ACT.activation(out=ge, in_=ge, func=AF.Exp)
                    i = ACT.activation(out=dd, in_=dd, func=AF.Exp)
                    kb.wait(DVE, M(ACT, i))
                    DVE.tensor_reduce(out=sume, in_=ge, axis=AX.X, op=ALU.add)
                    DVE.reciprocal(out=psel, in_=sume)
                    DVE.tensor_scalar(out=w1, in0=dd, scalar1=1.0, scalar2=None, op0=ALU.add)
                    DVE.reciprocal(out=w1, in_=w1)
                    DVE.tensor_tensor(out=w2, in0=dd, in1=w1, op=ALU.mult)
                    DVE.tensor_tensor(out=w1, in0=w1, in1=psel, op=ALU.mult)
                    DVE.tensor_tensor(out=w2, in0=w2, in1=psel, op=ALU.mult)
                    for g in range(4):
                        DVE.tensor_scalar(out=M1a[:, n, g * 8:(g + 1) * 8], in0=eq1, scalar1=oh[:, g:g + 1], scalar2=None, op0=ALU.mult)
                        DVE.tensor_scalar(out=M2a[:, n, g * 8:(g + 1) * 8], in0=eq2, scalar1=oh[:, g:g + 1], scalar2=None, op0=ALU.mult)
                    DVE.tensor_copy(out=g12[:, 0, n:n + 1], in_=w1)
                    DVE.tensor_copy(out=g12[:, 1, n:n + 1], in_=w2)

                a2_load(0)
                t_hs = {}
                for n in range(NT + 2):
                    if n + 1 < NT:
                        a2_load(n + 1)
                    if n < NT:
                        a2_pe_op(n)
                        a2_dve_ln(n)
                    if 1 <= n <= NT:
                        a2_pe_tp(n - 1)
                        t_hs[n - 1] = a2_act_tp(n - 1)
                    if 2 <= n:
                        a2_pe_route(n - 2, t_hs[n - 2])
                        a2_route_math(n - 2)
                barrier()

            if DEBUG and b == DEBUG_B:
                with contextlib.ExitStack() as esd:
                    kd = KB(nc, esd, kb.waited, kb.prog)
                    sd = kd.sem("dbg2")
                    kd.inc(SP.dma_start(out=dbg_y, in_=ybuf[:]), sd, 16)
                    kd.wait(SP, kd.inc(SP.dma_start(out=dbg_gate, in_=M1a[:]), sd, 16))
                    barrier()
            if not DEBUG or b == DEBUG_B:
                stop_if("A2")

            with contextlib.ExitStack() as es5:
                k5 = KB(nc, es5, kb.waited, kb.prog)
                NWS = 3
                wgu_sb = [catT[:, 2 * i:2 * i + 2, :].rearrange("p a (k f) -> p (a k) f", f=512) for i in range(3)]
                wdn_t = [k5.sb(f"wdn_t{b}_{i}", [128, 2, D], BF16) for i in range(2)]
                wdn_sb = [t_[:] for t_ in wdn_t] + [catT[:, 6, :].rearrange("p (k f) -> p k f", f=1024)]
                thr = k5.sb(f"thr{b}", [128, NJ, 32], F32)
                Mb = k5.sb(f"Mb{b}", [128, NT * 32], BF16)
                Ms = k5.sb(f"Ms{b}", [128, NT, 32], F32)
                cs = k5.sb(f"cs{b}", [128, NT, 32], F32)
                off = k5.sb(f"off{b}", [128, NT, 32], F32)
                Sf = k5.sb(f"Sf{b}", [128, NT, 32], F32)
                tS = Ms
                sc_a = k5.sb(f"sc_a{b}", [128, 32], F32)
                sc_b = k5.sb(f"sc_b{b}", [128, 32], F32)
                pt = k5.sb(f"pt{b}", [128, 32], F32)
                base = k5.sb(f"base{b}", [128, 32], F32)
                qi = k5.sb(f"qi{b}", [128, 32], I32)
                sl_f = k5.sb(f"sl_f{b}", [128, 2, NT], F32)
                sl_i = k5.sb(f"sl_i{b}", [128, 2, NT], I32)
                ej_f = k5.sb(f"ej_f{b}", [128, NJ], F32)
                wi_f = thr[:].rearrange("p j e -> p (j e)")[:, 0:NJ * 5].rearrange("p (j c) -> p j c", c=5)
                wi_i = k5.sb(f"wi_i{b}", [128, NJ, 5], I32)
                pidx = k5.sb(f"pidx{b}", [128, 1], F32)
                ko = k5.sb(f"ko{b}", [128, 8], F32)
                Lst = k5.sb(f"Lst{b}", [128, 128], BF16)
                xt_sb = [k5.sb(f"xt_sb{b}_{i}", [128, 2, D], BF16) for i in range(2)]
                xsT = [k5.sb(f"xsT{b}_{i}", [128, 8, 256], BF16) for i in range(2)]
                sl_sb = [k5.sb(f"sl_sb{b}_{i}", [128, 512], F32) for i in range(2)]
                at_sb = [k5.sb(f"at_sb{b}_{i}", [128, 2, 256], BF16) for i in range(2)]
                yo_sb = [k5.sb(f"yo_sb{b}_{i}", [128, D], F32) for i in range(2)]
                r_sb = yo_sb

                POOL.affine_select(out=Lst[:], in_=ones_bf[:], pattern=[[1, 128]], compare_op=ALU.is_gt,
                                   fill=0.0, base=0, channel_multiplier=-1)
                t_thr = M(POOL, POOL.iota(thr[:], pattern=[[256, NJ], [0, 32]], base=0, channel_multiplier=0,
                                          allow_small_or_imprecise_dtypes=True))
                POOL.iota(pidx[:], pattern=[[0, 1]], base=0, channel_multiplier=1, allow_small_or_imprecise_dtypes=True)
                t_io = M(POOL, POOL.iota(ko[:], pattern=[[128, 8]], base=0, channel_multiplier=0, allow_small_or_imprecise_dtypes=True))
                DVE.tensor_tensor(out=Ms[:], in0=M1a[:], in1=M2a[:], op=ALU.add)
                t = M(DVE, DVE.tensor_copy(out=Mb[:], in_=Ms[:].rearrange("p n e -> p (n e)")))
                kb.wait(PE, t)
                kb.wait(PE, t_thr)
                PE.matmul(banks[0][:], Lst[:], Mb[:], start=True, stop=True)
                t = M(PE, PE.matmul(banks[1][:], ones_bf[:], Mb[:], start=True, stop=True))
                kb.wait(DVE, t)
                DVE.tensor_copy(out=cs[:], in_=banks[1][:].rearrange("p (n e) -> p n e", e=32))
                DVE.memset(off[:, 0, :], 0.0)
                for n in range(1, NT):
                    DVE.tensor_tensor(out=off[:, n, :], in0=off[:, n - 1, :], in1=cs[:, n - 1, :], op=ALU.add)
                DVE.tensor_tensor(out=sc_a[:], in0=off[:, NT - 1, :], in1=cs[:, NT - 1, :], op=ALU.add)
                DVE.tensor_scalar(out=sc_b[:], in0=sc_a[:], scalar1=127.5, scalar2=1.0 / 256.0, op0=ALU.add, op1=ALU.mult)
                DVE.tensor_copy(out=qi[:], in_=sc_b[:])
                DVE.tensor_scalar(out=pt[:], in0=qi[:], scalar1=256.0, scalar2=None, op0=ALU.mult)
                DVE.tensor_copy(out=sc_a[:], in_=pt[:])
                pa, pb = sc_a, sc_b
                for sh in (1, 2, 4, 8, 16):
                    DVE.tensor_copy(out=pb[:, 0:sh], in_=pa[:, 0:sh])
                    DVE.tensor_tensor(out=pb[:, sh:32], in0=pa[:, sh:32], in1=pa[:, 0:32 - sh], op=ALU.add)
                    pa, pb = pb, pa
                incl = pa
                DVE.tensor_tensor(out=base[:], in0=incl[:], in1=pt[:], op=ALU.subtract)
                DVE.tensor_tensor(out=Sf[:], in0=banks[0][:].rearrange("p (n e) -> p n e", e=32), in1=off[:], op=ALU.add)
                DVE.tensor_tensor(out=Sf[:], in0=Sf[:], in1=base[:].unsqueeze(1).broadcast_to([128, NT, 32]), op=ALU.add)
                DVE.tensor_tensor(out=tS[:], in0=Sf[:], in1=M1a[:], op=ALU.mult)
                DVE.tensor_reduce(out=sl_f[:, 0, :], in_=tS[:], axis=AX.X, op=ALU.add)
                DVE.tensor_tensor(out=tS[:], in0=Sf[:], in1=M2a[:], op=ALU.mult)
                DVE.tensor_reduce(out=sl_f[:, 1, :], in_=tS[:], axis=AX.X, op=ALU.add)
                DVE.tensor_copy(out=sl_i[:], in_=sl_f[:])
                kb.wait(DVE, t_thr)
                DVE.tensor_tensor(out=thr[:], in0=thr[:], in1=incl[:].unsqueeze(1).broadcast_to([128, NJ, 32]), op=ALU.is_ge)
                DVE.tensor_reduce(out=ej_f[:], in_=thr[:], axis=AX.X, op=ALU.add)
                kb.wait(DVE, t_io)
                DVE.tensor_scalar(out=ej_f[:], in0=ej_f[:], scalar1=512.0, scalar2=pidx[:, 0:1], op0=ALU.mult, op1=ALU.add)
                DVE.tensor_tensor(out=wi_f[:, :, 0:4], in0=ej_f[:].unsqueeze(2).broadcast_to([128, NJ, 4]),
                                  in1=ko[:, 0:4].unsqueeze(1).broadcast_to([128, NJ, 4]), op=ALU.add)
                DVE.tensor_scalar(out=ej_f[:], in0=ej_f[:], scalar1=pidx[:, 0:1], scalar2=0.25, op0=ALU.subtract, op1=ALU.mult)
                DVE.tensor_scalar(out=wi_f[:, :, 4:5], in0=ej_f[:].unsqueeze(2), scalar1=pidx[:, 0:1], scalar2=None, op0=ALU.add)
                t_disp = M(DVE, DVE.tensor_copy(out=wi_i[:], in_=wi_f))

                kb.wait(POOL, t_disp)
                for n in range(NT):
                    for kk in range(2):
                        i = POOL.indirect_dma_start(out=xs_d, out_offset=bass.IndirectOffsetOnAxis(ap=sl_i[:, kk, n:n + 1], axis=0),
                                                    in_=hb_all[:, n, :], in_offset=None)
                        kb.inc(i, s_sc, 16)
                tok_sc = (s_sc, s_sc.n)
                kb.wait(SP, tok_sc)
                kb.wait(POOL, tok_sc)

                wgu_rows = w_gu.rearrange("e (d2 i) f -> (e d2) (i f)", i=2)
                wdn_rows = w_dn.rearrange("e (f2 i) d -> (e f2) (i d)", i=2)

                def m_load_w(j):
                    sw = ch_wt.pslot(POOL)
                    gv = wgu_sb[sw].rearrange("p (k2 i) f -> p k2 (i f)", i=2)
                    for k2 in range(4):
                        i = POOL.indirect_dma_start(out=gv[:, k2, :], out_offset=None, in_=wgu_rows,
                                                    in_offset=bass.IndirectOffsetOnAxis(ap=wi_i[:, j, k2:k2 + 1], axis=0),
                                                    bounds_check=rb_gu, oob_is_err=False)
                        t1 = ring_m.start(i, sw)
                    i = POOL.indirect_dma_start(out=wdn_sb[sw].rearrange("p i d -> p (i d)"), out_offset=None, in_=wdn_rows,
                                                in_offset=bass.IndirectOffsetOnAxis(ap=wi_i[:, j, 4:5], axis=0),
                                                bounds_check=rb_dn, oob_is_err=False)
                    t1 = ring_m.start(i, sw)
                    ch_wt.produced(t1)

                def m_load_x(j):
                    sx = ch_xs.pslot(SP)
                    i = SP.dma_start(out=xt_sb[sx][:], in_=xs_d[256 * j:256 * (j + 1), :].rearrange("(a p) d -> p a d", p=128))
                    ch_xs.produced(ring_xs.start(i, sx))

                def m_tp(j):
                    sx = ch_xs.cslot(PE)
                    ch_tp.pslot(PE)
                    ch_tp.pslot(PE)
                    for half in range(2):
                        tpv = banks[6 + half][:].bitcast(BF16).rearrange("p (k a t) -> p k a t", k=4, a=2)
                        for k4 in range(4):
                            for a_ in range(2):
                                k = half * 4 + k4
                                c0 = 256 * (k // 2) + (k % 2)
                                i = PE.transpose(tpv[:, k4, a_, :], xt_sb[sx][:, a_, c0:c0 + 255:2], ident[:])
                    t = M(PE, i)
                    ch_xs.consumed(t)
                    ch_tp.produced(t)
                    ch_tp.produced(t)
                    sT = ch_xT.pslot(ACT, DVE)
                    ch_tp.cslot(ACT)
                    ta = M(ACT, ACT.activation(out=xsT[sT][:, 0:4, :], in_=banks[6][:].bitcast(BF16).rearrange("p (k t) -> p k t", k=4), func=AF.Copy))
                    ch_tp.consumed(ta)
                    ch_tp.cslot(DVE)
                    td = M(DVE, DVE.tensor_copy(out=xsT[sT][:, 4:8, :], in_=banks[7][:].bitcast(BF16).rearrange("p (k t) -> p k t", k=4)))
                    ch_tp.consumed(td)
                    ch_xT.produced(ta, td)

                def m_gu(j):
                    sw = j % NWS
                    sT = ch_xT.cslot(PE)
                    for tok in ch_wt.ready[j]:
                        kb.wait(PE, tok)
                    sg = ch_g2.pslot(PE)
                    for part in range(2):
                        for fcp in range(2):
                            fc = part * 256 + fcp
                            for k in range(8):
                                i = PE.matmul(banks[2 * sg + part][:, fcp * 256:(fcp + 1) * 256], wgu_sb[sw][:, k, fc:fc + 255:2], xsT[sT][:, k, :],
                                              start=(fcp == 0 and k == 0), stop=(k == 7), skip_group_check=True)
                    t = M(PE, i)
                    ch_xT.consumed(t)
                    ch_g2.produced(t)
                    sg = ch_g2.cslot(ACT, 0)
                    ss = ch_sl.pslot(ACT)
                    t = M(ACT, ACT.activation(out=sl_sb[ss][:], in_=banks[2 * sg][:], func=AF.Silu))
                    ch_g2.consumed(t, 0)
                    ch_sl.produced(t)
                    sg = ch_g2.cslot(DVE, 1)
                    ss = ch_sl.cslot(DVE)
                    sa = ch_at.pslot(DVE)
                    t = M(DVE, DVE.tensor_tensor(out=at_sb[sa][:].rearrange("p c t -> p (c t)"), in0=banks[2 * sg + 1][:], in1=sl_sb[ss][:], op=ALU.mult))
                    ch_g2.consumed(t, 1)
                    ch_sl.consumed(t)
                    ch_at.produced(t)

                def m_dn(j):
                    sw = j % NWS
                    sa = ch_at.cslot(PE)
                    for a_ in range(2):
                        ch_dn.pslot(PE)
                        for hf in range(2):
                            for fc in range(2):
                                i = PE.matmul(banks[4 + hf][:], at_sb[sa][:, fc, a_ * 128:(a_ + 1) * 128],
                                              wdn_sb[sw][:, fc, hf * 512:(hf + 1) * 512], start=(fc == 0), stop=(fc == 1))
                        t = M(PE, i)
                        ch_dn.produced(t)
                        so = ch_yo.pslot(ACT, DVE)
                        ch_dn.cslot(ACT, 0)
                        ta = M(ACT, ACT.activation(out=yo_sb[so][:, 0:512], in_=banks[4][:], func=AF.Copy))
                        ch_dn.consumed(ta, 0)
                        ch_dn.cslot(DVE, 1)
                        td = M(DVE, DVE.tensor_copy(out=yo_sb[so][:, 512:1024], in_=banks[5][:]))
                        ch_dn.consumed(td, 1)
                        ch_yo.produced(ta, td)
                        so = ch_yo.cslot(SP)
                        i = SP.dma_start(out=ys_d[256 * j + 128 * a_:256 * j + 128 * (a_ + 1), :], in_=yo_sb[so][:])
                        ch_yo.consumed(ring_ys.start(i, so))
                        last_ys[so] = (ring_ys.sems[so], ring_ys.sems[so].n)
                    ch_at.consumed(t)
                    ch_wt.consumed(t)

                last_ys = {}
                m_load_w(0)
                m_load_w(1)
                m_load_x(0)
                m_load_x(1)
                m_tp(0)
                for j in range(NJ):
                    if j + 2 < NJ:
                        m_load_x(j + 2)
                    if j + 1 < NJ:
                        m_tp(j + 1)
                    if j >= 1:
                        m_dn(j - 1)
                    if j + 2 < NJ:
                        m_load_w(j + 2)
                    m_gu(j)
                m_dn(NJ - 1)
                for so, tk in last_ys.items():
                    kb.wait(POOL, tk)
                for n in range(NT):
                    for kk in range(2):
                        sr = ch_rg.pslot(POOL)
                        i = POOL.indirect_dma_start(out=r_sb[sr][:], out_offset=None, in_=ys_d,
                                                    in_offset=bass.IndirectOffsetOnAxis(ap=sl_i[:, kk, n:n + 1], axis=0))
                        ch_rg.produced(ring_g.start(i, sr))
                        sr = ch_rg.cslot(DVE)
                        t = M(DVE, DVE.scalar_tensor_tensor(out=ybuf[:, n, :], in0=r_sb[sr][:], scalar=g12[:, kk, n:n + 1], in1=ybuf[:, n, :],
                                                            op0=ALU.mult, op1=ALU.add))
                        ch_rg.consumed(t)
                barrier()
                stop_if("M")

            with contextlib.ExitStack() as es6:
                k6 = KB(nc, es6, kb.waited, kb.prog)
                o_sb = [k6.sb(f"o_sb{b}_{i}", [128, D], F32) for i in range(2)]
                st3 = k6.sb(f"st3_{b}", [128, 2, 6], F32)
                mv3 = k6.sb(f"mv3_{b}", [128, 2], F32)
                rstd3 = k6.sb(f"rstd3_{b}", [128, 1], F32)
                g2_bc = k6.sb(f"g2_bc{b}", [128, D], F32)
                b2_bc = k6.sb(f"b2_bc{b}", [128, D], F32)
                s_g2 = k6.sem(f"f_g2{b}")
                k6.inc(SP.dma_start(out=g2_bc[:], in_=ln2_g.partition_broadcast(128)), s_g2, 16)
                k6.wait(DVE, k6.inc(SP.dma_start(out=b2_bc[:], in_=ln2_b.partition_broadcast(128)), s_g2, 16))
                last_store = None
                for n in range(NT):
                    for hf in range(2):
                        DVE.bn_stats(out=st3[:, hf, :], in_=ybuf[:, n, hf * 512:(hf + 1) * 512])
                    i = DVE.bn_aggr(out=mv3[:], in_=st3[:].rearrange("p a c -> p (a c)"))
                    rstd_via_pool(mv3, rstd3[:], i)
                    so = ch_ot.pslot(DVE)
                    DVE.tensor_scalar(out=o_sb[so][:], in0=ybuf[:, n, :], scalar1=mv3[:, 0:1], scalar2=rstd3[:, 0:1],
                                      op0=ALU.subtract, op1=ALU.mult)
                    DVE.tensor_tensor(out=o_sb[so][:], in0=o_sb[so][:], in1=g2_bc[:], op=ALU.mult)
                    i = DVE.tensor_tensor(out=o_sb[so][:], in0=o_sb[so][:], in1=b2_bc[:], op=ALU.add)
                    ch_ot.produced(M(DVE, i))
                    so = ch_ot.cslot(SP)
                    i = SP.dma_start(out=out[b, n * 128:(n + 1) * 128, :], in_=o_sb[so][:])
                    t = ring_o.start(i, so)
                    ch_ot.consumed(t)
                    if n >= NT - 2:
                        kb.wait(SP, t) if n == NT - 2 else None
                        last_store = t
                        if n == NT - 2:
                            prev_store = t
                kb.wait(SP, prev_store)
                kb.wait(SP, last_store)
                barrier()
            esp.close()
    return nc


def _prep_inputs(inputs):
    x = np.ascontiguousarray(inputs["x"], dtype=np.float32)
    pos = np.ascontiguousarray(inputs["positions"], dtype=np.int32)
    w_r = np.concatenate([inputs["w_group"][0], np.transpose(inputs["w_expert"][0], (1, 0, 2)).reshape(D, 32)], axis=1)
    b_r = np.concatenate([inputs["b_group"][0], inputs["b_expert"][0].reshape(32)])[None, :]
    ws = inputs["w_spatial"][0]
    shared = {
        "w_in": np.ascontiguousarray(inputs["w_in"][0]),
        "w_out": np.ascontiguousarray(inputs["w_out"][0]),
        "ws_tgs": np.ascontiguousarray(np.transpose(ws, (1, 0, 2))),
        "ws_sgt": np.ascontiguousarray(np.transpose(ws, (2, 0, 1))),
        "bspT": np.ascontiguousarray(inputs["b_spatial"][0].T),
        "sgu_g": np.ascontiguousarray(inputs["sgu_ln_g"]),
        "sgu_b": np.ascontiguousarray(inputs["sgu_ln_b"]),
        "ln1_g": np.ascontiguousarray(inputs["ln1_g"]),
        "ln1_b": np.ascontiguousarray(inputs["ln1_b"]),
        "ln2_g": np.ascontiguousarray(inputs["ln2_g"]),
        "ln2_b": np.ascontiguousarray(inputs["ln2_b"]),
        "w_r": np.ascontiguousarray(w_r, dtype=np.float32),
        "b_r": np.ascontiguousarray(b_r, dtype=np.float32),
        "w_gu": np.ascontiguousarray(inputs["w_gate_up"][0].reshape(NE, D, 512)),
        "w_dn": np.ascontiguousarray(inputs["w_down"][0].reshape(NE, 256, D)),
    }
    in_maps = []
    for c in range(NCORES):
        xs = x[c * NB:(c + 1) * NB]
        m = dict(shared)
        m["xT"] = np.ascontiguousarray(np.transpose(xs, (0, 2, 1)))
        m["xtm"] = xs
        m["posT"] = np.ascontiguousarray(np.transpose(pos[c * NB:(c + 1) * NB].reshape(NB, NT, 128), (0, 2, 1)))
        in_maps.append(m)
    return in_maps


def kernel(**inputs):
    in_maps = _prep_inputs(inputs)
    nc = build_nc()
    res = run_bass_kernel_spmd(nc, in_maps, core_ids=list(range(NCORES)))
    return np.concatenate([r["out"] for r in res.results], axis=0).astype(np.float32)
```

```python
import contextlib
import numpy as np
import concourse.bass as bass
import concourse.mybir as mybir
from concourse.bass_utils import run_bass_kernel_spmd

F32, BF16, I32 = mybir.dt.float32, mybir.dt.bfloat16, mybir.dt.int32
AF = mybir.ActivationFunctionType
ALU = mybir.AluOpType
AX = mybir.AxisListType

NCORES = 8
S = 2048
D = 1024
NB = 2
NT = S // 128
ALPHA = float(2.0 ** 0.25)
EPS = 1e-5
NEG = -30000.0
NE = 32
TWO_PI = float(2 * np.pi)
DEBUG = False
DEBUG_B = 0
STOP_AFTER = None


class _Stop(Exception):
    pass


class Sem:
    _serial = 0

    def __init__(self, h):
        self.h = h
        self.n = 0
        Sem._serial += 1
        self.uid = Sem._serial


class KB:
    def __init__(self, nc, es, waited=None, prog=None):
        self.nc = nc
        self.es = es
        self.waited = {} if waited is None else waited
        self.prog = {} if prog is None else prog

    sem_es = None

    def sem(self, name):
        return Sem(KB.sem_es.enter_context(self.nc.semaphore(name)))

    def sb(self, name, shape, dt):
        return self.es.enter_context(self.nc.sbuf_tensor(name, shape, dt))

    def ps(self, name, shape, dt):
        return self.es.enter_context(self.nc.psum_tensor(name, shape, dt))

    def inc(self, instr, sem, k=1):
        instr.then_inc(sem.h, k)
        sem.n += k
        return (sem, sem.n)

    def mark(self, eng, instr):
        if isinstance(instr, Tok):
            return instr.tok
        return self.inc(instr, self.prog[id(getattr(eng, "raw", eng))])

    def wait(self, eng, tok):
        if tok is None:
            return
        sem, val = tok
        if val <= 0:
            return
        raw = getattr(eng, "raw", eng)
        key = (id(raw), sem.uid)
        if self.waited.get(key, 0) >= val:
            return
        self.waited[key] = val
        raw.wait_ge(sem.h, val)


class Tok:
    def __init__(self, instr, tok):
        self.instr = instr
        self.tok = tok


_COMPUTE = {"activation", "tensor_tensor", "tensor_scalar", "scalar_tensor_tensor", "tensor_copy", "tensor_reduce",
            "reciprocal", "bn_stats", "bn_aggr", "memset", "affine_select", "iota"}


class EngProxy:
    def __init__(self, raw, kb):
        self.raw = raw
        self.kb = kb
        self.last = None

    def __getattr__(self, name):
        attr = getattr(self.raw, name)
        if name not in _COMPUTE:
            return attr

        def wrapper(*a, **k):
            if self.last is not None:
                self.kb.wait(self, self.last)
            instr = attr(*a, **k)
            tok = self.kb.inc(instr, self.kb.prog[id(self.raw)])
            self.last = tok
            return Tok(instr, tok)
        return wrapper


class Chan:
    def __init__(self, kb, name, depth, ncons=1):
        self.kb = kb
        self.depth = depth
        self.ready = []
        self.free = []
        self.ci = [0] * ncons

    def pslot(self, *engs):
        i = len(self.ready)
        if i >= self.depth:
            for tok in self.free[i - self.depth]:
                for e in engs:
                    self.kb.wait(e, tok)
        return i % self.depth

    def produced(self, *toks):
        self.ready.append(list(toks))

    def cslot(self, eng, c=0):
        i = self.ci[c]
        for tok in self.ready[i]:
            self.kb.wait(eng, tok)
        return i % self.depth

    def consumed(self, tok, c=0):
        i = self.ci[c]
        while len(self.free) <= i:
            self.free.append([])
        self.free[i].append(tok)
        self.ci[c] += 1


class DmaRing:
    def __init__(self, kb, name, depth):
        self.kb = kb
        self.sems = [kb.sem(f"{name}{i}") for i in range(depth)]

    def start(self, instr, slot):
        return self.kb.inc(instr, self.sems[slot], 16)


def build_nc():
    nc = bass.Bass("TRN2", target_bir_lowering=False)
    dr = lambda name, shape, dt=F32: nc.dram_tensor(name, shape, dt, kind="ExternalInput").ap()
    xT = dr("xT", [NB, D, S])
    xtm = dr("xtm", [NB, S, D])
    posT = dr("posT", [NB, 128, NT], I32)
    w_in = dr("w_in", [D, 2560])
    w_out = dr("w_out", [D, D])
    ws_tgs = dr("ws_tgs", [128, 8, 128])
    ws_sgt = dr("ws_sgt", [128, 8, 128])
    bspT = dr("bspT", [128, 8])
    sgu_g = dr("sgu_g", [1, 512])
    sgu_b = dr("sgu_b", [1, 512])
    ln1_g = dr("ln1_g", [1, D])
    ln1_b = dr("ln1_b", [1, D])
    ln2_g = dr("ln2_g", [1, D])
    ln2_b = dr("ln2_b", [1, D])
    w_r = dr("w_r", [D, 36])
    b_r = dr("b_r", [1, 36])
    w_gu = dr("w_gu", [NE, D, 512])
    w_dn = dr("w_dn", [NE, 256, D])
    out = nc.dram_tensor("out", [NB, S, D], F32, kind="ExternalOutput").ap()
    if DEBUG:
        dbg_cat = nc.dram_tensor("dbg_cat", [128, 8, S], F32, kind="ExternalOutput").ap()
        dbg_y = nc.dram_tensor("dbg_y", [128, NT, D], F32, kind="ExternalOutput").ap()
        dbg_gate = nc.dram_tensor("dbg_gate", [128, NT, 32], F32, kind="ExternalOutput").ap()

    PE, ACT, DVE, POOL, SP = nc.tensor, nc.scalar, nc.vector, nc.gpsimd, nc.sync
    engines = [PE, ACT, DVE, POOL, SP]

    with contextlib.suppress(_Stop), contextlib.ExitStack() as es:
        KB.sem_es = es
        kb = KB(nc, es)
        progs = [{id(getattr(e, "raw", e)): kb.sem(f"prog{bb}_{n}") for e, n in zip(engines, "pe act dve pool sp".split())} for bb in range(NB + 1)]
        kb.prog = progs[NB]
        M = kb.mark
        ACT, DVE, POOL = EngProxy(nc.scalar, kb), EngProxy(nc.vector, kb), EngProxy(nc.gpsimd, kb)
        engines = [PE, ACT, DVE, POOL, SP]
        banks = [kb.ps(f"bank{i}", [128, 512], F32) for i in range(8)]

        ident = kb.sb("ident", [128, 128], BF16)
        ones_bf = kb.sb("ones_bf", [128, 128], BF16)
        ones_z = kb.sb("ones_z", [128, 2, 128], BF16)
        zer = kb.sb("zer", [128, 512], F32)
        mhalf = kb.sb("mhalf", [128, 1], F32)
        m_cur = kb.sb("m_cur", [128, 512], BF16)
        m_prev = kb.sb("m_prev", [128, 512], BF16)
        m3 = kb.sb("m3", [128, 4, 512], BF16)
        wmT = kb.sb("wmT", [128, 8, 128], BF16)
        rs = kb.sb("rs", [128, 8], F32)
        bsp_sb = kb.sb("bsp_sb", [128, 8], F32)
        sg_bc = kb.sb("sg_bc", [128, 512], F32)
        sb_bc = kb.sb("sb_bc", [128, 512], F32)
        Bp = kb.sb("Bp", [128, 512], F32)
        br_bc = kb.sb("br_bc", [128, 36], F32)
        wr_hl = kb.sb("wr_hl", [128, 8, 72], BF16)
        invf = kb.sb("invf", [128, NT, 8], F32)
        catT = kb.sb("catT", [128, 8, S], BF16)

        s_c = kb.sem("const_dma")
        s_bar = kb.sem("barrier")

        def barrier():
            base = s_bar.n
            for e in engines:
                if isinstance(e, EngProxy) and e.last is not None:
                    kb.wait(e, e.last)
                kb.inc(e.nop(), s_bar)
            for e in engines:
                kb.wait(e, (s_bar, base + len(engines)))

        def stop_if(tag):
            if STOP_AFTER == tag:
                barrier()
                raise _Stop()

        def wait_all(tok):
            for e in engines:
                kb.wait(e, tok)

        with contextlib.ExitStack() as es0:
            k0 = KB(nc, es0, kb.waited, kb.prog)
            wtmp = k0.sb("wtmp", [128, 8, 128], F32)
            wtmp2 = k0.sb("wtmp2", [128, 8, 128], F32)
            wr_f = k0.sb("wr_f", [128, 8, 36], F32)
            wr_t = k0.sb("wr_t", [128, 8, 36], F32)

            def dma_c(o, i):
                kb.inc(SP.dma_start(out=o, in_=i), s_c, 16)

            dma_c(wtmp[:], ws_sgt)
            dma_c(wtmp2[:], ws_tgs)
            dma_c(bsp_sb[:], bspT)
            dma_c(sg_bc[:], sgu_g.partition_broadcast(128))
            dma_c(sb_bc[:], sgu_b.partition_broadcast(128))
            dma_c(br_bc[:], b_r.partition_broadcast(128))
            dma_c(wr_f[:], w_r.rearrange("(k p) c -> p k c", p=128))
            tok_cdma = (s_c, s_c.n)

            POOL.memset(zer[:], 0.0)
            POOL.memset(ones_bf[:], 1.0)
            POOL.memset(ones_z[:], 0.0)
            POOL.memset(ones_z[:, 0, 0:64], 1.0)
            POOL.memset(ones_z[:, 1, 64:128], 1.0)
            POOL.memset(mhalf[:], -0.5)
            POOL.affine_select(out=ident[:], in_=ones_bf[:], pattern=[[1, 128]], compare_op=ALU.is_equal,
                               fill=0.0, base=0, channel_multiplier=-1)
            POOL.affine_select(out=m_cur[:], in_=zer[:], pattern=[[0, 4], [1, 128]], compare_op=ALU.is_ge,
                               fill=NEG, base=0, channel_multiplier=-1)
            POOL.affine_select(out=m_prev[:], in_=zer[:], pattern=[[0, 4], [-1, 128]], compare_op=ALU.is_ge,
                               fill=NEG, base=0, channel_multiplier=1)
            for s in range(4):
                POOL.affine_select(out=m3[:, s, :], in_=zer[:], pattern=[[0, 16], [1, 32]], compare_op=ALU.is_ge,
                                   fill=NEG, base=32 * s, channel_multiplier=-1)
            inv = (np.float32(500000.0) ** (-(np.arange(0, 16, 2, dtype=np.float32)) / np.float32(16))).astype(np.float32)
            for i in range(8):
                POOL.memset(invf[:, :, i:i + 1], float(inv[i]))
            kb.wait(POOL, tok_cdma)
            POOL.affine_select(out=wmT[:], in_=wtmp[:], pattern=[[0, 8], [1, 128]], compare_op=ALU.is_ge,
                               fill=0.0, base=0, channel_multiplier=-1)
            i_last = POOL.affine_select(out=wtmp[:], in_=wtmp2[:], pattern=[[0, 8], [-1, 128]], compare_op=ALU.is_ge,
                                        fill=0.0, base=0, channel_multiplier=1)
            tok_cpool = M(POOL, i_last)

            kb.wait(DVE, tok_cdma)
            kb.wait(DVE, tok_cpool)
            DVE.tensor_reduce(out=rs[:], in_=wtmp[:], axis=AX.X, op=ALU.add)
            for g in range(8):
                DVE.tensor_scalar(out=Bp[:, g * 64:(g + 1) * 64], in0=sb_bc[:, g * 64:(g + 1) * 64],
                                  scalar1=rs[:, g:g + 1], scalar2=bsp_sb[:, g:g + 1], op0=ALU.mult, op1=ALU.add)
            DVE.tensor_copy(out=wr_hl[:, :, 0:36], in_=wr_f[:])
            DVE.tensor_copy(out=wr_t[:], in_=wr_hl[:, :, 0:36])
            DVE.tensor_tensor(out=wr_t[:], in0=wr_f[:], in1=wr_t[:], op=ALU.subtract)
            i_last = DVE.tensor_copy(out=wr_hl[:, :, 36:72], in_=wr_t[:])
            tok_cdve = M(DVE, i_last)
            wait_all(tok_cdma)
            wait_all(tok_cpool)
            wait_all(tok_cdve)
            barrier()
        stop_if("const")

        def rstd_via_pool(mv, dst, last_dve_instr):
            t = M(DVE, last_dve_instr)
            kb.wait(POOL, t)
            POOL.tensor_scalar(out=dst, in0=mv[:, 1:2], scalar1=EPS, scalar2=None, op0=ALU.add)
            i = POOL.tensor_tensor(out=dst, in0=dst, in1=mhalf[:], op=ALU.pow)
            kb.wait(DVE, M(POOL, i))

        ring_x = DmaRing(kb, "ring_x", 2)
        ring_w = DmaRing(kb, "ring_w", 2)
        ring_o = DmaRing(kb, "ring_o", 2)
        ring_m = DmaRing(kb, "ring_m", 4)
        ring_xs3 = DmaRing(kb, "ring_xs3", 3)
        ring_xs = DmaRing(kb, "ring_xs", 2)
        ring_ys = DmaRing(kb, "ring_ys", 2)
        ring_g = DmaRing(kb, "ring_g", 2)
        s_sc = kb.sem("scatter")
        rb_gu = es.enter_context(nc.gpsimd.register("rb_gu"))
        rb_dn = es.enter_context(nc.gpsimd.register("rb_dn"))
        nc.gpsimd.reg_mov(rb_gu, NE * 512 - 1)
        nc.gpsimd.reg_mov(rb_dn, NE * 128 - 1)
        NJ = 48
        xs_d = nc.dram_tensor("xs_scratch", [NJ * 256, D], BF16, kind="Internal").ap()
        ys_d = nc.dram_tensor("ys_scratch", [NJ * 256, D], F32, kind="Internal").ap()

        for b in range(NB):
            kb.prog = progs[b]
            ch_u = Chan(kb, "ch_u", 2)
            ch_v = Chan(kb, "ch_v", 2)
            ch_z = Chan(kb, "ch_z", 2)
            ch_tp = Chan(kb, "ch_tp", 2)
            ch_n = Chan(kb, "ch_n", 2)
            ch_gu = Chan(kb, "ch_gu", 2)
            ch_gv = Chan(kb, "ch_gv", 2)
            ch_sg = Chan(kb, "ch_sg", 2)
            ch_qk = Chan(kb, "ch_qk", 2)
            ch_rot = Chan(kb, "ch_rot", 2)
            ch_qs = Chan(kb, "ch_qs", 2)
            ch_vp = Chan(kb, "ch_vp", 2)
            ch_s = Chan(kb, "ch_s", 4)
            ch_p = Chan(kb, "ch_p", 6)
            ch_o = Chan(kb, "ch_o", 2)
            ch_op = Chan(kb, "ch_op", 2)
            ch_x = Chan(kb, "ch_x", 2)
            ch_hb = Chan(kb, "ch_hb", 2)
            ch_hf = Chan(kb, "ch_hf", 2)
            ch_lo = Chan(kb, "ch_lo", 2)
            ch_ht = Chan(kb, "ch_ht", 2)
            ch_r = Chan(kb, "ch_r", 1)
            ch_g2 = Chan(kb, "ch_g2", 2, ncons=2)
            ch_sl = Chan(kb, "ch_sl", 2)
            ch_at = Chan(kb, "ch_at", 2)
            ch_dn = Chan(kb, "ch_dn", 1, ncons=2)
            ch_wt = Chan(kb, "ch_wt", 3)
            ch_ot = Chan(kb, "ch_ot", 2)
            ch_xs = Chan(kb, "ch_xs", 2)
            ch_xT = Chan(kb, "ch_xT", 2)
            ch_yo = Chan(kb, "ch_yo", 2)
            ch_rg = Chan(kb, "ch_rg", 2)

            with contextlib.ExitStack() as es1:
                k1 = KB(nc, es1, kb.waited, kb.prog)
                xT_sb = k1.sb(f"xT_sb{b}", [128, 8, S], BF16)
                wsec = k1.sb(f"wsec{b}", [128, 8, 1024], BF16)
                pos_i = k1.sb(f"pos_i{b}", [128, NT], I32)
                pos_f = k1.sb(f"pos_f{b}", [128, NT], F32)
                ang = k1.sb(f"ang{b}", [128, 2, NT, 8], F32)
                kk_i = k1.sb(f"kk_i{b}", [128, 2, NT, 8], I32)
                kk_f = k1.sb(f"kk_f{b}", [128, 2, NT, 8], F32)
                rr = k1.sb(f"rr{b}", [128, 2, NT, 8], F32)
                mm = k1.sb(f"mm{b}", [128, 2, NT, 8], F32)
                cs_t = k1.sb(f"cs_t{b}", [128, 2, NT, 8], F32)
                s_ld = k1.sem(f"a1_ld{b}")
                s_w = k1.sem(f"a1_w{b}")

                for k in range(8):
                    k1.inc(POOL.dma_start(out=xT_sb[:, k, :], in_=xT[b, k * 128:(k + 1) * 128, :]), s_ld, 16)
                tok_x = (s_ld, s_ld.n)
                s_pos = k1.sem(f"a1_pos{b}")
                tok_pos = k1.inc(SP.dma_start(out=pos_i[:], in_=posT[b]), s_pos, 16)
                k1.inc(POOL.dma_start(out=wsec[:], in_=w_in[:, 1536:2560].rearrange("(k p) c -> p k c", p=128)), s_w, 16)
                tok_w = (s_w, s_w.n)

                k1.wait(DVE, tok_pos)
                DVE.tensor_copy(out=pos_f[:], in_=pos_i[:])
                DVE.tensor_tensor(out=ang[:, 1], in0=invf[:], in1=pos_f[:].unsqueeze(2).broadcast_to([128, NT, 8]), op=ALU.mult)
                DVE.tensor_scalar(out=ang[:, 0], in0=ang[:, 1], scalar1=float(np.pi / 2), scalar2=None, op0=ALU.add)
                DVE.tensor_scalar(out=kk_f[:], in0=ang[:], scalar1=float(1.0 / TWO_PI), scalar2=None, op0=ALU.mult)
                DVE.tensor_copy(out=kk_i[:], in_=kk_f[:])
                DVE.tensor_copy(out=kk_f[:], in_=kk_i[:])
                DVE.scalar_tensor_tensor(out=rr[:], in0=kk_f[:], scalar=-TWO_PI, in1=ang[:], op0=ALU.mult, op1=ALU.add)
                DVE.tensor_scalar(out=mm[:], in0=rr[:], scalar1=float(np.pi), scalar2=None, op0=ALU.is_gt)
                DVE.scalar_tensor_tensor(out=rr[:], in0=mm[:], scalar=-TWO_PI, in1=rr[:], op0=ALU.mult, op1=ALU.add)
                DVE.tensor_scalar(out=mm[:], in0=rr[:], scalar1=float(-np.pi), scalar2=None, op0=ALU.is_lt)
                i_l = DVE.scalar_tensor_tensor(out=rr[:], in0=mm[:], scalar=TWO_PI, in1=rr[:], op0=ALU.mult, op1=ALU.add)
                k1.wait(ACT, M(DVE, i_l))
                i_l = ACT.activation(out=cs_t[:], in_=rr[:], func=AF.Sin)
                tok_cs = M(ACT, i_l)
                stop_if("rope")

                with contextlib.ExitStack() as es2:
                    k2 = KB(nc, es2, kb.waited, kb.prog)
                    gu_sb = [k2.sb(f"gu_sb{b}_{i}", [128, 512], F32) for i in range(2)]
                    gv_sb = [k2.sb(f"gv_sb{b}_{i}", [128, 512], F32) for i in range(2)]
                    n_sb = [k2.sb(f"n_sb{b}_{i}", [128, 512], BF16) for i in range(2)]
                    t1_sb = k2.sb(f"t1_sb{b}", [128, 512], F32)
                    sg_sb = [k2.sb(f"sg_sb{b}_{i}", [128, 512], BF16) for i in range(2)]
                    st_sb = k2.sb(f"st_sb{b}", [128, 6], F32)
                    mv_sb = k2.sb(f"mv_sb{b}", [128, 2], F32)
                    rstd_sb = k2.sb(f"rstd_sb{b}", [128, 1], F32)

                    k2.wait(PE, tok_x)
                    k2.wait(PE, tok_w)

                    def sgu_pe_front(n):
                        su = ch_u.pslot(PE)
                        for k in range(8):
                            i = PE.matmul(banks[0 + su][:], xT_sb[:, k, n * 128:(n + 1) * 128], wsec[:, k, 0:512],
                                          start=(k == 0), stop=(k == 7))
                        ch_u.produced(M(PE, i))
                        sv = ch_v.pslot(PE)
                        for k in range(8):
                            i = PE.matmul(banks[2 + sv][:], xT_sb[:, k, n * 128:(n + 1) * 128], wsec[:, k, 512:1024],
                                          start=(k == 0), stop=(k == 7))
                        ch_v.produced(M(PE, i))

                    def sgu_act(n):
                        su = ch_u.cslot(ACT)
                        sg = ch_gu.pslot(ACT)
                        t = M(ACT, ACT.activation(out=gu_sb[sg][:], in_=banks[0 + su][:], func=AF.Gelu))
                        ch_u.consumed(t)
                        ch_gu.produced(t)
                        sv = ch_v.cslot(ACT)
                        sg = ch_gv.pslot(ACT)
                        t = M(ACT, ACT.activation(out=gv_sb[sg][:], in_=banks[2 + sv][:], func=AF.Gelu))
                        ch_v.consumed(t)
                        ch_gv.produced(t)

                    def sgu_dve_norm(n):
                        sg = ch_gv.cslot(DVE)
                        DVE.bn_stats(out=st_sb[:], in_=gv_sb[sg][:])
                        i = DVE.bn_aggr(out=mv_sb[:], in_=st_sb[:])
                        rstd_via_pool(mv_sb, rstd_sb[:], i)
                        sn = ch_n.pslot(DVE)
                        t = M(DVE, DVE.tensor_scalar(out=n_sb[sn][:], in0=gv_sb[sg][:], scalar1=mv_sb[:, 0:1], scalar2=rstd_sb[:, 0:1],
                                                     op0=ALU.subtract, op1=ALU.mult))
                        ch_gv.consumed(t)
                        ch_n.produced(t)

                    def sgu_pe_z(n):
                        sn = ch_n.cslot(PE)
                        sz = ch_z.pslot(PE)
                        for g in range(8):
                            i = PE.matmul(banks[4 + sz][:, g * 64:(g + 1) * 64], wmT[:, g, :], n_sb[sn][:, g * 64:(g + 1) * 64],
                                          start=True, stop=True, skip_group_check=True)
                        t = M(PE, i)
                        ch_n.consumed(t)
                        ch_z.produced(t)

                    def sgu_dve_out(n):
                        sz = ch_z.cslot(DVE)
                        t = M(DVE, DVE.tensor_tensor(out=t1_sb[:], in0=banks[4 + sz][:], in1=sg_bc[:], op=ALU.mult))
                        ch_z.consumed(t)
                        DVE.tensor_tensor(out=t1_sb[:], in0=t1_sb[:], in1=Bp[:], op=ALU.add)
                        sgu_ = ch_gu.cslot(DVE)
                        so = ch_sg.pslot(DVE)
                        t = M(DVE, DVE.tensor_tensor(out=sg_sb[so][:], in0=t1_sb[:], in1=gu_sb[sgu_][:], op=ALU.mult))
                        ch_gu.consumed(t)
                        ch_sg.produced(t)

                    def sgu_pe_tp(n):
                        so = ch_sg.cslot(PE)
                        st = ch_tp.pslot(PE)
                        tpv = banks[6 + st][:].bitcast(BF16)
                        for j in range(4):
                            i = PE.transpose(tpv[:, j * 128:(j + 1) * 128], sg_sb[so][:, j * 128:(j + 1) * 128], ident[:])
                        t = M(PE, i)
                        ch_sg.consumed(t)
                        ch_tp.produced(t)

                    def sgu_act_tp(n):
                        st = ch_tp.cslot(ACT)
                        tpv = banks[6 + st][:].bitcast(BF16)
                        i = ACT.activation(out=catT[:, 4:8, n * 128:(n + 1) * 128],
                                           in_=tpv[:, 0:512].rearrange("p (j t) -> p j t", j=4), func=AF.Copy)
                        ch_tp.consumed(M(ACT, i))

                    for n in range(NT + 2):
                        if n < NT:
                            sgu_pe_front(n)
                            sgu_act(n)
                            sgu_dve_norm(n)
                        if 1 <= n <= NT:
                            sgu_pe_z(n - 1)
                            sgu_dve_out(n - 1)
                        if 2 <= n:
                            sgu_pe_tp(n - 2)
                            sgu_act_tp(n - 2)
                    barrier()
                stop_if("sgu")

                with contextlib.ExitStack() as es3:
                    k3 = KB(nc, es3, kb.waited, kb.prog)
                    QTz = k3.sb(f"QTz{b}", [128, 2, S], BF16)
                    KT = k3.sb(f"KT{b}", [128, S], BF16)
                    Vz = k3.sb(f"Vz{b}", [128, 48, 2, 128], BF16)
                    qs_sb = [k3.sb(f"qs_sb{b}_{i}", [128, 4, 256], BF16) for i in range(2)]
                    ra = k3.sb(f"ra{b}", [128, 4, 4, 8], F32)
                    rb = k3.sb(f"rb{b}", [128, 4, 4, 8], F32)
                    rot_sb = [k3.sb(f"rot_sb{b}_{i}", [128, 4, 2, 2, 16], F32) for i in range(2)]
                    p_sb = [k3.sb(f"p_sb{b}_{i}", [128, 512], BF16) for i in range(6)]
                    rl_sb = k3.sb(f"rl_sb{b}", [128, 512], F32)
                    s_ws = k3.sem(f"a1_ws{b}")

                    POOL.memset(QTz[:], 0.0)
                    tok_zero = M(POOL, POOL.memset(Vz[:], 0.0))
                    for e in (ACT, DVE, PE):
                        k3.wait(e, tok_zero)
                    stop_if("att_ms")

                    def tv_blk(T, j):
                        return T[:, j * 128:(j + 1) * 128]

                    def tv_p2(T, s, r4):
                        return T[:, 512 * s:512 * (s + 1)].rearrange("p (i r) -> p r i", r=4)[:, r4, :]

                    def tv_p3k(T, r):
                        return T.rearrange("p (l r) -> p r l", r=16)[:, r, :]

                    def tv_p3q(T, s, r):
                        return T.rearrange("p (l r) -> p r l", r=16)[:, r, 32 * s:32 * (s + 1)]

                    vdefs = []
                    for j in range(16):
                        vdefs.append(lambda T, j=j: tv_blk(T, j))
                    for s in range(4):
                        for r4 in range(4):
                            vdefs.append(lambda T, s=s, r4=r4: tv_p2(T, s, r4))
                    for r in range(16):
                        vdefs.append(lambda T, r=r: tv_p3k(T, r))

                    for c in range(4):
                        for j, c0 in enumerate((c * 128, 512 + c * 128, 1024 + c * 128)):
                            k3.inc(POOL.dma_start(out=wsec[:, :, j * 128:(j + 1) * 128],
                                                  in_=w_in[:, c0:c0 + 128].rearrange("(k p) c -> p k c", p=128)), s_ws, 16)
                        k3.wait(PE, (s_ws, s_ws.n))
                        k3.wait(DVE, tok_cs)
                        stop_if("att_dma")

                        def qk_pe(gi):
                            sq = ch_qk.pslot(PE)
                            for w in range(2):
                                for tt in range(4):
                                    n = gi * 4 + tt
                                    for k in range(8):
                                        i = PE.matmul(banks[2 * sq + w][:, tt * 128:(tt + 1) * 128],
                                                      xT_sb[:, k, n * 128:(n + 1) * 128], wsec[:, k, w * 128:(w + 1) * 128],
                                                      start=(tt == 0 and k == 0), stop=(k == 7), skip_group_check=True)
                            ch_qk.produced(M(PE, i))

                        def qk_evac(gi):
                            sq = ch_qk.cslot(ACT, 0)
                            so = ch_qs.pslot(ACT, DVE)
                            sr = ch_rot.pslot(ACT)
                            qv = banks[2 * sq + 0][:].rearrange("p (t h e) -> p t h e", t=4, h=2)
                            kv = banks[2 * sq + 1][:].rearrange("p (t h e) -> p t h e", t=4, h=2)
                            ov = qs_sb[so][:].rearrange("p t (w h e) -> p t w h e", w=2, h=2)
                            ACT.activation(out=ov[:, :, 0, :, 16:64], in_=qv[:, :, :, 16:64], func=AF.Copy)
                            ACT.activation(out=ov[:, :, 1, :, 16:64], in_=kv[:, :, :, 16:64], func=AF.Copy)
                            ACT.activation(out=rot_sb[sr][:, :, 0, :, :], in_=qv[:, :, :, 0:16], func=AF.Copy)
                            ta = M(ACT, ACT.activation(out=rot_sb[sr][:, :, 1, :, :], in_=kv[:, :, :, 0:16], func=AF.Copy))
                            ch_qk.consumed(ta, 0)
                            ch_rot.produced(ta)
                            sr = ch_rot.cslot(DVE)
                            rv = rot_sb[sr][:].rearrange("p t w h e -> p t (w h) e")
                            t1 = rv[:, :, :, 0:8]
                            t2 = rv[:, :, :, 8:16]
                            og = qs_sb[so][:].rearrange("p t (g e) -> p t g e", g=4)
                            cosb = cs_t[:, 0, gi * 4:(gi + 1) * 4, :].unsqueeze(2).broadcast_to([128, 4, 4, 8])
                            sinb = cs_t[:, 1, gi * 4:(gi + 1) * 4, :].unsqueeze(2).broadcast_to([128, 4, 4, 8])
                            DVE.tensor_tensor(out=ra[:], in0=t1, in1=cosb, op=ALU.mult)
                            DVE.tensor_tensor(out=rb[:], in0=t2, in1=sinb, op=ALU.mult)
                            DVE.tensor_tensor(out=og[:, :, :, 0:8], in0=ra[:], in1=rb[:], op=ALU.subtract)
                            DVE.tensor_tensor(out=ra[:], in0=t2, in1=cosb, op=ALU.mult)
                            DVE.tensor_tensor(out=rb[:], in0=t1, in1=sinb, op=ALU.mult)
                            td = M(DVE, DVE.tensor_tensor(out=og[:, :, :, 8:16], in0=ra[:], in1=rb[:], op=ALU.add))
                            ch_rot.consumed(td)
                            ch_qs.produced(ta, td)

                        def qk_tp(gi):
                            so = ch_qs.cslot(PE)
                            st = ch_tp.pslot(PE)
                            tpv = banks[6 + st][:].bitcast(BF16).rearrange("p (t w e) -> p t w e", t=4, w=2)
                            for tt in range(4):
                                for w in range(2):
                                    i = PE.transpose(tpv[:, tt, w, :], qs_sb[so][:, tt, w * 128:(w + 1) * 128], ident[:])
                            t = M(PE, i)
                            ch_qs.consumed(t)
                            ch_tp.produced(t)

                        def qk_tp_evac(gi):
                            st = ch_tp.cslot(ACT)
                            tpv = banks[6 + st][:].bitcast(BF16).rearrange("p (t w e) -> p t w e", t=4, w=2)
                            cols = slice(gi * 512, (gi + 1) * 512)
                            ACT.activation(out=QTz[0:64, 0, cols].rearrange("p (t e) -> p t e", t=4), in_=tpv[0:64, :, 0, :], func=AF.Copy)
                            ACT.activation(out=QTz[64:128, 1, cols].rearrange("p (t e) -> p t e", t=4), in_=tpv[64:128, :, 0, :], func=AF.Copy)
                            i = ACT.activation(out=KT[:, cols].rearrange("p (t e) -> p t e", t=4), in_=tpv[:, :, 1, :], func=AF.Copy)
                            ch_tp.consumed(M(ACT, i))

                        for gi in range(4 + 2):
                            if gi < 4:
                                qk_pe(gi)
                                stop_if("qk_pe0")
                                qk_evac(gi)
                                stop_if("qk_ev0")
                            if 1 <= gi <= 4:
                                qk_tp(gi - 1)
                                stop_if("qk_tp0")
                            if 2 <= gi:
                                qk_tp_evac(gi - 2)
                                stop_if("qk_te0")
                        stop_if("qk")

                        for gi in range(12):
                            sv = ch_vp.pslot(PE)
                            for tt in range(4):
                                vd = vdefs[gi * 4 + tt]
                                for k in range(8):
                                    i = PE.matmul(banks[4 + sv][:, tt * 128:(tt + 1) * 128], vd(xT_sb[:, k, :]), wsec[:, k, 256:384],
                                                  start=(tt == 0 and k == 0), stop=(k == 7), skip_group_check=True)
                            ch_vp.produced(M(PE, i))
                            eng = ACT if gi % 2 == 0 else DVE
                            sv = ch_vp.cslot(eng)
                            src = banks[4 + sv][:].rearrange("p (t h e) -> p t h e", t=4, h=2)
                            dst = Vz[:, gi * 4:(gi + 1) * 4, :, :]
                            if eng is ACT:
                                ACT.activation(out=dst[:, :, 0, 0:64], in_=src[:, :, 0, :], func=AF.Copy)
                                i = ACT.activation(out=dst[:, :, 1, 64:128], in_=src[:, :, 1, :], func=AF.Copy)
                            else:
                                DVE.tensor_copy(out=dst[:, :, 0, 0:64], in_=src[:, :, 0, :])
                                i = DVE.tensor_copy(out=dst[:, :, 1, 64:128], in_=src[:, :, 1, :])
                            ch_vp.consumed(M(eng, i))
                        barrier()
                        stop_if("v")

                        V1 = lambda j, hh: Vz[:, j, hh, :]
                        V2 = lambda s, r4, hh: Vz[:, 16 + 4 * s + r4, hh, :]
                        V3 = lambda r, hh: Vz[:, 32 + r, hh, :]

                        def emit_s(maskt, mms, lo=0, hi=512):
                            ss = ch_s.pslot(PE)
                            PE.matmul(banks[ss][:], ident[:], maskt, start=True, stop=False, skip_group_check=True)
                            for (oc, lhsT, rhs) in mms:
                                i = PE.matmul(banks[ss][:, oc[0]:oc[1]], lhsT, rhs, start=False, stop=True, skip_group_check=True)
                            ch_s.produced(M(PE, i))
                            ss2 = ch_s.cslot(ACT)
                            sp = ch_p.pslot(ACT)
                            t = M(ACT, ACT.activation(out=p_sb[sp][:, lo:hi], in_=banks[ss2][:, lo:hi], func=AF.Exp, scale=0.125))
                            ch_s.consumed(t)
                            ch_p.produced(t)
                            return sp

                        def oview(Bk, oc):
                            if oc[0] == "blk":
                                return Bk[:, oc[1] * 128:(oc[1] + 1) * 128]
                            if oc[0] == "p2":
                                return Bk[:].rearrange("p (i r) -> p r i", r=4)[:, oc[1], :]
                            return Bk[:].rearrange("p (i r) -> p r i", r=16)[:, oc[1], :]

                        for s in range(4):
                            so_ = ch_o.pslot(PE)
                            Ob = banks[4 + 2 * so_]
                            Lb = banks[5 + 2 * so_]
                            first_pv = True
                            for hh in range(2):
                                Q = QTz[:, hh, :]
                                pend = []
                                mms = [((j * 128, (j + 1) * 128), tv_blk(KT, 4 * s + j), tv_blk(Q, 4 * s + j)) for j in range(4)]
                                sp = emit_s(m_cur[:], mms)
                                pend.append((sp, [(V1(4 * s + j, hh), (j * 128, (j + 1) * 128), ("blk", j)) for j in range(4)]))
                                js = [j for j in range(4) if 4 * s + j >= 1]
                                mms = [((j * 128, (j + 1) * 128), tv_blk(KT, 4 * s + j - 1), tv_blk(Q, 4 * s + j)) for j in js]
                                sp = emit_s(m_prev[:], mms, lo=js[0] * 128)
                                pend.append((sp, [(V1(4 * s + j - 1, hh), (j * 128, (j + 1) * 128), ("blk", j)) for j in js]))
                                mms = [((r4 * 128, (r4 + 1) * 128), tv_p2(KT, s, r4), tv_p2(Q, s, r4)) for r4 in range(4)]
                                sp = emit_s(m_cur[:], mms)
                                pend.append((sp, [(V2(s, r4, hh), (r4 * 128, (r4 + 1) * 128), ("p2", r4)) for r4 in range(4)]))
                                if s >= 1:
                                    mms = [((r4 * 128, (r4 + 1) * 128), tv_p2(KT, s - 1, r4), tv_p2(Q, s, r4)) for r4 in range(4)]
                                    sp = emit_s(m_prev[:], mms)
                                    pend.append((sp, [(V2(s - 1, r4, hh), (r4 * 128, (r4 + 1) * 128), ("p2", r4)) for r4 in range(4)]))
                                mms = [((r * 32, (r + 1) * 32), tv_p3k(KT, r), tv_p3q(Q, s, r)) for r in range(16)]
                                sp = emit_s(m3[:, s, :], mms)
                                pend.append((sp, [(V3(r, hh), (r * 32, (r + 1) * 32), ("p3", r)) for r in range(16)]))

                                for (sp, pvs) in pend:
                                    sp2 = ch_p.cslot(PE)
                                    assert sp2 == sp
                                    for (vt, pc, oc) in pvs:
                                        PE.matmul(oview(Ob, oc), vt, p_sb[sp][:, pc[0]:pc[1]], start=first_pv, stop=False, skip_group_check=True)
                                        i = PE.matmul(oview(Lb, oc), ones_z[:, hh, :], p_sb[sp][:, pc[0]:pc[1]], start=first_pv, stop=False,
                                                      skip_group_check=True)
                                        first_pv = False
                                    t_last = M(PE, i)
                                    ch_p.consumed(t_last)
                            ch_o.produced(t_last)
                            so2 = ch_o.cslot(DVE)
                            DVE.reciprocal(out=rl_sb[:], in_=banks[5 + 2 * so2][:])
                            i = DVE.tensor_tensor(out=catT[:, c, 512 * s:512 * (s + 1)], in0=banks[4 + 2 * so2][:], in1=rl_sb[:], op=ALU.mult)
                            ch_o.consumed(M(DVE, i))
                            stop_if("attn_s0")
                        barrier()
                        if DEBUG and b == DEBUG_B and STOP_AFTER == "attn":
                            with contextlib.ExitStack() as esd:
                                kd = KB(nc, esd, kb.waited, kb.prog)
                                dtmp = kd.sb("dtmpa", [128, 4, S], F32)
                                sd = kd.sem("dbga")
                                kd.wait(SP, M(DVE, DVE.tensor_copy(out=dtmp[:], in_=catT[:, 0:4, :])))
                                kd.wait(SP, kd.inc(SP.dma_start(out=dbg_cat[:, 0:4, :], in_=dtmp[:]), sd, 16))
                            stop_if("attn")

                if DEBUG and b == DEBUG_B:
                    with contextlib.ExitStack() as esd:
                        kd = KB(nc, esd, kb.waited, kb.prog)
                        dtmp = kd.sb("dtmp", [128, 8, S], F32)
                        sd = kd.sem("dbg1")
                        kd.wait(SP, M(DVE, DVE.tensor_copy(out=dtmp[:], in_=catT[:])))
                        kd.wait(SP, kd.inc(SP.dma_start(out=dbg_cat, in_=dtmp[:]), sd, 16))
                        barrier()

            esp = contextlib.ExitStack()
            kp = KB(nc, esp, kb.waited, kb.prog)
            hb_all = kp.sb(f"hb_all{b}", [128, NT, D], BF16)
            ybuf = kp.sb(f"ybuf{b}", [128, NT, D], F32)
            M1a = kp.sb(f"M1a{b}", [128, NT, 32], F32)
            M2a = kp.sb(f"M2a{b}", [128, NT, 32], F32)
            g12 = kp.sb(f"g12{b}", [128, 2, NT], F32)
            with contextlib.ExitStack() as es4:
                k4 = KB(nc, es4, kb.waited, kb.prog)
                wo_sb = k4.sb(f"wo_sb{b}", [128, 8, D], BF16)
                x_sb = [k4.sb(f"x_sb{b}_{i}", [128, D], F32) for i in range(2)]
                hT_sb = [k4.sb(f"hT_sb{b}_{i}", [128, 8, 128], BF16) for i in range(2)]
                hf_sb = [k4.sb(f"hf_sb{b}_{i}", [128, D], F32) for i in range(2)]
                eps_t = k4.sb(f"eps_t{b}", [128, 1], F32)
                k4.wait(ACT, M(POOL, POOL.memset(eps_t[:], EPS)))
                math_done = [None]
                pend_D = []
                NH = 8
                lg_all = k4.sb(f"lg_all{b}", [128, NH, 72], F32)
                rq = k4.sb(f"rq{b}", [128, NH, 64], F32)
                lo_sb = [k4.sb(f"lo_sb{b}_{i}", [128, D], BF16) for i in range(2)]
                loT_sb = [k4.sb(f"loT_sb{b}_{i}", [128, 8, 128], BF16) for i in range(2)]
                st2 = k4.sb(f"st2_{b}", [128, 2, 6], F32)
                mv2 = k4.sb(f"mv2_{b}", [128, 2], F32)
                rstd2 = k4.sb(f"rstd2_{b}", [128, 1], F32)
                s_wo = k4.sem(f"a2_wo{b}")
                g1_bc = k4.sb(f"g1_bc{b}", [128, D], F32)
                b1_bc = k4.sb(f"b1_bc{b}", [128, D], F32)
                s_g1 = k4.sem(f"a2_g1{b}")
                k4.inc(SP.dma_start(out=g1_bc[:], in_=ln1_g.partition_broadcast(128)), s_g1, 16)
                k4.wait(DVE, k4.inc(SP.dma_start(out=b1_bc[:], in_=ln1_b.partition_broadcast(128)), s_g1, 16))

                t_wo = k4.inc(POOL.dma_start(out=wo_sb[:], in_=w_out.rearrange("(k p) c -> p k c", p=128)), s_wo, 16)
                k4.wait(PE, t_wo)
                k4.wait(DVE, t_wo)

                def a2_load(n):
                    sx = ch_x.pslot(SP)
                    i = SP.dma_start(out=x_sb[sx][:], in_=xtm[b, n * 128:(n + 1) * 128, :])
                    ch_x.produced(ring_x.start(i, sx))

                def a2_pe_op(n):
                    so = ch_op.pslot(PE)
                    for hf in range(2):
                        for k in range(8):
                            i = PE.matmul(banks[2 * so + hf][:], catT[:, k, n * 128:(n + 1) * 128], wo_sb[:, k, hf * 512:(hf + 1) * 512],
                                          start=(k == 0), stop=(k == 7))
                    ch_op.produced(M(PE, i))

                def a2_dve_A(n):
                    so = ch_op.cslot(DVE)
                    sx = ch_x.cslot(DVE)
                    sh = n % 2
                    hf_ = hf_sb[sh]
                    for hf in range(2):
                        i = DVE.scalar_tensor_tensor(out=hf_[:, hf * 512:(hf + 1) * 512], in0=x_sb[sx][:, hf * 512:(hf + 1) * 512],
                                                     scalar=ALPHA, in1=banks[2 * so + hf][:], op0=ALU.mult, op1=ALU.add)
                    t = M(DVE, i)
                    ch_op.consumed(t)
                    ch_x.consumed(t)
                    for hf in range(2):
                        DVE.bn_stats(out=st2[:, hf, :], in_=hf_[:, hf * 512:(hf + 1) * 512])
                    i = DVE.bn_aggr(out=mv2[:], in_=st2[:].rearrange("p a c -> p (a c)"))
                    kb.wait(ACT, M(DVE, i))
                    i = ACT.activation(out=rstd2[:], in_=mv2[:, 1:2], func=AF.Sqrt, bias=eps_t[:, 0:1], scale=1.0)
                    kb.wait(DVE, M(ACT, i))
                    DVE.reciprocal(out=rstd2[:], in_=rstd2[:])
                    t = M(DVE, DVE.tensor_scalar(out=hf_[:], in0=hf_[:], scalar1=mv2[:, 0:1], scalar2=rstd2[:, 0:1],
                                                 op0=ALU.subtract, op1=ALU.mult))
                    kb.wait(POOL, t)
                    POOL.tensor_tensor(out=hf_[:], in0=hf_[:], in1=g1_bc[:], op=ALU.mult)
                    t = M(POOL, POOL.tensor_tensor(out=hf_[:], in0=hf_[:], in1=b1_bc[:], op=ALU.add))
                    kb.wait(ACT, t)
                    ACT.activation(out=hb_all[:, n, :], in_=hf_[:], func=AF.Copy)
                    tC = M(ACT, ACT.activation(out=ybuf[:, n, :], in_=hf_[:], func=AF.Copy, scale=ALPHA))
                    pend_D.append((n, sh, tC))

                def a2_dve_D():
                    n, sh, tC = pend_D.pop(0)
                    kb.wait(DVE, tC)
                    sl_ = ch_hb.pslot(DVE)
                    t = M(DVE, DVE.tensor_tensor(out=lo_sb[sl_][:], in0=hf_sb[sh][:], in1=hb_all[:, n, :], op=ALU.subtract))
                    ch_hb.produced(t)

                def a2_pe_tp(n):
                    sh = ch_hb.cslot(PE)
                    st = ch_tp.pslot(PE)
                    tpv = banks[6 + st][:].bitcast(BF16)
                    for k in range(8):
                        i = PE.transpose(tpv[:, k * 128:(k + 1) * 128], hb_all[:, n, k * 128:(k + 1) * 128], ident[:])
                    ch_tp.produced(M(PE, i))
                    st = ch_tp.pslot(PE)
                    tpv = banks[6 + st][:].bitcast(BF16)
                    for k in range(8):
                        i = PE.transpose(tpv[:, k * 128:(k + 1) * 128], lo_sb[sh][:, k * 128:(k + 1) * 128], ident[:])
                    t = M(PE, i)
                    ch_hb.consumed(t)
                    ch_tp.produced(t)

                def a2_act_tp(n):
                    st = ch_tp.cslot(ACT)
                    tpv = banks[6 + st][:].bitcast(BF16)
                    sht = ch_ht.pslot(ACT)
                    i = ACT.activation(out=hT_sb[sht][:], in_=tpv.rearrange("p (k t) -> p k t", k=8), func=AF.Copy)
                    t_h = M(ACT, i)
                    ch_tp.consumed(t_h)
                    ch_ht.produced(t_h)
                    st = ch_tp.cslot(ACT)
                    tpv = banks[6 + st][:].bitcast(BF16)
                    sl = ch_lo.pslot(ACT)
                    i = ACT.activation(out=loT_sb[sl][:], in_=tpv.rearrange("p (k t) -> p k t", k=8), func=AF.Copy)
                    t = M(ACT, i)
                    ch_tp.consumed(t)
                    ch_lo.produced(t)
                    return t_h

                def a2_pe_route(n, t_h):
                    sl = ch_lo.cslot(PE)
                    sht = ch_ht.cslot(PE)
                    ch_r.pslot(PE)
                    rp = banks[4]
                    for k in range(8):
                        PE.matmul(rp[:, 0:72], hT_sb[sht][:, k, :], wr_hl[:, k, 0:72], start=(k == 0), stop=False,
                                  skip_group_check=True)
                    for k in range(8):
                        i = PE.matmul(rp[:, 0:36], loT_sb[sl][:, k, :], wr_hl[:, k, 0:36], start=False, stop=(k == 7),
                                      skip_group_check=True)
                    t = M(PE, i)
                    ch_lo.consumed(t)
                    ch_ht.consumed(t)
                    ch_r.produced(t)

                def a2_route_copy(n):
                    ch_r.cslot(ACT)
                    kb.wait(ACT, math_done[0])
                    t = M(ACT, ACT.activation(out=lg_all[:, n % NH, :], in_=banks[4][:, 0:72], func=AF.Copy))
                    ch_r.consumed(t)
                    return t

                def a2_route_math(n0, t_last):
                    kb.wait(DVE, t_last)
                    sl = slice(n0, n0 + NH)
                    L = lg_all[:, :, 0:36]
                    DVE.tensor_tensor(out=L, in0=L, in1=lg_all[:, :, 36:72], op=ALU.add)
                    DVE.tensor_tensor(out=L, in0=L, in1=br_bc[:].unsqueeze(1).broadcast_to([128, NH, 36]), op=ALU.add)
                    gmax, oh, ge, sume = rq[:, :, 0:1], rq[:, :, 1:5], rq[:, :, 5:9], rq[:, :, 9:10]
                    esel, m1, eq1, e2 = rq[:, :, 11:19], rq[:, :, 19:20], rq[:, :, 20:28], rq[:, :, 28:36]
                    m2, eq2, dd, w1, w2, tmp8 = rq[:, :, 36:37], rq[:, :, 37:45], rq[:, :, 45:46], rq[:, :, 46:47], rq[:, :, 47:48], rq[:, :, 48:56]
                    bc = lambda ap, k: ap.broadcast_to([128, NH, k])
                    DVE.tensor_reduce(out=gmax, in_=lg_all[:, :, 0:4], axis=AX.X, op=ALU.max)
                    DVE.tensor_tensor(out=oh, in0=lg_all[:, :, 0:4], in1=bc(gmax, 4), op=ALU.is_equal)
                    DVE.tensor_tensor(out=ge, in0=lg_all[:, :, 0:4], in1=bc(gmax, 4), op=ALU.subtract)
                    DVE.tensor_tensor(out=esel, in0=lg_all[:, :, 4:12], in1=bc(oh[:, :, 0:1], 8), op=ALU.mult)
                    for g in range(1, 4):
                        DVE.tensor_tensor(out=tmp8, in0=lg_all[:, :, 4 + 8 * g:12 + 8 * g], in1=bc(oh[:, :, g:g + 1], 8), op=ALU.mult)
                        DVE.tensor_tensor(out=esel, in0=esel, in1=tmp8, op=ALU.add)
                    DVE.tensor_reduce(out=m1, in_=esel, axis=AX.X, op=ALU.max)
                    DVE.tensor_tensor(out=eq1, in0=esel, in1=bc(m1, 8), op=ALU.is_equal)
                    DVE.scalar_tensor_tensor(out=e2, in0=eq1, scalar=-1e30, in1=esel, op0=ALU.mult, op1=ALU.add)
                    DVE.tensor_reduce(out=m2, in_=e2, axis=AX.X, op=ALU.max)
                    DVE.tensor_tensor(out=eq2, in0=e2, in1=bc(m2, 8), op=ALU.is_equal)
                    i = DVE.tensor_tensor(out=dd, in0=m2, in1=m1, op=ALU.subtract)
                    kb.wait(ACT, M(DVE, i))
                    ACT.activation(out=ge, in_=ge, func=AF.Exp)
                    i = ACT.activation(out=dd, in_=dd, func=AF.Exp)
                    kb.wait(DVE, M(ACT, i))
                    DVE.tensor_reduce(out=sume, in_=ge, axis=AX.X, op=ALU.add)
                    DVE.reciprocal(out=sume, in_=sume)
                    DVE.tensor_scalar(out=w1, in0=dd, scalar1=1.0, scalar2=None, op0=ALU.add)
                    DVE.reciprocal(out=w1, in_=w1)
                    DVE.tensor_tensor(out=w2, in0=dd, in1=w1, op=ALU.mult)
                    DVE.tensor_tensor(out=g12[:, 0, sl].unsqueeze(2), in0=w1, in1=sume, op=ALU.mult)
                    DVE.tensor_tensor(out=g12[:, 1, sl].unsqueeze(2), in0=w2, in1=sume, op=ALU.mult)
                    for g in range(4):
                        DVE.tensor_tensor(out=M1a[:, sl, g * 8:(g + 1) * 8], in0=eq1, in1=bc(oh[:, :, g:g + 1], 8), op=ALU.mult)
                        tm = DVE.tensor_tensor(out=M2a[:, sl, g * 8:(g + 1) * 8], in0=eq2, in1=bc(oh[:, :, g:g + 1], 8), op=ALU.mult)
                    math_done[0] = M(DVE, tm)

                a2_load(0)
                t_hs = {}
                for n in range(NT + 3):
                    if n + 1 < NT:
                        a2_load(n + 1)
                    if n < NT:
                        a2_pe_op(n)
                        a2_dve_A(n)
                    if 1 <= n <= NT:
                        a2_dve_D()
                    if 2 <= n <= NT + 1:
                        a2_pe_tp(n - 2)
                        t_hs[n - 2] = a2_act_tp(n - 2)
                    if 3 <= n:
                        a2_pe_route(n - 3, t_hs[n - 3])
                        t_c = a2_route_copy(n - 3)
                        if (n - 3) % NH == NH - 1:
                            a2_route_math(n - 3 - (NH - 1), t_c)
                barrier()

            if DEBUG and b == DEBUG_B:
                with contextlib.ExitStack() as esd:
                    kd = KB(nc, esd, kb.waited, kb.prog)
                    sd = kd.sem("dbg2")
                    kd.inc(SP.dma_start(out=dbg_y, in_=ybuf[:]), sd, 16)
                    kd.wait(SP, kd.inc(SP.dma_start(out=dbg_gate, in_=M1a[:]), sd, 16))
                    barrier()
            if not DEBUG or b == DEBUG_B:
                stop_if("A2")

            with contextlib.ExitStack() as es5:
                k5 = KB(nc, es5, kb.waited, kb.prog)
                NWS = 3
                wgu_sb = [catT[:, 2 * i:2 * i + 2, :].rearrange("p a (k f) -> p (a k) f", f=512) for i in range(3)]
                wdn_t = [k5.sb(f"wdn_t{b}_{i}", [128, 2, D], BF16) for i in range(2)]
                wdn_sb = [t_[:] for t_ in wdn_t] + [catT[:, 6, :].rearrange("p (k f) -> p k f", f=1024)]
                thr = k5.sb(f"thr{b}", [128, NJ, 32], F32)
                Mb = k5.sb(f"Mb{b}", [128, NT * 32], BF16)
                Ms = k5.sb(f"Ms{b}", [128, NT, 32], F32)
                cs = k5.sb(f"cs{b}", [128, NT, 32], F32)
                off = k5.sb(f"off{b}", [128, NT, 32], F32)
                Sf = k5.sb(f"Sf{b}", [128, NT, 32], F32)
                tS = Ms
                sc_a = k5.sb(f"sc_a{b}", [128, 32], F32)
                sc_b = k5.sb(f"sc_b{b}", [128, 32], F32)
                pt = k5.sb(f"pt{b}", [128, 32], F32)
                base = k5.sb(f"base{b}", [128, 32], F32)
                qi = k5.sb(f"qi{b}", [128, 32], I32)
                sl_f = k5.sb(f"sl_f{b}", [128, 2, NT], F32)
                sl_i = k5.sb(f"sl_i{b}", [128, 2, NT], I32)
                ej_f = k5.sb(f"ej_f{b}", [128, NJ], F32)
                wi_f = thr[:].rearrange("p j e -> p (j e)")[:, 0:NJ * 5].rearrange("p (j c) -> p j c", c=5)
                wi_i = k5.sb(f"wi_i{b}", [128, NJ, 5], I32)
                pidx = k5.sb(f"pidx{b}", [128, 1], F32)
                ko = k5.sb(f"ko{b}", [128, 8], F32)
                Lst = k5.sb(f"Lst{b}", [128, 128], BF16)
                xt_sb = [k5.sb(f"xt_sb{b}_{i}", [128, 2, D], BF16) for i in range(2)]
                xsT = [k5.sb(f"xsT{b}_{i}", [128, 8, 256], BF16) for i in range(2)]
                sl_sb = [k5.sb(f"sl_sb{b}_{i}", [128, 512], F32) for i in range(2)]
                at_sb = [k5.sb(f"at_sb{b}_{i}", [128, 2, 256], BF16) for i in range(2)]
                yo_sb = [k5.sb(f"yo_sb{b}_{i}", [128, D], F32) for i in range(2)]
                r_sb = yo_sb

                POOL.affine_select(out=Lst[:], in_=ones_bf[:], pattern=[[1, 128]], compare_op=ALU.is_gt,
                                   fill=0.0, base=0, channel_multiplier=-1)
                t_thr = M(POOL, POOL.iota(thr[:], pattern=[[256, NJ], [0, 32]], base=0, channel_multiplier=0,
                                          allow_small_or_imprecise_dtypes=True))
                POOL.iota(pidx[:], pattern=[[0, 1]], base=0, channel_multiplier=1, allow_small_or_imprecise_dtypes=True)
                t_io = M(POOL, POOL.iota(ko[:], pattern=[[128, 8]], base=0, channel_multiplier=0, allow_small_or_imprecise_dtypes=True))
                DVE.tensor_tensor(out=Ms[:], in0=M1a[:], in1=M2a[:], op=ALU.add)
                t = M(DVE, DVE.tensor_copy(out=Mb[:], in_=Ms[:].rearrange("p n e -> p (n e)")))
                kb.wait(PE, t)
                kb.wait(PE, t_thr)
                PE.matmul(banks[0][:], Lst[:], Mb[:], start=True, stop=True)
                t = M(PE, PE.matmul(banks[1][:], ones_bf[:], Mb[:], start=True, stop=True))
                kb.wait(DVE, t)
                DVE.tensor_copy(out=cs[:], in_=banks[1][:].rearrange("p (n e) -> p n e", e=32))
                DVE.memset(off[:, 0, :], 0.0)
                for n in range(1, NT):
                    DVE.tensor_tensor(out=off[:, n, :], in0=off[:, n - 1, :], in1=cs[:, n - 1, :], op=ALU.add)
                DVE.tensor_tensor(out=sc_a[:], in0=off[:, NT - 1, :], in1=cs[:, NT - 1, :], op=ALU.add)
                DVE.tensor_scalar(out=sc_b[:], in0=sc_a[:], scalar1=127.5, scalar2=1.0 / 256.0, op0=ALU.add, op1=ALU.mult)
                DVE.tensor_copy(out=qi[:], in_=sc_b[:])
                DVE.tensor_scalar(out=pt[:], in0=qi[:], scalar1=256.0, scalar2=None, op0=ALU.mult)
                DVE.tensor_copy(out=sc_a[:], in_=pt[:])
                pa, pb = sc_a, sc_b
                for sh in (1, 2, 4, 8, 16):
                    DVE.tensor_copy(out=pb[:, 0:sh], in_=pa[:, 0:sh])
                    DVE.tensor_tensor(out=pb[:, sh:32], in0=pa[:, sh:32], in1=pa[:, 0:32 - sh], op=ALU.add)
                    pa, pb = pb, pa
                incl = pa
                DVE.tensor_tensor(out=base[:], in0=incl[:], in1=pt[:], op=ALU.subtract)
                DVE.tensor_tensor(out=Sf[:], in0=banks[0][:].rearrange("p (n e) -> p n e", e=32), in1=off[:], op=ALU.add)
                DVE.tensor_tensor(out=Sf[:], in0=Sf[:], in1=base[:].unsqueeze(1).broadcast_to([128, NT, 32]), op=ALU.add)
                DVE.tensor_tensor(out=tS[:], in0=Sf[:], in1=M1a[:], op=ALU.mult)
                DVE.tensor_reduce(out=sl_f[:, 0, :], in_=tS[:], axis=AX.X, op=ALU.add)
                DVE.tensor_tensor(out=tS[:], in0=Sf[:], in1=M2a[:], op=ALU.mult)
                DVE.tensor_reduce(out=sl_f[:, 1, :], in_=tS[:], axis=AX.X, op=ALU.add)
                DVE.tensor_copy(out=sl_i[:], in_=sl_f[:])
                kb.wait(DVE, t_thr)
                DVE.tensor_tensor(out=thr[:], in0=thr[:], in1=incl[:].unsqueeze(1).broadcast_to([128, NJ, 32]), op=ALU.is_ge)
                DVE.tensor_reduce(out=ej_f[:], in_=thr[:], axis=AX.X, op=ALU.add)
                kb.wait(DVE, t_io)
                DVE.tensor_scalar(out=ej_f[:], in0=ej_f[:], scalar1=512.0, scalar2=pidx[:, 0:1], op0=ALU.mult, op1=ALU.add)
                DVE.tensor_tensor(out=wi_f[:, :, 0:4], in0=ej_f[:].unsqueeze(2).broadcast_to([128, NJ, 4]),
                                  in1=ko[:, 0:4].unsqueeze(1).broadcast_to([128, NJ, 4]), op=ALU.add)
                DVE.tensor_scalar(out=ej_f[:], in0=ej_f[:], scalar1=pidx[:, 0:1], scalar2=0.25, op0=ALU.subtract, op1=ALU.mult)
                DVE.tensor_scalar(out=wi_f[:, :, 4:5], in0=ej_f[:].unsqueeze(2), scalar1=pidx[:, 0:1], scalar2=None, op0=ALU.add)
                t_disp = M(DVE, DVE.tensor_copy(out=wi_i[:], in_=wi_f))

                kb.wait(POOL, t_disp)
                for n in range(NT):
                    for kk in range(2):
                        i = POOL.indirect_dma_start(out=xs_d, out_offset=bass.IndirectOffsetOnAxis(ap=sl_i[:, kk, n:n + 1], axis=0),
                                                    in_=hb_all[:, n, :], in_offset=None)
                        kb.inc(i, s_sc, 16)
                tok_sc = (s_sc, s_sc.n)
                kb.wait(SP, tok_sc)
                kb.wait(POOL, tok_sc)

                wgu_rows = w_gu.rearrange("e (d2 i) f -> (e d2) (i f)", i=2)
                wdn_rows = w_dn.rearrange("e (f2 i) d -> (e f2) (i d)", i=2)

                def m_load_w(j):
                    sw = ch_wt.pslot(POOL)
                    gv = wgu_sb[sw].rearrange("p (k2 i) f -> p k2 (i f)", i=2)
                    for k2 in range(4):
                        i = POOL.indirect_dma_start(out=gv[:, k2, :], out_offset=None, in_=wgu_rows,
                                                    in_offset=bass.IndirectOffsetOnAxis(ap=wi_i[:, j, k2:k2 + 1], axis=0),
                                                    bounds_check=rb_gu, oob_is_err=False)
                        t1 = ring_m.start(i, sw)
                    i = POOL.indirect_dma_start(out=wdn_sb[sw].rearrange("p i d -> p (i d)"), out_offset=None, in_=wdn_rows,
                                                in_offset=bass.IndirectOffsetOnAxis(ap=wi_i[:, j, 4:5], axis=0),
                                                bounds_check=rb_dn, oob_is_err=False)
                    t1 = ring_m.start(i, sw)
                    ch_wt.produced(t1)

                def m_load_x(j):
                    sx = ch_xs.pslot(SP)
                    i = SP.dma_start(out=xt_sb[sx][:], in_=xs_d[256 * j:256 * (j + 1), :].rearrange("(a p) d -> p a d", p=128))
                    ch_xs.produced(ring_xs.start(i, sx))

                def m_tp(j):
                    sx = ch_xs.cslot(PE)
                    ch_tp.pslot(PE)
                    ch_tp.pslot(PE)
                    for half in range(2):
                        tpv = banks[6 + half][:].bitcast(BF16).rearrange("p (k a t) -> p k a t", k=4, a=2)
                        for k4 in range(4):
                            for a_ in range(2):
                                k = half * 4 + k4
                                c0 = 256 * (k // 2) + (k % 2)
                                i = PE.transpose(tpv[:, k4, a_, :], xt_sb[sx][:, a_, c0:c0 + 255:2], ident[:])
                    t = M(PE, i)
                    ch_xs.consumed(t)
                    ch_tp.produced(t)
                    ch_tp.produced(t)
                    sT = ch_xT.pslot(ACT, DVE)
                    ch_tp.cslot(ACT)
                    ta = M(ACT, ACT.activation(out=xsT[sT][:, 0:4, :], in_=banks[6][:].bitcast(BF16).rearrange("p (k t) -> p k t", k=4), func=AF.Copy))
                    ch_tp.consumed(ta)
                    ch_tp.cslot(DVE)
                    td = M(DVE, DVE.tensor_copy(out=xsT[sT][:, 4:8, :], in_=banks[7][:].bitcast(BF16).rearrange("p (k t) -> p k t", k=4)))
                    ch_tp.consumed(td)
                    ch_xT.produced(ta, td)

                def m_gu(j):
                    sw = j % NWS
                    sT = ch_xT.cslot(PE)
                    for tok in ch_wt.ready[j]:
                        kb.wait(PE, tok)
                    sg = ch_g2.pslot(PE)
                    for part in range(2):
                        for fcp in range(2):
                            fc = part * 256 + fcp
                            for k in range(8):
                                i = PE.matmul(banks[2 * sg + part][:, fcp * 256:(fcp + 1) * 256], wgu_sb[sw][:, k, fc:fc + 255:2], xsT[sT][:, k, :],
                                              start=(fcp == 0 and k == 0), stop=(k == 7), skip_group_check=True)
                    t = M(PE, i)
                    ch_xT.consumed(t)
                    ch_g2.produced(t)
                    sg = ch_g2.cslot(ACT, 0)
                    ss = ch_sl.pslot(ACT)
                    t = M(ACT, ACT.activation(out=sl_sb[ss][:], in_=banks[2 * sg][:], func=AF.Silu))
                    ch_g2.consumed(t, 0)
                    ch_sl.produced(t)
                    sg = ch_g2.cslot(DVE, 1)
                    ss = ch_sl.cslot(DVE)
                    sa = ch_at.pslot(DVE)
                    t = M(DVE, DVE.tensor_tensor(out=at_sb[sa][:].rearrange("p c t -> p (c t)"), in0=banks[2 * sg + 1][:], in1=sl_sb[ss][:], op=ALU.mult))
                    ch_g2.consumed(t, 1)
                    ch_sl.consumed(t)
                    ch_at.produced(t)

                def m_dn(j):
                    sw = j % NWS
                    sa = ch_at.cslot(PE)
                    for a_ in range(2):
                        ch_dn.pslot(PE)
                        for hf in range(2):
                            for fc in range(2):
                                i = PE.matmul(banks[4 + hf][:], at_sb[sa][:, fc, a_ * 128:(a_ + 1) * 128],
                                              wdn_sb[sw][:, fc, hf * 512:(hf + 1) * 512], start=(fc == 0), stop=(fc == 1))
                        t = M(PE, i)
                        ch_dn.produced(t)
                        so = ch_yo.pslot(ACT, DVE)
                        ch_dn.cslot(ACT, 0)
                        ta = M(ACT, ACT.activation(out=yo_sb[so][:, 0:512], in_=banks[4][:], func=AF.Copy))
                        ch_dn.consumed(ta, 0)
                        ch_dn.cslot(DVE, 1)
                        td = M(DVE, DVE.tensor_copy(out=yo_sb[so][:, 512:1024], in_=banks[5][:]))
                        ch_dn.consumed(td, 1)
                        ch_yo.produced(ta, td)
                        so = ch_yo.cslot(SP)
                        i = SP.dma_start(out=ys_d[256 * j + 128 * a_:256 * j + 128 * (a_ + 1), :], in_=yo_sb[so][:])
                        ch_yo.consumed(ring_ys.start(i, so))
                        last_ys[so] = (ring_ys.sems[so], ring_ys.sems[so].n)
                    ch_at.consumed(t)
                    ch_wt.consumed(t)

                last_ys = {}
                m_load_w(0)
                m_load_w(1)
                m_load_x(0)
                m_load_x(1)
                m_tp(0)
                for j in range(NJ):
                    if j + 2 < NJ:
                        m_load_x(j + 2)
                    if j + 1 < NJ:
                        m_tp(j + 1)
                    if j >= 1:
                        m_dn(j - 1)
                    if j + 2 < NJ:
                        m_load_w(j + 2)
                    m_gu(j)
                m_dn(NJ - 1)
                for so, tk in last_ys.items():
                    kb.wait(POOL, tk)
                for n in range(NT):
                    for kk in range(2):
                        sr = ch_rg.pslot(POOL)
                        i = POOL.indirect_dma_start(out=r_sb[sr][:], out_offset=None, in_=ys_d,
                                                    in_offset=bass.IndirectOffsetOnAxis(ap=sl_i[:, kk, n:n + 1], axis=0))
                        ch_rg.produced(ring_g.start(i, sr))
                        sr = ch_rg.cslot(DVE)
                        t = M(DVE, DVE.scalar_tensor_tensor(out=ybuf[:, n, :], in0=r_sb[sr][:], scalar=g12[:, kk, n:n + 1], in1=ybuf[:, n, :],
                                                            op0=ALU.mult, op1=ALU.add))
                        ch_rg.consumed(t)
                barrier()
                stop_if("M")

            with contextlib.ExitStack() as es6:
                k6 = KB(nc, es6, kb.waited, kb.prog)
                o_sb = [k6.sb(f"o_sb{b}_{i}", [128, D], F32) for i in range(2)]
                st3 = k6.sb(f"st3_{b}", [128, 2, 6], F32)
                mv3 = k6.sb(f"mv3_{b}", [128, 2], F32)
                rstd3 = k6.sb(f"rstd3_{b}", [128, 1], F32)
                g2_bc = k6.sb(f"g2_bc{b}", [128, D], F32)
                b2_bc = k6.sb(f"b2_bc{b}", [128, D], F32)
                s_g2 = k6.sem(f"f_g2{b}")
                k6.inc(SP.dma_start(out=g2_bc[:], in_=ln2_g.partition_broadcast(128)), s_g2, 16)
                k6.wait(DVE, k6.inc(SP.dma_start(out=b2_bc[:], in_=ln2_b.partition_broadcast(128)), s_g2, 16))
                last_store = None
                for n in range(NT):
                    for hf in range(2):
                        DVE.bn_stats(out=st3[:, hf, :], in_=ybuf[:, n, hf * 512:(hf + 1) * 512])
                    i = DVE.bn_aggr(out=mv3[:], in_=st3[:].rearrange("p a c -> p (a c)"))
                    rstd_via_pool(mv3, rstd3[:], i)
                    so = ch_ot.pslot(DVE)
                    DVE.tensor_scalar(out=o_sb[so][:], in0=ybuf[:, n, :], scalar1=mv3[:, 0:1], scalar2=rstd3[:, 0:1],
                                      op0=ALU.subtract, op1=ALU.mult)
                    DVE.tensor_tensor(out=o_sb[so][:], in0=o_sb[so][:], in1=g2_bc[:], op=ALU.mult)
                    i = DVE.tensor_tensor(out=o_sb[so][:], in0=o_sb[so][:], in1=b2_bc[:], op=ALU.add)
                    ch_ot.produced(M(DVE, i))
                    so = ch_ot.cslot(SP)
                    i = SP.dma_start(out=out[b, n * 128:(n + 1) * 128, :], in_=o_sb[so][:])
                    t = ring_o.start(i, so)
                    ch_ot.consumed(t)
                    if n >= NT - 2:
                        kb.wait(SP, t) if n == NT - 2 else None
                        last_store = t
                        if n == NT - 2:
                            prev_store = t
                kb.wait(SP, prev_store)
                kb.wait(SP, last_store)
                barrier()
            esp.close()
    return nc


def _prep_inputs(inputs):
    x = np.ascontiguousarray(inputs["x"], dtype=np.float32)
    pos = np.ascontiguousarray(inputs["positions"], dtype=np.int32)
    w_r = np.concatenate([inputs["w_group"][0], np.transpose(inputs["w_expert"][0], (1, 0, 2)).reshape(D, 32)], axis=1)
    b_r = np.concatenate([inputs["b_group"][0], inputs["b_expert"][0].reshape(32)])[None, :]
    ws = inputs["w_spatial"][0]
    shared = {
        "w_in": np.ascontiguousarray(inputs["w_in"][0]),
        "w_out": np.ascontiguousarray(inputs["w_out"][0]),
        "ws_tgs": np.ascontiguousarray(np.transpose(ws, (1, 0, 2))),
        "ws_sgt": np.ascontiguousarray(np.transpose(ws, (2, 0, 1))),
        "bspT": np.ascontiguousarray(inputs["b_spatial"][0].T),
        "sgu_g": np.ascontiguousarray(inputs["sgu_ln_g"]),
        "sgu_b": np.ascontiguousarray(inputs["sgu_ln_b"]),
        "ln1_g": np.ascontiguousarray(inputs["ln1_g"]),
        "ln1_b": np.ascontiguousarray(inputs["ln1_b"]),
        "ln2_g": np.ascontiguousarray(inputs["ln2_g"]),
        "ln2_b": np.ascontiguousarray(inputs["ln2_b"]),
        "w_r": np.ascontiguousarray(w_r, dtype=np.float32),
        "b_r": np.ascontiguousarray(b_r, dtype=np.float32),
        "w_gu": np.ascontiguousarray(inputs["w_gate_up"][0].reshape(NE, D, 512)),
        "w_dn": np.ascontiguousarray(inputs["w_down"][0].reshape(NE, 256, D)),
    }
    in_maps = []
    for c in range(NCORES):
        xs = x[c * NB:(c + 1) * NB]
        m = dict(shared)
        m["xT"] = np.ascontiguousarray(np.transpose(xs, (0, 2, 1)))
        m["xtm"] = xs
        m["posT"] = np.ascontiguousarray(np.transpose(pos[c * NB:(c + 1) * NB].reshape(NB, NT, 128), (0, 2, 1)))
        in_maps.append(m)
    return in_maps


def kernel(**inputs):
    in_maps = _prep_inputs(inputs)
    nc = build_nc()
    res = run_bass_kernel_spmd(nc, in_maps, core_ids=list(range(NCORES)))
    return np.concatenate([r["out"] for r in res.results], axis=0).astype(np.float32)
```

```python
import contextlib
import numpy as np
import concourse.bass as bass
import concourse.mybir as mybir
from concourse.bass_utils import run_bass_kernel_spmd

F32, BF16, I32 = mybir.dt.float32, mybir.dt.bfloat16, mybir.dt.int32
AF = mybir.ActivationFunctionType
ALU = mybir.AluOpType
AX = mybir.AxisListType

NCORES = 8
S = 2048
D = 1024
NB = 2
NT = S // 128
ALPHA = float(2.0 ** 0.25)
EPS = 1e-5
NEG = -30000.0
NE = 32
TWO_PI = float(2 * np.pi)
DEBUG = False
DEBUG_B = 0
STOP_AFTER = None


class _Stop(Exception):
    pass


class Sem:
    _serial = 0

    def __init__(self, h):
        self.h = h
        self.n = 0
        Sem._serial += 1
        self.uid = Sem._serial


class KB:
    def __init__(self, nc, es, waited=None, prog=None):
        self.nc = nc
        self.es = es
        self.waited = {} if waited is None else waited
        self.prog = {} if prog is None else prog

    sem_es = None

    def sem(self, name):
        return Sem(KB.sem_es.enter_context(self.nc.semaphore(name)))

    def sb(self, name, shape, dt):
        return self.es.enter_context(self.nc.sbuf_tensor(name, shape, dt))

    def ps(self, name, shape, dt):
        return self.es.enter_context(self.nc.psum_tensor(name, shape, dt))

    def inc(self, instr, sem, k=1):
        instr.then_inc(sem.h, k)
        sem.n += k
        return (sem, sem.n)

    def mark(self, eng, instr):
        if isinstance(instr, Tok):
            return instr.tok
        return self.inc(instr, self.prog[id(getattr(eng, "raw", eng))])

    def wait(self, eng, tok):
        if tok is None:
            return
        sem, val = tok
        if val <= 0:
            return
        raw = getattr(eng, "raw", eng)
        key = (id(raw), sem.uid)
        if self.waited.get(key, 0) >= val:
            return
        self.waited[key] = val
        raw.wait_ge(sem.h, val)


class Tok:
    def __init__(self, instr, tok):
        self.instr = instr
        self.tok = tok


_COMPUTE = {"activation", "tensor_tensor", "tensor_scalar", "scalar_tensor_tensor", "tensor_copy", "tensor_reduce",
            "reciprocal", "bn_stats", "bn_aggr", "memset", "affine_select", "iota"}


class EngProxy:
    def __init__(self, raw, kb):
        self.raw = raw
        self.kb = kb
        self.last = None

    def __getattr__(self, name):
        attr = getattr(self.raw, name)
        if name not in _COMPUTE:
            return attr

        def wrapper(*a, **k):
            if self.last is not None:
                self.kb.wait(self, self.last)
            instr = attr(*a, **k)
            tok = self.kb.inc(instr, self.kb.prog[id(self.raw)])
            self.last = tok
            return Tok(instr, tok)
        return wrapper


class Chan:
    def __init__(self, kb, name, depth, ncons=1):
        self.kb = kb
        self.depth = depth
        self.ready = []
        self.free = []
        self.ci = [0] * ncons

    def pslot(self, *engs):
        i = len(self.ready)
        if i >= self.depth:
            for tok in self.free[i - self.depth]:
                for e in engs:
                    self.kb.wait(e, tok)
        return i % self.depth

    def produced(self, *toks):
        self.ready.append(list(toks))

    def cslot(self, eng, c=0):
        i = self.ci[c]
        for tok in self.ready[i]:
            self.kb.wait(eng, tok)
        return i % self.depth

    def consumed(self, tok, c=0):
        i = self.ci[c]
        while len(self.free) <= i:
            self.free.append([])
        self.free[i].append(tok)
        self.ci[c] += 1


class DmaRing:
    def __init__(self, kb, name, depth):
        self.kb = kb
        self.sems = [kb.sem(f"{name}{i}") for i in range(depth)]

    def start(self, instr, slot):
        return self.kb.inc(instr, self.sems[slot], 16)


def build_nc():
    nc = bass.Bass("TRN2", target_bir_lowering=False)
    dr = lambda name, shape, dt=F32: nc.dram_tensor(name, shape, dt, kind="ExternalInput").ap()
    xT = dr("xT", [NB, D, S])
    xtm = dr("xtm", [NB, S, D])
    posT = dr("posT", [NB, 128, NT], I32)
    w_in = dr("w_in", [D, 2560])
    w_out = dr("w_out", [D, D])
    ws_tgs = dr("ws_tgs", [128, 8, 128])
    ws_sgt = dr("ws_sgt", [128, 8, 128])
    bspT = dr("bspT", [128, 8])
    sgu_g = dr("sgu_g", [1, 512])
    sgu_b = dr("sgu_b", [1, 512])
    ln1_g = dr("ln1_g", [1, D])
    ln1_b = dr("ln1_b", [1, D])
    ln2_g = dr("ln2_g", [1, D])
    ln2_b = dr("ln2_b", [1, D])
    w_r = dr("w_r", [D, 36])
    b_r = dr("b_r", [1, 36])
    w_gu = dr("w_gu", [NE, D, 512])
    w_dn = dr("w_dn", [NE, 256, D])
    out = nc.dram_tensor("out", [NB, S, D], F32, kind="ExternalOutput").ap()
    if DEBUG:
        dbg_cat = nc.dram_tensor("dbg_cat", [128, 8, S], F32, kind="ExternalOutput").ap()
        dbg_y = nc.dram_tensor("dbg_y", [128, NT, D], F32, kind="ExternalOutput").ap()
        dbg_gate = nc.dram_tensor("dbg_gate", [128, NT, 32], F32, kind="ExternalOutput").ap()

    PE, ACT, DVE, POOL, SP = nc.tensor, nc.scalar, nc.vector, nc.gpsimd, nc.sync
    engines = [PE, ACT, DVE, POOL, SP]

    with contextlib.suppress(_Stop), contextlib.ExitStack() as es:
        KB.sem_es = es
        kb = KB(nc, es)
        progs = [{id(getattr(e, "raw", e)): kb.sem(f"prog{bb}_{n}") for e, n in zip(engines, "pe act dve pool sp".split())} for bb in range(NB + 1)]
        kb.prog = progs[NB]
        M = kb.mark
        ACT, DVE, POOL = EngProxy(nc.scalar, kb), EngProxy(nc.vector, kb), EngProxy(nc.gpsimd, kb)
        engines = [PE, ACT, DVE, POOL, SP]
        banks = [kb.ps(f"bank{i}", [128, 512], F32) for i in range(8)]

        ident = kb.sb("ident", [128, 128], BF16)
        ones_bf = kb.sb("ones_bf", [128, 128], BF16)
        ones_z = kb.sb("ones_z", [128, 2, 128], BF16)
        zer = kb.sb("zer", [128, 512], F32)
        mhalf = kb.sb("mhalf", [128, 1], F32)
        m_cur = kb.sb("m_cur", [128, 512], BF16)
        m_prev = kb.sb("m_prev", [128, 512], BF16)
        m3 = kb.sb("m3", [128, 4, 512], BF16)
        wmT = kb.sb("wmT", [128, 8, 128], BF16)
        rs = kb.sb("rs", [128, 8], F32)
        bsp_sb = kb.sb("bsp_sb", [128, 8], F32)
        sg_bc = kb.sb("sg_bc", [128, 512], F32)
        sb_bc = kb.sb("sb_bc", [128, 512], F32)
        Bp = kb.sb("Bp", [128, 512], F32)
        br_bc = kb.sb("br_bc", [128, 36], F32)
        wr_hl = kb.sb("wr_hl", [128, 8, 72], BF16)
        invf = kb.sb("invf", [128, NT, 8], F32)
        catT = kb.sb("catT", [128, 8, S], BF16)

        s_c = kb.sem("const_dma")
        s_bar = kb.sem("barrier")

        def barrier():
            base = s_bar.n
            for e in engines:
                if isinstance(e, EngProxy) and e.last is not None:
                    kb.wait(e, e.last)
                kb.inc(e.nop(), s_bar)
            for e in engines:
                kb.wait(e, (s_bar, base + len(engines)))

        def stop_if(tag):
            if STOP_AFTER == tag:
                barrier()
                raise _Stop()

        def wait_all(tok):
            for e in engines:
                kb.wait(e, tok)

        with contextlib.ExitStack() as es0:
            k0 = KB(nc, es0, kb.waited, kb.prog)
            wtmp = k0.sb("wtmp", [128, 8, 128], F32)
            wtmp2 = k0.sb("wtmp2", [128, 8, 128], F32)
            wr_f = k0.sb("wr_f", [128, 8, 36], F32)
            wr_t = k0.sb("wr_t", [128, 8, 36], F32)

            def dma_c(o, i):
                kb.inc(SP.dma_start(out=o, in_=i), s_c, 16)

            dma_c(wtmp[:], ws_sgt)
            dma_c(wtmp2[:], ws_tgs)
            dma_c(bsp_sb[:], bspT)
            dma_c(sg_bc[:], sgu_g.partition_broadcast(128))
            dma_c(sb_bc[:], sgu_b.partition_broadcast(128))
            dma_c(br_bc[:], b_r.partition_broadcast(128))
            dma_c(wr_f[:], w_r.rearrange("(k p) c -> p k c", p=128))
            tok_cdma = (s_c, s_c.n)

            POOL.memset(zer[:], 0.0)
            POOL.memset(ones_bf[:], 1.0)
            POOL.memset(ones_z[:], 0.0)
            POOL.memset(ones_z[:, 0, 0:64], 1.0)
            POOL.memset(ones_z[:, 1, 64:128], 1.0)
            POOL.memset(mhalf[:], -0.5)
            POOL.affine_select(out=ident[:], in_=ones_bf[:], pattern=[[1, 128]], compare_op=ALU.is_equal,
                               fill=0.0, base=0, channel_multiplier=-1)
            POOL.affine_select(out=m_cur[:], in_=zer[:], pattern=[[0, 4], [1, 128]], compare_op=ALU.is_ge,
                               fill=NEG, base=0, channel_multiplier=-1)
            POOL.affine_select(out=m_prev[:], in_=zer[:], pattern=[[0, 4], [-1, 128]], compare_op=ALU.is_ge,
                               fill=NEG, base=0, channel_multiplier=1)
            for s in range(4):
                POOL.affine_select(out=m3[:, s, :], in_=zer[:], pattern=[[0, 16], [1, 32]], compare_op=ALU.is_ge,
                                   fill=NEG, base=32 * s, channel_multiplier=-1)
            inv = (np.float32(500000.0) ** (-(np.arange(0, 16, 2, dtype=np.float32)) / np.float32(16))).astype(np.float32)
            for i in range(8):
                POOL.memset(invf[:, :, i:i + 1], float(inv[i]))
            kb.wait(POOL, tok_cdma)
            POOL.affine_select(out=wmT[:], in_=wtmp[:], pattern=[[0, 8], [1, 128]], compare_op=ALU.is_ge,
                               fill=0.0, base=0, channel_multiplier=-1)
            i_last = POOL.affine_select(out=wtmp[:], in_=wtmp2[:], pattern=[[0, 8], [-1, 128]], compare_op=ALU.is_ge,
                                        fill=0.0, base=0, channel_multiplier=1)
            tok_cpool = M(POOL, i_last)

            kb.wait(DVE, tok_cdma)
            kb.wait(DVE, tok_cpool)
            DVE.tensor_reduce(out=rs[:], in_=wtmp[:], axis=AX.X, op=ALU.add)
            for g in range(8):
                DVE.tensor_scalar(out=Bp[:, g * 64:(g + 1) * 64], in0=sb_bc[:, g * 64:(g + 1) * 64],
                                  scalar1=rs[:, g:g + 1], scalar2=bsp_sb[:, g:g + 1], op0=ALU.mult, op1=ALU.add)
            DVE.tensor_copy(out=wr_hl[:, :, 0:36], in_=wr_f[:])
            DVE.tensor_copy(out=wr_t[:], in_=wr_hl[:, :, 0:36])
            DVE.tensor_tensor(out=wr_t[:], in0=wr_f[:], in1=wr_t[:], op=ALU.subtract)
            i_last = DVE.tensor_copy(out=wr_hl[:, :, 36:72], in_=wr_t[:])
            tok_cdve = M(DVE, i_last)
            wait_all(tok_cdma)
            wait_all(tok_cpool)
            wait_all(tok_cdve)
            barrier()
        stop_if("const")

        def rstd_via_pool(mv, dst, last_dve_instr):
            t = M(DVE, last_dve_instr)
            kb.wait(POOL, t)
            POOL.tensor_scalar(out=dst, in0=mv[:, 1:2], scalar1=EPS, scalar2=None, op0=ALU.add)
            i = POOL.tensor_tensor(out=dst, in0=dst, in1=mhalf[:], op=ALU.pow)
            kb.wait(DVE, M(POOL, i))

        ring_x = DmaRing(kb, "ring_x", 2)
        ring_w = DmaRing(kb, "ring_w", 2)
        ring_o = DmaRing(kb, "ring_o", 2)
        ring_m = DmaRing(kb, "ring_m", 4)
        ring_wg = DmaRing(kb, "ring_wg", 3)
        ring_wd = DmaRing(kb, "ring_wd", 4)
        ring_xs3 = DmaRing(kb, "ring_xs3", 3)
        ring_xs = DmaRing(kb, "ring_xs", 2)
        ring_ys = DmaRing(kb, "ring_ys", 2)
        ring_g = DmaRing(kb, "ring_g", 2)
        ring_g4 = DmaRing(kb, "ring_g4", 4)
        s_sc = kb.sem("scatter")
        rb_gu = es.enter_context(nc.gpsimd.register("rb_gu"))
        rb_dn = es.enter_context(nc.gpsimd.register("rb_dn"))
        nc.gpsimd.reg_mov(rb_gu, NE * 256 - 1)
        nc.gpsimd.reg_mov(rb_dn, NE * 128 - 1)
        NJ = 48
        xs_d = nc.dram_tensor("xs_scratch", [NJ * 256, D], BF16, kind="Internal").ap()
        ys_d = nc.dram_tensor("ys_scratch", [NJ * 256, D], F32, kind="Internal").ap()

        for b in range(NB):
            kb.prog = progs[b]
            ch_u = Chan(kb, "ch_u", 2)
            ch_v = Chan(kb, "ch_v", 2)
            ch_z = Chan(kb, "ch_z", 2)
            ch_tp = Chan(kb, "ch_tp", 2)
            ch_n = Chan(kb, "ch_n", 2)
            ch_gu = Chan(kb, "ch_gu", 2)
            ch_gv = Chan(kb, "ch_gv", 2)
            ch_sg = Chan(kb, "ch_sg", 2)
            ch_qk = Chan(kb, "ch_qk", 2)
            ch_rot = Chan(kb, "ch_rot", 2)
            ch_qs = Chan(kb, "ch_qs", 2)
            ch_vp = Chan(kb, "ch_vp", 2)
            ch_s = Chan(kb, "ch_s", 4)
            ch_p = Chan(kb, "ch_p", 6)
            ch_o = Chan(kb, "ch_o", 2)
            ch_op = Chan(kb, "ch_op", 2)
            ch_x = Chan(kb, "ch_x", 2)
            ch_hb = Chan(kb, "ch_hb", 2)
            ch_hf = Chan(kb, "ch_hf", 2)
            ch_lo = Chan(kb, "ch_lo", 2)
            ch_ht = Chan(kb, "ch_ht", 2)
            ch_r = Chan(kb, "ch_r", 1)
            ch_g2 = Chan(kb, "ch_g2", 2, ncons=2)
            ch_sl = Chan(kb, "ch_sl", 2)
            ch_at = Chan(kb, "ch_at", 3)
            ch_wg = Chan(kb, "ch_wg", 3)
            ch_wd = Chan(kb, "ch_wd", 4)
            ch_dn = Chan(kb, "ch_dn", 1, ncons=2)
            ch_wt = Chan(kb, "ch_wt", 3)
            ch_ot = Chan(kb, "ch_ot", 2)
            ch_xs = Chan(kb, "ch_xs", 2)
            ch_xT = Chan(kb, "ch_xT", 2)
            ch_yo = Chan(kb, "ch_yo", 2)
            ch_rg = Chan(kb, "ch_rg", 4)

            with contextlib.ExitStack() as es1:
                k1 = KB(nc, es1, kb.waited, kb.prog)
                xT_sb = k1.sb(f"xT_sb{b}", [128, 8, S], BF16)
                wsec = k1.sb(f"wsec{b}", [128, 8, 1024], BF16)
                pos_i = k1.sb(f"pos_i{b}", [128, NT], I32)
                pos_f = k1.sb(f"pos_f{b}", [128, NT], F32)
                ang = k1.sb(f"ang{b}", [128, 2, NT, 8], F32)
                kk_i = k1.sb(f"kk_i{b}", [128, 2, NT, 8], I32)
                kk_f = k1.sb(f"kk_f{b}", [128, 2, NT, 8], F32)
                rr = k1.sb(f"rr{b}", [128, 2, NT, 8], F32)
                mm = k1.sb(f"mm{b}", [128, 2, NT, 8], F32)
                cs_t = k1.sb(f"cs_t{b}", [128, 2, NT, 8], F32)
                s_ld = k1.sem(f"a1_ld{b}")
                s_w = k1.sem(f"a1_w{b}")

                for k in range(8):
                    k1.inc(POOL.dma_start(out=xT_sb[:, k, :], in_=xT[b, k * 128:(k + 1) * 128, :]), s_ld, 16)
                tok_x = (s_ld, s_ld.n)
                s_pos = k1.sem(f"a1_pos{b}")
                tok_pos = k1.inc(SP.dma_start(out=pos_i[:], in_=posT[b]), s_pos, 16)
                k1.inc(POOL.dma_start(out=wsec[:], in_=w_in[:, 1536:2560].rearrange("(k p) c -> p k c", p=128)), s_w, 16)
                tok_w = (s_w, s_w.n)

                k1.wait(DVE, tok_pos)
                DVE.tensor_copy(out=pos_f[:], in_=pos_i[:])
                DVE.tensor_tensor(out=ang[:, 1], in0=invf[:], in1=pos_f[:].unsqueeze(2).broadcast_to([128, NT, 8]), op=ALU.mult)
                DVE.tensor_scalar(out=ang[:, 0], in0=ang[:, 1], scalar1=float(np.pi / 2), scalar2=None, op0=ALU.add)
                DVE.tensor_scalar(out=kk_f[:], in0=ang[:], scalar1=float(1.0 / TWO_PI), scalar2=None, op0=ALU.mult)
                DVE.tensor_copy(out=kk_i[:], in_=kk_f[:])
                DVE.tensor_copy(out=kk_f[:], in_=kk_i[:])
                DVE.scalar_tensor_tensor(out=rr[:], in0=kk_f[:], scalar=-TWO_PI, in1=ang[:], op0=ALU.mult, op1=ALU.add)
                DVE.tensor_scalar(out=mm[:], in0=rr[:], scalar1=float(np.pi), scalar2=None, op0=ALU.is_gt)
                DVE.scalar_tensor_tensor(out=rr[:], in0=mm[:], scalar=-TWO_PI, in1=rr[:], op0=ALU.mult, op1=ALU.add)
                DVE.tensor_scalar(out=mm[:], in0=rr[:], scalar1=float(-np.pi), scalar2=None, op0=ALU.is_lt)
                i_l = DVE.scalar_tensor_tensor(out=rr[:], in0=mm[:], scalar=TWO_PI, in1=rr[:], op0=ALU.mult, op1=ALU.add)
                k1.wait(ACT, M(DVE, i_l))
                i_l = ACT.activation(out=cs_t[:], in_=rr[:], func=AF.Sin)
                tok_cs = M(ACT, i_l)
                stop_if("rope")

                with contextlib.ExitStack() as es2:
                    k2 = KB(nc, es2, kb.waited, kb.prog)
                    gu_sb = [k2.sb(f"gu_sb{b}_{i}", [128, 512], F32) for i in range(2)]
                    gv_sb = [k2.sb(f"gv_sb{b}_{i}", [128, 512], F32) for i in range(2)]
                    n_sb = [k2.sb(f"n_sb{b}_{i}", [128, 512], BF16) for i in range(2)]
                    t1_sb = k2.sb(f"t1_sb{b}", [128, 512], F32)
                    sg_sb = [k2.sb(f"sg_sb{b}_{i}", [128, 512], BF16) for i in range(2)]
                    st_sb = k2.sb(f"st_sb{b}", [128, 6], F32)
                    mv_sb = k2.sb(f"mv_sb{b}", [128, 2], F32)
                    rstd_sb = k2.sb(f"rstd_sb{b}", [128, 1], F32)

                    k2.wait(PE, tok_x)
                    k2.wait(PE, tok_w)

                    def sgu_pe_front(n):
                        su = ch_u.pslot(PE)
                        for k in range(8):
                            i = PE.matmul(banks[0 + su][:], xT_sb[:, k, n * 128:(n + 1) * 128], wsec[:, k, 0:512],
                                          start=(k == 0), stop=(k == 7))
                        ch_u.produced(M(PE, i))
                        sv = ch_v.pslot(PE)
                        for k in range(8):
                            i = PE.matmul(banks[2 + sv][:], xT_sb[:, k, n * 128:(n + 1) * 128], wsec[:, k, 512:1024],
                                          start=(k == 0), stop=(k == 7))
                        ch_v.produced(M(PE, i))

                    def sgu_act(n):
                        su = ch_u.cslot(ACT)
                        sg = ch_gu.pslot(ACT)
                        t = M(ACT, ACT.activation(out=gu_sb[sg][:], in_=banks[0 + su][:], func=AF.Gelu))
                        ch_u.consumed(t)
                        ch_gu.produced(t)
                        sv = ch_v.cslot(ACT)
                        sg = ch_gv.pslot(ACT)
                        t = M(ACT, ACT.activation(out=gv_sb[sg][:], in_=banks[2 + sv][:], func=AF.Gelu))
                        ch_v.consumed(t)
                        ch_gv.produced(t)

                    def sgu_dve_norm(n):
                        sg = ch_gv.cslot(DVE)
                        DVE.bn_stats(out=st_sb[:], in_=gv_sb[sg][:])
                        i = DVE.bn_aggr(out=mv_sb[:], in_=st_sb[:])
                        rstd_via_pool(mv_sb, rstd_sb[:], i)
                        sn = ch_n.pslot(DVE)
                        t = M(DVE, DVE.tensor_scalar(out=n_sb[sn][:], in0=gv_sb[sg][:], scalar1=mv_sb[:, 0:1], scalar2=rstd_sb[:, 0:1],
                                                     op0=ALU.subtract, op1=ALU.mult))
                        ch_gv.consumed(t)
                        ch_n.produced(t)

                    def sgu_pe_z(n):
                        sn = ch_n.cslot(PE)
                        sz = ch_z.pslot(PE)
                        for g in range(8):
                            i = PE.matmul(banks[4 + sz][:, g * 64:(g + 1) * 64], wmT[:, g, :], n_sb[sn][:, g * 64:(g + 1) * 64],
                                          start=True, stop=True, skip_group_check=True)
                        t = M(PE, i)
                        ch_n.consumed(t)
                        ch_z.produced(t)

                    def sgu_dve_out(n):
                        sz = ch_z.cslot(DVE)
                        t = M(DVE, DVE.tensor_tensor(out=t1_sb[:], in0=banks[4 + sz][:], in1=sg_bc[:], op=ALU.mult))
                        ch_z.consumed(t)
                        DVE.tensor_tensor(out=t1_sb[:], in0=t1_sb[:], in1=Bp[:], op=ALU.add)
                        sgu_ = ch_gu.cslot(DVE)
                        so = ch_sg.pslot(DVE)
                        t = M(DVE, DVE.tensor_tensor(out=sg_sb[so][:], in0=t1_sb[:], in1=gu_sb[sgu_][:], op=ALU.mult))
                        ch_gu.consumed(t)
                        ch_sg.produced(t)

                    def sgu_pe_tp(n):
                        so = ch_sg.cslot(PE)
                        st = ch_tp.pslot(PE)
                        tpv = banks[6 + st][:].bitcast(BF16)
                        for j in range(4):
                            i = PE.transpose(tpv[:, j * 128:(j + 1) * 128], sg_sb[so][:, j * 128:(j + 1) * 128], ident[:])
                        t = M(PE, i)
                        ch_sg.consumed(t)
                        ch_tp.produced(t)

                    def sgu_act_tp(n):
                        st = ch_tp.cslot(ACT)
                        tpv = banks[6 + st][:].bitcast(BF16)
                        i = ACT.activation(out=catT[:, 4:8, n * 128:(n + 1) * 128],
                                           in_=tpv[:, 0:512].rearrange("p (j t) -> p j t", j=4), func=AF.Copy)
                        ch_tp.consumed(M(ACT, i))

                    for n in range(NT + 2):
                        if n < NT:
                            sgu_pe_front(n)
                            sgu_act(n)
                            sgu_dve_norm(n)
                        if 1 <= n <= NT:
                            sgu_pe_z(n - 1)
                            sgu_dve_out(n - 1)
                        if 2 <= n:
                            sgu_pe_tp(n - 2)
                            sgu_act_tp(n - 2)
                    barrier()
                stop_if("sgu")

                with contextlib.ExitStack() as es3:
                    k3 = KB(nc, es3, kb.waited, kb.prog)
                    QTz = k3.sb(f"QTz{b}", [128, 2, S], BF16)
                    KT = k3.sb(f"KT{b}", [128, S], BF16)
                    Vz = k3.sb(f"Vz{b}", [128, 48, 2, 128], BF16)
                    qs_sb = [k3.sb(f"qs_sb{b}_{i}", [128, 4, 256], BF16) for i in range(2)]
                    ra = k3.sb(f"ra{b}", [128, 4, 4, 8], F32)
                    rb = k3.sb(f"rb{b}", [128, 4, 4, 8], F32)
                    rot_sb = [k3.sb(f"rot_sb{b}_{i}", [128, 4, 2, 2, 16], F32) for i in range(2)]
                    p_sb = [k3.sb(f"p_sb{b}_{i}", [128, 512], BF16) for i in range(6)]
                    rl_sb = k3.sb(f"rl_sb{b}", [128, 512], F32)
                    s_ws = k3.sem(f"a1_ws{b}")

                    POOL.memset(QTz[:], 0.0)
                    tok_zero = M(POOL, POOL.memset(Vz[:], 0.0))
                    for e in (ACT, DVE, PE):
                        k3.wait(e, tok_zero)
                    stop_if("att_ms")

                    def tv_blk(T, j):
                        return T[:, j * 128:(j + 1) * 128]

                    def tv_p2(T, s, r4):
                        return T[:, 512 * s:512 * (s + 1)].rearrange("p (i r) -> p r i", r=4)[:, r4, :]

                    def tv_p3k(T, r):
                        return T.rearrange("p (l r) -> p r l", r=16)[:, r, :]

                    def tv_p3q(T, s, r):
                        return T.rearrange("p (l r) -> p r l", r=16)[:, r, 32 * s:32 * (s + 1)]

                    vdefs = []
                    for j in range(16):
                        vdefs.append(lambda T, j=j: tv_blk(T, j))
                    for s in range(4):
                        for r4 in range(4):
                            vdefs.append(lambda T, s=s, r4=r4: tv_p2(T, s, r4))
                    for r in range(16):
                        vdefs.append(lambda T, r=r: tv_p3k(T, r))

                    for c in range(4):
                        for j, c0 in enumerate((c * 128, 512 + c * 128, 1024 + c * 128)):
                            k3.inc(POOL.dma_start(out=wsec[:, :, j * 128:(j + 1) * 128],
                                                  in_=w_in[:, c0:c0 + 128].rearrange("(k p) c -> p k c", p=128)), s_ws, 16)
                        k3.wait(PE, (s_ws, s_ws.n))
                        k3.wait(DVE, tok_cs)
                        stop_if("att_dma")

                        def qk_pe(gi):
                            sq = ch_qk.pslot(PE)
                            for w in range(2):
                                for tt in range(4):
                                    n = gi * 4 + tt
                                    for k in range(8):
                                        i = PE.matmul(banks[2 * sq + w][:, tt * 128:(tt + 1) * 128],
                                                      xT_sb[:, k, n * 128:(n + 1) * 128], wsec[:, k, w * 128:(w + 1) * 128],
                                                      start=(tt == 0 and k == 0), stop=(k == 7), skip_group_check=True)
                            ch_qk.produced(M(PE, i))

                        def qk_evac(gi):
                            sq = ch_qk.cslot(ACT, 0)
                            so = ch_qs.pslot(ACT, DVE)
                            sr = ch_rot.pslot(ACT)
                            qv = banks[2 * sq + 0][:].rearrange("p (t h e) -> p t h e", t=4, h=2)
                            kv = banks[2 * sq + 1][:].rearrange("p (t h e) -> p t h e", t=4, h=2)
                            ov = qs_sb[so][:].rearrange("p t (w h e) -> p t w h e", w=2, h=2)
                            ACT.activation(out=ov[:, :, 0, :, 16:64], in_=qv[:, :, :, 16:64], func=AF.Copy)
                            ACT.activation(out=ov[:, :, 1, :, 16:64], in_=kv[:, :, :, 16:64], func=AF.Copy)
                            ACT.activation(out=rot_sb[sr][:, :, 0, :, :], in_=qv[:, :, :, 0:16], func=AF.Copy)
                            ta = M(ACT, ACT.activation(out=rot_sb[sr][:, :, 1, :, :], in_=kv[:, :, :, 0:16], func=AF.Copy))
                            ch_qk.consumed(ta, 0)
                            ch_rot.produced(ta)
                            sr = ch_rot.cslot(DVE)
                            rv = rot_sb[sr][:].rearrange("p t w h e -> p t (w h) e")
                            t1 = rv[:, :, :, 0:8]
                            t2 = rv[:, :, :, 8:16]
                            og = qs_sb[so][:].rearrange("p t (g e) -> p t g e", g=4)
                            cosb = cs_t[:, 0, gi * 4:(gi + 1) * 4, :].unsqueeze(2).broadcast_to([128, 4, 4, 8])
                            sinb = cs_t[:, 1, gi * 4:(gi + 1) * 4, :].unsqueeze(2).broadcast_to([128, 4, 4, 8])
                            DVE.tensor_tensor(out=ra[:], in0=t1, in1=cosb, op=ALU.mult)
                            DVE.tensor_tensor(out=rb[:], in0=t2, in1=sinb, op=ALU.mult)
                            DVE.tensor_tensor(out=og[:, :, :, 0:8], in0=ra[:], in1=rb[:], op=ALU.subtract)
                            DVE.tensor_tensor(out=ra[:], in0=t2, in1=cosb, op=ALU.mult)
                            DVE.tensor_tensor(out=rb[:], in0=t1, in1=sinb, op=ALU.mult)
                            td = M(DVE, DVE.tensor_tensor(out=og[:, :, :, 8:16], in0=ra[:], in1=rb[:], op=ALU.add))
                            ch_rot.consumed(td)
                            ch_qs.produced(ta, td)

                        def qk_tp(gi):
                            so = ch_qs.cslot(PE)
                            st = ch_tp.pslot(PE)
                            tpv = banks[6 + st][:].bitcast(BF16).rearrange("p (t w e) -> p t w e", t=4, w=2)
                            for tt in range(4):
                                for w in range(2):
                                    i = PE.transpose(tpv[:, tt, w, :], qs_sb[so][:, tt, w * 128:(w + 1) * 128], ident[:])
                            t = M(PE, i)
                            ch_qs.consumed(t)
                            ch_tp.produced(t)

                        def qk_tp_evac(gi):
                            st = ch_tp.cslot(ACT)
                            tpv = banks[6 + st][:].bitcast(BF16).rearrange("p (t w e) -> p t w e", t=4, w=2)
                            cols = slice(gi * 512, (gi + 1) * 512)
                            ACT.activation(out=QTz[0:64, 0, cols].rearrange("p (t e) -> p t e", t=4), in_=tpv[0:64, :, 0, :], func=AF.Copy)
                            ACT.activation(out=QTz[64:128, 1, cols].rearrange("p (t e) -> p t e", t=4), in_=tpv[64:128, :, 0, :], func=AF.Copy)
                            i = ACT.activation(out=KT[:, cols].rearrange("p (t e) -> p t e", t=4), in_=tpv[:, :, 1, :], func=AF.Copy)
                            ch_tp.consumed(M(ACT, i))

                        for gi in range(4 + 2):
                            if gi < 4:
                                qk_pe(gi)
                                stop_if("qk_pe0")
                                qk_evac(gi)
                                stop_if("qk_ev0")
                            if 1 <= gi <= 4:
                                qk_tp(gi - 1)
                                stop_if("qk_tp0")
                            if 2 <= gi:
                                qk_tp_evac(gi - 2)
                                stop_if("qk_te0")
                        stop_if("qk")

                        for gi in range(12):
                            sv = ch_vp.pslot(PE)
                            for tt in range(4):
                                vd = vdefs[gi * 4 + tt]
                                for k in range(8):
                                    i = PE.matmul(banks[4 + sv][:, tt * 128:(tt + 1) * 128], vd(xT_sb[:, k, :]), wsec[:, k, 256:384],
                                                  start=(tt == 0 and k == 0), stop=(k == 7), skip_group_check=True)
                            ch_vp.produced(M(PE, i))
                            eng = ACT if gi % 2 == 0 else DVE
                            sv = ch_vp.cslot(eng)
                            src = banks[4 + sv][:].rearrange("p (t h e) -> p t h e", t=4, h=2)
                            dst = Vz[:, gi * 4:(gi + 1) * 4, :, :]
                            if eng is ACT:
                                ACT.activation(out=dst[:, :, 0, 0:64], in_=src[:, :, 0, :], func=AF.Copy)
                                i = ACT.activation(out=dst[:, :, 1, 64:128], in_=src[:, :, 1, :], func=AF.Copy)
                            else:
                                DVE.tensor_copy(out=dst[:, :, 0, 0:64], in_=src[:, :, 0, :])
                                i = DVE.tensor_copy(out=dst[:, :, 1, 64:128], in_=src[:, :, 1, :])
                            ch_vp.consumed(M(eng, i))
                        barrier()
                        stop_if("v")

                        V1 = lambda j, hh: Vz[:, j, hh, :]
                        V2 = lambda s, r4, hh: Vz[:, 16 + 4 * s + r4, hh, :]
                        V3 = lambda r, hh: Vz[:, 32 + r, hh, :]

                        def emit_s(maskt, mms, lo=0, hi=512):
                            ss = ch_s.pslot(PE)
                            PE.matmul(banks[ss][:], ident[:], maskt, start=True, stop=False, skip_group_check=True)
                            for (oc, lhsT, rhs) in mms:
                                i = PE.matmul(banks[ss][:, oc[0]:oc[1]], lhsT, rhs, start=False, stop=True, skip_group_check=True)
                            ch_s.produced(M(PE, i))
                            ss2 = ch_s.cslot(ACT)
                            sp = ch_p.pslot(ACT)
                            t = M(ACT, ACT.activation(out=p_sb[sp][:, lo:hi], in_=banks[ss2][:, lo:hi], func=AF.Exp, scale=0.125))
                            ch_s.consumed(t)
                            ch_p.produced(t)
                            return sp

                        def oview(Bk, oc):
                            if oc[0] == "blk":
                                return Bk[:, oc[1] * 128:(oc[1] + 1) * 128]
                            if oc[0] == "p2":
                                return Bk[:].rearrange("p (i r) -> p r i", r=4)[:, oc[1], :]
                            return Bk[:].rearrange("p (i r) -> p r i", r=16)[:, oc[1], :]

                        for s in range(4):
                            so_ = ch_o.pslot(PE)
                            Ob = banks[4 + 2 * so_]
                            Lb = banks[5 + 2 * so_]
                            first_pv = True
                            for hh in range(2):
                                Q = QTz[:, hh, :]
                                pend = []
                                mms = [((j * 128, (j + 1) * 128), tv_blk(KT, 4 * s + j), tv_blk(Q, 4 * s + j)) for j in range(4)]
                                sp = emit_s(m_cur[:], mms)
                                pend.append((sp, [(V1(4 * s + j, hh), (j * 128, (j + 1) * 128), ("blk", j)) for j in range(4)]))
                                js = [j for j in range(4) if 4 * s + j >= 1]
                                mms = [((j * 128, (j + 1) * 128), tv_blk(KT, 4 * s + j - 1), tv_blk(Q, 4 * s + j)) for j in js]
                                sp = emit_s(m_prev[:], mms, lo=js[0] * 128)
                                pend.append((sp, [(V1(4 * s + j - 1, hh), (j * 128, (j + 1) * 128), ("blk", j)) for j in js]))
                                mms = [((r4 * 128, (r4 + 1) * 128), tv_p2(KT, s, r4), tv_p2(Q, s, r4)) for r4 in range(4)]
                                sp = emit_s(m_cur[:], mms)
                                pend.append((sp, [(V2(s, r4, hh), (r4 * 128, (r4 + 1) * 128), ("p2", r4)) for r4 in range(4)]))
                                if s >= 1:
                                    mms = [((r4 * 128, (r4 + 1) * 128), tv_p2(KT, s - 1, r4), tv_p2(Q, s, r4)) for r4 in range(4)]
                                    sp = emit_s(m_prev[:], mms)
                                    pend.append((sp, [(V2(s - 1, r4, hh), (r4 * 128, (r4 + 1) * 128), ("p2", r4)) for r4 in range(4)]))
                                mms = [((r * 32, (r + 1) * 32), tv_p3k(KT, r), tv_p3q(Q, s, r)) for r in range(16)]
                                sp = emit_s(m3[:, s, :], mms)
                                pend.append((sp, [(V3(r, hh), (r * 32, (r + 1) * 32), ("p3", r)) for r in range(16)]))

                                for (sp, pvs) in pend:
                                    sp2 = ch_p.cslot(PE)
                                    assert sp2 == sp
                                    for (vt, pc, oc) in pvs:
                                        PE.matmul(oview(Ob, oc), vt, p_sb[sp][:, pc[0]:pc[1]], start=first_pv, stop=False, skip_group_check=True)
                                        i = PE.matmul(oview(Lb, oc), ones_z[:, hh, :], p_sb[sp][:, pc[0]:pc[1]], start=first_pv, stop=False,
                                                      skip_group_check=True)
                                        first_pv = False
                                    t_last = M(PE, i)
                                    ch_p.consumed(t_last)
                            ch_o.produced(t_last)
                            so2 = ch_o.cslot(DVE)
                            DVE.reciprocal(out=rl_sb[:], in_=banks[5 + 2 * so2][:])
                            i = DVE.tensor_tensor(out=catT[:, c, 512 * s:512 * (s + 1)], in0=banks[4 + 2 * so2][:], in1=rl_sb[:], op=ALU.mult)
                            ch_o.consumed(M(DVE, i))
                            stop_if("attn_s0")
                        barrier()
                        if DEBUG and b == DEBUG_B and STOP_AFTER == "attn":
                            with contextlib.ExitStack() as esd:
                                kd = KB(nc, esd, kb.waited, kb.prog)
                                dtmp = kd.sb("dtmpa", [128, 4, S], F32)
                                sd = kd.sem("dbga")
                                kd.wait(SP, M(DVE, DVE.tensor_copy(out=dtmp[:], in_=catT[:, 0:4, :])))
                                kd.wait(SP, kd.inc(SP.dma_start(out=dbg_cat[:, 0:4, :], in_=dtmp[:]), sd, 16))
                            stop_if("attn")

                if DEBUG and b == DEBUG_B:
                    with contextlib.ExitStack() as esd:
                        kd = KB(nc, esd, kb.waited, kb.prog)
                        dtmp = kd.sb("dtmp", [128, 8, S], F32)
                        sd = kd.sem("dbg1")
                        kd.wait(SP, M(DVE, DVE.tensor_copy(out=dtmp[:], in_=catT[:])))
                        kd.wait(SP, kd.inc(SP.dma_start(out=dbg_cat, in_=dtmp[:]), sd, 16))
                        barrier()

            esp = contextlib.ExitStack()
            kp = KB(nc, esp, kb.waited, kb.prog)
            hb_all = kp.sb(f"hb_all{b}", [128, NT, D], BF16)
            ybuf = kp.sb(f"ybuf{b}", [128, NT, D], F32)
            M1a = kp.sb(f"M1a{b}", [128, NT, 32], F32)
            M2a = kp.sb(f"M2a{b}", [128, NT, 32], F32)
            g12 = kp.sb(f"g12{b}", [128, 2, NT], F32)
            sl_i = kp.sb(f"sl_i{b}", [128, 2, NT], I32)
            with contextlib.ExitStack() as es4:
                k4 = KB(nc, es4, kb.waited, kb.prog)
                wo_sb = k4.sb(f"wo_sb{b}", [128, 8, D], BF16)
                x_sb = [k4.sb(f"x_sb{b}_{i}", [128, D], F32) for i in range(2)]
                hT_sb = [k4.sb(f"hT_sb{b}_{i}", [128, 8, 128], BF16) for i in range(2)]
                hf_sb = [k4.sb(f"hf_sb{b}_{i}", [128, D], F32) for i in range(2)]
                eps_t = k4.sb(f"eps_t{b}", [128, 1], F32)
                k4.wait(ACT, M(POOL, POOL.memset(eps_t[:], EPS)))
                math_done = [None]
                pend_D = []
                NH = 8
                lg_all = k4.sb(f"lg_all{b}", [128, NH, 72], F32)
                rq = k4.sb(f"rq{b}", [128, NH, 64], F32)
                lo_sb = [k4.sb(f"lo_sb{b}_{i}", [128, D], BF16) for i in range(2)]
                loT_sb = [k4.sb(f"loT_sb{b}_{i}", [128, 8, 128], BF16) for i in range(2)]
                st2 = k4.sb(f"st2_{b}", [128, 2, 6], F32)
                mv2 = k4.sb(f"mv2_{b}", [128, 2], F32)
                rstd2 = k4.sb(f"rstd2_{b}", [128, 1], F32)
                s_wo = k4.sem(f"a2_wo{b}")
                g1_bc = k4.sb(f"g1_bc{b}", [128, D], F32)
                b1_bc = k4.sb(f"b1_bc{b}", [128, D], F32)
                s_g1 = k4.sem(f"a2_g1{b}")
                k4.inc(SP.dma_start(out=g1_bc[:], in_=ln1_g.partition_broadcast(128)), s_g1, 16)
                k4.wait(DVE, k4.inc(SP.dma_start(out=b1_bc[:], in_=ln1_b.partition_broadcast(128)), s_g1, 16))

                t_wo = k4.inc(POOL.dma_start(out=wo_sb[:], in_=w_out.rearrange("(k p) c -> p k c", p=128)), s_wo, 16)
                k4.wait(PE, t_wo)
                k4.wait(DVE, t_wo)

                def a2_load(n):
                    sx = ch_x.pslot(SP)
                    i = SP.dma_start(out=x_sb[sx][:], in_=xtm[b, n * 128:(n + 1) * 128, :])
                    ch_x.produced(ring_x.start(i, sx))

                def a2_pe_op(n):
                    so = ch_op.pslot(PE)
                    for hf in range(2):
                        for k in range(8):
                            i = PE.matmul(banks[2 * so + hf][:], catT[:, k, n * 128:(n + 1) * 128], wo_sb[:, k, hf * 512:(hf + 1) * 512],
                                          start=(k == 0), stop=(k == 7))
                    ch_op.produced(M(PE, i))

                def a2_dve_A(n):
                    so = ch_op.cslot(DVE)
                    sx = ch_x.cslot(DVE)
                    sh = n % 2
                    hf_ = hf_sb[sh]
                    for hf in range(2):
                        i = DVE.scalar_tensor_tensor(out=hf_[:, hf * 512:(hf + 1) * 512], in0=x_sb[sx][:, hf * 512:(hf + 1) * 512],
                                                     scalar=ALPHA, in1=banks[2 * so + hf][:], op0=ALU.mult, op1=ALU.add)
                    t = M(DVE, i)
                    ch_op.consumed(t)
                    ch_x.consumed(t)
                    for hf in range(2):
                        DVE.bn_stats(out=st2[:, hf, :], in_=hf_[:, hf * 512:(hf + 1) * 512])
                    i = DVE.bn_aggr(out=mv2[:], in_=st2[:].rearrange("p a c -> p (a c)"))
                    kb.wait(ACT, M(DVE, i))
                    i = ACT.activation(out=rstd2[:], in_=mv2[:, 1:2], func=AF.Sqrt, bias=eps_t[:, 0:1], scale=1.0)
                    kb.wait(DVE, M(ACT, i))
                    DVE.reciprocal(out=rstd2[:], in_=rstd2[:])
                    t = M(DVE, DVE.tensor_scalar(out=hf_[:], in0=hf_[:], scalar1=mv2[:, 0:1], scalar2=rstd2[:, 0:1],
                                                 op0=ALU.subtract, op1=ALU.mult))
                    kb.wait(POOL, t)
                    POOL.tensor_tensor(out=hf_[:], in0=hf_[:], in1=g1_bc[:], op=ALU.mult)
                    t = M(POOL, POOL.tensor_tensor(out=hf_[:], in0=hf_[:], in1=b1_bc[:], op=ALU.add))
                    kb.wait(ACT, t)
                    ACT.activation(out=hb_all[:, n, :], in_=hf_[:], func=AF.Copy)
                    tC = M(ACT, ACT.activation(out=ybuf[:, n, :], in_=hf_[:], func=AF.Copy, scale=ALPHA))
                    pend_D.append((n, sh, tC))

                def a2_dve_D():
                    n, sh, tC = pend_D.pop(0)
                    kb.wait(DVE, tC)
                    sl_ = ch_hb.pslot(DVE)
                    t = M(DVE, DVE.tensor_tensor(out=lo_sb[sl_][:], in0=hf_sb[sh][:], in1=hb_all[:, n, :], op=ALU.subtract))
                    ch_hb.produced(t)

                def a2_pe_tp(n):
                    sh = ch_hb.cslot(PE)
                    st = ch_tp.pslot(PE)
                    tpv = banks[6 + st][:].bitcast(BF16)
                    for k in range(8):
                        i = PE.transpose(tpv[:, k * 128:(k + 1) * 128], hb_all[:, n, k * 128:(k + 1) * 128], ident[:])
                    ch_tp.produced(M(PE, i))
                    st = ch_tp.pslot(PE)
                    tpv = banks[6 + st][:].bitcast(BF16)
                    for k in range(8):
                        i = PE.transpose(tpv[:, k * 128:(k + 1) * 128], lo_sb[sh][:, k * 128:(k + 1) * 128], ident[:])
                    t = M(PE, i)
                    ch_hb.consumed(t)
                    ch_tp.produced(t)

                def a2_act_tp(n):
                    st = ch_tp.cslot(ACT)
                    tpv = banks[6 + st][:].bitcast(BF16)
                    sht = ch_ht.pslot(ACT)
                    i = ACT.activation(out=hT_sb[sht][:], in_=tpv.rearrange("p (k t) -> p k t", k=8), func=AF.Copy)
                    t_h = M(ACT, i)
                    ch_tp.consumed(t_h)
                    ch_ht.produced(t_h)
                    st = ch_tp.cslot(ACT)
                    tpv = banks[6 + st][:].bitcast(BF16)
                    sl = ch_lo.pslot(ACT)
                    i = ACT.activation(out=loT_sb[sl][:], in_=tpv.rearrange("p (k t) -> p k t", k=8), func=AF.Copy)
                    t = M(ACT, i)
                    ch_tp.consumed(t)
                    ch_lo.produced(t)
                    return t_h

                def a2_pe_route(n, t_h):
                    sl = ch_lo.cslot(PE)
                    sht = ch_ht.cslot(PE)
                    ch_r.pslot(PE)
                    rp = banks[4]
                    for k in range(8):
                        PE.matmul(rp[:, 0:72], hT_sb[sht][:, k, :], wr_hl[:, k, 0:72], start=(k == 0), stop=False,
                                  skip_group_check=True)
                    for k in range(8):
                        i = PE.matmul(rp[:, 0:36], loT_sb[sl][:, k, :], wr_hl[:, k, 0:36], start=False, stop=(k == 7),
                                      skip_group_check=True)
                    t = M(PE, i)
                    ch_lo.consumed(t)
                    ch_ht.consumed(t)
                    ch_r.produced(t)

                def a2_route_copy(n):
                    ch_r.cslot(ACT)
                    kb.wait(ACT, math_done[0])
                    t = M(ACT, ACT.activation(out=lg_all[:, n % NH, :], in_=banks[4][:, 0:72], func=AF.Copy))
                    ch_r.consumed(t)
                    return t

                def a2_route_math(n0, t_last):
                    kb.wait(DVE, t_last)
                    sl = slice(n0, n0 + NH)
                    L = lg_all[:, :, 0:36]
                    DVE.tensor_tensor(out=L, in0=L, in1=lg_all[:, :, 36:72], op=ALU.add)
                    DVE.tensor_tensor(out=L, in0=L, in1=br_bc[:].unsqueeze(1).broadcast_to([128, NH, 36]), op=ALU.add)
                    gmax, oh, ge, sume = rq[:, :, 0:1], rq[:, :, 1:5], rq[:, :, 5:9], rq[:, :, 9:10]
                    esel, m1, eq1, e2 = rq[:, :, 11:19], rq[:, :, 19:20], rq[:, :, 20:28], rq[:, :, 28:36]
                    m2, eq2, dd, w1, w2, tmp8 = rq[:, :, 36:37], rq[:, :, 37:45], rq[:, :, 45:46], rq[:, :, 46:47], rq[:, :, 47:48], rq[:, :, 48:56]
                    bc = lambda ap, k: ap.broadcast_to([128, NH, k])
                    DVE.tensor_reduce(out=gmax, in_=lg_all[:, :, 0:4], axis=AX.X, op=ALU.max)
                    DVE.tensor_tensor(out=oh, in0=lg_all[:, :, 0:4], in1=bc(gmax, 4), op=ALU.is_equal)
                    DVE.tensor_tensor(out=ge, in0=lg_all[:, :, 0:4], in1=bc(gmax, 4), op=ALU.subtract)
                    DVE.tensor_tensor(out=esel, in0=lg_all[:, :, 4:12], in1=bc(oh[:, :, 0:1], 8), op=ALU.mult)
                    for g in range(1, 4):
                        DVE.tensor_tensor(out=tmp8, in0=lg_all[:, :, 4 + 8 * g:12 + 8 * g], in1=bc(oh[:, :, g:g + 1], 8), op=ALU.mult)
                        DVE.tensor_tensor(out=esel, in0=esel, in1=tmp8, op=ALU.add)
                    DVE.tensor_reduce(out=m1, in_=esel, axis=AX.X, op=ALU.max)
                    DVE.tensor_tensor(out=eq1, in0=esel, in1=bc(m1, 8), op=ALU.is_equal)
                    DVE.scalar_tensor_tensor(out=e2, in0=eq1, scalar=-1e30, in1=esel, op0=ALU.mult, op1=ALU.add)
                    DVE.tensor_reduce(out=m2, in_=e2, axis=AX.X, op=ALU.max)
                    DVE.tensor_tensor(out=eq2, in0=e2, in1=bc(m2, 8), op=ALU.is_equal)
                    i = DVE.tensor_tensor(out=dd, in0=m2, in1=m1, op=ALU.subtract)
                    kb.wait(ACT, M(DVE, i))
                    ACT.activation(out=ge, in_=ge, func=AF.Exp)
                    i = ACT.activation(out=dd, in_=dd, func=AF.Exp)
                    kb.wait(DVE, M(ACT, i))
                    DVE.tensor_reduce(out=sume, in_=ge, axis=AX.X, op=ALU.add)
                    DVE.reciprocal(out=sume, in_=sume)
                    DVE.tensor_scalar(out=w1, in0=dd, scalar1=1.0, scalar2=None, op0=ALU.add)
                    DVE.reciprocal(out=w1, in_=w1)
                    DVE.tensor_tensor(out=w2, in0=dd, in1=w1, op=ALU.mult)
                    DVE.tensor_tensor(out=g12[:, 0, sl].unsqueeze(2), in0=w1, in1=sume, op=ALU.mult)
                    DVE.tensor_tensor(out=g12[:, 1, sl].unsqueeze(2), in0=w2, in1=sume, op=ALU.mult)
                    for g in range(4):
                        DVE.tensor_tensor(out=M1a[:, sl, g * 8:(g + 1) * 8], in0=eq1, in1=bc(oh[:, :, g:g + 1], 8), op=ALU.mult)
                        tm = DVE.tensor_tensor(out=M2a[:, sl, g * 8:(g + 1) * 8], in0=eq2, in1=bc(oh[:, :, g:g + 1], 8), op=ALU.mult)
                    math_done[0] = M(DVE, tm)

                a2_load(0)
                t_hs = {}
                for n in range(NT + 3):
                    if n + 1 < NT:
                        a2_load(n + 1)
                    if n < NT:
                        a2_pe_op(n)
                        a2_dve_A(n)
                    if 1 <= n <= NT:
                        a2_dve_D()
                    if 2 <= n <= NT + 1:
                        a2_pe_tp(n - 2)
                        t_hs[n - 2] = a2_act_tp(n - 2)
                    if 3 <= n:
                        a2_pe_route(n - 3, t_hs[n - 3])
                        t_c = a2_route_copy(n - 3)
                        if (n - 3) % NH == NH - 1:
                            a2_route_math(n - 3 - (NH - 1), t_c)
                barrier()

            if DEBUG and b == DEBUG_B:
                with contextlib.ExitStack() as esd:
                    kd = KB(nc, esd, kb.waited, kb.prog)
                    sd = kd.sem("dbg2")
                    kd.inc(SP.dma_start(out=dbg_y, in_=ybuf[:]), sd, 16)
                    kd.wait(SP, kd.inc(SP.dma_start(out=dbg_gate, in_=M1a[:]), sd, 16))
                    barrier()
            if not DEBUG or b == DEBUG_B:
                stop_if("A2")

            with contextlib.ExitStack() as es5:
                k5 = KB(nc, es5, kb.waited, kb.prog)
                NWS = 3
                wgu_sb = [catT[:, 2 * i:2 * i + 2, :].rearrange("p a (k f) -> p (a k) f", f=512) for i in range(3)]
                wdn_t = [k5.sb(f"wdn_t{b}_{i}", [128, 2, D], BF16) for i in range(2)]
                wdn_sb = [t_[:] for t_ in wdn_t] + [catT[:, 6 + i, :].rearrange("p (k f) -> p k f", f=1024) for i in range(2)]
                thr = k5.sb(f"thr{b}", [128, NJ, 32], F32)
                Mb = k5.sb(f"Mb{b}", [128, NT * 32], BF16)
                Ms = k5.sb(f"Ms{b}", [128, NT, 32], F32)
                cs = k5.sb(f"cs{b}", [128, NT, 32], F32)
                off = k5.sb(f"off{b}", [128, NT, 32], F32)
                Sf = k5.sb(f"Sf{b}", [128, NT, 32], F32)
                tS = Ms
                sc_a = k5.sb(f"sc_a{b}", [128, 32], F32)
                sc_b = k5.sb(f"sc_b{b}", [128, 32], F32)
                pt = k5.sb(f"pt{b}", [128, 32], F32)
                base = k5.sb(f"base{b}", [128, 32], F32)
                qi = k5.sb(f"qi{b}", [128, 32], I32)
                sl_f = k5.sb(f"sl_f{b}", [128, 2, NT], F32)
                ej_f = k5.sb(f"ej_f{b}", [128, NJ], F32)
                wi_f = thr[:].rearrange("p j e -> p (j e)")[:, 0:NJ * 3].rearrange("p (j c) -> p j c", c=3)
                wi_i = k5.sb(f"wi_i{b}", [128, NJ, 3], I32)
                pidx = k5.sb(f"pidx{b}", [128, 1], F32)
                ko = k5.sb(f"ko{b}", [128, 8], F32)
                Lst = k5.sb(f"Lst{b}", [128, 128], BF16)
                xt_sb = [k5.sb(f"xt_sb{b}_{i}", [128, 2, D], BF16) for i in range(2)]
                xsT = [k5.sb(f"xsT{b}_{i}", [128, 8, 256], BF16) for i in range(2)]
                sl_sb = [k5.sb(f"sl_sb{b}_{i}", [128, 512], F32) for i in range(2)]
                at_sb = [k5.sb(f"at_sb{b}_{i}", [128, 2, 256], BF16) for i in range(3)]
                yo_sb = [k5.sb(f"yo_sb{b}_{i}", [128, D], F32) for i in range(2)]

                POOL.affine_select(out=Lst[:], in_=ones_bf[:], pattern=[[1, 128]], compare_op=ALU.is_gt,
                                   fill=0.0, base=0, channel_multiplier=-1)
                t_thr = M(POOL, POOL.iota(thr[:], pattern=[[256, NJ], [0, 32]], base=0, channel_multiplier=0,
                                          allow_small_or_imprecise_dtypes=True))
                POOL.iota(pidx[:], pattern=[[0, 1]], base=0, channel_multiplier=1, allow_small_or_imprecise_dtypes=True)
                t_io = M(POOL, POOL.iota(ko[:], pattern=[[128, 8]], base=0, channel_multiplier=0, allow_small_or_imprecise_dtypes=True))
                DVE.tensor_tensor(out=Ms[:], in0=M1a[:], in1=M2a[:], op=ALU.add)
                t = M(DVE, DVE.tensor_copy(out=Mb[:], in_=Ms[:].rearrange("p n e -> p (n e)")))
                kb.wait(PE, t)
                kb.wait(PE, t_thr)
                PE.matmul(banks[0][:], Lst[:], Mb[:], start=True, stop=True)
                t = M(PE, PE.matmul(banks[1][:], ones_bf[:], Mb[:], start=True, stop=True))
                kb.wait(DVE, t)
                DVE.tensor_copy(out=cs[:], in_=banks[1][:].rearrange("p (n e) -> p n e", e=32))
                DVE.memset(off[:, 0, :], 0.0)
                for n in range(1, NT):
                    DVE.tensor_tensor(out=off[:, n, :], in0=off[:, n - 1, :], in1=cs[:, n - 1, :], op=ALU.add)
                DVE.tensor_tensor(out=sc_a[:], in0=off[:, NT - 1, :], in1=cs[:, NT - 1, :], op=ALU.add)
                DVE.tensor_scalar(out=sc_b[:], in0=sc_a[:], scalar1=127.5, scalar2=1.0 / 256.0, op0=ALU.add, op1=ALU.mult)
                DVE.tensor_copy(out=qi[:], in_=sc_b[:])
                DVE.tensor_scalar(out=pt[:], in0=qi[:], scalar1=256.0, scalar2=None, op0=ALU.mult)
                DVE.tensor_copy(out=sc_a[:], in_=pt[:])
                pa, pb = sc_a, sc_b
                for sh in (1, 2, 4, 8, 16):
                    DVE.tensor_copy(out=pb[:, 0:sh], in_=pa[:, 0:sh])
                    DVE.tensor_tensor(out=pb[:, sh:32], in0=pa[:, sh:32], in1=pa[:, 0:32 - sh], op=ALU.add)
                    pa, pb = pb, pa
                incl = pa
                DVE.tensor_tensor(out=base[:], in0=incl[:], in1=pt[:], op=ALU.subtract)
                DVE.tensor_tensor(out=Sf[:], in0=banks[0][:].rearrange("p (n e) -> p n e", e=32), in1=off[:], op=ALU.add)
                DVE.tensor_tensor(out=Sf[:], in0=Sf[:], in1=base[:].unsqueeze(1).broadcast_to([128, NT, 32]), op=ALU.add)
                DVE.tensor_tensor(out=tS[:], in0=Sf[:], in1=M1a[:], op=ALU.mult)
                DVE.tensor_reduce(out=sl_f[:, 0, :], in_=tS[:], axis=AX.X, op=ALU.add)
                DVE.tensor_tensor(out=tS[:], in0=Sf[:], in1=M2a[:], op=ALU.mult)
                DVE.tensor_reduce(out=sl_f[:, 1, :], in_=tS[:], axis=AX.X, op=ALU.add)
                DVE.tensor_copy(out=sl_i[:], in_=sl_f[:])
                kb.wait(DVE, t_thr)
                DVE.tensor_tensor(out=thr[:], in0=thr[:], in1=incl[:].unsqueeze(1).broadcast_to([128, NJ, 32]), op=ALU.is_ge)
                DVE.tensor_reduce(out=ej_f[:], in_=thr[:], axis=AX.X, op=ALU.add)
                kb.wait(DVE, t_io)
                DVE.tensor_scalar(out=ej_f[:], in0=ej_f[:], scalar1=256.0, scalar2=pidx[:, 0:1], op0=ALU.mult, op1=ALU.add)
                DVE.tensor_tensor(out=wi_f[:, :, 0:2], in0=ej_f[:].unsqueeze(2).broadcast_to([128, NJ, 2]),
                                  in1=ko[:, 0:2].unsqueeze(1).broadcast_to([128, NJ, 2]), op=ALU.add)
                DVE.tensor_scalar(out=ej_f[:], in0=ej_f[:], scalar1=pidx[:, 0:1], scalar2=0.5, op0=ALU.subtract, op1=ALU.mult)
                DVE.tensor_scalar(out=wi_f[:, :, 2:3], in0=ej_f[:].unsqueeze(2), scalar1=pidx[:, 0:1], scalar2=None, op0=ALU.add)
                t_disp = M(DVE, DVE.tensor_copy(out=wi_i[:], in_=wi_f))

                kb.wait(POOL, t_disp)
                for n in range(NT):
                    for kk in range(2):
                        i = POOL.indirect_dma_start(out=xs_d, out_offset=bass.IndirectOffsetOnAxis(ap=sl_i[:, kk, n:n + 1], axis=0),
                                                    in_=hb_all[:, n, :], in_offset=None)
                        kb.inc(i, s_sc, 16)
                tok_sc = (s_sc, s_sc.n)
                kb.wait(SP, tok_sc)
                kb.wait(POOL, tok_sc)

                wgu_rows = w_gu.rearrange("e (d4 i) f -> (e d4) (i f)", i=4)
                wdn_rows = w_dn.rearrange("e (f2 i) d -> (e f2) (i d)", i=2)

                def m_load_w(j):
                    sg_ = ch_wg.pslot(POOL)
                    gv = wgu_sb[sg_].rearrange("p (k4 i) f -> p k4 (i f)", i=4)
                    for k4 in range(2):
                        i = POOL.indirect_dma_start(out=gv[:, k4, :], out_offset=None, in_=wgu_rows,
                                                    in_offset=bass.IndirectOffsetOnAxis(ap=wi_i[:, j, k4:k4 + 1], axis=0),
                                                    bounds_check=rb_gu, oob_is_err=False)
                        t1 = ring_wg.start(i, sg_)
                    ch_wg.produced(t1)
                    sd_ = ch_wd.pslot(POOL)
                    i = POOL.indirect_dma_start(out=wdn_sb[sd_].rearrange("p i d -> p (i d)"), out_offset=None, in_=wdn_rows,
                                                in_offset=bass.IndirectOffsetOnAxis(ap=wi_i[:, j, 2:3], axis=0),
                                                bounds_check=rb_dn, oob_is_err=False)
                    ch_wd.produced(ring_wd.start(i, sd_))

                def m_load_x(j):
                    sx = ch_xs.pslot(SP)
                    i = SP.dma_start(out=xt_sb[sx][:], in_=xs_d[256 * j:256 * (j + 1), :].rearrange("(a p) d -> p a d", p=128))
                    ch_xs.produced(ring_xs.start(i, sx))

                def m_tp(j):
                    sx = ch_xs.cslot(PE)
                    ch_tp.pslot(PE)
                    ch_tp.pslot(PE)
                    for half in range(2):
                        tpv = banks[6 + half][:].bitcast(BF16).rearrange("p (k a t) -> p k a t", k=4, a=2)
                        for k4 in range(4):
                            for a_ in range(2):
                                k = half * 4 + k4
                                c0 = 512 * (k // 4) + (k % 4)
                                i = PE.transpose(tpv[:, k4, a_, :], xt_sb[sx][:, a_, c0:c0 + 509:4], ident[:])
                    t = M(PE, i)
                    ch_xs.consumed(t)
                    ch_tp.produced(t)
                    ch_tp.produced(t)
                    sT = ch_xT.pslot(ACT, DVE)
                    ch_tp.cslot(ACT)
                    ta = M(ACT, ACT.activation(out=xsT[sT][:, 0:4, :], in_=banks[6][:].bitcast(BF16).rearrange("p (k t) -> p k t", k=4), func=AF.Copy))
                    ch_tp.consumed(ta)
                    ch_tp.cslot(DVE)
                    td = M(DVE, DVE.tensor_copy(out=xsT[sT][:, 4:8, :], in_=banks[7][:].bitcast(BF16).rearrange("p (k t) -> p k t", k=4)))
                    ch_tp.consumed(td)
                    ch_xT.produced(ta, td)

                def m_gu(j):
                    sw = j % 3
                    sT = ch_xT.cslot(PE)
                    for tok in ch_wg.ready[j]:
                        kb.wait(PE, tok)
                    sg = ch_g2.pslot(PE)
                    for part in range(2):
                        for fcp in range(2):
                            fc = part * 256 + fcp * 128
                            for k in range(8):
                                i = PE.matmul(banks[2 * sg + part][:, fcp * 256:(fcp + 1) * 256], wgu_sb[sw][:, k, fc:fc + 128], xsT[sT][:, k, :],
                                              start=(fcp == 0 and k == 0), stop=(k == 7), skip_group_check=True)
                    t = M(PE, i)
                    ch_xT.consumed(t)
                    ch_wg.consumed(t)
                    ch_g2.produced(t)
                    sg = ch_g2.cslot(ACT, 0)
                    ss = ch_sl.pslot(ACT)
                    t = M(ACT, ACT.activation(out=sl_sb[ss][:], in_=banks[2 * sg][:], func=AF.Silu))
                    ch_g2.consumed(t, 0)
                    ch_sl.produced(t)
                    sg = ch_g2.cslot(DVE, 1)
                    ss = ch_sl.cslot(DVE)
                    sa = ch_at.pslot(DVE)
                    t = M(DVE, DVE.tensor_tensor(out=at_sb[sa][:].rearrange("p c t -> p (c t)"), in0=banks[2 * sg + 1][:], in1=sl_sb[ss][:], op=ALU.mult))
                    ch_g2.consumed(t, 1)
                    ch_sl.consumed(t)
                    ch_at.produced(t)

                def m_dn(j):
                    sw = j % 4
                    sa = ch_at.cslot(PE)
                    for tok in ch_wd.ready[j]:
                        kb.wait(PE, tok)
                    for a_ in range(2):
                        ch_dn.pslot(PE)
                        for hf in range(2):
                            for fc in range(2):
                                i = PE.matmul(banks[4 + hf][:], at_sb[sa][:, fc, a_ * 128:(a_ + 1) * 128],
                                              wdn_sb[sw][:, fc, hf * 512:(hf + 1) * 512], start=(fc == 0), stop=(fc == 1))
                        t = M(PE, i)
                        ch_dn.produced(t)
                        so = ch_yo.pslot(ACT, DVE)
                        ch_dn.cslot(ACT, 0)
                        ta = M(ACT, ACT.activation(out=yo_sb[so][:, 0:512], in_=banks[4][:], func=AF.Copy))
                        ch_dn.consumed(ta, 0)
                        ch_dn.cslot(DVE, 1)
                        td = M(DVE, DVE.tensor_copy(out=yo_sb[so][:, 512:1024], in_=banks[5][:]))
                        ch_dn.consumed(td, 1)
                        ch_yo.produced(ta, td)
                        so = ch_yo.cslot(SP)
                        i = SP.dma_start(out=ys_d[256 * j + 128 * a_:256 * j + 128 * (a_ + 1), :], in_=yo_sb[so][:])
                        ch_yo.consumed(ring_ys.start(i, so))
                        last_ys[so] = (ring_ys.sems[so], ring_ys.sems[so].n)
                    ch_at.consumed(t)
                    ch_wd.consumed(t)

                last_ys = {}
                m_load_w(0)
                m_load_w(1)
                m_load_x(0)
                m_load_x(1)
                m_tp(0)
                for j in range(NJ):
                    if j + 2 < NJ:
                        m_load_x(j + 2)
                    if j + 1 < NJ:
                        m_tp(j + 1)
                    if j >= 2:
                        m_dn(j - 2)
                    if j + 2 < NJ:
                        m_load_w(j + 2)
                    m_gu(j)
                m_dn(NJ - 2)
                m_dn(NJ - 1)
                barrier()
                stop_if("M")

            with contextlib.ExitStack() as es6:
                k6 = KB(nc, es6, kb.waited, kb.prog)
                o_sb = [k6.sb(f"o_sb{b}_{i}", [128, D], F32) for i in range(2)]
                rg_sb = [k6.sb(f"rg_sb{b}_{i}", [128, D], F32) for i in range(4)]
                st3 = k6.sb(f"st3_{b}", [128, 2, 6], F32)
                mv3 = k6.sb(f"mv3_{b}", [128, 2], F32)
                rstd3 = k6.sb(f"rstd3_{b}", [128, 1], F32)
                eps3 = k6.sb(f"eps3_{b}", [128, 1], F32)
                g2_bc = k6.sb(f"g2_bc{b}", [128, D], F32)
                b2_bc = k6.sb(f"b2_bc{b}", [128, D], F32)
                s_g2 = k6.sem(f"f_g2{b}")
                k6.inc(SP.dma_start(out=g2_bc[:], in_=ln2_g.partition_broadcast(128)), s_g2, 16)
                k6.wait(DVE, k6.inc(SP.dma_start(out=b2_bc[:], in_=ln2_b.partition_broadcast(128)), s_g2, 16))
                k6.wait(ACT, M(POOL, POOL.memset(eps3[:], EPS)))
                for so, tk in last_ys.items():
                    kb.wait(POOL, tk)

                def f_gather(n):
                    for kk in range(2):
                        sr = ch_rg.pslot(POOL)
                        i = POOL.indirect_dma_start(out=rg_sb[sr][:], out_offset=None, in_=ys_d,
                                                    in_offset=bass.IndirectOffsetOnAxis(ap=sl_i[:, kk, n:n + 1], axis=0))
                        ch_rg.produced(ring_g4.start(i, sr))

                last2 = []
                f_gather(0)
                for n in range(NT):
                    if n + 1 < NT:
                        f_gather(n + 1)
                    for kk in range(2):
                        sr = ch_rg.cslot(DVE)
                        t = M(DVE, DVE.scalar_tensor_tensor(out=ybuf[:, n, :], in0=rg_sb[sr][:], scalar=g12[:, kk, n:n + 1], in1=ybuf[:, n, :],
                                                            op0=ALU.mult, op1=ALU.add))
                        ch_rg.consumed(t)
                    for hf in range(2):
                        DVE.bn_stats(out=st3[:, hf, :], in_=ybuf[:, n, hf * 512:(hf + 1) * 512])
                    i = DVE.bn_aggr(out=mv3[:], in_=st3[:].rearrange("p a c -> p (a c)"))
                    kb.wait(ACT, M(DVE, i))
                    i = ACT.activation(out=rstd3[:], in_=mv3[:, 1:2], func=AF.Sqrt, bias=eps3[:, 0:1], scale=1.0)
                    kb.wait(DVE, M(ACT, i))
                    DVE.reciprocal(out=rstd3[:], in_=rstd3[:])
                    so = ch_ot.pslot(DVE)
                    DVE.tensor_scalar(out=o_sb[so][:], in0=ybuf[:, n, :], scalar1=mv3[:, 0:1], scalar2=rstd3[:, 0:1],
                                      op0=ALU.subtract, op1=ALU.mult)
                    DVE.tensor_tensor(out=o_sb[so][:], in0=o_sb[so][:], in1=g2_bc[:], op=ALU.mult)
                    i = DVE.tensor_tensor(out=o_sb[so][:], in0=o_sb[so][:], in1=b2_bc[:], op=ALU.add)
                    ch_ot.produced(M(DVE, i))
                    so = ch_ot.cslot(SP)
                    i = SP.dma_start(out=out[b, n * 128:(n + 1) * 128, :], in_=o_sb[so][:])
                    t = ring_o.start(i, so)
                    ch_ot.consumed(t)
                    last2 = (last2 + [t])[-2:]
                for t in last2:
                    kb.wait(SP, t)
                barrier()
            esp.close()
    return nc


def _prep_inputs(inputs):
    x = np.ascontiguousarray(inputs["x"], dtype=np.float32)
    pos = np.ascontiguousarray(inputs["positions"], dtype=np.int32)
    w_r = np.concatenate([inputs["w_group"][0], np.transpose(inputs["w_expert"][0], (1, 0, 2)).reshape(D, 32)], axis=1)
    b_r = np.concatenate([inputs["b_group"][0], inputs["b_expert"][0].reshape(32)])[None, :]
    ws = inputs["w_spatial"][0]
    shared = {
        "w_in": np.ascontiguousarray(inputs["w_in"][0]),
        "w_out": np.ascontiguousarray(inputs["w_out"][0]),
        "ws_tgs": np.ascontiguousarray(np.transpose(ws, (1, 0, 2))),
        "ws_sgt": np.ascontiguousarray(np.transpose(ws, (2, 0, 1))),
        "bspT": np.ascontiguousarray(inputs["b_spatial"][0].T),
        "sgu_g": np.ascontiguousarray(inputs["sgu_ln_g"]),
        "sgu_b": np.ascontiguousarray(inputs["sgu_ln_b"]),
        "ln1_g": np.ascontiguousarray(inputs["ln1_g"]),
        "ln1_b": np.ascontiguousarray(inputs["ln1_b"]),
        "ln2_g": np.ascontiguousarray(inputs["ln2_g"]),
        "ln2_b": np.ascontiguousarray(inputs["ln2_b"]),
        "w_r": np.ascontiguousarray(w_r, dtype=np.float32),
        "b_r": np.ascontiguousarray(b_r, dtype=np.float32),
        "w_gu": np.ascontiguousarray(np.transpose(inputs["w_gate_up"][0].reshape(NE, D, 2, 128, 2), (0, 1, 2, 4, 3)).reshape(NE, D, 512)),
        "w_dn": np.ascontiguousarray(inputs["w_down"][0].reshape(NE, 256, D)),
    }
    in_maps = []
    for c in range(NCORES):
        xs = x[c * NB:(c + 1) * NB]
        m = dict(shared)
        m["xT"] = np.ascontiguousarray(np.transpose(xs, (0, 2, 1)))
        m["xtm"] = xs
        m["posT"] = np.ascontiguousarray(np.transpose(pos[c * NB:(c + 1) * NB].reshape(NB, NT, 128), (0, 2, 1)))
        in_maps.append(m)
    return in_maps


def kernel(**inputs):
    in_maps = _prep_inputs(inputs)
    nc = build_nc()
    res = run_bass_kernel_spmd(nc, in_maps, core_ids=list(range(NCORES)))
    return np.concatenate([r["out"] for r in res.results], axis=0).astype(np.float32)
```

```python
import contextlib
import numpy as np
import concourse.bass as bass
import concourse.mybir as mybir
from concourse.bass_utils import run_bass_kernel_spmd

F32, BF16, I32 = mybir.dt.float32, mybir.dt.bfloat16, mybir.dt.int32
AF = mybir.ActivationFunctionType
ALU = mybir.AluOpType
AX = mybir.AxisListType

NCORES = 8
S = 2048
D = 1024
NB = 2
NT = S // 128
ALPHA = float(2.0 ** 0.25)
EPS = 1e-5
NEG = -30000.0
NE = 32
TWO_PI = float(2 * np.pi)
DEBUG = False
DEBUG_B = 0
STOP_AFTER = None


class _Stop(Exception):
    pass


class Sem:
    _serial = 0

    def __init__(self, h):
        self.h = h
        self.n = 0
        Sem._serial += 1
        self.uid = Sem._serial


class KB:
    def __init__(self, nc, es, waited=None, prog=None):
        self.nc = nc
        self.es = es
        self.waited = {} if waited is None else waited
        self.prog = {} if prog is None else prog

    sem_es = None

    def sem(self, name):
        return Sem(KB.sem_es.enter_context(self.nc.semaphore(name)))

    def sb(self, name, shape, dt):
        return self.es.enter_context(self.nc.sbuf_tensor(name, shape, dt))

    def ps(self, name, shape, dt):
        return self.es.enter_context(self.nc.psum_tensor(name, shape, dt))

    def inc(self, instr, sem, k=1):
        instr.then_inc(sem.h, k)
        sem.n += k
        return (sem, sem.n)

    def mark(self, eng, instr):
        if isinstance(instr, Tok):
            return instr.tok
        return self.inc(instr, self.prog[id(getattr(eng, "raw", eng))])

    def wait(self, eng, tok):
        if tok is None:
            return
        sem, val = tok
        if val <= 0:
            return
        raw = getattr(eng, "raw", eng)
        key = (id(raw), sem.uid)
        if self.waited.get(key, 0) >= val:
            return
        self.waited[key] = val
        raw.wait_ge(sem.h, val)


class Tok:
    def __init__(self, instr, tok):
        self.instr = instr
        self.tok = tok


_COMPUTE = {"activation", "tensor_tensor", "tensor_scalar", "scalar_tensor_tensor", "tensor_copy", "tensor_reduce",
            "reciprocal", "bn_stats", "bn_aggr", "memset", "affine_select", "iota"}


class EngProxy:
    def __init__(self, raw, kb):
        self.raw = raw
        self.kb = kb
        self.last = None

    def __getattr__(self, name):
        attr = getattr(self.raw, name)
        if name not in _COMPUTE:
            return attr

        def wrapper(*a, **k):
            if self.last is not None:
                self.kb.wait(self, self.last)
            instr = attr(*a, **k)
            tok = self.kb.inc(instr, self.kb.prog[id(self.raw)])
            self.last = tok
            return Tok(instr, tok)
        return wrapper


class Chan:
    def __init__(self, kb, name, depth, ncons=1):
        self.kb = kb
        self.depth = depth
        self.ready = []
        self.free = []
        self.ci = [0] * ncons

    def pslot(self, *engs):
        i = len(self.ready)
        if i >= self.depth:
            for tok in self.free[i - self.depth]:
                for e in engs:
                    self.kb.wait(e, tok)
        return i % self.depth

    def produced(self, *toks):
        self.ready.append(list(toks))

    def cslot(self, eng, c=0):
        i = self.ci[c]
        for tok in self.ready[i]:
            self.kb.wait(eng, tok)
        return i % self.depth

    def consumed(self, tok, c=0):
        i = self.ci[c]
        while len(self.free) <= i:
            self.free.append([])
        self.free[i].append(tok)
        self.ci[c] += 1


class DmaRing:
    def __init__(self, kb, name, depth):
        self.kb = kb
        self.sems = [kb.sem(f"{name}{i}") for i in range(depth)]

    def start(self, instr, slot):
        return self.kb.inc(instr, self.sems[slot], 16)


def build_nc():
    nc = bass.Bass("TRN2", target_bir_lowering=False)
    dr = lambda name, shape, dt=F32: nc.dram_tensor(name, shape, dt, kind="ExternalInput").ap()
    xT = dr("xT", [NB, D, S])
    xtm = dr("xtm", [NB, S, D])
    posT = dr("posT", [NB, 128, NT], I32)
    w_in = dr("w_in", [D, 2560])
    w_out = dr("w_out", [D, D])
    ws_tgs = dr("ws_tgs", [128, 8, 128])
    ws_sgt = dr("ws_sgt", [128, 8, 128])
    bspT = dr("bspT", [128, 8])
    sgu_g = dr("sgu_g", [1, 512])
    sgu_b = dr("sgu_b", [1, 512])
    ln1_g = dr("ln1_g", [1, D])
    ln1_b = dr("ln1_b", [1, D])
    ln2_g = dr("ln2_g", [1, D])
    ln2_b = dr("ln2_b", [1, D])
    w_r = dr("w_r", [D, 36])
    b_r = dr("b_r", [1, 36])
    w_gu = dr("w_gu", [NE, D, 512])
    w_dn = dr("w_dn", [NE, 256, D])
    out = nc.dram_tensor("out", [NB, S, D], F32, kind="ExternalOutput").ap()
    if DEBUG:
        dbg_cat = nc.dram_tensor("dbg_cat", [128, 8, S], F32, kind="ExternalOutput").ap()
        dbg_y = nc.dram_tensor("dbg_y", [128, NT, D], F32, kind="ExternalOutput").ap()
        dbg_gate = nc.dram_tensor("dbg_gate", [128, NT, 32], F32, kind="ExternalOutput").ap()

    PE, ACT, DVE, POOL, SP = nc.tensor, nc.scalar, nc.vector, nc.gpsimd, nc.sync
    engines = [PE, ACT, DVE, POOL, SP]

    with contextlib.suppress(_Stop), contextlib.ExitStack() as es:
        KB.sem_es = es
        kb = KB(nc, es)
        progs = [{id(getattr(e, "raw", e)): kb.sem(f"prog{bb}_{n}") for e, n in zip(engines, "pe act dve pool sp".split())} for bb in range(NB + 1)]
        kb.prog = progs[NB]
        M = kb.mark
        ACT, DVE, POOL = EngProxy(nc.scalar, kb), EngProxy(nc.vector, kb), EngProxy(nc.gpsimd, kb)
        engines = [PE, ACT, DVE, POOL, SP]
        banks = [kb.ps(f"bank{i}", [128, 512], F32) for i in range(8)]

        ident = kb.sb("ident", [128, 128], BF16)
        ones_bf = kb.sb("ones_bf", [128, 128], BF16)
        ones_z = kb.sb("ones_z", [128, 2, 128], BF16)
        zer = kb.sb("zer", [128, 512], F32)
        mhalf = kb.sb("mhalf", [128, 1], F32)
        m_cur = kb.sb("m_cur", [128, 512], BF16)
        m_prev = kb.sb("m_prev", [128, 512], BF16)
        m3 = kb.sb("m3", [128, 4, 512], BF16)
        wmT = kb.sb("wmT", [128, 8, 128], BF16)
        rs = kb.sb("rs", [128, 8], F32)
        bsp_sb = kb.sb("bsp_sb", [128, 8], F32)
        sg_bc = kb.sb("sg_bc", [128, 512], F32)
        sb_bc = kb.sb("sb_bc", [128, 512], F32)
        Bp = kb.sb("Bp", [128, 512], F32)
        br_bc = kb.sb("br_bc", [128, 36], F32)
        wr_hl = kb.sb("wr_hl", [128, 8, 72], BF16)
        invf = kb.sb("invf", [128, NT, 8], F32)
        catT = kb.sb("catT", [128, 8, S], BF16)

        s_c = kb.sem("const_dma")
        s_bar = kb.sem("barrier")

        def barrier():
            base = s_bar.n
            for e in engines:
                if isinstance(e, EngProxy) and e.last is not None:
                    kb.wait(e, e.last)
                kb.inc(e.nop(), s_bar)
            for e in engines:
                kb.wait(e, (s_bar, base + len(engines)))

        def stop_if(tag):
            if STOP_AFTER == tag:
                barrier()
                raise _Stop()

        def wait_all(tok):
            for e in engines:
                kb.wait(e, tok)

        with contextlib.ExitStack() as es0:
            k0 = KB(nc, es0, kb.waited, kb.prog)
            wtmp = k0.sb("wtmp", [128, 8, 128], F32)
            wtmp2 = k0.sb("wtmp2", [128, 8, 128], F32)
            wr_f = k0.sb("wr_f", [128, 8, 36], F32)
            wr_t = k0.sb("wr_t", [128, 8, 36], F32)

            def dma_c(o, i):
                kb.inc(SP.dma_start(out=o, in_=i), s_c, 16)

            dma_c(wtmp[:], ws_sgt)
            dma_c(wtmp2[:], ws_tgs)
            dma_c(bsp_sb[:], bspT)
            dma_c(sg_bc[:], sgu_g.partition_broadcast(128))
            dma_c(sb_bc[:], sgu_b.partition_broadcast(128))
            dma_c(br_bc[:], b_r.partition_broadcast(128))
            dma_c(wr_f[:], w_r.rearrange("(k p) c -> p k c", p=128))
            tok_cdma = (s_c, s_c.n)

            POOL.memset(zer[:], 0.0)
            POOL.memset(ones_bf[:], 1.0)
            POOL.memset(ones_z[:], 0.0)
            POOL.memset(ones_z[:, 0, 0:64], 1.0)
            POOL.memset(ones_z[:, 1, 64:128], 1.0)
            POOL.memset(mhalf[:], -0.5)
            POOL.affine_select(out=ident[:], in_=ones_bf[:], pattern=[[1, 128]], compare_op=ALU.is_equal,
                               fill=0.0, base=0, channel_multiplier=-1)
            POOL.affine_select(out=m_cur[:], in_=zer[:], pattern=[[0, 4], [1, 128]], compare_op=ALU.is_ge,
                               fill=NEG, base=0, channel_multiplier=-1)
            POOL.affine_select(out=m_prev[:], in_=zer[:], pattern=[[0, 4], [-1, 128]], compare_op=ALU.is_ge,
                               fill=NEG, base=0, channel_multiplier=1)
            for s in range(4):
                POOL.affine_select(out=m3[:, s, :], in_=zer[:], pattern=[[0, 16], [1, 32]], compare_op=ALU.is_ge,
                                   fill=NEG, base=32 * s, channel_multiplier=-1)
            inv = (np.float32(500000.0) ** (-(np.arange(0, 16, 2, dtype=np.float32)) / np.float32(16))).astype(np.float32)
            for i in range(8):
                POOL.memset(invf[:, :, i:i + 1], float(inv[i]))
            kb.wait(POOL, tok_cdma)
            POOL.affine_select(out=wmT[:], in_=wtmp[:], pattern=[[0, 8], [1, 128]], compare_op=ALU.is_ge,
                               fill=0.0, base=0, channel_multiplier=-1)
            i_last = POOL.affine_select(out=wtmp[:], in_=wtmp2[:], pattern=[[0, 8], [-1, 128]], compare_op=ALU.is_ge,
                                        fill=0.0, base=0, channel_multiplier=1)
            tok_cpool = M(POOL, i_last)

            kb.wait(DVE, tok_cdma)
            kb.wait(DVE, tok_cpool)
            DVE.tensor_reduce(out=rs[:], in_=wtmp[:], axis=AX.X, op=ALU.add)
            for g in range(8):
                DVE.tensor_scalar(out=Bp[:, g * 64:(g + 1) * 64], in0=sb_bc[:, g * 64:(g + 1) * 64],
                                  scalar1=rs[:, g:g + 1], scalar2=bsp_sb[:, g:g + 1], op0=ALU.mult, op1=ALU.add)
            DVE.tensor_copy(out=wr_hl[:, :, 0:36], in_=wr_f[:])
            DVE.tensor_copy(out=wr_t[:], in_=wr_hl[:, :, 0:36])
            DVE.tensor_tensor(out=wr_t[:], in0=wr_f[:], in1=wr_t[:], op=ALU.subtract)
            i_last = DVE.tensor_copy(out=wr_hl[:, :, 36:72], in_=wr_t[:])
            tok_cdve = M(DVE, i_last)
            wait_all(tok_cdma)
            wait_all(tok_cpool)
            wait_all(tok_cdve)
            barrier()
        stop_if("const")

        def rstd_via_pool(mv, dst, last_dve_instr):
            t = M(DVE, last_dve_instr)
            kb.wait(POOL, t)
            POOL.tensor_scalar(out=dst, in0=mv[:, 1:2], scalar1=EPS, scalar2=None, op0=ALU.add)
            i = POOL.tensor_tensor(out=dst, in0=dst, in1=mhalf[:], op=ALU.pow)
            kb.wait(DVE, M(POOL, i))

        ring_x = DmaRing(kb, "ring_x", 2)
        ring_w = DmaRing(kb, "ring_w", 2)
        ring_o = DmaRing(kb, "ring_o", 2)
        ring_m = DmaRing(kb, "ring_m", 4)
        ring_wg = DmaRing(kb, "ring_wg", 3)
        ring_wd = DmaRing(kb, "ring_wd", 4)
        ring_xs3 = DmaRing(kb, "ring_xs3", 3)
        ring_xs = DmaRing(kb, "ring_xs", 2)
        ring_ys = DmaRing(kb, "ring_ys", 2)
        ring_g = DmaRing(kb, "ring_g", 2)
        ring_g4 = DmaRing(kb, "ring_g4", 4)
        s_sc = kb.sem("scatter")
        rb_gu = es.enter_context(nc.gpsimd.register("rb_gu"))
        rb_dn = es.enter_context(nc.gpsimd.register("rb_dn"))
        nc.gpsimd.reg_mov(rb_gu, NE * 256 - 1)
        nc.gpsimd.reg_mov(rb_dn, NE * 128 - 1)
        NJ = 48
        xs_d = nc.dram_tensor("xs_scratch", [NJ * 256, D], BF16, kind="Internal").ap()
        ys_d = nc.dram_tensor("ys_scratch", [NJ * 256, D], F32, kind="Internal").ap()

        for b in range(NB):
            kb.prog = progs[b]
            ch_u = Chan(kb, "ch_u", 2)
            ch_v = Chan(kb, "ch_v", 2)
            ch_z = Chan(kb, "ch_z", 2)
            ch_tp = Chan(kb, "ch_tp", 2)
            ch_n = Chan(kb, "ch_n", 2)
            ch_gu = Chan(kb, "ch_gu", 2)
            ch_gv = Chan(kb, "ch_gv", 2)
            ch_sg = Chan(kb, "ch_sg", 2)
            ch_qk = Chan(kb, "ch_qk", 2)
            ch_rot = Chan(kb, "ch_rot", 2)
            ch_qs = Chan(kb, "ch_qs", 2)
            ch_vp = Chan(kb, "ch_vp", 2)
            ch_s = Chan(kb, "ch_s", 4)
            ch_p = Chan(kb, "ch_p", 6)
            ch_o = Chan(kb, "ch_o", 2)
            ch_op = Chan(kb, "ch_op", 2)
            ch_x = Chan(kb, "ch_x", 2)
            ch_hb = Chan(kb, "ch_hb", 2)
            ch_hf = Chan(kb, "ch_hf", 2)
            ch_lo = Chan(kb, "ch_lo", 2)
            ch_ht = Chan(kb, "ch_ht", 2)
            ch_r = Chan(kb, "ch_r", 1)
            ch_g2 = Chan(kb, "ch_g2", 2, ncons=2)
            ch_sl = Chan(kb, "ch_sl", 2)
            ch_at = Chan(kb, "ch_at", 3)
            ch_wg = Chan(kb, "ch_wg", 3)
            ch_wd = Chan(kb, "ch_wd", 4)
            ch_dn = Chan(kb, "ch_dn", 1, ncons=2)
            ch_wt = Chan(kb, "ch_wt", 3)
            ch_ot = Chan(kb, "ch_ot", 2)
            ch_xs = Chan(kb, "ch_xs", 2)
            ch_xT = Chan(kb, "ch_xT", 2)
            ch_yo = Chan(kb, "ch_yo", 2)
            ch_rg = Chan(kb, "ch_rg", 4)

            with contextlib.ExitStack() as es1:
                k1 = KB(nc, es1, kb.waited, kb.prog)
                xT_sb = k1.sb(f"xT_sb{b}", [128, 8, S], BF16)
                wsec = k1.sb(f"wsec{b}", [128, 8, 1024], BF16)
                pos_i = k1.sb(f"pos_i{b}", [128, NT], I32)
                pos_f = k1.sb(f"pos_f{b}", [128, NT], F32)
                ang = k1.sb(f"ang{b}", [128, 2, NT, 8], F32)
                kk_i = k1.sb(f"kk_i{b}", [128, 2, NT, 8], I32)
                kk_f = k1.sb(f"kk_f{b}", [128, 2, NT, 8], F32)
                rr = k1.sb(f"rr{b}", [128, 2, NT, 8], F32)
                mm = k1.sb(f"mm{b}", [128, 2, NT, 8], F32)
                cs_t = k1.sb(f"cs_t{b}", [128, 2, NT, 8], F32)
                s_ld = k1.sem(f"a1_ld{b}")
                s_w = k1.sem(f"a1_w{b}")

                k1.inc(POOL.dma_start(out=wsec[:], in_=w_in[:, 1536:2560].rearrange("(k p) c -> p k c", p=128)), s_w, 16)
                tok_w = (s_w, s_w.n)
                s_ldq = [k1.sem(f"a1_ldq{b}_{q}") for q in range(4)]
                tok_xq = []
                for q in range(4):
                    tok_xq.append(k1.inc(POOL.dma_start(out=xT_sb[:, :, q * 512:(q + 1) * 512],
                                                        in_=xT[b, :, q * 512:(q + 1) * 512].rearrange("(k p) t -> p k t", p=128)), s_ldq[q], 16))
                s_pos = k1.sem(f"a1_pos{b}")
                tok_pos = k1.inc(SP.dma_start(out=pos_i[:], in_=posT[b]), s_pos, 16)
                wq_sb = [k1.sb(f"wq_sb{b}_{i}", [128, 8, 384], BF16) for i in range(2)]
                s_ws = k1.sem(f"a1_ws{b}")
                tok_wq = {}

                def load_wq(c):
                    for j, c0 in enumerate((c * 128, 512 + c * 128, 1024 + c * 128)):
                        t_ = k1.inc(POOL.dma_start(out=wq_sb[c % 2][:, :, j * 128:(j + 1) * 128],
                                                   in_=w_in[:, c0:c0 + 128].rearrange("(k p) c -> p k c", p=128)), s_ws, 16)
                    tok_wq[c] = t_

                load_wq(0)

                k1.wait(DVE, tok_pos)
                DVE.tensor_copy(out=pos_f[:], in_=pos_i[:])
                DVE.tensor_tensor(out=ang[:, 1], in0=invf[:], in1=pos_f[:].unsqueeze(2).broadcast_to([128, NT, 8]), op=ALU.mult)
                DVE.tensor_scalar(out=ang[:, 0], in0=ang[:, 1], scalar1=float(np.pi / 2), scalar2=None, op0=ALU.add)
                DVE.tensor_scalar(out=kk_f[:], in0=ang[:], scalar1=float(1.0 / TWO_PI), scalar2=None, op0=ALU.mult)
                DVE.tensor_copy(out=kk_i[:], in_=kk_f[:])
                DVE.tensor_copy(out=kk_f[:], in_=kk_i[:])
                DVE.scalar_tensor_tensor(out=rr[:], in0=kk_f[:], scalar=-TWO_PI, in1=ang[:], op0=ALU.mult, op1=ALU.add)
                DVE.tensor_scalar(out=mm[:], in0=rr[:], scalar1=float(np.pi), scalar2=None, op0=ALU.is_gt)
                DVE.scalar_tensor_tensor(out=rr[:], in0=mm[:], scalar=-TWO_PI, in1=rr[:], op0=ALU.mult, op1=ALU.add)
                DVE.tensor_scalar(out=mm[:], in0=rr[:], scalar1=float(-np.pi), scalar2=None, op0=ALU.is_lt)
                i_l = DVE.scalar_tensor_tensor(out=rr[:], in0=mm[:], scalar=TWO_PI, in1=rr[:], op0=ALU.mult, op1=ALU.add)
                k1.wait(ACT, M(DVE, i_l))
                i_l = ACT.activation(out=cs_t[:], in_=rr[:], func=AF.Sin)
                tok_cs = M(ACT, i_l)
                stop_if("rope")

                with contextlib.ExitStack() as es2:
                    k2 = KB(nc, es2, kb.waited, kb.prog)
                    gu_sb = [k2.sb(f"gu_sb{b}_{i}", [128, 512], F32) for i in range(2)]
                    gv_sb = [k2.sb(f"gv_sb{b}_{i}", [128, 512], F32) for i in range(2)]
                    n_sb = [k2.sb(f"n_sb{b}_{i}", [128, 512], BF16) for i in range(2)]
                    t1_sb = k2.sb(f"t1_sb{b}", [128, 512], F32)
                    sg_sb = [k2.sb(f"sg_sb{b}_{i}", [128, 512], BF16) for i in range(2)]
                    st_sb = k2.sb(f"st_sb{b}", [128, 6], F32)
                    mv_sb = k2.sb(f"mv_sb{b}", [128, 2], F32)
                    rstd_sb = k2.sb(f"rstd_sb{b}", [128, 1], F32)

                    k2.wait(PE, tok_w)

                    def sgu_pe_front(n):
                        k2.wait(PE, tok_xq[n // 4])
                        su = ch_u.pslot(PE)
                        for k in range(8):
                            i = PE.matmul(banks[0 + su][:], xT_sb[:, k, n * 128:(n + 1) * 128], wsec[:, k, 0:512],
                                          start=(k == 0), stop=(k == 7))
                        ch_u.produced(M(PE, i))
                        sv = ch_v.pslot(PE)
                        for k in range(8):
                            i = PE.matmul(banks[2 + sv][:], xT_sb[:, k, n * 128:(n + 1) * 128], wsec[:, k, 512:1024],
                                          start=(k == 0), stop=(k == 7))
                        ch_v.produced(M(PE, i))

                    def sgu_act(n):
                        su = ch_u.cslot(ACT)
                        sg = ch_gu.pslot(ACT)
                        t = M(ACT, ACT.activation(out=gu_sb[sg][:], in_=banks[0 + su][:], func=AF.Gelu))
                        ch_u.consumed(t)
                        ch_gu.produced(t)
                        sv = ch_v.cslot(ACT)
                        sg = ch_gv.pslot(ACT)
                        t = M(ACT, ACT.activation(out=gv_sb[sg][:], in_=banks[2 + sv][:], func=AF.Gelu))
                        ch_v.consumed(t)
                        ch_gv.produced(t)

                    def sgu_dve_norm(n):
                        sg = ch_gv.cslot(DVE)
                        DVE.bn_stats(out=st_sb[:], in_=gv_sb[sg][:])
                        i = DVE.bn_aggr(out=mv_sb[:], in_=st_sb[:])
                        rstd_via_pool(mv_sb, rstd_sb[:], i)
                        sn = ch_n.pslot(DVE)
                        t = M(DVE, DVE.tensor_scalar(out=n_sb[sn][:], in0=gv_sb[sg][:], scalar1=mv_sb[:, 0:1], scalar2=rstd_sb[:, 0:1],
                                                     op0=ALU.subtract, op1=ALU.mult))
                        ch_gv.consumed(t)
                        ch_n.produced(t)

                    def sgu_pe_z(n):
                        sn = ch_n.cslot(PE)
                        sz = ch_z.pslot(PE)
                        for g in range(8):
                            i = PE.matmul(banks[4 + sz][:, g * 64:(g + 1) * 64], wmT[:, g, :], n_sb[sn][:, g * 64:(g + 1) * 64],
                                          start=True, stop=True, skip_group_check=True)
                        t = M(PE, i)
                        ch_n.consumed(t)
                        ch_z.produced(t)

                    def sgu_dve_out(n):
                        sz = ch_z.cslot(DVE)
                        t = M(DVE, DVE.tensor_tensor(out=t1_sb[:], in0=banks[4 + sz][:], in1=sg_bc[:], op=ALU.mult))
                        ch_z.consumed(t)
                        DVE.tensor_tensor(out=t1_sb[:], in0=t1_sb[:], in1=Bp[:], op=ALU.add)
                        sgu_ = ch_gu.cslot(DVE)
                        so = ch_sg.pslot(DVE)
                        t = M(DVE, DVE.tensor_tensor(out=sg_sb[so][:], in0=t1_sb[:], in1=gu_sb[sgu_][:], op=ALU.mult))
                        ch_gu.consumed(t)
                        ch_sg.produced(t)

                    def sgu_pe_tp(n):
                        so = ch_sg.cslot(PE)
                        st = ch_tp.pslot(PE)
                        tpv = banks[6 + st][:].bitcast(BF16)
                        for j in range(4):
                            i = PE.transpose(tpv[:, j * 128:(j + 1) * 128], sg_sb[so][:, j * 128:(j + 1) * 128], ident[:])
                        t = M(PE, i)
                        ch_sg.consumed(t)
                        ch_tp.produced(t)

                    def sgu_act_tp(n):
                        st = ch_tp.cslot(ACT)
                        tpv = banks[6 + st][:].bitcast(BF16)
                        i = ACT.activation(out=catT[:, 4:8, n * 128:(n + 1) * 128],
                                           in_=tpv[:, 0:512].rearrange("p (j t) -> p j t", j=4), func=AF.Copy)
                        ch_tp.consumed(M(ACT, i))

                    for n in range(NT + 2):
                        if n < NT:
                            sgu_pe_front(n)
                            sgu_act(n)
                            sgu_dve_norm(n)
                        if 1 <= n <= NT:
                            sgu_pe_z(n - 1)
                            sgu_dve_out(n - 1)
                        if 2 <= n:
                            sgu_pe_tp(n - 2)
                            sgu_act_tp(n - 2)
                    barrier()
                stop_if("sgu")

                with contextlib.ExitStack() as es3:
                    k3 = KB(nc, es3, kb.waited, kb.prog)
                    QTz = k3.sb(f"QTz{b}", [128, 2, S], BF16)
                    KT = k3.sb(f"KT{b}", [128, S], BF16)
                    Vz = k3.sb(f"Vz{b}", [128, 48, 2, 128], BF16)
                    qs_sb = [k3.sb(f"qs_sb{b}_{i}", [128, 4, 256], BF16) for i in range(2)]
                    ra = k3.sb(f"ra{b}", [128, 4, 4, 8], F32)
                    rb = k3.sb(f"rb{b}", [128, 4, 4, 8], F32)
                    rot_sb = [k3.sb(f"rot_sb{b}_{i}", [128, 4, 2, 2, 16], F32) for i in range(2)]
                    p_sb = [k3.sb(f"p_sb{b}_{i}", [128, 512], BF16) for i in range(6)]
                    rl_sb = k3.sb(f"rl_sb{b}", [128, 512], F32)
                    tok_zero = M(POOL, POOL.memset(QTz[:], 0.0))
                    tok_one = M(DVE, DVE.memset(Vz[:], 1.0))
                    for e in (ACT, DVE, PE):
                        k3.wait(e, tok_zero)
                        k3.wait(e, tok_one)
                    for q in range(4):
                        k3.wait(PE, tok_xq[q])
                    stop_if("att_ms")

                    def tv_blk(T, j):
                        return T[:, j * 128:(j + 1) * 128]

                    def tv_p2(T, s, r4):
                        return T[:, 512 * s:512 * (s + 1)].rearrange("p (i r) -> p r i", r=4)[:, r4, :]

                    def tv_p3k(T, r):
                        return T.rearrange("p (l r) -> p r l", r=16)[:, r, :]

                    def tv_p3q(T, s, r):
                        return T.rearrange("p (l r) -> p r l", r=16)[:, r, 32 * s:32 * (s + 1)]

                    vdefs = []
                    for j in range(16):
                        vdefs.append(lambda T, j=j: tv_blk(T, j))
                    for s in range(4):
                        for r4 in range(4):
                            vdefs.append(lambda T, s=s, r4=r4: tv_p2(T, s, r4))
                    for r in range(16):
                        vdefs.append(lambda T, r=r: tv_p3k(T, r))

                    for c in range(4):
                        wq = wq_sb[c % 2]
                        k3.wait(PE, tok_wq[c])
                        k3.wait(DVE, tok_cs)
                        stop_if("att_dma")

                        def qk_pe(gi):
                            sq = ch_qk.pslot(PE)
                            for w in range(2):
                                for tt in range(4):
                                    n = gi * 4 + tt
                                    for k in range(8):
                                        i = PE.matmul(banks[2 * sq + w][:, tt * 128:(tt + 1) * 128],
                                                      xT_sb[:, k, n * 128:(n + 1) * 128], wq[:, k, w * 128:(w + 1) * 128],
                                                      start=(tt == 0 and k == 0), stop=(k == 7), skip_group_check=True)
                            ch_qk.produced(M(PE, i))

                        def qk_evac(gi):
                            sq = ch_qk.cslot(ACT, 0)
                            so = ch_qs.pslot(ACT, DVE)
                            sr = ch_rot.pslot(ACT)
                            qv = banks[2 * sq + 0][:].rearrange("p (t h e) -> p t h e", t=4, h=2)
                            kv = banks[2 * sq + 1][:].rearrange("p (t h e) -> p t h e", t=4, h=2)
                            ov = qs_sb[so][:].rearrange("p t (w h e) -> p t w h e", w=2, h=2)
                            ACT.activation(out=ov[:, :, 0, :, 16:64], in_=qv[:, :, :, 16:64], func=AF.Copy)
                            ACT.activation(out=ov[:, :, 1, :, 16:64], in_=kv[:, :, :, 16:64], func=AF.Copy)
                            ACT.activation(out=rot_sb[sr][:, :, 0, :, :], in_=qv[:, :, :, 0:16], func=AF.Copy)
                            ta = M(ACT, ACT.activation(out=rot_sb[sr][:, :, 1, :, :], in_=kv[:, :, :, 0:16], func=AF.Copy))
                            ch_qk.consumed(ta, 0)
                            ch_rot.produced(ta)
                            sr = ch_rot.cslot(DVE)
                            rv = rot_sb[sr][:].rearrange("p t w h e -> p t (w h) e")
                            t1 = rv[:, :, :, 0:8]
                            t2 = rv[:, :, :, 8:16]
                            og = qs_sb[so][:].rearrange("p t (g e) -> p t g e", g=4)
                            cosb = cs_t[:, 0, gi * 4:(gi + 1) * 4, :].unsqueeze(2).broadcast_to([128, 4, 4, 8])
                            sinb = cs_t[:, 1, gi * 4:(gi + 1) * 4, :].unsqueeze(2).broadcast_to([128, 4, 4, 8])
                            DVE.tensor_tensor(out=ra[:], in0=t1, in1=cosb, op=ALU.mult)
                            DVE.tensor_tensor(out=rb[:], in0=t2, in1=sinb, op=ALU.mult)
                            DVE.tensor_tensor(out=og[:, :, :, 0:8], in0=ra[:], in1=rb[:], op=ALU.subtract)
                            DVE.tensor_tensor(out=ra[:], in0=t2, in1=cosb, op=ALU.mult)
                            DVE.tensor_tensor(out=rb[:], in0=t1, in1=sinb, op=ALU.mult)
                            td = M(DVE, DVE.tensor_tensor(out=og[:, :, :, 8:16], in0=ra[:], in1=rb[:], op=ALU.add))
                            ch_rot.consumed(td)
                            ch_qs.produced(ta, td)

                        def qk_tp(gi):
                            so = ch_qs.cslot(PE)
                            st = ch_tp.pslot(PE)
                            tpv = banks[6 + st][:].bitcast(BF16).rearrange("p (t w e) -> p t w e", t=4, w=2)
                            for tt in range(4):
                                for w in range(2):
                                    i = PE.transpose(tpv[:, tt, w, :], qs_sb[so][:, tt, w * 128:(w + 1) * 128], ident[:])
                            t = M(PE, i)
                            ch_qs.consumed(t)
                            ch_tp.produced(t)

                        def qk_tp_evac(gi):
                            st = ch_tp.cslot(ACT)
                            tpv = banks[6 + st][:].bitcast(BF16).rearrange("p (t w e) -> p t w e", t=4, w=2)
                            cols = slice(gi * 512, (gi + 1) * 512)
                            ACT.activation(out=QTz[0:64, 0, cols].rearrange("p (t e) -> p t e", t=4), in_=tpv[0:64, :, 0, :], func=AF.Copy)
                            ACT.activation(out=QTz[64:128, 1, cols].rearrange("p (t e) -> p t e", t=4), in_=tpv[64:128, :, 0, :], func=AF.Copy)
                            i = ACT.activation(out=KT[:, cols].rearrange("p (t e) -> p t e", t=4), in_=tpv[:, :, 1, :], func=AF.Copy)
                            ch_tp.consumed(M(ACT, i))

                        for gi in range(4 + 2):
                            if gi < 4:
                                qk_pe(gi)
                                stop_if("qk_pe0")
                                qk_evac(gi)
                                stop_if("qk_ev0")
                            if 1 <= gi <= 4:
                                qk_tp(gi - 1)
                                stop_if("qk_tp0")
                            if 2 <= gi:
                                qk_tp_evac(gi - 2)
                                stop_if("qk_te0")
                        stop_if("qk")

                        for gi in range(12):
                            sv = ch_vp.pslot(PE)
                            for tt in range(4):
                                vd = vdefs[gi * 4 + tt]
                                for k in range(8):
                                    i = PE.matmul(banks[4 + sv][:, tt * 128:(tt + 1) * 128], vd(xT_sb[:, k, :]), wq[:, k, 256:384],
                                                  start=(tt == 0 and k == 0), stop=(k == 7), skip_group_check=True)
                            ch_vp.produced(M(PE, i))
                            eng = ACT if gi % 2 == 0 else DVE
                            sv = ch_vp.cslot(eng)
                            src = banks[4 + sv][:].rearrange("p (t h e) -> p t h e", t=4, h=2)
                            dst = Vz[:, gi * 4:(gi + 1) * 4, :, :]
                            if eng is ACT:
                                ACT.activation(out=dst[:, :, 0, 0:64], in_=src[:, :, 0, :], func=AF.Copy)
                                i = ACT.activation(out=dst[:, :, 1, 64:128], in_=src[:, :, 1, :], func=AF.Copy)
                            else:
                                DVE.tensor_copy(out=dst[:, :, 0, 0:64], in_=src[:, :, 0, :])
                                i = DVE.tensor_copy(out=dst[:, :, 1, 64:128], in_=src[:, :, 1, :])
                            ch_vp.consumed(M(eng, i))
                        barrier()
                        stop_if("v")
                        if c + 1 < 4:
                            load_wq(c + 1)

                        V1 = lambda j, hh: Vz[:, j, hh, :]
                        V2 = lambda s, r4, hh: Vz[:, 16 + 4 * s + r4, hh, :]
                        V3 = lambda r, hh: Vz[:, 32 + r, hh, :]

                        def emit_s(maskt, mms, lo=0, hi=512):
                            ss = ch_s.pslot(PE)
                            PE.matmul(banks[ss][:], ident[:], maskt, start=True, stop=False, skip_group_check=True)
                            for (oc, lhsT, rhs) in mms:
                                i = PE.matmul(banks[ss][:, oc[0]:oc[1]], lhsT, rhs, start=False, stop=True, skip_group_check=True)
                            ch_s.produced(M(PE, i))
                            ss2 = ch_s.cslot(ACT)
                            sp = ch_p.pslot(ACT)
                            t = M(ACT, ACT.activation(out=p_sb[sp][:, lo:hi], in_=banks[ss2][:, lo:hi], func=AF.Exp, scale=0.125))
                            ch_s.consumed(t)
                            ch_p.produced(t)
                            return sp

                        def oview(Bk, oc):
                            if oc[0] == "blk":
                                return Bk[:, oc[1] * 128:(oc[1] + 1) * 128]
                            if oc[0] == "p2":
                                return Bk[:].rearrange("p (i r) -> p r i", r=4)[:, oc[1], :]
                            return Bk[:].rearrange("p (i r) -> p r i", r=16)[:, oc[1], :]

                        for s in range(4):
                            so_ = ch_o.pslot(PE)
                            Ob = banks[4 + 2 * so_]
                            Lb = banks[5 + 2 * so_]
                            first_pv = [True, True]
                            for hh in range(2):
                                Q = QTz[:, hh, :]
                                pend = []
                                mms = [((j * 128, (j + 1) * 128), tv_blk(KT, 4 * s + j), tv_blk(Q, 4 * s + j)) for j in range(4)]
                                sp = emit_s(m_cur[:], mms)
                                pend.append((sp, [(V1(4 * s + j, hh), (j * 128, (j + 1) * 128), ("blk", j)) for j in range(4)]))
                                js = [j for j in range(4) if 4 * s + j >= 1]
                                mms = [((j * 128, (j + 1) * 128), tv_blk(KT, 4 * s + j - 1), tv_blk(Q, 4 * s + j)) for j in js]
                                sp = emit_s(m_prev[:], mms, lo=js[0] * 128)
                                pend.append((sp, [(V1(4 * s + j - 1, hh), (j * 128, (j + 1) * 128), ("blk", j)) for j in js]))
                                mms = [((r4 * 128, (r4 + 1) * 128), tv_p2(KT, s, r4), tv_p2(Q, s, r4)) for r4 in range(4)]
                                sp = emit_s(m_cur[:], mms)
                                pend.append((sp, [(V2(s, r4, hh), (r4 * 128, (r4 + 1) * 128), ("p2", r4)) for r4 in range(4)]))
                                if s >= 1:
                                    mms = [((r4 * 128, (r4 + 1) * 128), tv_p2(KT, s - 1, r4), tv_p2(Q, s, r4)) for r4 in range(4)]
                                    sp = emit_s(m_prev[:], mms)
                                    pend.append((sp, [(V2(s - 1, r4, hh), (r4 * 128, (r4 + 1) * 128), ("p2", r4)) for r4 in range(4)]))
                                mms = [((r * 32, (r + 1) * 32), tv_p3k(KT, r), tv_p3q(Q, s, r)) for r in range(16)]
                                sp = emit_s(m3[:, s, :], mms)
                                pend.append((sp, [(V3(r, hh), (r * 32, (r + 1) * 32), ("p3", r)) for r in range(16)]))

                                for (sp, pvs) in pend:
                                    sp2 = ch_p.cslot(PE)
                                    assert sp2 == sp
                                    for (vt, pc, oc) in pvs:
                                        i = PE.matmul(oview(Lb if hh else Ob, oc), vt, p_sb[sp][:, pc[0]:pc[1]], start=first_pv[hh], stop=False,
                                                      skip_group_check=True)
                                        first_pv[hh] = False
                                    t_last = M(PE, i)
                                    ch_p.consumed(t_last)
                            ch_o.produced(t_last)
                            so2 = ch_o.cslot(DVE)
                            B0, B1 = banks[4 + 2 * so2], banks[5 + 2 * so2]
                            DVE.reciprocal(out=rl_sb[0:64, :], in_=B0[64:128, :])
                            DVE.tensor_tensor(out=catT[0:64, c, 512 * s:512 * (s + 1)], in0=B0[0:64, :], in1=rl_sb[0:64, :], op=ALU.mult)
                            DVE.reciprocal(out=rl_sb[64:128, :], in_=B1[0:64, :])
                            i = DVE.tensor_tensor(out=catT[64:128, c, 512 * s:512 * (s + 1)], in0=B1[64:128, :], in1=rl_sb[64:128, :], op=ALU.mult)
                            ch_o.consumed(M(DVE, i))
                            stop_if("attn_s0")
                        barrier()
                        if DEBUG and b == DEBUG_B and STOP_AFTER == "attn":
                            with contextlib.ExitStack() as esd:
                                kd = KB(nc, esd, kb.waited, kb.prog)
                                dtmp = kd.sb("dtmpa", [128, 4, S], F32)
                                sd = kd.sem("dbga")
                                kd.wait(SP, M(DVE, DVE.tensor_copy(out=dtmp[:], in_=catT[:, 0:4, :])))
                                kd.wait(SP, kd.inc(SP.dma_start(out=dbg_cat[:, 0:4, :], in_=dtmp[:]), sd, 16))
                            stop_if("attn")

                if DEBUG and b == DEBUG_B:
                    with contextlib.ExitStack() as esd:
                        kd = KB(nc, esd, kb.waited, kb.prog)
                        dtmp = kd.sb("dtmp", [128, 8, S], F32)
                        sd = kd.sem("dbg1")
                        kd.wait(SP, M(DVE, DVE.tensor_copy(out=dtmp[:], in_=catT[:])))
                        kd.wait(SP, kd.inc(SP.dma_start(out=dbg_cat, in_=dtmp[:]), sd, 16))
                        barrier()

            esp = contextlib.ExitStack()
            kp = KB(nc, esp, kb.waited, kb.prog)
            hb_all = kp.sb(f"hb_all{b}", [128, NT, D], BF16)
            ybuf = kp.sb(f"ybuf{b}", [128, NT, D], F32)
            M1a = kp.sb(f"M1a{b}", [128, NT, 32], F32)
            M2a = kp.sb(f"M2a{b}", [128, NT, 32], F32)
            g12 = kp.sb(f"g12{b}", [128, 2, NT], F32)
            sl_i = kp.sb(f"sl_i{b}", [128, 2, NT], I32)
            with contextlib.ExitStack() as es4:
                k4 = KB(nc, es4, kb.waited, kb.prog)
                wo_sb = k4.sb(f"wo_sb{b}", [128, 8, D], BF16)
                x_sb = [k4.sb(f"x_sb{b}_{i}", [128, D], F32) for i in range(2)]
                hT_sb = [k4.sb(f"hT_sb{b}_{i}", [128, 8, 128], BF16) for i in range(2)]
                hf_sb = [k4.sb(f"hf_sb{b}_{i}", [128, D], F32) for i in range(2)]
                eps_t = k4.sb(f"eps_t{b}", [128, 1], F32)
                k4.wait(ACT, M(POOL, POOL.memset(eps_t[:], EPS)))
                math_done = [None]
                pend_D = []
                NH = 8
                lg_all = k4.sb(f"lg_all{b}", [128, NH, 72], F32)
                rq = k4.sb(f"rq{b}", [128, NH, 64], F32)
                lo_sb = [k4.sb(f"lo_sb{b}_{i}", [128, D], BF16) for i in range(2)]
                loT_sb = [k4.sb(f"loT_sb{b}_{i}", [128, 8, 128], BF16) for i in range(2)]
                st2 = k4.sb(f"st2_{b}", [128, 2, 6], F32)
                mv2 = k4.sb(f"mv2_{b}", [128, 2], F32)
                rstd2 = k4.sb(f"rstd2_{b}", [128, 1], F32)
                s_wo = k4.sem(f"a2_wo{b}")
                g1_bc = k4.sb(f"g1_bc{b}", [128, D], F32)
                b1_bc = k4.sb(f"b1_bc{b}", [128, D], F32)
                s_g1 = k4.sem(f"a2_g1{b}")
                k4.inc(SP.dma_start(out=g1_bc[:], in_=ln1_g.partition_broadcast(128)), s_g1, 16)
                k4.wait(DVE, k4.inc(SP.dma_start(out=b1_bc[:], in_=ln1_b.partition_broadcast(128)), s_g1, 16))

                t_wo = k4.inc(POOL.dma_start(out=wo_sb[:], in_=w_out.rearrange("(k p) c -> p k c", p=128)), s_wo, 16)
                k4.wait(PE, t_wo)
                k4.wait(DVE, t_wo)

                def a2_load(n):
                    sx = ch_x.pslot(SP)
                    i = SP.dma_start(out=x_sb[sx][:], in_=xtm[b, n * 128:(n + 1) * 128, :])
                    ch_x.produced(ring_x.start(i, sx))

                def a2_pe_op(n):
                    so = ch_op.pslot(PE)
                    for hf in range(2):
                        for k in range(8):
                            i = PE.matmul(banks[2 * so + hf][:], catT[:, k, n * 128:(n + 1) * 128], wo_sb[:, k, hf * 512:(hf + 1) * 512],
                                          start=(k == 0), stop=(k == 7))
                    ch_op.produced(M(PE, i))

                def a2_dve_A(n):
                    so = ch_op.cslot(DVE)
                    sx = ch_x.cslot(DVE)
                    sh = n % 2
                    hf_ = hf_sb[sh]
                    for hf in range(2):
                        i = DVE.scalar_tensor_tensor(out=hf_[:, hf * 512:(hf + 1) * 512], in0=x_sb[sx][:, hf * 512:(hf + 1) * 512],
                                                     scalar=ALPHA, in1=banks[2 * so + hf][:], op0=ALU.mult, op1=ALU.add)
                    t = M(DVE, i)
                    ch_op.consumed(t)
                    ch_x.consumed(t)
                    for hf in range(2):
                        DVE.bn_stats(out=st2[:, hf, :], in_=hf_[:, hf * 512:(hf + 1) * 512])
                    i = DVE.bn_aggr(out=mv2[:], in_=st2[:].rearrange("p a c -> p (a c)"))
                    kb.wait(ACT, M(DVE, i))
                    i = ACT.activation(out=rstd2[:], in_=mv2[:, 1:2], func=AF.Sqrt, bias=eps_t[:, 0:1], scale=1.0)
                    kb.wait(DVE, M(ACT, i))
                    DVE.reciprocal(out=rstd2[:], in_=rstd2[:])
                    t = M(DVE, DVE.tensor_scalar(out=hf_[:], in0=hf_[:], scalar1=mv2[:, 0:1], scalar2=rstd2[:, 0:1],
                                                 op0=ALU.subtract, op1=ALU.mult))
                    kb.wait(POOL, t)
                    POOL.tensor_tensor(out=hf_[:], in0=hf_[:], in1=g1_bc[:], op=ALU.mult)
                    t = M(POOL, POOL.tensor_tensor(out=hf_[:], in0=hf_[:], in1=b1_bc[:], op=ALU.add))
                    kb.wait(ACT, t)
                    ACT.activation(out=hb_all[:, n, :], in_=hf_[:], func=AF.Copy)
                    tC = M(ACT, ACT.activation(out=ybuf[:, n, :], in_=hf_[:], func=AF.Copy, scale=ALPHA))
                    pend_D.append((n, sh, tC))

                def a2_dve_D():
                    n, sh, tC = pend_D.pop(0)
                    kb.wait(DVE, tC)
                    sl_ = ch_hb.pslot(DVE)
                    t = M(DVE, DVE.tensor_tensor(out=lo_sb[sl_][:], in0=hf_sb[sh][:], in1=hb_all[:, n, :], op=ALU.subtract))
                    ch_hb.produced(t)

                def a2_pe_tp(n):
                    sh = ch_hb.cslot(PE)
                    st = ch_tp.pslot(PE)
                    tpv = banks[6 + st][:].bitcast(BF16)
                    for k in range(8):
                        i = PE.transpose(tpv[:, k * 128:(k + 1) * 128], hb_all[:, n, k * 128:(k + 1) * 128], ident[:])
                    ch_tp.produced(M(PE, i))
                    st = ch_tp.pslot(PE)
                    tpv = banks[6 + st][:].bitcast(BF16)
                    for k in range(8):
                        i = PE.transpose(tpv[:, k * 128:(k + 1) * 128], lo_sb[sh][:, k * 128:(k + 1) * 128], ident[:])
                    t = M(PE, i)
                    ch_hb.consumed(t)
                    ch_tp.produced(t)

                def a2_act_tp(n):
                    st = ch_tp.cslot(ACT)
                    tpv = banks[6 + st][:].bitcast(BF16)
                    sht = ch_ht.pslot(ACT)
                    i = ACT.activation(out=hT_sb[sht][:], in_=tpv.rearrange("p (k t) -> p k t", k=8), func=AF.Copy)
                    t_h = M(ACT, i)
                    ch_tp.consumed(t_h)
                    ch_ht.produced(t_h)
                    st = ch_tp.cslot(ACT)
                    tpv = banks[6 + st][:].bitcast(BF16)
                    sl = ch_lo.pslot(ACT)
                    i = ACT.activation(out=loT_sb[sl][:], in_=tpv.rearrange("p (k t) -> p k t", k=8), func=AF.Copy)
                    t = M(ACT, i)
                    ch_tp.consumed(t)
                    ch_lo.produced(t)
                    return t_h

                def a2_pe_route(n, t_h):
                    sl = ch_lo.cslot(PE)
                    sht = ch_ht.cslot(PE)
                    ch_r.pslot(PE)
                    rp = banks[4]
                    for k in range(8):
                        PE.matmul(rp[:, 0:72], hT_sb[sht][:, k, :], wr_hl[:, k, 0:72], start=(k == 0), stop=False,
                                  skip_group_check=True)
                    for k in range(8):
                        i = PE.matmul(rp[:, 0:36], loT_sb[sl][:, k, :], wr_hl[:, k, 0:36], start=False, stop=(k == 7),
                                      skip_group_check=True)
                    t = M(PE, i)
                    ch_lo.consumed(t)
                    ch_ht.consumed(t)
                    ch_r.produced(t)

                def a2_route_copy(n):
                    ch_r.cslot(ACT)
                    kb.wait(ACT, math_done[0])
                    t = M(ACT, ACT.activation(out=lg_all[:, n % NH, :], in_=banks[4][:, 0:72], func=AF.Copy))
                    ch_r.consumed(t)
                    return t

                def a2_route_math(n0, t_last):
                    kb.wait(DVE, t_last)
                    sl = slice(n0, n0 + NH)
                    L = lg_all[:, :, 0:36]
                    DVE.tensor_tensor(out=L, in0=L, in1=lg_all[:, :, 36:72], op=ALU.add)
                    DVE.tensor_tensor(out=L, in0=L, in1=br_bc[:].unsqueeze(1).broadcast_to([128, NH, 36]), op=ALU.add)
                    gmax, oh, ge, sume = rq[:, :, 0:1], rq[:, :, 1:5], rq[:, :, 5:9], rq[:, :, 9:10]
                    esel, m1, eq1, e2 = rq[:, :, 11:19], rq[:, :, 19:20], rq[:, :, 20:28], rq[:, :, 28:36]
                    m2, eq2, dd, w1, w2, tmp8 = rq[:, :, 36:37], rq[:, :, 37:45], rq[:, :, 45:46], rq[:, :, 46:47], rq[:, :, 47:48], rq[:, :, 48:56]
                    bc = lambda ap, k: ap.broadcast_to([128, NH, k])
                    DVE.tensor_reduce(out=gmax, in_=lg_all[:, :, 0:4], axis=AX.X, op=ALU.max)
                    DVE.tensor_tensor(out=oh, in0=lg_all[:, :, 0:4], in1=bc(gmax, 4), op=ALU.is_equal)
                    DVE.tensor_tensor(out=ge, in0=lg_all[:, :, 0:4], in1=bc(gmax, 4), op=ALU.subtract)
                    DVE.tensor_tensor(out=esel, in0=lg_all[:, :, 4:12], in1=bc(oh[:, :, 0:1], 8), op=ALU.mult)
                    for g in range(1, 4):
                        DVE.tensor_tensor(out=tmp8, in0=lg_all[:, :, 4 + 8 * g:12 + 8 * g], in1=bc(oh[:, :, g:g + 1], 8), op=ALU.mult)
                        DVE.tensor_tensor(out=esel, in0=esel, in1=tmp8, op=ALU.add)
                    DVE.tensor_reduce(out=m1, in_=esel, axis=AX.X, op=ALU.max)
                    DVE.tensor_tensor(out=eq1, in0=esel, in1=bc(m1, 8), op=ALU.is_equal)
                    DVE.scalar_tensor_tensor(out=e2, in0=eq1, scalar=-1e30, in1=esel, op0=ALU.mult, op1=ALU.add)
                    DVE.tensor_reduce(out=m2, in_=e2, axis=AX.X, op=ALU.max)
                    DVE.tensor_tensor(out=eq2, in0=e2, in1=bc(m2, 8), op=ALU.is_equal)
                    i = DVE.tensor_tensor(out=dd, in0=m2, in1=m1, op=ALU.subtract)
                    kb.wait(ACT, M(DVE, i))
                    ACT.activation(out=ge, in_=ge, func=AF.Exp)
                    i = ACT.activation(out=dd, in_=dd, func=AF.Exp)
                    kb.wait(DVE, M(ACT, i))
                    DVE.tensor_reduce(out=sume, in_=ge, axis=AX.X, op=ALU.add)
                    DVE.reciprocal(out=sume, in_=sume)
                    DVE.tensor_scalar(out=w1, in0=dd, scalar1=1.0, scalar2=None, op0=ALU.add)
                    DVE.reciprocal(out=w1, in_=w1)
                    DVE.tensor_tensor(out=w2, in0=dd, in1=w1, op=ALU.mult)
                    DVE.tensor_tensor(out=g12[:, 0, sl].unsqueeze(2), in0=w1, in1=sume, op=ALU.mult)
                    DVE.tensor_tensor(out=g12[:, 1, sl].unsqueeze(2), in0=w2, in1=sume, op=ALU.mult)
                    for g in range(4):
                        DVE.tensor_tensor(out=M1a[:, sl, g * 8:(g + 1) * 8], in0=eq1, in1=bc(oh[:, :, g:g + 1], 8), op=ALU.mult)
                        tm = DVE.tensor_tensor(out=M2a[:, sl, g * 8:(g + 1) * 8], in0=eq2, in1=bc(oh[:, :, g:g + 1], 8), op=ALU.mult)
                    math_done[0] = M(DVE, tm)

                a2_load(0)
                t_hs = {}
                for n in range(NT + 3):
                    if n + 1 < NT:
                        a2_load(n + 1)
                    if n < NT:
                        a2_pe_op(n)
                        a2_dve_A(n)
                    if 1 <= n <= NT:
                        a2_dve_D()
                    if 2 <= n <= NT + 1:
                        a2_pe_tp(n - 2)
                        t_hs[n - 2] = a2_act_tp(n - 2)
                    if 3 <= n:
                        a2_pe_route(n - 3, t_hs[n - 3])
                        t_c = a2_route_copy(n - 3)
                        if (n - 3) % NH == NH - 1:
                            a2_route_math(n - 3 - (NH - 1), t_c)
                barrier()

            if DEBUG and b == DEBUG_B:
                with contextlib.ExitStack() as esd:
                    kd = KB(nc, esd, kb.waited, kb.prog)
                    sd = kd.sem("dbg2")
                    kd.inc(SP.dma_start(out=dbg_y, in_=ybuf[:]), sd, 16)
                    kd.wait(SP, kd.inc(SP.dma_start(out=dbg_gate, in_=M1a[:]), sd, 16))
                    barrier()
            if not DEBUG or b == DEBUG_B:
                stop_if("A2")

            with contextlib.ExitStack() as es5:
                k5 = KB(nc, es5, kb.waited, kb.prog)
                NWS = 3
                wgu_sb = [catT[:, 2 * i:2 * i + 2, :].rearrange("p a (k f) -> p (a k) f", f=512) for i in range(3)]
                wdn_t = [k5.sb(f"wdn_t{b}_{i}", [128, 2, D], BF16) for i in range(2)]
                wdn_sb = [t_[:] for t_ in wdn_t] + [catT[:, 6 + i, :].rearrange("p (k f) -> p k f", f=1024) for i in range(2)]
                thr = k5.sb(f"thr{b}", [128, NJ, 32], F32)
                Mb = k5.sb(f"Mb{b}", [128, NT * 32], BF16)
                Ms = k5.sb(f"Ms{b}", [128, NT, 32], F32)
                cs = k5.sb(f"cs{b}", [128, NT, 32], F32)
                off = k5.sb(f"off{b}", [128, NT, 32], F32)
                Sf = k5.sb(f"Sf{b}", [128, NT, 32], F32)
                tS = Ms
                sc_a = k5.sb(f"sc_a{b}", [128, 32], F32)
                sc_b = k5.sb(f"sc_b{b}", [128, 32], F32)
                pt = k5.sb(f"pt{b}", [128, 32], F32)
                base = k5.sb(f"base{b}", [128, 32], F32)
                qi = k5.sb(f"qi{b}", [128, 32], I32)
                sl_f = k5.sb(f"sl_f{b}", [128, 2, NT], F32)
                ej_f = k5.sb(f"ej_f{b}", [128, NJ], F32)
                wi_f = thr[:].rearrange("p j e -> p (j e)")[:, 0:NJ * 3].rearrange("p (j c) -> p j c", c=3)
                wi_i = k5.sb(f"wi_i{b}", [128, NJ, 3], I32)
                pidx = k5.sb(f"pidx{b}", [128, 1], F32)
                ko = k5.sb(f"ko{b}", [128, 8], F32)
                Lst = k5.sb(f"Lst{b}", [128, 128], BF16)
                xt_sb = [k5.sb(f"xt_sb{b}_{i}", [128, 2, D], BF16) for i in range(2)]
                xsT = [k5.sb(f"xsT{b}_{i}", [128, 8, 256], BF16) for i in range(2)]
                sl_sb = [k5.sb(f"sl_sb{b}_{i}", [128, 512], F32) for i in range(2)]
                at_sb = [k5.sb(f"at_sb{b}_{i}", [128, 2, 256], BF16) for i in range(3)]
                yo_sb = [k5.sb(f"yo_sb{b}_{i}", [128, D], F32) for i in range(2)]

                POOL.affine_select(out=Lst[:], in_=ones_bf[:], pattern=[[1, 128]], compare_op=ALU.is_gt,
                                   fill=0.0, base=0, channel_multiplier=-1)
                t_thr = M(POOL, POOL.iota(thr[:], pattern=[[256, NJ], [0, 32]], base=0, channel_multiplier=0,
                                          allow_small_or_imprecise_dtypes=True))
                POOL.iota(pidx[:], pattern=[[0, 1]], base=0, channel_multiplier=1, allow_small_or_imprecise_dtypes=True)
                t_io = M(POOL, POOL.iota(ko[:], pattern=[[128, 8]], base=0, channel_multiplier=0, allow_small_or_imprecise_dtypes=True))
                DVE.tensor_tensor(out=Ms[:], in0=M1a[:], in1=M2a[:], op=ALU.add)
                t = M(DVE, DVE.tensor_copy(out=Mb[:], in_=Ms[:].rearrange("p n e -> p (n e)")))
                kb.wait(PE, t)
                kb.wait(PE, t_thr)
                PE.matmul(banks[0][:], Lst[:], Mb[:], start=True, stop=True)
                t = M(PE, PE.matmul(banks[1][:], ones_bf[:], Mb[:], start=True, stop=True))
                kb.wait(DVE, t)
                DVE.tensor_copy(out=cs[:], in_=banks[1][:].rearrange("p (n e) -> p n e", e=32))
                DVE.memset(off[:, 0, :], 0.0)
                for n in range(1, NT):
                    DVE.tensor_tensor(out=off[:, n, :], in0=off[:, n - 1, :], in1=cs[:, n - 1, :], op=ALU.add)
                DVE.tensor_tensor(out=sc_a[:], in0=off[:, NT - 1, :], in1=cs[:, NT - 1, :], op=ALU.add)
                DVE.tensor_scalar(out=sc_b[:], in0=sc_a[:], scalar1=127.5, scalar2=1.0 / 256.0, op0=ALU.add, op1=ALU.mult)
                DVE.tensor_copy(out=qi[:], in_=sc_b[:])
                DVE.tensor_scalar(out=pt[:], in0=qi[:], scalar1=256.0, scalar2=None, op0=ALU.mult)
                DVE.tensor_copy(out=sc_a[:], in_=pt[:])
                pa, pb = sc_a, sc_b
                for sh in (1, 2, 4, 8, 16):
                    DVE.tensor_copy(out=pb[:, 0:sh], in_=pa[:, 0:sh])
                    DVE.tensor_tensor(out=pb[:, sh:32], in0=pa[:, sh:32], in1=pa[:, 0:32 - sh], op=ALU.add)
                    pa, pb = pb, pa
                incl = pa
                DVE.tensor_tensor(out=base[:], in0=incl[:], in1=pt[:], op=ALU.subtract)
                DVE.tensor_tensor(out=Sf[:], in0=banks[0][:].rearrange("p (n e) -> p n e", e=32), in1=off[:], op=ALU.add)
                DVE.tensor_tensor(out=Sf[:], in0=Sf[:], in1=base[:].unsqueeze(1).broadcast_to([128, NT, 32]), op=ALU.add)
                DVE.tensor_tensor(out=tS[:], in0=Sf[:], in1=M1a[:], op=ALU.mult)
                DVE.tensor_reduce(out=sl_f[:, 0, :], in_=tS[:], axis=AX.X, op=ALU.add)
                DVE.tensor_tensor(out=tS[:], in0=Sf[:], in1=M2a[:], op=ALU.mult)
                DVE.tensor_reduce(out=sl_f[:, 1, :], in_=tS[:], axis=AX.X, op=ALU.add)
                DVE.tensor_copy(out=sl_i[:], in_=sl_f[:])
                kb.wait(DVE, t_thr)
                DVE.tensor_tensor(out=thr[:], in0=thr[:], in1=incl[:].unsqueeze(1).broadcast_to([128, NJ, 32]), op=ALU.is_ge)
                DVE.tensor_reduce(out=ej_f[:], in_=thr[:], axis=AX.X, op=ALU.add)
                kb.wait(DVE, t_io)
                DVE.tensor_scalar(out=ej_f[:], in0=ej_f[:], scalar1=256.0, scalar2=pidx[:, 0:1], op0=ALU.mult, op1=ALU.add)
                DVE.tensor_tensor(out=wi_f[:, :, 0:2], in0=ej_f[:].unsqueeze(2).broadcast_to([128, NJ, 2]),
                                  in1=ko[:, 0:2].unsqueeze(1).broadcast_to([128, NJ, 2]), op=ALU.add)
                DVE.tensor_scalar(out=ej_f[:], in0=ej_f[:], scalar1=pidx[:, 0:1], scalar2=0.5, op0=ALU.subtract, op1=ALU.mult)
                DVE.tensor_scalar(out=wi_f[:, :, 2:3], in0=ej_f[:].unsqueeze(2), scalar1=pidx[:, 0:1], scalar2=None, op0=ALU.add)
                t_disp = M(DVE, DVE.tensor_copy(out=wi_i[:], in_=wi_f))

                kb.wait(POOL, t_disp)
                for n in range(NT):
                    for kk in range(2):
                        i = POOL.indirect_dma_start(out=xs_d, out_offset=bass.IndirectOffsetOnAxis(ap=sl_i[:, kk, n:n + 1], axis=0),
                                                    in_=hb_all[:, n, :], in_offset=None)
                        kb.inc(i, s_sc, 16)
                tok_sc = (s_sc, s_sc.n)
                kb.wait(SP, tok_sc)
                kb.wait(POOL, tok_sc)

                wgu_rows = w_gu.rearrange("e (d4 i) f -> (e d4) (i f)", i=4)
                wdn_rows = w_dn.rearrange("e (f2 i) d -> (e f2) (i d)", i=2)

                def m_load_w(j):
                    sg_ = ch_wg.pslot(POOL)
                    gv = wgu_sb[sg_].rearrange("p (k4 i) f -> p k4 (i f)", i=4)
                    for k4 in range(2):
                        i = POOL.indirect_dma_start(out=gv[:, k4, :], out_offset=None, in_=wgu_rows,
                                                    in_offset=bass.IndirectOffsetOnAxis(ap=wi_i[:, j, k4:k4 + 1], axis=0),
                                                    bounds_check=rb_gu, oob_is_err=False)
                        t1 = ring_wg.start(i, sg_)
                    ch_wg.produced(t1)
                    sd_ = ch_wd.pslot(POOL)
                    i = POOL.indirect_dma_start(out=wdn_sb[sd_].rearrange("p i d -> p (i d)"), out_offset=None, in_=wdn_rows,
                                                in_offset=bass.IndirectOffsetOnAxis(ap=wi_i[:, j, 2:3], axis=0),
                                                bounds_check=rb_dn, oob_is_err=False)
                    ch_wd.produced(ring_wd.start(i, sd_))

                def m_load_x(j):
                    sx = ch_xs.pslot(SP)
                    i = SP.dma_start(out=xt_sb[sx][:], in_=xs_d[256 * j:256 * (j + 1), :].rearrange("(a p) d -> p a d", p=128))
                    ch_xs.produced(ring_xs.start(i, sx))

                def m_tp(j):
                    sx = ch_xs.cslot(PE)
                    ch_tp.pslot(PE)
                    ch_tp.pslot(PE)
                    for half in range(2):
                        tpv = banks[6 + half][:].bitcast(BF16).rearrange("p (k a t) -> p k a t", k=4, a=2)
                        for k4 in range(4):
                            for a_ in range(2):
                                k = half * 4 + k4
                                c0 = 512 * (k // 4) + (k % 4)
                                i = PE.transpose(tpv[:, k4, a_, :], xt_sb[sx][:, a_, c0:c0 + 509:4], ident[:])
                    t = M(PE, i)
                    ch_xs.consumed(t)
                    ch_tp.produced(t)
                    ch_tp.produced(t)
                    sT = ch_xT.pslot(ACT, DVE)
                    ch_tp.cslot(ACT)
                    ta = M(ACT, ACT.activation(out=xsT[sT][:, 0:4, :], in_=banks[6][:].bitcast(BF16).rearrange("p (k t) -> p k t", k=4), func=AF.Copy))
                    ch_tp.consumed(ta)
                    ch_tp.cslot(DVE)
                    td = M(DVE, DVE.tensor_copy(out=xsT[sT][:, 4:8, :], in_=banks[7][:].bitcast(BF16).rearrange("p (k t) -> p k t", k=4)))
                    ch_tp.consumed(td)
                    ch_xT.produced(ta, td)

                def m_gu(j):
                    sw = j % 3
                    sT = ch_xT.cslot(PE)
                    for tok in ch_wg.ready[j]:
                        kb.wait(PE, tok)
                    sg = ch_g2.pslot(PE)
                    for part in range(2):
                        for fcp in range(2):
                            fc = part * 256 + fcp * 128
                            for k in range(8):
                                i = PE.matmul(banks[2 * sg + part][:, fcp * 256:(fcp + 1) * 256], wgu_sb[sw][:, k, fc:fc + 128], xsT[sT][:, k, :],
                                              start=(fcp == 0 and k == 0), stop=(k == 7), skip_group_check=True)
                    t = M(PE, i)
                    ch_xT.consumed(t)
                    ch_wg.consumed(t)
                    ch_g2.produced(t)
                    sg = ch_g2.cslot(ACT, 0)
                    ss = ch_sl.pslot(ACT)
                    t = M(ACT, ACT.activation(out=sl_sb[ss][:], in_=banks[2 * sg][:], func=AF.Silu))
                    ch_g2.consumed(t, 0)
                    ch_sl.produced(t)
                    sg = ch_g2.cslot(DVE, 1)
                    ss = ch_sl.cslot(DVE)
                    sa = ch_at.pslot(DVE)
                    t = M(DVE, DVE.tensor_tensor(out=at_sb[sa][:].rearrange("p c t -> p (c t)"), in0=banks[2 * sg + 1][:], in1=sl_sb[ss][:], op=ALU.mult))
                    ch_g2.consumed(t, 1)
                    ch_sl.consumed(t)
                    ch_at.produced(t)

                def m_dn(j):
                    sw = j % 4
                    sa = ch_at.cslot(PE)
                    for tok in ch_wd.ready[j]:
                        kb.wait(PE, tok)
                    for a_ in range(2):
                        ch_dn.pslot(PE)
                        for hf in range(2):
                            for fc in range(2):
                                i = PE.matmul(banks[4 + hf][:], at_sb[sa][:, fc, a_ * 128:(a_ + 1) * 128],
                                              wdn_sb[sw][:, fc, hf * 512:(hf + 1) * 512], start=(fc == 0), stop=(fc == 1))
                        t = M(PE, i)
                        ch_dn.produced(t)
                        so = ch_yo.pslot(ACT, DVE)
                        ch_dn.cslot(ACT, 0)
                        ta = M(ACT, ACT.activation(out=yo_sb[so][:, 0:512], in_=banks[4][:], func=AF.Copy))
                        ch_dn.consumed(ta, 0)
                        ch_dn.cslot(DVE, 1)
                        td = M(DVE, DVE.tensor_copy(out=yo_sb[so][:, 512:1024], in_=banks[5][:]))
                        ch_dn.consumed(td, 1)
                        ch_yo.produced(ta, td)
                        so = ch_yo.cslot(SP)
                        i = SP.dma_start(out=ys_d[256 * j + 128 * a_:256 * j + 128 * (a_ + 1), :], in_=yo_sb[so][:])
                        ch_yo.consumed(ring_ys.start(i, so))
                        last_ys[so] = (ring_ys.sems[so], ring_ys.sems[so].n)
                    ch_at.consumed(t)
                    ch_wd.consumed(t)

                last_ys = {}
                m_load_w(0)
                m_load_w(1)
                m_load_x(0)
                m_load_x(1)
                m_tp(0)
                for j in range(NJ):
                    if j + 2 < NJ:
                        m_load_x(j + 2)
                    if j + 1 < NJ:
                        m_tp(j + 1)
                    if j >= 2:
                        m_dn(j - 2)
                    if j + 2 < NJ:
                        m_load_w(j + 2)
                    m_gu(j)
                m_dn(NJ - 2)
                m_dn(NJ - 1)
                barrier()
                stop_if("M")

            with contextlib.ExitStack() as es6:
                k6 = KB(nc, es6, kb.waited, kb.prog)
                o_sb = [k6.sb(f"o_sb{b}_{i}", [128, D], F32) for i in range(2)]
                rg_sb = [k6.sb(f"rg_sb{b}_{i}", [128, D], F32) for i in range(4)]
                st3 = k6.sb(f"st3_{b}", [128, 2, 6], F32)
                mv3 = k6.sb(f"mv3_{b}", [128, 2], F32)
                rstd3 = k6.sb(f"rstd3_{b}", [128, 1], F32)
                eps3 = k6.sb(f"eps3_{b}", [128, 1], F32)
                g2_bc = k6.sb(f"g2_bc{b}", [128, D], F32)
                b2_bc = k6.sb(f"b2_bc{b}", [128, D], F32)
                s_g2 = k6.sem(f"f_g2{b}")
                k6.inc(SP.dma_start(out=g2_bc[:], in_=ln2_g.partition_broadcast(128)), s_g2, 16)
                k6.wait(DVE, k6.inc(SP.dma_start(out=b2_bc[:], in_=ln2_b.partition_broadcast(128)), s_g2, 16))
                k6.wait(ACT, M(POOL, POOL.memset(eps3[:], EPS)))
                for so, tk in last_ys.items():
                    kb.wait(POOL, tk)

                def f_gather(n):
                    for kk in range(2):
                        sr = ch_rg.pslot(POOL)
                        i = POOL.indirect_dma_start(out=rg_sb[sr][:], out_offset=None, in_=ys_d,
                                                    in_offset=bass.IndirectOffsetOnAxis(ap=sl_i[:, kk, n:n + 1], axis=0))
                        ch_rg.produced(ring_g4.start(i, sr))

                last2 = []
                f_gather(0)
                for n in range(NT):
                    if n + 1 < NT:
                        f_gather(n + 1)
                    for kk in range(2):
                        sr = ch_rg.cslot(DVE)
                        t = M(DVE, DVE.scalar_tensor_tensor(out=ybuf[:, n, :], in0=rg_sb[sr][:], scalar=g12[:, kk, n:n + 1], in1=ybuf[:, n, :],
                                                            op0=ALU.mult, op1=ALU.add))
                        ch_rg.consumed(t)
                    for hf in range(2):
                        DVE.bn_stats(out=st3[:, hf, :], in_=ybuf[:, n, hf * 512:(hf + 1) * 512])
                    i = DVE.bn_aggr(out=mv3[:], in_=st3[:].rearrange("p a c -> p (a c)"))
                    kb.wait(ACT, M(DVE, i))
                    i = ACT.activation(out=rstd3[:], in_=mv3[:, 1:2], func=AF.Sqrt, bias=eps3[:, 0:1], scale=1.0)
                    kb.wait(DVE, M(ACT, i))
                    DVE.reciprocal(out=rstd3[:], in_=rstd3[:])
                    so = ch_ot.pslot(DVE)
                    DVE.tensor_scalar(out=o_sb[so][:], in0=ybuf[:, n, :], scalar1=mv3[:, 0:1], scalar2=rstd3[:, 0:1],
                                      op0=ALU.subtract, op1=ALU.mult)
                    DVE.tensor_tensor(out=o_sb[so][:], in0=o_sb[so][:], in1=g2_bc[:], op=ALU.mult)
                    i = DVE.tensor_tensor(out=o_sb[so][:], in0=o_sb[so][:], in1=b2_bc[:], op=ALU.add)
                    ch_ot.produced(M(DVE, i))
                    so = ch_ot.cslot(SP)
                    i = SP.dma_start(out=out[b, n * 128:(n + 1) * 128, :], in_=o_sb[so][:])
                    t = ring_o.start(i, so)
                    ch_ot.consumed(t)
                    last2 = (last2 + [t])[-2:]
                for t in last2:
                    kb.wait(SP, t)
                barrier()
            esp.close()
    return nc


def _prep_inputs(inputs):
    x = np.ascontiguousarray(inputs["x"], dtype=np.float32)
    pos = np.ascontiguousarray(inputs["positions"], dtype=np.int32)
    w_r = np.concatenate([inputs["w_group"][0], np.transpose(inputs["w_expert"][0], (1, 0, 2)).reshape(D, 32)], axis=1)
    b_r = np.concatenate([inputs["b_group"][0], inputs["b_expert"][0].reshape(32)])[None, :]
    ws = inputs["w_spatial"][0]
    shared = {
        "w_in": np.ascontiguousarray(inputs["w_in"][0]),
        "w_out": np.ascontiguousarray(inputs["w_out"][0]),
        "ws_tgs": np.ascontiguousarray(np.transpose(ws, (1, 0, 2))),
        "ws_sgt": np.ascontiguousarray(np.transpose(ws, (2, 0, 1))),
        "bspT": np.ascontiguousarray(inputs["b_spatial"][0].T),
        "sgu_g": np.ascontiguousarray(inputs["sgu_ln_g"]),
        "sgu_b": np.ascontiguousarray(inputs["sgu_ln_b"]),
        "ln1_g": np.ascontiguousarray(inputs["ln1_g"]),
        "ln1_b": np.ascontiguousarray(inputs["ln1_b"]),
        "ln2_g": np.ascontiguousarray(inputs["ln2_g"]),
        "ln2_b": np.ascontiguousarray(inputs["ln2_b"]),
        "w_r": np.ascontiguousarray(w_r, dtype=np.float32),
        "b_r": np.ascontiguousarray(b_r, dtype=np.float32),
        "w_gu": np.ascontiguousarray(np.transpose(inputs["w_gate_up"][0].reshape(NE, D, 2, 128, 2), (0, 1, 2, 4, 3)).reshape(NE, D, 512)),
        "w_dn": np.ascontiguousarray(inputs["w_down"][0].reshape(NE, 256, D)),
    }
    in_maps = []
    for c in range(NCORES):
        xs = x[c * NB:(c + 1) * NB]
        m = dict(shared)
        m["xT"] = np.ascontiguousarray(np.transpose(xs, (0, 2, 1)))
        m["xtm"] = xs
        m["posT"] = np.ascontiguousarray(np.transpose(pos[c * NB:(c + 1) * NB].reshape(NB, NT, 128), (0, 2, 1)))
        in_maps.append(m)
    return in_maps


def kernel(**inputs):
    in_maps = _prep_inputs(inputs)
    nc = build_nc()
    res = run_bass_kernel_spmd(nc, in_maps, core_ids=list(range(NCORES)))
    return np.concatenate([r["out"] for r in res.results], axis=0).astype(np.float32)
```

```python
import contextlib
import numpy as np
import concourse.bass as bass
import concourse.mybir as mybir
from concourse.bass_utils import run_bass_kernel_spmd

F32, BF16, I32 = mybir.dt.float32, mybir.dt.bfloat16, mybir.dt.int32
AF = mybir.ActivationFunctionType
ALU = mybir.AluOpType
AX = mybir.AxisListType

NCORES = 8
S = 2048
D = 1024
NB = 2
NT = S // 128
ALPHA = float(2.0 ** 0.25)
EPS = 1e-5
NEG = -30000.0
NE = 32
TWO_PI = float(2 * np.pi)
DEBUG = False
DEBUG_B = 0
STOP_AFTER = None


class _Stop(Exception):
    pass


class Sem:
    _serial = 0

    def __init__(self, h):
        self.h = h
        self.n = 0
        Sem._serial += 1
        self.uid = Sem._serial


class KB:
    def __init__(self, nc, es, waited=None, prog=None):
        self.nc = nc
        self.es = es
        self.waited = {} if waited is None else waited
        self.prog = {} if prog is None else prog

    sem_es = None

    def sem(self, name):
        return Sem(KB.sem_es.enter_context(self.nc.semaphore(name)))

    def sb(self, name, shape, dt):
        return self.es.enter_context(self.nc.sbuf_tensor(name, shape, dt))

    def ps(self, name, shape, dt):
        return self.es.enter_context(self.nc.psum_tensor(name, shape, dt))

    def inc(self, instr, sem, k=1):
        instr.then_inc(sem.h, k)
        sem.n += k
        return (sem, sem.n)

    def mark(self, eng, instr):
        if isinstance(instr, Tok):
            return instr.tok
        return self.inc(instr, self.prog[id(getattr(eng, "raw", eng))])

    def wait(self, eng, tok):
        if tok is None:
            return
        sem, val = tok
        if val <= 0:
            return
        raw = getattr(eng, "raw", eng)
        key = (id(raw), sem.uid)
        if self.waited.get(key, 0) >= val:
            return
        self.waited[key] = val
        raw.wait_ge(sem.h, val)


class Tok:
    def __init__(self, instr, tok):
        self.instr = instr
        self.tok = tok


_COMPUTE = {"activation", "tensor_tensor", "tensor_scalar", "scalar_tensor_tensor", "tensor_copy", "tensor_reduce",
            "reciprocal", "bn_stats", "bn_aggr", "memset", "affine_select", "iota"}


class EngProxy:
    def __init__(self, raw, kb):
        self.raw = raw
        self.kb = kb
        self.last = None

    def __getattr__(self, name):
        attr = getattr(self.raw, name)
        if name not in _COMPUTE:
            return attr

        def wrapper(*a, **k):
            if self.last is not None:
                self.kb.wait(self, self.last)
            instr = attr(*a, **k)
            tok = self.kb.inc(instr, self.kb.prog[id(self.raw)])
            self.last = tok
            return Tok(instr, tok)
        return wrapper


class Chan:
    def __init__(self, kb, name, depth, ncons=1):
        self.kb = kb
        self.depth = depth
        self.ready = []
        self.free = []
        self.ci = [0] * ncons

    def pslot(self, *engs):
        i = len(self.ready)
        if i >= self.depth:
            for tok in self.free[i - self.depth]:
                for e in engs:
                    self.kb.wait(e, tok)
        return i % self.depth

    def produced(self, *toks):
        self.ready.append(list(toks))

    def cslot(self, eng, c=0):
        i = self.ci[c]
        for tok in self.ready[i]:
            self.kb.wait(eng, tok)
        return i % self.depth

    def consumed(self, tok, c=0):
        i = self.ci[c]
        while len(self.free) <= i:
            self.free.append([])
        self.free[i].append(tok)
        self.ci[c] += 1


class DmaRing:
    def __init__(self, kb, name, depth):
        self.kb = kb
        self.sems = [kb.sem(f"{name}{i}") for i in range(depth)]

    def start(self, instr, slot):
        return self.kb.inc(instr, self.sems[slot], 16)


def build_nc():
    nc = bass.Bass("TRN2", target_bir_lowering=False)
    dr = lambda name, shape, dt=F32: nc.dram_tensor(name, shape, dt, kind="ExternalInput").ap()
    xT = dr("xT", [NB, D, S])
    xtm = dr("xtm", [NB, S, D])
    posT = dr("posT", [NB, 128, NT], I32)
    w_in = dr("w_in", [D, 2560])
    w_out = dr("w_out", [D, D])
    ws_tgs = dr("ws_tgs", [128, 8, 128])
    ws_sgt = dr("ws_sgt", [128, 8, 128])
    bspT = dr("bspT", [128, 8])
    sgu_g = dr("sgu_g", [1, 512])
    sgu_b = dr("sgu_b", [1, 512])
    ln1_g = dr("ln1_g", [1, D])
    ln1_b = dr("ln1_b", [1, D])
    ln2_g = dr("ln2_g", [1, D])
    ln2_b = dr("ln2_b", [1, D])
    w_r = dr("w_r", [D, 36])
    b_r = dr("b_r", [1, 36])
    w_gu = dr("w_gu", [NE, D, 512])
    w_dn = dr("w_dn", [NE, 256, D])
    out = nc.dram_tensor("out", [NB, S, D], F32, kind="ExternalOutput").ap()
    if DEBUG:
        dbg_cat = nc.dram_tensor("dbg_cat", [128, 8, S], F32, kind="ExternalOutput").ap()
        dbg_y = nc.dram_tensor("dbg_y", [128, NT, D], F32, kind="ExternalOutput").ap()
        dbg_gate = nc.dram_tensor("dbg_gate", [128, NT, 32], F32, kind="ExternalOutput").ap()

    PE, ACT, DVE, POOL, SP = nc.tensor, nc.scalar, nc.vector, nc.gpsimd, nc.sync
    engines = [PE, ACT, DVE, POOL, SP]

    with contextlib.suppress(_Stop), contextlib.ExitStack() as es:
        KB.sem_es = es
        kb = KB(nc, es)
        progs = [{id(getattr(e, "raw", e)): kb.sem(f"prog{bb}_{n}") for e, n in zip(engines, "pe act dve pool sp".split())} for bb in range(NB + 1)]
        kb.prog = progs[NB]
        M = kb.mark
        ACT, DVE, POOL = EngProxy(nc.scalar, kb), EngProxy(nc.vector, kb), EngProxy(nc.gpsimd, kb)
        engines = [PE, ACT, DVE, POOL, SP]
        banks = [kb.ps(f"bank{i}", [128, 512], F32) for i in range(8)]

        ident = kb.sb("ident", [128, 128], BF16)
        ones_bf = kb.sb("ones_bf", [128, 128], BF16)
        ones_z = kb.sb("ones_z", [128, 2, 128], BF16)
        zer = kb.sb("zer", [128, 512], F32)
        mhalf = kb.sb("mhalf", [128, 1], F32)
        m_cur = kb.sb("m_cur", [128, 512], BF16)
        m_prev = kb.sb("m_prev", [128, 512], BF16)
        m3 = kb.sb("m3", [128, 4, 512], BF16)
        wmT = kb.sb("wmT", [128, 8, 128], BF16)
        rs = kb.sb("rs", [128, 8], F32)
        bsp_sb = kb.sb("bsp_sb", [128, 8], F32)
        sg_bc = kb.sb("sg_bc", [128, 512], F32)
        sb_bc = kb.sb("sb_bc", [128, 512], F32)
        Bp = kb.sb("Bp", [128, 512], F32)
        br_bc = kb.sb("br_bc", [128, 36], F32)
        wr_hl = kb.sb("wr_hl", [128, 8, 72], BF16)
        invf = kb.sb("invf", [128, NT, 8], F32)
        catT = kb.sb("catT", [128, 8, S], BF16)

        s_c = kb.sem("const_dma")
        s_bar = kb.sem("barrier")

        def barrier():
            base = s_bar.n
            for e in engines:
                if isinstance(e, EngProxy) and e.last is not None:
                    kb.wait(e, e.last)
                kb.inc(e.nop(), s_bar)
            for e in engines:
                kb.wait(e, (s_bar, base + len(engines)))

        def stop_if(tag):
            if STOP_AFTER == tag:
                barrier()
                raise _Stop()

        def wait_all(tok):
            for e in engines:
                kb.wait(e, tok)

        with contextlib.ExitStack() as es0:
            k0 = KB(nc, es0, kb.waited, kb.prog)
            wtmp = k0.sb("wtmp", [128, 8, 128], F32)
            wtmp2 = k0.sb("wtmp2", [128, 8, 128], F32)
            wr_f = k0.sb("wr_f", [128, 8, 36], F32)
            wr_t = k0.sb("wr_t", [128, 8, 36], F32)

            def dma_c(o, i):
                kb.inc(SP.dma_start(out=o, in_=i), s_c, 16)

            dma_c(wtmp[:], ws_sgt)
            dma_c(wtmp2[:], ws_tgs)
            dma_c(bsp_sb[:], bspT)
            dma_c(sg_bc[:], sgu_g.partition_broadcast(128))
            dma_c(sb_bc[:], sgu_b.partition_broadcast(128))
            dma_c(br_bc[:], b_r.partition_broadcast(128))
            dma_c(wr_f[:], w_r.rearrange("(k p) c -> p k c", p=128))
            tok_cdma = (s_c, s_c.n)

            POOL.memset(zer[:], 0.0)
            POOL.memset(ones_bf[:], 1.0)
            POOL.memset(ones_z[:], 0.0)
            POOL.memset(ones_z[:, 0, 0:64], 1.0)
            POOL.memset(ones_z[:, 1, 64:128], 1.0)
            POOL.memset(mhalf[:], -0.5)
            POOL.affine_select(out=ident[:], in_=ones_bf[:], pattern=[[1, 128]], compare_op=ALU.is_equal,
                               fill=0.0, base=0, channel_multiplier=-1)
            POOL.affine_select(out=m_cur[:], in_=zer[:], pattern=[[0, 4], [1, 128]], compare_op=ALU.is_ge,
                               fill=NEG, base=0, channel_multiplier=-1)
            POOL.affine_select(out=m_prev[:], in_=zer[:], pattern=[[0, 4], [-1, 128]], compare_op=ALU.is_ge,
                               fill=NEG, base=0, channel_multiplier=1)
            for s in range(4):
                POOL.affine_select(out=m3[:, s, :], in_=zer[:], pattern=[[0, 16], [1, 32]], compare_op=ALU.is_ge,
                                   fill=NEG, base=32 * s, channel_multiplier=-1)
            inv = (np.float32(500000.0) ** (-(np.arange(0, 16, 2, dtype=np.float32)) / np.float32(16))).astype(np.float32)
            for i in range(8):
                POOL.memset(invf[:, :, i:i + 1], float(inv[i]))
            kb.wait(POOL, tok_cdma)
            POOL.affine_select(out=wmT[:], in_=wtmp[:], pattern=[[0, 8], [1, 128]], compare_op=ALU.is_ge,
                               fill=0.0, base=0, channel_multiplier=-1)
            i_last = POOL.affine_select(out=wtmp[:], in_=wtmp2[:], pattern=[[0, 8], [-1, 128]], compare_op=ALU.is_ge,
                                        fill=0.0, base=0, channel_multiplier=1)
            tok_cpool = M(POOL, i_last)

            kb.wait(DVE, tok_cdma)
            kb.wait(DVE, tok_cpool)
            DVE.tensor_reduce(out=rs[:], in_=wtmp[:], axis=AX.X, op=ALU.add)
            for g in range(8):
                DVE.tensor_scalar(out=Bp[:, g * 64:(g + 1) * 64], in0=sb_bc[:, g * 64:(g + 1) * 64],
                                  scalar1=rs[:, g:g + 1], scalar2=bsp_sb[:, g:g + 1], op0=ALU.mult, op1=ALU.add)
            DVE.tensor_copy(out=wr_hl[:, :, 0:36], in_=wr_f[:])
            DVE.tensor_copy(out=wr_t[:], in_=wr_hl[:, :, 0:36])
            DVE.tensor_tensor(out=wr_t[:], in0=wr_f[:], in1=wr_t[:], op=ALU.subtract)
            i_last = DVE.tensor_copy(out=wr_hl[:, :, 36:72], in_=wr_t[:])
            tok_cdve = M(DVE, i_last)
            wait_all(tok_cdma)
            wait_all(tok_cpool)
            wait_all(tok_cdve)
            barrier()
        stop_if("const")

        def rstd_via_pool(mv, dst, last_dve_instr):
            t = M(DVE, last_dve_instr)
            kb.wait(POOL, t)
            POOL.tensor_scalar(out=dst, in0=mv[:, 1:2], scalar1=EPS, scalar2=None, op0=ALU.add)
            i = POOL.tensor_tensor(out=dst, in0=dst, in1=mhalf[:], op=ALU.pow)
            kb.wait(DVE, M(POOL, i))

        ring_x = DmaRing(kb, "ring_x", 2)
        ring_w = DmaRing(kb, "ring_w", 2)
        ring_o = DmaRing(kb, "ring_o", 2)
        ring_m = DmaRing(kb, "ring_m", 4)
        ring_wg = DmaRing(kb, "ring_wg", 3)
        ring_wd = DmaRing(kb, "ring_wd", 4)
        ring_xs3 = DmaRing(kb, "ring_xs3", 3)
        ring_xs = DmaRing(kb, "ring_xs", 2)
        ring_ys = DmaRing(kb, "ring_ys", 2)
        ring_g = DmaRing(kb, "ring_g", 2)
        ring_g4 = DmaRing(kb, "ring_g4", 4)
        s_sc = kb.sem("scatter")
        rb_gu = es.enter_context(nc.gpsimd.register("rb_gu"))
        rb_dn = es.enter_context(nc.gpsimd.register("rb_dn"))
        nc.gpsimd.reg_mov(rb_gu, NE * 256 - 1)
        nc.gpsimd.reg_mov(rb_dn, NE * 128 - 1)
        NJ = 48
        xs_d = nc.dram_tensor("xs_scratch", [NJ * 256, D], BF16, kind="Internal").ap()
        ys_d = nc.dram_tensor("ys_scratch", [NJ * 256, D], F32, kind="Internal").ap()

        for b in range(NB):
            kb.prog = progs[b]
            ch_u = Chan(kb, "ch_u", 2)
            ch_v = Chan(kb, "ch_v", 2)
            ch_z = Chan(kb, "ch_z", 2)
            ch_tp = Chan(kb, "ch_tp", 2)
            ch_n = Chan(kb, "ch_n", 2)
            ch_gu = Chan(kb, "ch_gu", 2)
            ch_gv = Chan(kb, "ch_gv", 2)
            ch_sg = Chan(kb, "ch_sg", 2)
            ch_qk = Chan(kb, "ch_qk", 2)
            ch_rot = Chan(kb, "ch_rot", 2)
            ch_qs = Chan(kb, "ch_qs", 2)
            ch_vp = Chan(kb, "ch_vp", 2)
            ch_s = Chan(kb, "ch_s", 4)
            ch_p = Chan(kb, "ch_p", 6)
            ch_o = Chan(kb, "ch_o", 2)
            ch_op = Chan(kb, "ch_op", 2)
            ch_x = Chan(kb, "ch_x", 2)
            ch_hb = Chan(kb, "ch_hb", 2)
            ch_hf = Chan(kb, "ch_hf", 2)
            ch_lo = Chan(kb, "ch_lo", 2)
            ch_ht = Chan(kb, "ch_ht", 2)
            ch_r = Chan(kb, "ch_r", 1)
            ch_g2 = Chan(kb, "ch_g2", 2, ncons=2)
            ch_sl = Chan(kb, "ch_sl", 2)
            ch_at = Chan(kb, "ch_at", 3)
            ch_wg = Chan(kb, "ch_wg", 3)
            ch_wd = Chan(kb, "ch_wd", 4)
            ch_dn = Chan(kb, "ch_dn", 1, ncons=2)
            ch_wt = Chan(kb, "ch_wt", 3)
            ch_ot = Chan(kb, "ch_ot", 2)
            ch_xs = Chan(kb, "ch_xs", 2)
            ch_xT = Chan(kb, "ch_xT", 2)
            ch_yo = Chan(kb, "ch_yo", 2)
            ch_rg = Chan(kb, "ch_rg", 4)

            with contextlib.ExitStack() as es1:
                k1 = KB(nc, es1, kb.waited, kb.prog)
                xT_sb = k1.sb(f"xT_sb{b}", [128, 8, S], BF16)
                wsec = k1.sb(f"wsec{b}", [128, 8, 1024], BF16)
                pos_i = k1.sb(f"pos_i{b}", [128, NT], I32)
                pos_f = k1.sb(f"pos_f{b}", [128, NT], F32)
                ang = k1.sb(f"ang{b}", [128, 2, NT, 8], F32)
                kk_i = k1.sb(f"kk_i{b}", [128, 2, NT, 8], I32)
                kk_f = k1.sb(f"kk_f{b}", [128, 2, NT, 8], F32)
                rr = k1.sb(f"rr{b}", [128, 2, NT, 8], F32)
                mm = k1.sb(f"mm{b}", [128, 2, NT, 8], F32)
                cs_t = k1.sb(f"cs_t{b}", [128, 2, NT, 8], F32)
                s_ld = k1.sem(f"a1_ld{b}")
                s_w = k1.sem(f"a1_w{b}")

                k1.inc(POOL.dma_start(out=wsec[:], in_=w_in[:, 1536:2560].rearrange("(k p) c -> p k c", p=128)), s_w, 16)
                tok_w = (s_w, s_w.n)
                s_ldq = [k1.sem(f"a1_ldq{b}_{q}") for q in range(4)]
                tok_xq = []
                for q in range(4):
                    tok_xq.append(k1.inc(POOL.dma_start(out=xT_sb[:, :, q * 512:(q + 1) * 512],
                                                        in_=xT[b, :, q * 512:(q + 1) * 512].rearrange("(k p) t -> p k t", p=128)), s_ldq[q], 16))
                s_pos = k1.sem(f"a1_pos{b}")
                tok_pos = k1.inc(SP.dma_start(out=pos_i[:], in_=posT[b]), s_pos, 16)
                wq_sb = [k1.sb(f"wq_sb{b}_{i}", [128, 8, 384], BF16) for i in range(2)]
                s_ws = k1.sem(f"a1_ws{b}")
                tok_wq = {}

                def load_wq(c):
                    for j, c0 in enumerate((c * 128, 512 + c * 128, 1024 + c * 128)):
                        t_ = k1.inc(POOL.dma_start(out=wq_sb[c % 2][:, :, j * 128:(j + 1) * 128],
                                                   in_=w_in[:, c0:c0 + 128].rearrange("(k p) c -> p k c", p=128)), s_ws, 16)
                    tok_wq[c] = t_

                load_wq(0)

                k1.wait(DVE, tok_pos)
                DVE.tensor_copy(out=pos_f[:], in_=pos_i[:])
                DVE.tensor_tensor(out=ang[:, 1], in0=invf[:], in1=pos_f[:].unsqueeze(2).broadcast_to([128, NT, 8]), op=ALU.mult)
                DVE.tensor_scalar(out=ang[:, 0], in0=ang[:, 1], scalar1=float(np.pi / 2), scalar2=None, op0=ALU.add)
                DVE.tensor_scalar(out=kk_f[:], in0=ang[:], scalar1=float(1.0 / TWO_PI), scalar2=None, op0=ALU.mult)
                DVE.tensor_copy(out=kk_i[:], in_=kk_f[:])
                DVE.tensor_copy(out=kk_f[:], in_=kk_i[:])
                DVE.scalar_tensor_tensor(out=rr[:], in0=kk_f[:], scalar=-TWO_PI, in1=ang[:], op0=ALU.mult, op1=ALU.add)
                DVE.tensor_scalar(out=mm[:], in0=rr[:], scalar1=float(np.pi), scalar2=None, op0=ALU.is_gt)
                DVE.scalar_tensor_tensor(out=rr[:], in0=mm[:], scalar=-TWO_PI, in1=rr[:], op0=ALU.mult, op1=ALU.add)
                DVE.tensor_scalar(out=mm[:], in0=rr[:], scalar1=float(-np.pi), scalar2=None, op0=ALU.is_lt)
                i_l = DVE.scalar_tensor_tensor(out=rr[:], in0=mm[:], scalar=TWO_PI, in1=rr[:], op0=ALU.mult, op1=ALU.add)
                k1.wait(ACT, M(DVE, i_l))
                i_l = ACT.activation(out=cs_t[:], in_=rr[:], func=AF.Sin)
                tok_cs = M(ACT, i_l)
                stop_if("rope")

                with contextlib.ExitStack() as es2:
                    k2 = KB(nc, es2, kb.waited, kb.prog)
                    gu_sb = [k2.sb(f"gu_sb{b}_{i}", [128, 512], F32) for i in range(2)]
                    gv_sb = [k2.sb(f"gv_sb{b}_{i}", [128, 512], F32) for i in range(2)]
                    n_sb = [k2.sb(f"n_sb{b}_{i}", [128, 512], BF16) for i in range(2)]
                    t1_sb = k2.sb(f"t1_sb{b}", [128, 512], F32)
                    sg_sb = [k2.sb(f"sg_sb{b}_{i}", [128, 512], BF16) for i in range(2)]
                    st_sb = k2.sb(f"st_sb{b}", [128, 6], F32)
                    mv_sb = k2.sb(f"mv_sb{b}", [128, 2], F32)
                    rstd_sb = k2.sb(f"rstd_sb{b}", [128, 1], F32)

                    k2.wait(PE, tok_w)

                    def sgu_pe_front(n):
                        k2.wait(PE, tok_xq[n // 4])
                        su = ch_u.pslot(PE)
                        for k in range(8):
                            i = PE.matmul(banks[0 + su][:], xT_sb[:, k, n * 128:(n + 1) * 128], wsec[:, k, 0:512],
                                          start=(k == 0), stop=(k == 7))
                        ch_u.produced(M(PE, i))
                        sv = ch_v.pslot(PE)
                        for k in range(8):
                            i = PE.matmul(banks[2 + sv][:], xT_sb[:, k, n * 128:(n + 1) * 128], wsec[:, k, 512:1024],
                                          start=(k == 0), stop=(k == 7))
                        ch_v.produced(M(PE, i))

                    def sgu_act(n):
                        su = ch_u.cslot(ACT)
                        sg = ch_gu.pslot(ACT)
                        t = M(ACT, ACT.activation(out=gu_sb[sg][:], in_=banks[0 + su][:], func=AF.Gelu))
                        ch_u.consumed(t)
                        ch_gu.produced(t)
                        sv = ch_v.cslot(ACT)
                        sg = ch_gv.pslot(ACT)
                        t = M(ACT, ACT.activation(out=gv_sb[sg][:], in_=banks[2 + sv][:], func=AF.Gelu))
                        ch_v.consumed(t)
                        ch_gv.produced(t)

                    def sgu_dve_norm(n):
                        sg = ch_gv.cslot(DVE)
                        DVE.bn_stats(out=st_sb[:], in_=gv_sb[sg][:])
                        i = DVE.bn_aggr(out=mv_sb[:], in_=st_sb[:])
                        rstd_via_pool(mv_sb, rstd_sb[:], i)
                        sn = ch_n.pslot(DVE)
                        t = M(DVE, DVE.tensor_scalar(out=n_sb[sn][:], in0=gv_sb[sg][:], scalar1=mv_sb[:, 0:1], scalar2=rstd_sb[:, 0:1],
                                                     op0=ALU.subtract, op1=ALU.mult))
                        ch_gv.consumed(t)
                        ch_n.produced(t)

                    def sgu_pe_z(n):
                        sn = ch_n.cslot(PE)
                        sz = ch_z.pslot(PE)
                        for g in range(8):
                            i = PE.matmul(banks[4 + sz][:, g * 64:(g + 1) * 64], wmT[:, g, :], n_sb[sn][:, g * 64:(g + 1) * 64],
                                          start=True, stop=True, skip_group_check=True)
                        t = M(PE, i)
                        ch_n.consumed(t)
                        ch_z.produced(t)

                    def sgu_dve_out(n):
                        sz = ch_z.cslot(DVE)
                        t = M(DVE, DVE.tensor_tensor(out=t1_sb[:], in0=banks[4 + sz][:], in1=sg_bc[:], op=ALU.mult))
                        ch_z.consumed(t)
                        DVE.tensor_tensor(out=t1_sb[:], in0=t1_sb[:], in1=Bp[:], op=ALU.add)
                        sgu_ = ch_gu.cslot(DVE)
                        so = ch_sg.pslot(DVE)
                        t = M(DVE, DVE.tensor_tensor(out=sg_sb[so][:], in0=t1_sb[:], in1=gu_sb[sgu_][:], op=ALU.mult))
                        ch_gu.consumed(t)
                        ch_sg.produced(t)

                    def sgu_pe_tp(n):
                        so = ch_sg.cslot(PE)
                        st = ch_tp.pslot(PE)
                        tpv = banks[6 + st][:].bitcast(BF16)
                        for j in range(4):
                            i = PE.transpose(tpv[:, j * 128:(j + 1) * 128], sg_sb[so][:, j * 128:(j + 1) * 128], ident[:])
                        t = M(PE, i)
                        ch_sg.consumed(t)
                        ch_tp.produced(t)

                    def sgu_act_tp(n):
                        st = ch_tp.cslot(ACT)
                        tpv = banks[6 + st][:].bitcast(BF16)
                        i = ACT.activation(out=catT[:, 4:8, n * 128:(n + 1) * 128],
                                           in_=tpv[:, 0:512].rearrange("p (j t) -> p j t", j=4), func=AF.Copy)
                        ch_tp.consumed(M(ACT, i))

                    for n in range(NT + 2):
                        if n < NT:
                            sgu_pe_front(n)
                            sgu_act(n)
                            sgu_dve_norm(n)
                        if 1 <= n <= NT:
                            sgu_pe_z(n - 1)
                            sgu_dve_out(n - 1)
                        if 2 <= n:
                            sgu_pe_tp(n - 2)
                            sgu_act_tp(n - 2)
                    barrier()
                stop_if("sgu")

                with contextlib.ExitStack() as es3:
                    k3 = KB(nc, es3, kb.waited, kb.prog)
                    QTz = k3.sb(f"QTz{b}", [128, 2, S], BF16)
                    KT = k3.sb(f"KT{b}", [128, S], BF16)
                    Vz = k3.sb(f"Vz{b}", [128, 48, 2, 128], BF16)
                    qs_sb = [k3.sb(f"qs_sb{b}_{i}", [128, 4, 256], BF16) for i in range(2)]
                    ra = k3.sb(f"ra{b}", [128, 4, 4, 8], F32)
                    rb = k3.sb(f"rb{b}", [128, 4, 4, 8], F32)
                    rot_sb = [k3.sb(f"rot_sb{b}_{i}", [128, 4, 2, 2, 16], F32) for i in range(2)]
                    p_sb = [k3.sb(f"p_sb{b}_{i}", [128, 512], BF16) for i in range(6)]
                    rl_sb = k3.sb(f"rl_sb{b}", [128, 512], F32)
                    tok_zero = M(POOL, POOL.memset(QTz[:], 0.0))
                    tok_one = M(DVE, DVE.memset(Vz[:], 1.0))
                    for e in (ACT, DVE, PE):
                        k3.wait(e, tok_zero)
                        k3.wait(e, tok_one)
                    for q in range(4):
                        k3.wait(PE, tok_xq[q])
                    stop_if("att_ms")

                    def tv_blk(T, j):
                        return T[:, j * 128:(j + 1) * 128]

                    def tv_p2(T, s, r4):
                        return T[:, 512 * s:512 * (s + 1)].rearrange("p (i r) -> p r i", r=4)[:, r4, :]

                    def tv_p3k(T, r):
                        return T.rearrange("p (l r) -> p r l", r=16)[:, r, :]

                    def tv_p3q(T, s, r):
                        return T.rearrange("p (l r) -> p r l", r=16)[:, r, 32 * s:32 * (s + 1)]

                    vdefs = []
                    for j in range(16):
                        vdefs.append(lambda T, j=j: tv_blk(T, j))
                    for s in range(4):
                        for r4 in range(4):
                            vdefs.append(lambda T, s=s, r4=r4: tv_p2(T, s, r4))
                    for r in range(16):
                        vdefs.append(lambda T, r=r: tv_p3k(T, r))

                    for c in range(4):
                        wq = wq_sb[c % 2]
                        k3.wait(PE, tok_wq[c])
                        k3.wait(DVE, tok_cs)
                        stop_if("att_dma")

                        def qk_pe(gi):
                            sq = ch_qk.pslot(PE)
                            for w in range(2):
                                for tt in range(4):
                                    n = gi * 4 + tt
                                    for k in range(8):
                                        i = PE.matmul(banks[2 * sq + w][:, tt * 128:(tt + 1) * 128],
                                                      xT_sb[:, k, n * 128:(n + 1) * 128], wq[:, k, w * 128:(w + 1) * 128],
                                                      start=(tt == 0 and k == 0), stop=(k == 7), skip_group_check=True)
                            ch_qk.produced(M(PE, i))

                        def qk_evac(gi):
                            sq = ch_qk.cslot(ACT, 0)
                            so = ch_qs.pslot(ACT, DVE)
                            sr = ch_rot.pslot(ACT)
                            qv = banks[2 * sq + 0][:].rearrange("p (t h e) -> p t h e", t=4, h=2)
                            kv = banks[2 * sq + 1][:].rearrange("p (t h e) -> p t h e", t=4, h=2)
                            ov = qs_sb[so][:].rearrange("p t (w h e) -> p t w h e", w=2, h=2)
                            ACT.activation(out=ov[:, :, 0, :, 16:64], in_=qv[:, :, :, 16:64], func=AF.Copy)
                            ACT.activation(out=ov[:, :, 1, :, 16:64], in_=kv[:, :, :, 16:64], func=AF.Copy)
                            ACT.activation(out=rot_sb[sr][:, :, 0, :, :], in_=qv[:, :, :, 0:16], func=AF.Copy)
                            ta = M(ACT, ACT.activation(out=rot_sb[sr][:, :, 1, :, :], in_=kv[:, :, :, 0:16], func=AF.Copy))
                            ch_qk.consumed(ta, 0)
                            ch_rot.produced(ta)
                            sr = ch_rot.cslot(DVE)
                            rv = rot_sb[sr][:].rearrange("p t w h e -> p t (w h) e")
                            t1 = rv[:, :, :, 0:8]
                            t2 = rv[:, :, :, 8:16]
                            og = qs_sb[so][:].rearrange("p t (g e) -> p t g e", g=4)
                            cosb = cs_t[:, 0, gi * 4:(gi + 1) * 4, :].unsqueeze(2).broadcast_to([128, 4, 4, 8])
                            sinb = cs_t[:, 1, gi * 4:(gi + 1) * 4, :].unsqueeze(2).broadcast_to([128, 4, 4, 8])
                            DVE.tensor_tensor(out=ra[:], in0=t1, in1=cosb, op=ALU.mult)
                            DVE.tensor_tensor(out=rb[:], in0=t2, in1=sinb, op=ALU.mult)
                            DVE.tensor_tensor(out=og[:, :, :, 0:8], in0=ra[:], in1=rb[:], op=ALU.subtract)
                            DVE.tensor_tensor(out=ra[:], in0=t2, in1=cosb, op=ALU.mult)
                            DVE.tensor_tensor(out=rb[:], in0=t1, in1=sinb, op=ALU.mult)
                            td = M(DVE, DVE.tensor_tensor(out=og[:, :, :, 8:16], in0=ra[:], in1=rb[:], op=ALU.add))
                            ch_rot.consumed(td)
                            ch_qs.produced(ta, td)

                        def qk_tp(gi):
                            so = ch_qs.cslot(PE)
                            st = ch_tp.pslot(PE)
                            tpv = banks[6 + st][:].bitcast(BF16).rearrange("p (t w e) -> p t w e", t=4, w=2)
                            for tt in range(4):
                                for w in range(2):
                                    i = PE.transpose(tpv[:, tt, w, :], qs_sb[so][:, tt, w * 128:(w + 1) * 128], ident[:])
                            t = M(PE, i)
                            ch_qs.consumed(t)
                            ch_tp.produced(t)

                        def qk_tp_evac(gi):
                            st = ch_tp.cslot(ACT)
                            tpv = banks[6 + st][:].bitcast(BF16).rearrange("p (t w e) -> p t w e", t=4, w=2)
                            cols = slice(gi * 512, (gi + 1) * 512)
                            ACT.activation(out=QTz[0:64, 0, cols].rearrange("p (t e) -> p t e", t=4), in_=tpv[0:64, :, 0, :], func=AF.Copy)
                            ACT.activation(out=QTz[64:128, 1, cols].rearrange("p (t e) -> p t e", t=4), in_=tpv[64:128, :, 0, :], func=AF.Copy)
                            i = ACT.activation(out=KT[:, cols].rearrange("p (t e) -> p t e", t=4), in_=tpv[:, :, 1, :], func=AF.Copy)
                            ch_tp.consumed(M(ACT, i))

                        for gi in range(4 + 2):
                            if gi < 4:
                                qk_pe(gi)
                                stop_if("qk_pe0")
                                qk_evac(gi)
                                stop_if("qk_ev0")
                            if 1 <= gi <= 4:
                                qk_tp(gi - 1)
                                stop_if("qk_tp0")
                            if 2 <= gi:
                                qk_tp_evac(gi - 2)
                                stop_if("qk_te0")
                        stop_if("qk")

                        for gi in range(12):
                            sv = ch_vp.pslot(PE)
                            for tt in range(4):
                                vd = vdefs[gi * 4 + tt]
                                for k in range(8):
                                    i = PE.matmul(banks[4 + sv][:, tt * 128:(tt + 1) * 128], vd(xT_sb[:, k, :]), wq[:, k, 256:384],
                                                  start=(tt == 0 and k == 0), stop=(k == 7), skip_group_check=True)
                            ch_vp.produced(M(PE, i))
                            eng = ACT if gi % 2 == 0 else DVE
                            sv = ch_vp.cslot(eng)
                            src = banks[4 + sv][:].rearrange("p (t h e) -> p t h e", t=4, h=2)
                            dst = Vz[:, gi * 4:(gi + 1) * 4, :, :]
                            if eng is ACT:
                                ACT.activation(out=dst[:, :, 0, 0:64], in_=src[:, :, 0, :], func=AF.Copy)
                                i = ACT.activation(out=dst[:, :, 1, 64:128], in_=src[:, :, 1, :], func=AF.Copy)
                            else:
                                DVE.tensor_copy(out=dst[:, :, 0, 0:64], in_=src[:, :, 0, :])
                                i = DVE.tensor_copy(out=dst[:, :, 1, 64:128], in_=src[:, :, 1, :])
                            ch_vp.consumed(M(eng, i))
                        barrier()
                        stop_if("v")
                        if c + 1 < 4:
                            load_wq(c + 1)

                        V1 = lambda j, hh: Vz[:, j, hh, :]
                        V2 = lambda s, r4, hh: Vz[:, 16 + 4 * s + r4, hh, :]
                        V3 = lambda r, hh: Vz[:, 32 + r, hh, :]

                        def emit_s(maskt, mms, lo=0, hi=512):
                            ss = ch_s.pslot(PE)
                            PE.matmul(banks[ss][:], ident[:], maskt, start=True, stop=False, skip_group_check=True)
                            for (oc, lhsT, rhs) in mms:
                                i = PE.matmul(banks[ss][:, oc[0]:oc[1]], lhsT, rhs, start=False, stop=True, skip_group_check=True)
                            ch_s.produced(M(PE, i))
                            ss2 = ch_s.cslot(ACT)
                            sp = ch_p.pslot(ACT)
                            t = M(ACT, ACT.activation(out=p_sb[sp][:, lo:hi], in_=banks[ss2][:, lo:hi], func=AF.Exp, scale=0.125))
                            ch_s.consumed(t)
                            ch_p.produced(t)
                            return sp

                        def oview(Bk, oc):
                            if oc[0] == "blk":
                                return Bk[:, oc[1] * 128:(oc[1] + 1) * 128]
                            if oc[0] == "p2":
                                return Bk[:].rearrange("p (i r) -> p r i", r=4)[:, oc[1], :]
                            return Bk[:].rearrange("p (i r) -> p r i", r=16)[:, oc[1], :]

                        for s in range(4):
                            so_ = ch_o.pslot(PE)
                            Ob = banks[4 + 2 * so_]
                            Lb = banks[5 + 2 * so_]
                            first_pv = [True, True]
                            for hh in range(2):
                                Q = QTz[:, hh, :]
                                pend = []
                                mms = [((j * 128, (j + 1) * 128), tv_blk(KT, 4 * s + j), tv_blk(Q, 4 * s + j)) for j in range(4)]
                                sp = emit_s(m_cur[:], mms)
                                pend.append((sp, [(V1(4 * s + j, hh), (j * 128, (j + 1) * 128), ("blk", j)) for j in range(4)]))
                                js = [j for j in range(4) if 4 * s + j >= 1]
                                mms = [((j * 128, (j + 1) * 128), tv_blk(KT, 4 * s + j - 1), tv_blk(Q, 4 * s + j)) for j in js]
                                sp = emit_s(m_prev[:], mms, lo=js[0] * 128)
                                pend.append((sp, [(V1(4 * s + j - 1, hh), (j * 128, (j + 1) * 128), ("blk", j)) for j in js]))
                                mms = [((r4 * 128, (r4 + 1) * 128), tv_p2(KT, s, r4), tv_p2(Q, s, r4)) for r4 in range(4)]
                                sp = emit_s(m_cur[:], mms)
                                pend.append((sp, [(V2(s, r4, hh), (r4 * 128, (r4 + 1) * 128), ("p2", r4)) for r4 in range(4)]))
                                if s >= 1:
                                    mms = [((r4 * 128, (r4 + 1) * 128), tv_p2(KT, s - 1, r4), tv_p2(Q, s, r4)) for r4 in range(4)]
                                    sp = emit_s(m_prev[:], mms)
                                    pend.append((sp, [(V2(s - 1, r4, hh), (r4 * 128, (r4 + 1) * 128), ("p2", r4)) for r4 in range(4)]))
                                mms = [((r * 32, (r + 1) * 32), tv_p3k(KT, r), tv_p3q(Q, s, r)) for r in range(16)]
                                sp = emit_s(m3[:, s, :], mms)
                                pend.append((sp, [(V3(r, hh), (r * 32, (r + 1) * 32), ("p3", r)) for r in range(16)]))

                                for (sp, pvs) in pend:
                                    sp2 = ch_p.cslot(PE)
                                    assert sp2 == sp
                                    for (vt, pc, oc) in pvs:
                                        i = PE.matmul(oview(Lb if hh else Ob, oc), vt, p_sb[sp][:, pc[0]:pc[1]], start=first_pv[hh], stop=False,
                                                      skip_group_check=True)
                                        first_pv[hh] = False
                                    t_last = M(PE, i)
                                    ch_p.consumed(t_last)
                            ch_o.produced(t_last)
                            so2 = ch_o.cslot(DVE)
                            B0, B1 = banks[4 + 2 * so2], banks[5 + 2 * so2]
                            DVE.reciprocal(out=rl_sb[0:64, :], in_=B0[64:128, :])
                            DVE.tensor_tensor(out=catT[0:64, c, 512 * s:512 * (s + 1)], in0=B0[0:64, :], in1=rl_sb[0:64, :], op=ALU.mult)
                            DVE.reciprocal(out=rl_sb[64:128, :], in_=B1[0:64, :])
                            i = DVE.tensor_tensor(out=catT[64:128, c, 512 * s:512 * (s + 1)], in0=B1[64:128, :], in1=rl_sb[64:128, :], op=ALU.mult)
                            ch_o.consumed(M(DVE, i))
                            stop_if("attn_s0")
                        barrier()
                        if DEBUG and b == DEBUG_B and STOP_AFTER == "attn":
                            with contextlib.ExitStack() as esd:
                                kd = KB(nc, esd, kb.waited, kb.prog)
                                dtmp = kd.sb("dtmpa", [128, 4, S], F32)
                                sd = kd.sem("dbga")
                                kd.wait(SP, M(DVE, DVE.tensor_copy(out=dtmp[:], in_=catT[:, 0:4, :])))
                                kd.wait(SP, kd.inc(SP.dma_start(out=dbg_cat[:, 0:4, :], in_=dtmp[:]), sd, 16))
                            stop_if("attn")

                if DEBUG and b == DEBUG_B:
                    with contextlib.ExitStack() as esd:
                        kd = KB(nc, esd, kb.waited, kb.prog)
                        dtmp = kd.sb("dtmp", [128, 8, S], F32)
                        sd = kd.sem("dbg1")
                        kd.wait(SP, M(DVE, DVE.tensor_copy(out=dtmp[:], in_=catT[:])))
                        kd.wait(SP, kd.inc(SP.dma_start(out=dbg_cat, in_=dtmp[:]), sd, 16))
                        barrier()

            esp = contextlib.ExitStack()
            kp = KB(nc, esp, kb.waited, kb.prog)
            hb_all = kp.sb(f"hb_all{b}", [128, NT, D], BF16)
            ybuf = kp.sb(f"ybuf{b}", [128, NT, D], F32)
            M1a = kp.sb(f"M1a{b}", [128, NT, 32], F32)
            M2a = kp.sb(f"M2a{b}", [128, NT, 32], F32)
            g12 = kp.sb(f"g12{b}", [128, 2, NT], F32)
            sl_i = kp.sb(f"sl_i{b}", [128, 2, NT], I32)
            with contextlib.ExitStack() as es4:
                k4 = KB(nc, es4, kb.waited, kb.prog)
                wo_sb = k4.sb(f"wo_sb{b}", [128, 8, D], BF16)
                x_sb = [k4.sb(f"x_sb{b}_{i}", [128, D], F32) for i in range(2)]
                hT_sb = [k4.sb(f"hT_sb{b}_{i}", [128, 8, 128], BF16) for i in range(2)]
                hf_sb = [k4.sb(f"hf_sb{b}_{i}", [128, D], F32) for i in range(2)]
                eps_t = k4.sb(f"eps_t{b}", [128, 1], F32)
                k4.wait(ACT, M(POOL, POOL.memset(eps_t[:], EPS)))
                math_done = [None]
                pend_D = []
                tD = {}
                NH = 8
                lg_all = k4.sb(f"lg_all{b}", [128, NH, 72], F32)
                rq = k4.sb(f"rq{b}", [128, NH, 64], F32)
                lo_sb = [k4.sb(f"lo_sb{b}_{i}", [128, D], BF16) for i in range(2)]
                loT_sb = [k4.sb(f"loT_sb{b}_{i}", [128, 8, 128], BF16) for i in range(2)]
                st2 = k4.sb(f"st2_{b}", [128, 2, 6], F32)
                mv2 = k4.sb(f"mv2_{b}", [128, 2], F32)
                rstd2 = k4.sb(f"rstd2_{b}", [128, 1], F32)
                s_wo = k4.sem(f"a2_wo{b}")
                g1_bc = k4.sb(f"g1_bc{b}", [128, D], F32)
                b1_bc = k4.sb(f"b1_bc{b}", [128, D], F32)
                s_g1 = k4.sem(f"a2_g1{b}")
                k4.inc(SP.dma_start(out=g1_bc[:], in_=ln1_g.partition_broadcast(128)), s_g1, 16)
                k4.wait(DVE, k4.inc(SP.dma_start(out=b1_bc[:], in_=ln1_b.partition_broadcast(128)), s_g1, 16))

                t_wo = k4.inc(POOL.dma_start(out=wo_sb[:], in_=w_out.rearrange("(k p) c -> p k c", p=128)), s_wo, 16)
                k4.wait(PE, t_wo)
                k4.wait(DVE, t_wo)

                def a2_load(n):
                    sx = ch_x.pslot(SP)
                    i = SP.dma_start(out=x_sb[sx][:], in_=xtm[b, n * 128:(n + 1) * 128, :])
                    ch_x.produced(ring_x.start(i, sx))

                def a2_pe_op(n):
                    so = ch_op.pslot(PE)
                    for hf in range(2):
                        for k in range(8):
                            i = PE.matmul(banks[2 * so + hf][:], catT[:, k, n * 128:(n + 1) * 128], wo_sb[:, k, hf * 512:(hf + 1) * 512],
                                          start=(k == 0), stop=(k == 7))
                    ch_op.produced(M(PE, i))

                def a2_dve_A(n):
                    so = ch_op.cslot(DVE)
                    sx = ch_x.cslot(DVE)
                    sh = n % 2
                    hf_ = hf_sb[sh]
                    if n >= 2:
                        kb.wait(DVE, tD[n - 2])
                    for hf in range(2):
                        i = DVE.scalar_tensor_tensor(out=hf_[:, hf * 512:(hf + 1) * 512], in0=x_sb[sx][:, hf * 512:(hf + 1) * 512],
                                                     scalar=ALPHA, in1=banks[2 * so + hf][:], op0=ALU.mult, op1=ALU.add)
                    t = M(DVE, i)
                    ch_op.consumed(t)
                    ch_x.consumed(t)
                    for hf in range(2):
                        DVE.bn_stats(out=st2[:, hf, :], in_=hf_[:, hf * 512:(hf + 1) * 512])
                    i = DVE.bn_aggr(out=mv2[:], in_=st2[:].rearrange("p a c -> p (a c)"))
                    kb.wait(ACT, M(DVE, i))
                    i = ACT.activation(out=rstd2[:], in_=mv2[:, 1:2], func=AF.Sqrt, bias=eps_t[:, 0:1], scale=1.0)
                    kb.wait(DVE, M(ACT, i))
                    DVE.reciprocal(out=rstd2[:], in_=rstd2[:])
                    DVE.tensor_scalar(out=hf_[:], in0=hf_[:], scalar1=mv2[:, 0:1], scalar2=rstd2[:, 0:1],
                                      op0=ALU.subtract, op1=ALU.mult)
                    DVE.tensor_tensor(out=hf_[:], in0=hf_[:], in1=g1_bc[:], op=ALU.mult)
                    t = M(DVE, DVE.tensor_tensor(out=hf_[:], in0=hf_[:], in1=b1_bc[:], op=ALU.add))
                    kb.wait(ACT, t)
                    ACT.activation(out=hb_all[:, n, :], in_=hf_[:], func=AF.Copy)
                    tC = M(ACT, ACT.activation(out=ybuf[:, n, :], in_=hf_[:], func=AF.Copy, scale=ALPHA))
                    pend_D.append((n, sh, tC))

                def a2_dve_D():
                    n, sh, tC = pend_D.pop(0)
                    kb.wait(POOL, tC)
                    sl_ = ch_hb.pslot(POOL)
                    t = M(POOL, POOL.tensor_tensor(out=lo_sb[sl_][:], in0=hf_sb[sh][:], in1=hb_all[:, n, :], op=ALU.subtract))
                    ch_hb.produced(t)
                    tD[n] = t

                def a2_pe_tp(n):
                    sh = ch_hb.cslot(PE)
                    st = ch_tp.pslot(PE)
                    tpv = banks[6 + st][:].bitcast(BF16)
                    for k in range(8):
                        i = PE.transpose(tpv[:, k * 128:(k + 1) * 128], hb_all[:, n, k * 128:(k + 1) * 128], ident[:])
                    ch_tp.produced(M(PE, i))
                    st = ch_tp.pslot(PE)
                    tpv = banks[6 + st][:].bitcast(BF16)
                    for k in range(8):
                        i = PE.transpose(tpv[:, k * 128:(k + 1) * 128], lo_sb[sh][:, k * 128:(k + 1) * 128], ident[:])
                    t = M(PE, i)
                    ch_hb.consumed(t)
                    ch_tp.produced(t)

                def a2_act_tp(n):
                    st = ch_tp.cslot(ACT)
                    tpv = banks[6 + st][:].bitcast(BF16)
                    sht = ch_ht.pslot(ACT)
                    i = ACT.activation(out=hT_sb[sht][:], in_=tpv.rearrange("p (k t) -> p k t", k=8), func=AF.Copy)
                    t_h = M(ACT, i)
                    ch_tp.consumed(t_h)
                    ch_ht.produced(t_h)
                    st = ch_tp.cslot(ACT)
                    tpv = banks[6 + st][:].bitcast(BF16)
                    sl = ch_lo.pslot(ACT)
                    i = ACT.activation(out=loT_sb[sl][:], in_=tpv.rearrange("p (k t) -> p k t", k=8), func=AF.Copy)
                    t = M(ACT, i)
                    ch_tp.consumed(t)
                    ch_lo.produced(t)
                    return t_h

                def a2_pe_route(n, t_h):
                    sl = ch_lo.cslot(PE)
                    sht = ch_ht.cslot(PE)
                    ch_r.pslot(PE)
                    rp = banks[4]
                    for k in range(8):
                        PE.matmul(rp[:, 0:72], hT_sb[sht][:, k, :], wr_hl[:, k, 0:72], start=(k == 0), stop=False,
                                  skip_group_check=True)
                    for k in range(8):
                        i = PE.matmul(rp[:, 0:36], loT_sb[sl][:, k, :], wr_hl[:, k, 0:36], start=False, stop=(k == 7),
                                      skip_group_check=True)
                    t = M(PE, i)
                    ch_lo.consumed(t)
                    ch_ht.consumed(t)
                    ch_r.produced(t)

                def a2_route_copy(n):
                    ch_r.cslot(ACT)
                    kb.wait(ACT, math_done[0])
                    t = M(ACT, ACT.activation(out=lg_all[:, n % NH, :], in_=banks[4][:, 0:72], func=AF.Copy))
                    ch_r.consumed(t)
                    return t

                def a2_route_math(n0, t_last):
                    kb.wait(DVE, t_last)
                    sl = slice(n0, n0 + NH)
                    L = lg_all[:, :, 0:36]
                    DVE.tensor_tensor(out=L, in0=L, in1=lg_all[:, :, 36:72], op=ALU.add)
                    DVE.tensor_tensor(out=L, in0=L, in1=br_bc[:].unsqueeze(1).broadcast_to([128, NH, 36]), op=ALU.add)
                    gmax, oh, ge, sume = rq[:, :, 0:1], rq[:, :, 1:5], rq[:, :, 5:9], rq[:, :, 9:10]
                    esel, m1, eq1, e2 = rq[:, :, 11:19], rq[:, :, 19:20], rq[:, :, 20:28], rq[:, :, 28:36]
                    m2, eq2, dd, w1, w2, tmp8 = rq[:, :, 36:37], rq[:, :, 37:45], rq[:, :, 45:46], rq[:, :, 46:47], rq[:, :, 47:48], rq[:, :, 48:56]
                    bc = lambda ap, k: ap.broadcast_to([128, NH, k])
                    DVE.tensor_reduce(out=gmax, in_=lg_all[:, :, 0:4], axis=AX.X, op=ALU.max)
                    DVE.tensor_tensor(out=oh, in0=lg_all[:, :, 0:4], in1=bc(gmax, 4), op=ALU.is_equal)
                    DVE.tensor_tensor(out=ge, in0=lg_all[:, :, 0:4], in1=bc(gmax, 4), op=ALU.subtract)
                    DVE.tensor_tensor(out=esel, in0=lg_all[:, :, 4:12], in1=bc(oh[:, :, 0:1], 8), op=ALU.mult)
                    for g in range(1, 4):
                        DVE.tensor_tensor(out=tmp8, in0=lg_all[:, :, 4 + 8 * g:12 + 8 * g], in1=bc(oh[:, :, g:g + 1], 8), op=ALU.mult)
                        DVE.tensor_tensor(out=esel, in0=esel, in1=tmp8, op=ALU.add)
                    DVE.tensor_reduce(out=m1, in_=esel, axis=AX.X, op=ALU.max)
                    DVE.tensor_tensor(out=eq1, in0=esel, in1=bc(m1, 8), op=ALU.is_equal)
                    DVE.scalar_tensor_tensor(out=e2, in0=eq1, scalar=-1e30, in1=esel, op0=ALU.mult, op1=ALU.add)
                    DVE.tensor_reduce(out=m2, in_=e2, axis=AX.X, op=ALU.max)
                    DVE.tensor_tensor(out=eq2, in0=e2, in1=bc(m2, 8), op=ALU.is_equal)
                    i = DVE.tensor_tensor(out=dd, in0=m2, in1=m1, op=ALU.subtract)
                    kb.wait(ACT, M(DVE, i))
                    ACT.activation(out=ge, in_=ge, func=AF.Exp)
                    i = ACT.activation(out=dd, in_=dd, func=AF.Exp)
                    kb.wait(DVE, M(ACT, i))
                    DVE.tensor_reduce(out=sume, in_=ge, axis=AX.X, op=ALU.add)
                    DVE.reciprocal(out=sume, in_=sume)
                    DVE.tensor_scalar(out=w1, in0=dd, scalar1=1.0, scalar2=None, op0=ALU.add)
                    DVE.reciprocal(out=w1, in_=w1)
                    DVE.tensor_tensor(out=w2, in0=dd, in1=w1, op=ALU.mult)
                    DVE.tensor_tensor(out=g12[:, 0, sl].unsqueeze(2), in0=w1, in1=sume, op=ALU.mult)
                    DVE.tensor_tensor(out=g12[:, 1, sl].unsqueeze(2), in0=w2, in1=sume, op=ALU.mult)
                    for g in range(4):
                        DVE.tensor_tensor(out=M1a[:, sl, g * 8:(g + 1) * 8], in0=eq1, in1=bc(oh[:, :, g:g + 1], 8), op=ALU.mult)
                        tm = DVE.tensor_tensor(out=M2a[:, sl, g * 8:(g + 1) * 8], in0=eq2, in1=bc(oh[:, :, g:g + 1], 8), op=ALU.mult)
                    math_done[0] = M(DVE, tm)

                a2_load(0)
                t_hs = {}
                for n in range(NT + 3):
                    if n + 1 < NT:
                        a2_load(n + 1)
                    if n < NT:
                        a2_pe_op(n)
                        a2_dve_A(n)
                    if 1 <= n <= NT:
                        a2_dve_D()
                    if 2 <= n <= NT + 1:
                        a2_pe_tp(n - 2)
                        t_hs[n - 2] = a2_act_tp(n - 2)
                    if 3 <= n:
                        a2_pe_route(n - 3, t_hs[n - 3])
                        t_c = a2_route_copy(n - 3)
                        if (n - 3) % NH == NH - 1:
                            a2_route_math(n - 3 - (NH - 1), t_c)
                barrier()

            if DEBUG and b == DEBUG_B:
                with contextlib.ExitStack() as esd:
                    kd = KB(nc, esd, kb.waited, kb.prog)
                    sd = kd.sem("dbg2")
                    kd.inc(SP.dma_start(out=dbg_y, in_=ybuf[:]), sd, 16)
                    kd.wait(SP, kd.inc(SP.dma_start(out=dbg_gate, in_=M1a[:]), sd, 16))
                    barrier()
            if not DEBUG or b == DEBUG_B:
                stop_if("A2")

            with contextlib.ExitStack() as es5:
                k5 = KB(nc, es5, kb.waited, kb.prog)
                NWS = 3
                wgu_sb = [catT[:, 2 * i:2 * i + 2, :].rearrange("p a (k f) -> p (a k) f", f=512) for i in range(3)]
                wdn_t = [k5.sb(f"wdn_t{b}_{i}", [128, 2, D], BF16) for i in range(2)]
                wdn_sb = [t_[:] for t_ in wdn_t] + [catT[:, 6 + i, :].rearrange("p (k f) -> p k f", f=1024) for i in range(2)]
                thr = k5.sb(f"thr{b}", [128, NJ, 32], F32)
                Mb = k5.sb(f"Mb{b}", [128, NT * 32], BF16)
                Ms = k5.sb(f"Ms{b}", [128, NT, 32], F32)
                cs = k5.sb(f"cs{b}", [128, NT, 32], F32)
                off = k5.sb(f"off{b}", [128, NT, 32], F32)
                Sf = k5.sb(f"Sf{b}", [128, NT, 32], F32)
                tS = Ms
                sc_a = k5.sb(f"sc_a{b}", [128, 32], F32)
                sc_b = k5.sb(f"sc_b{b}", [128, 32], F32)
                pt = k5.sb(f"pt{b}", [128, 32], F32)
                base = k5.sb(f"base{b}", [128, 32], F32)
                qi = k5.sb(f"qi{b}", [128, 32], I32)
                sl_f = k5.sb(f"sl_f{b}", [128, 2, NT], F32)
                ej_f = k5.sb(f"ej_f{b}", [128, NJ], F32)
                wi_f = thr[:].rearrange("p j e -> p (j e)")[:, 0:NJ * 3].rearrange("p (j c) -> p j c", c=3)
                wi_i = k5.sb(f"wi_i{b}", [128, NJ, 3], I32)
                pidx = k5.sb(f"pidx{b}", [128, 1], F32)
                ko = k5.sb(f"ko{b}", [128, 8], F32)
                Lst = k5.sb(f"Lst{b}", [128, 128], BF16)
                xt_sb = [k5.sb(f"xt_sb{b}_{i}", [128, 2, D], BF16) for i in range(2)]
                xsT = [k5.sb(f"xsT{b}_{i}", [128, 8, 256], BF16) for i in range(2)]
                sl_sb = [k5.sb(f"sl_sb{b}_{i}", [128, 512], F32) for i in range(2)]
                at_sb = [k5.sb(f"at_sb{b}_{i}", [128, 2, 256], BF16) for i in range(3)]
                yo_sb = [k5.sb(f"yo_sb{b}_{i}", [128, D], F32) for i in range(2)]

                POOL.affine_select(out=Lst[:], in_=ones_bf[:], pattern=[[1, 128]], compare_op=ALU.is_gt,
                                   fill=0.0, base=0, channel_multiplier=-1)
                t_thr = M(POOL, POOL.iota(thr[:], pattern=[[256, NJ], [0, 32]], base=0, channel_multiplier=0,
                                          allow_small_or_imprecise_dtypes=True))
                POOL.iota(pidx[:], pattern=[[0, 1]], base=0, channel_multiplier=1, allow_small_or_imprecise_dtypes=True)
                t_io = M(POOL, POOL.iota(ko[:], pattern=[[128, 8]], base=0, channel_multiplier=0, allow_small_or_imprecise_dtypes=True))
                DVE.tensor_tensor(out=Ms[:], in0=M1a[:], in1=M2a[:], op=ALU.add)
                t = M(DVE, DVE.tensor_copy(out=Mb[:], in_=Ms[:].rearrange("p n e -> p (n e)")))
                kb.wait(PE, t)
                kb.wait(PE, t_thr)
                PE.matmul(banks[0][:], Lst[:], Mb[:], start=True, stop=True)
                t = M(PE, PE.matmul(banks[1][:], ones_bf[:], Mb[:], start=True, stop=True))
                kb.wait(DVE, t)
                DVE.tensor_copy(out=cs[:], in_=banks[1][:].rearrange("p (n e) -> p n e", e=32))
                DVE.memset(off[:, 0, :], 0.0)
                for n in range(1, NT):
                    DVE.tensor_tensor(out=off[:, n, :], in0=off[:, n - 1, :], in1=cs[:, n - 1, :], op=ALU.add)
                DVE.tensor_tensor(out=sc_a[:], in0=off[:, NT - 1, :], in1=cs[:, NT - 1, :], op=ALU.add)
                DVE.tensor_scalar(out=sc_b[:], in0=sc_a[:], scalar1=127.5, scalar2=1.0 / 256.0, op0=ALU.add, op1=ALU.mult)
                DVE.tensor_copy(out=qi[:], in_=sc_b[:])
                DVE.tensor_scalar(out=pt[:], in0=qi[:], scalar1=256.0, scalar2=None, op0=ALU.mult)
                DVE.tensor_copy(out=sc_a[:], in_=pt[:])
                pa, pb = sc_a, sc_b
                for sh in (1, 2, 4, 8, 16):
                    DVE.tensor_copy(out=pb[:, 0:sh], in_=pa[:, 0:sh])
                    DVE.tensor_tensor(out=pb[:, sh:32], in0=pa[:, sh:32], in1=pa[:, 0:32 - sh], op=ALU.add)
                    pa, pb = pb, pa
                incl = pa
                DVE.tensor_tensor(out=base[:], in0=incl[:], in1=pt[:], op=ALU.subtract)
                DVE.tensor_tensor(out=Sf[:], in0=banks[0][:].rearrange("p (n e) -> p n e", e=32), in1=off[:], op=ALU.add)
                DVE.tensor_tensor(out=Sf[:], in0=Sf[:], in1=base[:].unsqueeze(1).broadcast_to([128, NT, 32]), op=ALU.add)
                DVE.tensor_tensor(out=tS[:], in0=Sf[:], in1=M1a[:], op=ALU.mult)
                DVE.tensor_reduce(out=sl_f[:, 0, :], in_=tS[:], axis=AX.X, op=ALU.add)
                DVE.tensor_tensor(out=tS[:], in0=Sf[:], in1=M2a[:], op=ALU.mult)
                DVE.tensor_reduce(out=sl_f[:, 1, :], in_=tS[:], axis=AX.X, op=ALU.add)
                DVE.tensor_copy(out=sl_i[:], in_=sl_f[:])
                kb.wait(DVE, t_thr)
                DVE.tensor_tensor(out=thr[:], in0=thr[:], in1=incl[:].unsqueeze(1).broadcast_to([128, NJ, 32]), op=ALU.is_ge)
                DVE.tensor_reduce(out=ej_f[:], in_=thr[:], axis=AX.X, op=ALU.add)
                kb.wait(DVE, t_io)
                DVE.tensor_scalar(out=ej_f[:], in0=ej_f[:], scalar1=256.0, scalar2=pidx[:, 0:1], op0=ALU.mult, op1=ALU.add)
                DVE.tensor_tensor(out=wi_f[:, :, 0:2], in0=ej_f[:].unsqueeze(2).broadcast_to([128, NJ, 2]),
                                  in1=ko[:, 0:2].unsqueeze(1).broadcast_to([128, NJ, 2]), op=ALU.add)
                DVE.tensor_scalar(out=ej_f[:], in0=ej_f[:], scalar1=pidx[:, 0:1], scalar2=0.5, op0=ALU.subtract, op1=ALU.mult)
                DVE.tensor_scalar(out=wi_f[:, :, 2:3], in0=ej_f[:].unsqueeze(2), scalar1=pidx[:, 0:1], scalar2=None, op0=ALU.add)
                t_disp = M(DVE, DVE.tensor_copy(out=wi_i[:], in_=wi_f))

                kb.wait(POOL, t_disp)
                for n in range(NT):
                    for kk in range(2):
                        i = POOL.indirect_dma_start(out=xs_d, out_offset=bass.IndirectOffsetOnAxis(ap=sl_i[:, kk, n:n + 1], axis=0),
                                                    in_=hb_all[:, n, :], in_offset=None)
                        kb.inc(i, s_sc, 16)
                tok_sc = (s_sc, s_sc.n)
                kb.wait(SP, tok_sc)
                kb.wait(POOL, tok_sc)

                wgu_rows = w_gu.rearrange("e (d4 i) f -> (e d4) (i f)", i=4)
                wdn_rows = w_dn.rearrange("e (f2 i) d -> (e f2) (i d)", i=2)

                def m_load_w(j):
                    sg_ = ch_wg.pslot(POOL)
                    gv = wgu_sb[sg_].rearrange("p (k4 i) f -> p k4 (i f)", i=4)
                    for k4 in range(2):
                        i = POOL.indirect_dma_start(out=gv[:, k4, :], out_offset=None, in_=wgu_rows,
                                                    in_offset=bass.IndirectOffsetOnAxis(ap=wi_i[:, j, k4:k4 + 1], axis=0),
                                                    bounds_check=rb_gu, oob_is_err=False)
                        t1 = ring_wg.start(i, sg_)
                    ch_wg.produced(t1)
                    sd_ = ch_wd.pslot(POOL)
                    i = POOL.indirect_dma_start(out=wdn_sb[sd_].rearrange("p i d -> p (i d)"), out_offset=None, in_=wdn_rows,
                                                in_offset=bass.IndirectOffsetOnAxis(ap=wi_i[:, j, 2:3], axis=0),
                                                bounds_check=rb_dn, oob_is_err=False)
                    ch_wd.produced(ring_wd.start(i, sd_))

                def m_load_x(j):
                    sx = ch_xs.pslot(SP)
                    i = SP.dma_start(out=xt_sb[sx][:], in_=xs_d[256 * j:256 * (j + 1), :].rearrange("(a p) d -> p a d", p=128))
                    ch_xs.produced(ring_xs.start(i, sx))

                def m_tp(j):
                    sx = ch_xs.cslot(PE)
                    ch_tp.pslot(PE)
                    ch_tp.pslot(PE)
                    for half in range(2):
                        tpv = banks[6 + half][:].bitcast(BF16).rearrange("p (k a t) -> p k a t", k=4, a=2)
                        for k4 in range(4):
                            for a_ in range(2):
                                k = half * 4 + k4
                                c0 = 512 * (k // 4) + (k % 4)
                                i = PE.transpose(tpv[:, k4, a_, :], xt_sb[sx][:, a_, c0:c0 + 509:4], ident[:])
                    t = M(PE, i)
                    ch_xs.consumed(t)
                    ch_tp.produced(t)
                    ch_tp.produced(t)
                    sT = ch_xT.pslot(ACT, DVE)
                    ch_tp.cslot(ACT)
                    ta = M(ACT, ACT.activation(out=xsT[sT][:, 0:4, :], in_=banks[6][:].bitcast(BF16).rearrange("p (k t) -> p k t", k=4), func=AF.Copy))
                    ch_tp.consumed(ta)
                    ch_tp.cslot(DVE)
                    td = M(DVE, DVE.tensor_copy(out=xsT[sT][:, 4:8, :], in_=banks[7][:].bitcast(BF16).rearrange("p (k t) -> p k t", k=4)))
                    ch_tp.consumed(td)
                    ch_xT.produced(ta, td)

                def m_gu(j):
                    sw = j % 3
                    sT = ch_xT.cslot(PE)
                    for tok in ch_wg.ready[j]:
                        kb.wait(PE, tok)
                    sg = ch_g2.pslot(PE)
                    for part in range(2):
                        for fcp in range(2):
                            fc = part * 256 + fcp * 128
                            for k in range(8):
                                i = PE.matmul(banks[2 * sg + part][:, fcp * 256:(fcp + 1) * 256], wgu_sb[sw][:, k, fc:fc + 128], xsT[sT][:, k, :],
                                              start=(fcp == 0 and k == 0), stop=(k == 7), skip_group_check=True)
                    t = M(PE, i)
                    ch_xT.consumed(t)
                    ch_wg.consumed(t)
                    ch_g2.produced(t)
                    sg = ch_g2.cslot(ACT, 0)
                    ss = ch_sl.pslot(ACT)
                    t = M(ACT, ACT.activation(out=sl_sb[ss][:], in_=banks[2 * sg][:], func=AF.Silu))
                    ch_g2.consumed(t, 0)
                    ch_sl.produced(t)
                    sg = ch_g2.cslot(DVE, 1)
                    ss = ch_sl.cslot(DVE)
                    sa = ch_at.pslot(DVE)
                    t = M(DVE, DVE.tensor_tensor(out=at_sb[sa][:].rearrange("p c t -> p (c t)"), in0=banks[2 * sg + 1][:], in1=sl_sb[ss][:], op=ALU.mult))
                    ch_g2.consumed(t, 1)
                    ch_sl.consumed(t)
                    ch_at.produced(t)

                def m_dn(j):
                    sw = j % 4
                    sa = ch_at.cslot(PE)
                    for tok in ch_wd.ready[j]:
                        kb.wait(PE, tok)
                    for a_ in range(2):
                        ch_dn.pslot(PE)
                        for hf in range(2):
                            for fc in range(2):
                                i = PE.matmul(banks[4 + hf][:], at_sb[sa][:, fc, a_ * 128:(a_ + 1) * 128],
                                              wdn_sb[sw][:, fc, hf * 512:(hf + 1) * 512], start=(fc == 0), stop=(fc == 1))
                        t = M(PE, i)
                        ch_dn.produced(t)
                        so = ch_yo.pslot(ACT, DVE)
                        ch_dn.cslot(ACT, 0)
                        ta = M(ACT, ACT.activation(out=yo_sb[so][:, 0:512], in_=banks[4][:], func=AF.Copy))
                        ch_dn.consumed(ta, 0)
                        ch_dn.cslot(DVE, 1)
                        td = M(DVE, DVE.tensor_copy(out=yo_sb[so][:, 512:1024], in_=banks[5][:]))
                        ch_dn.consumed(td, 1)
                        ch_yo.produced(ta, td)
                        so = ch_yo.cslot(SP)
                        i = SP.dma_start(out=ys_d[256 * j + 128 * a_:256 * j + 128 * (a_ + 1), :], in_=yo_sb[so][:])
                        ch_yo.consumed(ring_ys.start(i, so))
                        last_ys[so] = (ring_ys.sems[so], ring_ys.sems[so].n)
                    ch_at.consumed(t)
                    ch_wd.consumed(t)

                last_ys = {}
                m_load_w(0)
                m_load_w(1)
                m_load_x(0)
                m_load_x(1)
                m_tp(0)
                for j in range(NJ):
                    if j + 2 < NJ:
                        m_load_x(j + 2)
                    if j + 1 < NJ:
                        m_tp(j + 1)
                    if j >= 2:
                        m_dn(j - 2)
                    if j + 2 < NJ:
                        m_load_w(j + 2)
                    m_gu(j)
                m_dn(NJ - 2)
                m_dn(NJ - 1)
                barrier()
                stop_if("M")

            with contextlib.ExitStack() as es6:
                k6 = KB(nc, es6, kb.waited, kb.prog)
                o_sb = [k6.sb(f"o_sb{b}_{i}", [128, D], F32) for i in range(2)]
                rg_sb = [k6.sb(f"rg_sb{b}_{i}", [128, D], F32) for i in range(4)]
                st3 = k6.sb(f"st3_{b}", [128, 2, 6], F32)
                mv3 = k6.sb(f"mv3_{b}", [128, 2], F32)
                rstd3 = k6.sb(f"rstd3_{b}", [128, 1], F32)
                eps3 = k6.sb(f"eps3_{b}", [128, 1], F32)
                g2_bc = k6.sb(f"g2_bc{b}", [128, D], F32)
                b2_bc = k6.sb(f"b2_bc{b}", [128, D], F32)
                s_g2 = k6.sem(f"f_g2{b}")
                k6.inc(SP.dma_start(out=g2_bc[:], in_=ln2_g.partition_broadcast(128)), s_g2, 16)
                k6.wait(DVE, k6.inc(SP.dma_start(out=b2_bc[:], in_=ln2_b.partition_broadcast(128)), s_g2, 16))
                k6.wait(ACT, M(POOL, POOL.memset(eps3[:], EPS)))
                for so, tk in last_ys.items():
                    kb.wait(POOL, tk)

                def f_gather(n):
                    for kk in range(2):
                        sr = ch_rg.pslot(POOL)
                        i = POOL.indirect_dma_start(out=rg_sb[sr][:], out_offset=None, in_=ys_d,
                                                    in_offset=bass.IndirectOffsetOnAxis(ap=sl_i[:, kk, n:n + 1], axis=0))
                        ch_rg.produced(ring_g4.start(i, sr))

                last2 = []
                f_gather(0)
                for n in range(NT):
                    if n + 1 < NT:
                        f_gather(n + 1)
                    for kk in range(2):
                        sr = ch_rg.cslot(DVE)
                        t = M(DVE, DVE.scalar_tensor_tensor(out=ybuf[:, n, :], in0=rg_sb[sr][:], scalar=g12[:, kk, n:n + 1], in1=ybuf[:, n, :],
                                                            op0=ALU.mult, op1=ALU.add))
                        ch_rg.consumed(t)
                    for hf in range(2):
                        DVE.bn_stats(out=st3[:, hf, :], in_=ybuf[:, n, hf * 512:(hf + 1) * 512])
                    i = DVE.bn_aggr(out=mv3[:], in_=st3[:].rearrange("p a c -> p (a c)"))
                    kb.wait(ACT, M(DVE, i))
                    i = ACT.activation(out=rstd3[:], in_=mv3[:, 1:2], func=AF.Sqrt, bias=eps3[:, 0:1], scale=1.0)
                    kb.wait(DVE, M(ACT, i))
                    DVE.reciprocal(out=rstd3[:], in_=rstd3[:])
                    so = ch_ot.pslot(DVE)
                    DVE.tensor_scalar(out=o_sb[so][:], in0=ybuf[:, n, :], scalar1=mv3[:, 0:1], scalar2=rstd3[:, 0:1],
                                      op0=ALU.subtract, op1=ALU.mult)
                    DVE.tensor_tensor(out=o_sb[so][:], in0=o_sb[so][:], in1=g2_bc[:], op=ALU.mult)
                    i = DVE.tensor_tensor(out=o_sb[so][:], in0=o_sb[so][:], in1=b2_bc[:], op=ALU.add)
                    ch_ot.produced(M(DVE, i))
                    so = ch_ot.cslot(SP)
                    i = SP.dma_start(out=out[b, n * 128:(n + 1) * 128, :], in_=o_sb[so][:])
                    t = ring_o.start(i, so)
                    ch_ot.consumed(t)
                    last2 = (last2 + [t])[-2:]
                for t in last2:
                    kb.wait(SP, t)
                barrier()
            esp.close()
    return nc


def _prep_inputs(inputs):
    x = np.ascontiguousarray(inputs["x"], dtype=np.float32)
    pos = np.ascontiguousarray(inputs["positions"], dtype=np.int32)
    w_r = np.concatenate([inputs["w_group"][0], np.transpose(inputs["w_expert"][0], (1, 0, 2)).reshape(D, 32)], axis=1)
    b_r = np.concatenate([inputs["b_group"][0], inputs["b_expert"][0].reshape(32)])[None, :]
    ws = inputs["w_spatial"][0]
    shared = {
        "w_in": np.ascontiguousarray(inputs["w_in"][0]),
        "w_out": np.ascontiguousarray(inputs["w_out"][0]),
        "ws_tgs": np.ascontiguousarray(np.transpose(ws, (1, 0, 2))),
        "ws_sgt": np.ascontiguousarray(np.transpose(ws, (2, 0, 1))),
        "bspT": np.ascontiguousarray(inputs["b_spatial"][0].T),
        "sgu_g": np.ascontiguousarray(inputs["sgu_ln_g"]),
        "sgu_b": np.ascontiguousarray(inputs["sgu_ln_b"]),
        "ln1_g": np.ascontiguousarray(inputs["ln1_g"]),
        "ln1_b": np.ascontiguousarray(inputs["ln1_b"]),
        "ln2_g": np.ascontiguousarray(inputs["ln2_g"]),
        "ln2_b": np.ascontiguousarray(inputs["ln2_b"]),
        "w_r": np.ascontiguousarray(w_r, dtype=np.float32),
        "b_r": np.ascontiguousarray(b_r, dtype=np.float32),
        "w_gu": np.ascontiguousarray(np.transpose(inputs["w_gate_up"][0].reshape(NE, D, 2, 128, 2), (0, 1, 2, 4, 3)).reshape(NE, D, 512)),
        "w_dn": np.ascontiguousarray(inputs["w_down"][0].reshape(NE, 256, D)),
    }
    in_maps = []
    for c in range(NCORES):
        xs = x[c * NB:(c + 1) * NB]
        m = dict(shared)
        m["xT"] = np.ascontiguousarray(np.transpose(xs, (0, 2, 1)))
        m["xtm"] = xs
        m["posT"] = np.ascontiguousarray(np.transpose(pos[c * NB:(c + 1) * NB].reshape(NB, NT, 128), (0, 2, 1)))
        in_maps.append(m)
    return in_maps


def kernel(**inputs):
    in_maps = _prep_inputs(inputs)
    nc = build_nc()
    res = run_bass_kernel_spmd(nc, in_maps, core_ids=list(range(NCORES)))
    return np.concatenate([r["out"] for r in res.results], axis=0).astype(np.float32)
```

```python
import contextlib
import numpy as np
import concourse.bass as bass
import concourse.mybir as mybir
from concourse.bass_utils import run_bass_kernel_spmd

F32, BF16, I32 = mybir.dt.float32, mybir.dt.bfloat16, mybir.dt.int32
AF = mybir.ActivationFunctionType
ALU = mybir.AluOpType
AX = mybir.AxisListType

NCORES = 8
S = 2048
D = 1024
NB = 2
NT = S // 128
ALPHA = float(2.0 ** 0.25)
EPS = 1e-5
NEG = -30000.0
NE = 32
TWO_PI = float(2 * np.pi)
DEBUG = False
DEBUG_B = 0
STOP_AFTER = None


class _Stop(Exception):
    pass


class Sem:
    _serial = 0

    def __init__(self, h):
        self.h = h
        self.n = 0
        Sem._serial += 1
        self.uid = Sem._serial


class KB:
    def __init__(self, nc, es, waited=None, prog=None):
        self.nc = nc
        self.es = es
        self.waited = {} if waited is None else waited
        self.prog = {} if prog is None else prog

    sem_es = None

    def sem(self, name):
        return Sem(KB.sem_es.enter_context(self.nc.semaphore(name)))

    def sb(self, name, shape, dt):
        return self.es.enter_context(self.nc.sbuf_tensor(name, shape, dt))

    def ps(self, name, shape, dt):
        return self.es.enter_context(self.nc.psum_tensor(name, shape, dt))

    def inc(self, instr, sem, k=1):
        instr.then_inc(sem.h, k)
        sem.n += k
        return (sem, sem.n)

    def mark(self, eng, instr):
        if isinstance(instr, Tok):
            return instr.tok
        return self.inc(instr, self.prog[id(getattr(eng, "raw", eng))])

    def wait(self, eng, tok):
        if tok is None:
            return
        sem, val = tok
        if val <= 0:
            return
        raw = getattr(eng, "raw", eng)
        key = (id(raw), sem.uid)
        if self.waited.get(key, 0) >= val:
            return
        self.waited[key] = val
        raw.wait_ge(sem.h, val)


class Tok:
    def __init__(self, instr, tok):
        self.instr = instr
        self.tok = tok


_COMPUTE = {"activation", "tensor_tensor", "tensor_scalar", "scalar_tensor_tensor", "tensor_copy", "tensor_reduce",
            "reciprocal", "bn_stats", "bn_aggr", "memset", "affine_select", "iota"}


class EngProxy:
    def __init__(self, raw, kb):
        self.raw = raw
        self.kb = kb
        self.last = None

    def __getattr__(self, name):
        attr = getattr(self.raw, name)
        if name not in _COMPUTE:
            return attr

        def wrapper(*a, **k):
            if self.last is not None:
                self.kb.wait(self, self.last)
            instr = attr(*a, **k)
            tok = self.kb.inc(instr, self.kb.prog[id(self.raw)])
            self.last = tok
            return Tok(instr, tok)
        return wrapper


class Chan:
    def __init__(self, kb, name, depth, ncons=1):
        self.kb = kb
        self.depth = depth
        self.ready = []
        self.free = []
        self.ci = [0] * ncons

    def pslot(self, *engs):
        i = len(self.ready)
        if i >= self.depth:
            for tok in self.free[i - self.depth]:
                for e in engs:
                    self.kb.wait(e, tok)
        return i % self.depth

    def produced(self, *toks):
        self.ready.append(list(toks))

    def cslot(self, eng, c=0):
        i = self.ci[c]
        for tok in self.ready[i]:
            self.kb.wait(eng, tok)
        return i % self.depth

    def consumed(self, tok, c=0):
        i = self.ci[c]
        while len(self.free) <= i:
            self.free.append([])
        self.free[i].append(tok)
        self.ci[c] += 1


class DmaRing:
    def __init__(self, kb, name, depth):
        self.kb = kb
        self.sems = [kb.sem(f"{name}{i}") for i in range(depth)]

    def start(self, instr, slot):
        return self.kb.inc(instr, self.sems[slot], 16)


def build_nc():
    nc = bass.Bass("TRN2", target_bir_lowering=False)
    dr = lambda name, shape, dt=F32: nc.dram_tensor(name, shape, dt, kind="ExternalInput").ap()
    xT = dr("xT", [NB, D, S])
    xtm = dr("xtm", [NB, S, D])
    posT = dr("posT", [NB, 128, NT], I32)
    w_in = dr("w_in", [D, 2560])
    w_out = dr("w_out", [D, D])
    ws_tgs = dr("ws_tgs", [128, 8, 128])
    ws_sgt = dr("ws_sgt", [128, 8, 128])
    bspT = dr("bspT", [128, 8])
    sgu_g = dr("sgu_g", [1, 512])
    sgu_b = dr("sgu_b", [1, 512])
    ln1_g = dr("ln1_g", [1, D])
    ln1_b = dr("ln1_b", [1, D])
    ln2_g = dr("ln2_g", [1, D])
    ln2_b = dr("ln2_b", [1, D])
    w_r = dr("w_r", [D, 36])
    b_r = dr("b_r", [1, 36])
    w_gu = dr("w_gu", [NE, D, 512])
    w_dn = dr("w_dn", [NE, 256, D])
    out = nc.dram_tensor("out", [NB, S, D], F32, kind="ExternalOutput").ap()
    if DEBUG:
        dbg_cat = nc.dram_tensor("dbg_cat", [128, 8, S], F32, kind="ExternalOutput").ap()
        dbg_y = nc.dram_tensor("dbg_y", [128, NT, D], F32, kind="ExternalOutput").ap()
        dbg_gate = nc.dram_tensor("dbg_gate", [128, NT, 32], F32, kind="ExternalOutput").ap()

    PE, ACT, DVE, POOL, SP = nc.tensor, nc.scalar, nc.vector, nc.gpsimd, nc.sync
    engines = [PE, ACT, DVE, POOL, SP]

    with contextlib.suppress(_Stop), contextlib.ExitStack() as es:
        KB.sem_es = es
        kb = KB(nc, es)
        progs = [{id(getattr(e, "raw", e)): kb.sem(f"prog{bb}_{n}") for e, n in zip(engines, "pe act dve pool sp".split())} for bb in range(NB + 1)]
        kb.prog = progs[NB]
        M = kb.mark
        ACT, DVE, POOL = EngProxy(nc.scalar, kb), EngProxy(nc.vector, kb), EngProxy(nc.gpsimd, kb)
        engines = [PE, ACT, DVE, POOL, SP]
        banks = [kb.ps(f"bank{i}", [128, 512], F32) for i in range(8)]

        ident = kb.sb("ident", [128, 128], BF16)
        ones_bf = kb.sb("ones_bf", [128, 128], BF16)
        ones_z = kb.sb("ones_z", [128, 2, 128], BF16)
        zer = kb.sb("zer", [128, 512], F32)
        mhalf = kb.sb("mhalf", [128, 1], F32)
        m_cur = kb.sb("m_cur", [128, 512], BF16)
        m_prev = kb.sb("m_prev", [128, 512], BF16)
        m3 = kb.sb("m3", [128, 4, 512], BF16)
        wmT = kb.sb("wmT", [128, 8, 128], BF16)
        rs = kb.sb("rs", [128, 8], F32)
        bsp_sb = kb.sb("bsp_sb", [128, 8], F32)
        sg_bc = kb.sb("sg_bc", [128, 512], F32)
        sb_bc = kb.sb("sb_bc", [128, 512], F32)
        Bp = kb.sb("Bp", [128, 512], F32)
        br_bc = kb.sb("br_bc", [128, 36], F32)
        wr_hl = kb.sb("wr_hl", [128, 8, 72], BF16)
        invf = kb.sb("invf", [128, NT, 8], F32)
        catT = kb.sb("catT", [128, 8, S], BF16)

        s_c = kb.sem("const_dma")
        s_bar = kb.sem("barrier")

        def barrier():
            base = s_bar.n
            for e in engines:
                if isinstance(e, EngProxy) and e.last is not None:
                    kb.wait(e, e.last)
                kb.inc(e.nop(), s_bar)
            for e in engines:
                kb.wait(e, (s_bar, base + len(engines)))

        def stop_if(tag):
            if STOP_AFTER == tag:
                barrier()
                raise _Stop()

        def wait_all(tok):
            for e in engines:
                kb.wait(e, tok)

        with contextlib.ExitStack() as es0:
            k0 = KB(nc, es0, kb.waited, kb.prog)
            wtmp = k0.sb("wtmp", [128, 8, 128], F32)
            wtmp2 = k0.sb("wtmp2", [128, 8, 128], F32)
            wr_f = k0.sb("wr_f", [128, 8, 36], F32)
            wr_t = k0.sb("wr_t", [128, 8, 36], F32)

            def dma_c(o, i):
                kb.inc(SP.dma_start(out=o, in_=i), s_c, 16)

            dma_c(wtmp[:], ws_sgt)
            dma_c(wtmp2[:], ws_tgs)
            dma_c(bsp_sb[:], bspT)
            dma_c(sg_bc[:], sgu_g.partition_broadcast(128))
            dma_c(sb_bc[:], sgu_b.partition_broadcast(128))
            dma_c(br_bc[:], b_r.partition_broadcast(128))
            dma_c(wr_f[:], w_r.rearrange("(k p) c -> p k c", p=128))
            tok_cdma = (s_c, s_c.n)

            POOL.memset(zer[:], 0.0)
            POOL.memset(ones_bf[:], 1.0)
            POOL.memset(ones_z[:], 0.0)
            POOL.memset(ones_z[:, 0, 0:64], 1.0)
            POOL.memset(ones_z[:, 1, 64:128], 1.0)
            POOL.memset(mhalf[:], -0.5)
            POOL.affine_select(out=ident[:], in_=ones_bf[:], pattern=[[1, 128]], compare_op=ALU.is_equal,
                               fill=0.0, base=0, channel_multiplier=-1)
            POOL.affine_select(out=m_cur[:], in_=zer[:], pattern=[[0, 4], [1, 128]], compare_op=ALU.is_ge,
                               fill=NEG, base=0, channel_multiplier=-1)
            POOL.affine_select(out=m_prev[:], in_=zer[:], pattern=[[0, 4], [-1, 128]], compare_op=ALU.is_ge,
                               fill=NEG, base=0, channel_multiplier=1)
            for s in range(4):
                POOL.affine_select(out=m3[:, s, :], in_=zer[:], pattern=[[0, 16], [1, 32]], compare_op=ALU.is_ge,
                                   fill=NEG, base=32 * s, channel_multiplier=-1)
            inv = (np.float32(500000.0) ** (-(np.arange(0, 16, 2, dtype=np.float32)) / np.float32(16))).astype(np.float32)
            for i in range(8):
                POOL.memset(invf[:, :, i:i + 1], float(inv[i]))
            kb.wait(POOL, tok_cdma)
            POOL.affine_select(out=wmT[:], in_=wtmp[:], pattern=[[0, 8], [1, 128]], compare_op=ALU.is_ge,
                               fill=0.0, base=0, channel_multiplier=-1)
            i_last = POOL.affine_select(out=wtmp[:], in_=wtmp2[:], pattern=[[0, 8], [-1, 128]], compare_op=ALU.is_ge,
                                        fill=0.0, base=0, channel_multiplier=1)
            tok_cpool = M(POOL, i_last)

            kb.wait(DVE, tok_cdma)
            kb.wait(DVE, tok_cpool)
            DVE.tensor_reduce(out=rs[:], in_=wtmp[:], axis=AX.X, op=ALU.add)
            for g in range(8):
                DVE.tensor_scalar(out=Bp[:, g * 64:(g + 1) * 64], in0=sb_bc[:, g * 64:(g + 1) * 64],
                                  scalar1=rs[:, g:g + 1], scalar2=bsp_sb[:, g:g + 1], op0=ALU.mult, op1=ALU.add)
            DVE.tensor_copy(out=wr_hl[:, :, 0:36], in_=wr_f[:])
            DVE.tensor_copy(out=wr_t[:], in_=wr_hl[:, :, 0:36])
            DVE.tensor_tensor(out=wr_t[:], in0=wr_f[:], in1=wr_t[:], op=ALU.subtract)
            i_last = DVE.tensor_copy(out=wr_hl[:, :, 36:72], in_=wr_t[:])
            tok_cdve = M(DVE, i_last)
            wait_all(tok_cdma)
            wait_all(tok_cpool)
            wait_all(tok_cdve)
            barrier()
        stop_if("const")

        def rstd_via_pool(mv, dst, last_dve_instr):
            t = M(DVE, last_dve_instr)
            kb.wait(POOL, t)
            POOL.tensor_scalar(out=dst, in0=mv[:, 1:2], scalar1=EPS, scalar2=None, op0=ALU.add)
            i = POOL.tensor_tensor(out=dst, in0=dst, in1=mhalf[:], op=ALU.pow)
            kb.wait(DVE, M(POOL, i))

        ring_x = DmaRing(kb, "ring_x", 2)
        ring_w = DmaRing(kb, "ring_w", 2)
        ring_o = DmaRing(kb, "ring_o", 2)
        ring_m = DmaRing(kb, "ring_m", 4)
        ring_wg = DmaRing(kb, "ring_wg", 3)
        ring_wd = DmaRing(kb, "ring_wd", 4)
        ring_xs3 = DmaRing(kb, "ring_xs3", 3)
        ring_xs = DmaRing(kb, "ring_xs", 2)
        ring_ys = DmaRing(kb, "ring_ys", 2)
        ring_g = DmaRing(kb, "ring_g", 2)
        ring_g4 = DmaRing(kb, "ring_g4", 4)
        s_sc = kb.sem("scatter")
        rb_gu = es.enter_context(nc.gpsimd.register("rb_gu"))
        rb_dn = es.enter_context(nc.gpsimd.register("rb_dn"))
        nc.gpsimd.reg_mov(rb_gu, NE * 256 - 1)
        nc.gpsimd.reg_mov(rb_dn, NE * 128 - 1)
        NJ = 48
        xs_d = nc.dram_tensor("xs_scratch", [NJ * 256, D], BF16, kind="Internal").ap()
        ys_d = nc.dram_tensor("ys_scratch", [NJ * 256, D], F32, kind="Internal").ap()

        s_zf = kb.sem("xs_zero")
        zrow = zer[:].bitcast(BF16)
        zf_state = {}

        for b in range(NB):
            kb.prog = progs[b]
            ch_u = Chan(kb, "ch_u", 2)
            ch_v = Chan(kb, "ch_v", 2)
            ch_z = Chan(kb, "ch_z", 2)
            ch_tp = Chan(kb, "ch_tp", 2)
            ch_n = Chan(kb, "ch_n", 2)
            ch_gu = Chan(kb, "ch_gu", 2)
            ch_gv = Chan(kb, "ch_gv", 2)
            ch_sg = Chan(kb, "ch_sg", 2)
            ch_qk = Chan(kb, "ch_qk", 2)
            ch_rot = Chan(kb, "ch_rot", 2)
            ch_qs = Chan(kb, "ch_qs", 2)
            ch_vp = Chan(kb, "ch_vp", 2)
            ch_s = Chan(kb, "ch_s", 4)
            ch_p = Chan(kb, "ch_p", 6)
            ch_o = Chan(kb, "ch_o", 2)
            ch_op = Chan(kb, "ch_op", 2)
            ch_x = Chan(kb, "ch_x", 2)
            ch_hb = Chan(kb, "ch_hb", 2)
            ch_hf = Chan(kb, "ch_hf", 2)
            ch_lo = Chan(kb, "ch_lo", 2)
            ch_ht = Chan(kb, "ch_ht", 2)
            ch_r = Chan(kb, "ch_r", 1)
            ch_g2 = Chan(kb, "ch_g2", 2, ncons=2)
            ch_sl = Chan(kb, "ch_sl", 2)
            ch_at = Chan(kb, "ch_at", 3)
            ch_wg = Chan(kb, "ch_wg", 3)
            ch_wd = Chan(kb, "ch_wd", 4)
            ch_dn = Chan(kb, "ch_dn", 1, ncons=2)
            ch_wt = Chan(kb, "ch_wt", 3)
            ch_ot = Chan(kb, "ch_ot", 2)
            ch_xs = Chan(kb, "ch_xs", 2)
            ch_xT = Chan(kb, "ch_xT", 2)
            ch_yo = Chan(kb, "ch_yo", 2)
            ch_rg = Chan(kb, "ch_rg", 4)

            with contextlib.ExitStack() as es1:
                k1 = KB(nc, es1, kb.waited, kb.prog)
                xT_sb = k1.sb(f"xT_sb{b}", [128, 8, S], BF16)
                wsec = k1.sb(f"wsec{b}", [128, 8, 1024], BF16)
                pos_i = k1.sb(f"pos_i{b}", [128, NT], I32)
                pos_f = k1.sb(f"pos_f{b}", [128, NT], F32)
                ang = k1.sb(f"ang{b}", [128, 2, NT, 8], F32)
                kk_i = k1.sb(f"kk_i{b}", [128, 2, NT, 8], I32)
                kk_f = k1.sb(f"kk_f{b}", [128, 2, NT, 8], F32)
                rr = k1.sb(f"rr{b}", [128, 2, NT, 8], F32)
                mm = k1.sb(f"mm{b}", [128, 2, NT, 8], F32)
                cs_t = k1.sb(f"cs_t{b}", [128, 2, NT, 8], F32)
                s_ld = k1.sem(f"a1_ld{b}")
                s_w = k1.sem(f"a1_w{b}")

                k1.inc(POOL.dma_start(out=wsec[:], in_=w_in[:, 1536:2560].rearrange("(k p) c -> p k c", p=128)), s_w, 16)
                tok_w = (s_w, s_w.n)
                s_ldq = [k1.sem(f"a1_ldq{b}_{q}") for q in range(4)]
                tok_xq = []
                for q in range(4):
                    tok_xq.append(k1.inc(POOL.dma_start(out=xT_sb[:, :, q * 512:(q + 1) * 512],
                                                        in_=xT[b, :, q * 512:(q + 1) * 512].rearrange("(k p) t -> p k t", p=128)), s_ldq[q], 16))
                s_pos = k1.sem(f"a1_pos{b}")
                tok_pos = k1.inc(SP.dma_start(out=pos_i[:], in_=posT[b]), s_pos, 16)
                wq_sb = [k1.sb(f"wq_sb{b}_{i}", [128, 8, 384], BF16) for i in range(2)]
                s_ws = k1.sem(f"a1_ws{b}")
                tok_wq = {}

                def load_wq(c):
                    for j, c0 in enumerate((c * 128, 512 + c * 128, 1024 + c * 128)):
                        t_ = k1.inc(POOL.dma_start(out=wq_sb[c % 2][:, :, j * 128:(j + 1) * 128],
                                                   in_=w_in[:, c0:c0 + 128].rearrange("(k p) c -> p k c", p=128)), s_ws, 16)
                    tok_wq[c] = t_

                load_wq(0)

                k1.wait(DVE, tok_pos)
                DVE.tensor_copy(out=pos_f[:], in_=pos_i[:])
                DVE.tensor_tensor(out=ang[:, 1], in0=invf[:], in1=pos_f[:].unsqueeze(2).broadcast_to([128, NT, 8]), op=ALU.mult)
                DVE.tensor_scalar(out=ang[:, 0], in0=ang[:, 1], scalar1=float(np.pi / 2), scalar2=None, op0=ALU.add)
                DVE.tensor_scalar(out=kk_f[:], in0=ang[:], scalar1=float(1.0 / TWO_PI), scalar2=None, op0=ALU.mult)
                DVE.tensor_copy(out=kk_i[:], in_=kk_f[:])
                DVE.tensor_copy(out=kk_f[:], in_=kk_i[:])
                DVE.scalar_tensor_tensor(out=rr[:], in0=kk_f[:], scalar=-TWO_PI, in1=ang[:], op0=ALU.mult, op1=ALU.add)
                DVE.tensor_scalar(out=mm[:], in0=rr[:], scalar1=float(np.pi), scalar2=None, op0=ALU.is_gt)
                DVE.scalar_tensor_tensor(out=rr[:], in0=mm[:], scalar=-TWO_PI, in1=rr[:], op0=ALU.mult, op1=ALU.add)
                DVE.tensor_scalar(out=mm[:], in0=rr[:], scalar1=float(-np.pi), scalar2=None, op0=ALU.is_lt)
                i_l = DVE.scalar_tensor_tensor(out=rr[:], in0=mm[:], scalar=TWO_PI, in1=rr[:], op0=ALU.mult, op1=ALU.add)
                k1.wait(ACT, M(DVE, i_l))
                i_l = ACT.activation(out=cs_t[:], in_=rr[:], func=AF.Sin)
                tok_cs = M(ACT, i_l)
                stop_if("rope")

                with contextlib.ExitStack() as es2:
                    k2 = KB(nc, es2, kb.waited, kb.prog)
                    gu_sb = [k2.sb(f"gu_sb{b}_{i}", [128, 512], F32) for i in range(2)]
                    gv_sb = [k2.sb(f"gv_sb{b}_{i}", [128, 512], F32) for i in range(2)]
                    n_sb = [k2.sb(f"n_sb{b}_{i}", [128, 512], BF16) for i in range(2)]
                    t1_sb = k2.sb(f"t1_sb{b}", [128, 512], F32)
                    sg_sb = [k2.sb(f"sg_sb{b}_{i}", [128, 512], BF16) for i in range(2)]
                    st_sb = k2.sb(f"st_sb{b}", [128, 6], F32)
                    mv_sb = k2.sb(f"mv_sb{b}", [128, 2], F32)
                    rstd_sb = k2.sb(f"rstd_sb{b}", [128, 1], F32)

                    k2.wait(PE, tok_w)

                    def sgu_pe_front(n):
                        k2.wait(PE, tok_xq[n // 4])
                        su = ch_u.pslot(PE)
                        for k in range(8):
                            i = PE.matmul(banks[0 + su][:], xT_sb[:, k, n * 128:(n + 1) * 128], wsec[:, k, 0:512],
                                          start=(k == 0), stop=(k == 7))
                        ch_u.produced(M(PE, i))
                        sv = ch_v.pslot(PE)
                        for k in range(8):
                            i = PE.matmul(banks[2 + sv][:], xT_sb[:, k, n * 128:(n + 1) * 128], wsec[:, k, 512:1024],
                                          start=(k == 0), stop=(k == 7))
                        ch_v.produced(M(PE, i))

                    def sgu_act(n):
                        su = ch_u.cslot(ACT)
                        sg = ch_gu.pslot(ACT)
                        t = M(ACT, ACT.activation(out=gu_sb[sg][:], in_=banks[0 + su][:], func=AF.Gelu))
                        ch_u.consumed(t)
                        ch_gu.produced(t)
                        sv = ch_v.cslot(ACT)
                        sg = ch_gv.pslot(ACT)
                        t = M(ACT, ACT.activation(out=gv_sb[sg][:], in_=banks[2 + sv][:], func=AF.Gelu))
                        ch_v.consumed(t)
                        ch_gv.produced(t)

                    def sgu_dve_norm(n):
                        sg = ch_gv.cslot(DVE)
                        DVE.bn_stats(out=st_sb[:], in_=gv_sb[sg][:])
                        i = DVE.bn_aggr(out=mv_sb[:], in_=st_sb[:])
                        rstd_via_pool(mv_sb, rstd_sb[:], i)
                        sn = ch_n.pslot(DVE)
                        t = M(DVE, DVE.tensor_scalar(out=n_sb[sn][:], in0=gv_sb[sg][:], scalar1=mv_sb[:, 0:1], scalar2=rstd_sb[:, 0:1],
                                                     op0=ALU.subtract, op1=ALU.mult))
                        ch_gv.consumed(t)
                        ch_n.produced(t)

                    def sgu_pe_z(n):
                        sn = ch_n.cslot(PE)
                        sz = ch_z.pslot(PE)
                        for g in range(8):
                            i = PE.matmul(banks[4 + sz][:, g * 64:(g + 1) * 64], wmT[:, g, :], n_sb[sn][:, g * 64:(g + 1) * 64],
                                          start=True, stop=True, skip_group_check=True)
                        t = M(PE, i)
                        ch_n.consumed(t)
                        ch_z.produced(t)

                    def sgu_dve_out(n):
                        sz = ch_z.cslot(DVE)
                        t = M(DVE, DVE.tensor_tensor(out=t1_sb[:], in0=banks[4 + sz][:], in1=sg_bc[:], op=ALU.mult))
                        ch_z.consumed(t)
                        DVE.tensor_tensor(out=t1_sb[:], in0=t1_sb[:], in1=Bp[:], op=ALU.add)
                        sgu_ = ch_gu.cslot(DVE)
                        so = ch_sg.pslot(DVE)
                        t = M(DVE, DVE.tensor_tensor(out=sg_sb[so][:], in0=t1_sb[:], in1=gu_sb[sgu_][:], op=ALU.mult))
                        ch_gu.consumed(t)
                        ch_sg.produced(t)

                    def sgu_pe_tp(n):
                        so = ch_sg.cslot(PE)
                        st = ch_tp.pslot(PE)
                        tpv = banks[6 + st][:].bitcast(BF16)
                        for j in range(4):
                            i = PE.transpose(tpv[:, j * 128:(j + 1) * 128], sg_sb[so][:, j * 128:(j + 1) * 128], ident[:])
                        t = M(PE, i)
                        ch_sg.consumed(t)
                        ch_tp.produced(t)

                    def sgu_act_tp(n):
                        st = ch_tp.cslot(ACT)
                        tpv = banks[6 + st][:].bitcast(BF16)
                        i = ACT.activation(out=catT[:, 4:8, n * 128:(n + 1) * 128],
                                           in_=tpv[:, 0:512].rearrange("p (j t) -> p j t", j=4), func=AF.Copy)
                        ch_tp.consumed(M(ACT, i))

                    for n in range(NT + 2):
                        if n < NT:
                            sgu_pe_front(n)
                            sgu_act(n)
                            sgu_dve_norm(n)
                        if 1 <= n <= NT:
                            sgu_pe_z(n - 1)
                            sgu_dve_out(n - 1)
                        if 2 <= n:
                            sgu_pe_tp(n - 2)
                            sgu_act_tp(n - 2)
                    barrier()
                stop_if("sgu")

                with contextlib.ExitStack() as es3:
                    k3 = KB(nc, es3, kb.waited, kb.prog)
                    QTz = k3.sb(f"QTz{b}", [128, 2, S], BF16)
                    KT = k3.sb(f"KT{b}", [128, S], BF16)
                    Vz = k3.sb(f"Vz{b}", [128, 48, 2, 128], BF16)
                    qs_sb = [k3.sb(f"qs_sb{b}_{i}", [128, 4, 256], BF16) for i in range(2)]
                    ra = k3.sb(f"ra{b}", [128, 4, 4, 8], F32)
                    rb = k3.sb(f"rb{b}", [128, 4, 4, 8], F32)
                    rot_sb = [k3.sb(f"rot_sb{b}_{i}", [128, 4, 2, 2, 16], F32) for i in range(2)]
                    p_sb = [k3.sb(f"p_sb{b}_{i}", [128, 512], BF16) for i in range(6)]
                    rl_sb = k3.sb(f"rl_sb{b}", [128, 512], F32)
                    if b == 0:
                        for r0 in range(0, NJ * 256, 128):
                            kb.inc(SP.dma_start(out=xs_d[r0:r0 + 128, :], in_=zrow), s_zf, 16)
                        zf_state["tok"] = (s_zf, s_zf.n)
                    tok_zero = M(POOL, POOL.memset(QTz[:], 0.0))
                    tok_one = M(DVE, DVE.memset(Vz[:], 1.0))
                    for e in (ACT, DVE, PE):
                        k3.wait(e, tok_zero)
                        k3.wait(e, tok_one)
                    for q in range(4):
                        k3.wait(PE, tok_xq[q])
                    stop_if("att_ms")

                    def tv_blk(T, j):
                        return T[:, j * 128:(j + 1) * 128]

                    def tv_p2(T, s, r4):
                        return T[:, 512 * s:512 * (s + 1)].rearrange("p (i r) -> p r i", r=4)[:, r4, :]

                    def tv_p3k(T, r):
                        return T.rearrange("p (l r) -> p r l", r=16)[:, r, :]

                    def tv_p3q(T, s, r):
                        return T.rearrange("p (l r) -> p r l", r=16)[:, r, 32 * s:32 * (s + 1)]

                    vdefs = []
                    for j in range(16):
                        vdefs.append(lambda T, j=j: tv_blk(T, j))
                    for s in range(4):
                        for r4 in range(4):
                            vdefs.append(lambda T, s=s, r4=r4: tv_p2(T, s, r4))
                    for r in range(16):
                        vdefs.append(lambda T, r=r: tv_p3k(T, r))

                    for c in range(4):
                        wq = wq_sb[c % 2]
                        k3.wait(PE, tok_wq[c])
                        k3.wait(DVE, tok_cs)
                        stop_if("att_dma")

                        def qk_pe(gi):
                            sq = ch_qk.pslot(PE)
                            for w in range(2):
                                for tt in range(4):
                                    n = gi * 4 + tt
                                    for k in range(8):
                                        i = PE.matmul(banks[2 * sq + w][:, tt * 128:(tt + 1) * 128],
                                                      xT_sb[:, k, n * 128:(n + 1) * 128], wq[:, k, w * 128:(w + 1) * 128],
                                                      start=(tt == 0 and k == 0), stop=(k == 7), skip_group_check=True)
                            ch_qk.produced(M(PE, i))

                        def qk_evac(gi):
                            sq = ch_qk.cslot(ACT, 0)
                            so = ch_qs.pslot(ACT, DVE)
                            sr = ch_rot.pslot(ACT)
                            qv = banks[2 * sq + 0][:].rearrange("p (t h e) -> p t h e", t=4, h=2)
                            kv = banks[2 * sq + 1][:].rearrange("p (t h e) -> p t h e", t=4, h=2)
                            ov = qs_sb[so][:].rearrange("p t (w h e) -> p t w h e", w=2, h=2)
                            ACT.activation(out=ov[:, :, 0, :, 16:64], in_=qv[:, :, :, 16:64], func=AF.Copy)
                            ACT.activation(out=ov[:, :, 1, :, 16:64], in_=kv[:, :, :, 16:64], func=AF.Copy)
                            ACT.activation(out=rot_sb[sr][:, :, 0, :, :], in_=qv[:, :, :, 0:16], func=AF.Copy)
                            ta = M(ACT, ACT.activation(out=rot_sb[sr][:, :, 1, :, :], in_=kv[:, :, :, 0:16], func=AF.Copy))
                            ch_qk.consumed(ta, 0)
                            ch_rot.produced(ta)
                            sr = ch_rot.cslot(DVE)
                            rv = rot_sb[sr][:].rearrange("p t w h e -> p t (w h) e")
                            t1 = rv[:, :, :, 0:8]
                            t2 = rv[:, :, :, 8:16]
                            og = qs_sb[so][:].rearrange("p t (g e) -> p t g e", g=4)
                            cosb = cs_t[:, 0, gi * 4:(gi + 1) * 4, :].unsqueeze(2).broadcast_to([128, 4, 4, 8])
                            sinb = cs_t[:, 1, gi * 4:(gi + 1) * 4, :].unsqueeze(2).broadcast_to([128, 4, 4, 8])
                            DVE.tensor_tensor(out=ra[:], in0=t1, in1=cosb, op=ALU.mult)
                            DVE.tensor_tensor(out=rb[:], in0=t2, in1=sinb, op=ALU.mult)
                            DVE.tensor_tensor(out=og[:, :, :, 0:8], in0=ra[:], in1=rb[:], op=ALU.subtract)
                            DVE.tensor_tensor(out=ra[:], in0=t2, in1=cosb, op=ALU.mult)
                            DVE.tensor_tensor(out=rb[:], in0=t1, in1=sinb, op=ALU.mult)
                            td = M(DVE, DVE.tensor_tensor(out=og[:, :, :, 8:16], in0=ra[:], in1=rb[:], op=ALU.add))
                            ch_rot.consumed(td)
                            ch_qs.produced(ta, td)

                        def qk_tp(gi):
                            so = ch_qs.cslot(PE)
                            st = ch_tp.pslot(PE)
                            tpv = banks[6 + st][:].bitcast(BF16).rearrange("p (t w e) -> p t w e", t=4, w=2)
                            for tt in range(4):
                                for w in range(2):
                                    i = PE.transpose(tpv[:, tt, w, :], qs_sb[so][:, tt, w * 128:(w + 1) * 128], ident[:])
                            t = M(PE, i)
                            ch_qs.consumed(t)
                            ch_tp.produced(t)

                        def qk_tp_evac(gi):
                            st = ch_tp.cslot(ACT)
                            tpv = banks[6 + st][:].bitcast(BF16).rearrange("p (t w e) -> p t w e", t=4, w=2)
                            cols = slice(gi * 512, (gi + 1) * 512)
                            ACT.activation(out=QTz[0:64, 0, cols].rearrange("p (t e) -> p t e", t=4), in_=tpv[0:64, :, 0, :], func=AF.Copy)
                            ACT.activation(out=QTz[64:128, 1, cols].rearrange("p (t e) -> p t e", t=4), in_=tpv[64:128, :, 0, :], func=AF.Copy)
                            i = ACT.activation(out=KT[:, cols].rearrange("p (t e) -> p t e", t=4), in_=tpv[:, :, 1, :], func=AF.Copy)
                            ch_tp.consumed(M(ACT, i))

                        for gi in range(4 + 2):
                            if gi < 4:
                                qk_pe(gi)
                                stop_if("qk_pe0")
                                qk_evac(gi)
                                stop_if("qk_ev0")
                            if 1 <= gi <= 4:
                                qk_tp(gi - 1)
                                stop_if("qk_tp0")
                            if 2 <= gi:
                                qk_tp_evac(gi - 2)
                                stop_if("qk_te0")
                        stop_if("qk")

                        for gi in range(12):
                            sv = ch_vp.pslot(PE)
                            for tt in range(4):
                                vd = vdefs[gi * 4 + tt]
                                for k in range(8):
                                    i = PE.matmul(banks[4 + sv][:, tt * 128:(tt + 1) * 128], vd(xT_sb[:, k, :]), wq[:, k, 256:384],
                                                  start=(tt == 0 and k == 0), stop=(k == 7), skip_group_check=True)
                            ch_vp.produced(M(PE, i))
                            eng = ACT if gi % 2 == 0 else DVE
                            sv = ch_vp.cslot(eng)
                            src = banks[4 + sv][:].rearrange("p (t h e) -> p t h e", t=4, h=2)
                            dst = Vz[:, gi * 4:(gi + 1) * 4, :, :]
                            if eng is ACT:
                                ACT.activation(out=dst[:, :, 0, 0:64], in_=src[:, :, 0, :], func=AF.Copy)
                                i = ACT.activation(out=dst[:, :, 1, 64:128], in_=src[:, :, 1, :], func=AF.Copy)
                            else:
                                DVE.tensor_copy(out=dst[:, :, 0, 0:64], in_=src[:, :, 0, :])
                                i = DVE.tensor_copy(out=dst[:, :, 1, 64:128], in_=src[:, :, 1, :])
                            ch_vp.consumed(M(eng, i))
                        barrier()
                        stop_if("v")
                        if c + 1 < 4:
                            load_wq(c + 1)

                        V1 = lambda j, hh: Vz[:, j, hh, :]
                        V2 = lambda s, r4, hh: Vz[:, 16 + 4 * s + r4, hh, :]
                        V3 = lambda r, hh: Vz[:, 32 + r, hh, :]

                        def emit_s(maskt, mms, lo=0, hi=512):
                            ss = ch_s.pslot(PE)
                            PE.matmul(banks[ss][:], ident[:], maskt, start=True, stop=False, skip_group_check=True)
                            for (oc, lhsT, rhs) in mms:
                                i = PE.matmul(banks[ss][:, oc[0]:oc[1]], lhsT, rhs, start=False, stop=True, skip_group_check=True)
                            ch_s.produced(M(PE, i))
                            ss2 = ch_s.cslot(ACT)
                            sp = ch_p.pslot(ACT)
                            t = M(ACT, ACT.activation(out=p_sb[sp][:, lo:hi], in_=banks[ss2][:, lo:hi], func=AF.Exp, scale=0.125))
                            ch_s.consumed(t)
                            ch_p.produced(t)
                            return sp

                        def oview(Bk, oc):
                            if oc[0] == "blk":
                                return Bk[:, oc[1] * 128:(oc[1] + 1) * 128]
                            if oc[0] == "p2":
                                return Bk[:].rearrange("p (i r) -> p r i", r=4)[:, oc[1], :]
                            return Bk[:].rearrange("p (i r) -> p r i", r=16)[:, oc[1], :]

                        for s in range(4):
                            so_ = ch_o.pslot(PE)
                            Ob = banks[4 + 2 * so_]
                            Lb = banks[5 + 2 * so_]
                            first_pv = [True, True]
                            for hh in range(2):
                                Q = QTz[:, hh, :]
                                pend = []
                                mms = [((j * 128, (j + 1) * 128), tv_blk(KT, 4 * s + j), tv_blk(Q, 4 * s + j)) for j in range(4)]
                                sp = emit_s(m_cur[:], mms)
                                pend.append((sp, [(V1(4 * s + j, hh), (j * 128, (j + 1) * 128), ("blk", j)) for j in range(4)]))
                                js = [j for j in range(4) if 4 * s + j >= 1]
                                mms = [((j * 128, (j + 1) * 128), tv_blk(KT, 4 * s + j - 1), tv_blk(Q, 4 * s + j)) for j in js]
                                sp = emit_s(m_prev[:], mms, lo=js[0] * 128)
                                pend.append((sp, [(V1(4 * s + j - 1, hh), (j * 128, (j + 1) * 128), ("blk", j)) for j in js]))
                                mms = [((r4 * 128, (r4 + 1) * 128), tv_p2(KT, s, r4), tv_p2(Q, s, r4)) for r4 in range(4)]
                                sp = emit_s(m_cur[:], mms)
                                pend.append((sp, [(V2(s, r4, hh), (r4 * 128, (r4 + 1) * 128), ("p2", r4)) for r4 in range(4)]))
                                if s >= 1:
                                    mms = [((r4 * 128, (r4 + 1) * 128), tv_p2(KT, s - 1, r4), tv_p2(Q, s, r4)) for r4 in range(4)]
                                    sp = emit_s(m_prev[:], mms)
                                    pend.append((sp, [(V2(s - 1, r4, hh), (r4 * 128, (r4 + 1) * 128), ("p2", r4)) for r4 in range(4)]))
                                mms = [((r * 32, (r + 1) * 32), tv_p3k(KT, r), tv_p3q(Q, s, r)) for r in range(16)]
                                sp = emit_s(m3[:, s, :], mms)
                                pend.append((sp, [(V3(r, hh), (r * 32, (r + 1) * 32), ("p3", r)) for r in range(16)]))

                                for (sp, pvs) in pend:
                                    sp2 = ch_p.cslot(PE)
                                    assert sp2 == sp
                                    for (vt, pc, oc) in pvs:
                                        i = PE.matmul(oview(Lb if hh else Ob, oc), vt, p_sb[sp][:, pc[0]:pc[1]], start=first_pv[hh], stop=False,
                                                      skip_group_check=True)
                                        first_pv[hh] = False
                                    t_last = M(PE, i)
                                    ch_p.consumed(t_last)
                            ch_o.produced(t_last)
                            so2 = ch_o.cslot(DVE)
                            B0, B1 = banks[4 + 2 * so2], banks[5 + 2 * so2]
                            DVE.reciprocal(out=rl_sb[0:64, :], in_=B0[64:128, :])
                            DVE.tensor_tensor(out=catT[0:64, c, 512 * s:512 * (s + 1)], in0=B0[0:64, :], in1=rl_sb[0:64, :], op=ALU.mult)
                            DVE.reciprocal(out=rl_sb[64:128, :], in_=B1[0:64, :])
                            i = DVE.tensor_tensor(out=catT[64:128, c, 512 * s:512 * (s + 1)], in0=B1[64:128, :], in1=rl_sb[64:128, :], op=ALU.mult)
                            ch_o.consumed(M(DVE, i))
                            stop_if("attn_s0")
                        barrier()
                        if DEBUG and b == DEBUG_B and STOP_AFTER == "attn":
                            with contextlib.ExitStack() as esd:
                                kd = KB(nc, esd, kb.waited, kb.prog)
                                dtmp = kd.sb("dtmpa", [128, 4, S], F32)
                                sd = kd.sem("dbga")
                                kd.wait(SP, M(DVE, DVE.tensor_copy(out=dtmp[:], in_=catT[:, 0:4, :])))
                                kd.wait(SP, kd.inc(SP.dma_start(out=dbg_cat[:, 0:4, :], in_=dtmp[:]), sd, 16))
                            stop_if("attn")

                if DEBUG and b == DEBUG_B:
                    with contextlib.ExitStack() as esd:
                        kd = KB(nc, esd, kb.waited, kb.prog)
                        dtmp = kd.sb("dtmp", [128, 8, S], F32)
                        sd = kd.sem("dbg1")
                        kd.wait(SP, M(DVE, DVE.tensor_copy(out=dtmp[:], in_=catT[:])))
                        kd.wait(SP, kd.inc(SP.dma_start(out=dbg_cat, in_=dtmp[:]), sd, 16))
                        barrier()

            esp = contextlib.ExitStack()
            kp = KB(nc, esp, kb.waited, kb.prog)
            hb_all = kp.sb(f"hb_all{b}", [128, NT, D], BF16)
            ybuf = kp.sb(f"ybuf{b}", [128, NT, D], F32)
            M1a = kp.sb(f"M1a{b}", [128, NT, 32], F32)
            M2a = kp.sb(f"M2a{b}", [128, NT, 32], F32)
            g12 = kp.sb(f"g12{b}", [128, 2, NT], F32)
            sl_i = kp.sb(f"sl_i{b}", [128, 2, NT], I32)
            with contextlib.ExitStack() as es4:
                k4 = KB(nc, es4, kb.waited, kb.prog)
                wo_sb = k4.sb(f"wo_sb{b}", [128, 8, D], BF16)
                x_sb = [k4.sb(f"x_sb{b}_{i}", [128, D], F32) for i in range(2)]
                hT_sb = [k4.sb(f"hT_sb{b}_{i}", [128, 8, 128], BF16) for i in range(2)]
                hf_sb = [k4.sb(f"hf_sb{b}_{i}", [128, D], F32) for i in range(2)]
                eps_t = k4.sb(f"eps_t{b}", [128, 1], F32)
                k4.wait(ACT, M(POOL, POOL.memset(eps_t[:], EPS)))
                math_done = [None]
                pend_D = []
                tD = {}
                NH = 8
                lg_all = k4.sb(f"lg_all{b}", [128, NH, 72], F32)
                rq = k4.sb(f"rq{b}", [128, NH, 64], F32)
                lo_sb = [k4.sb(f"lo_sb{b}_{i}", [128, D], BF16) for i in range(2)]
                loT_sb = [k4.sb(f"loT_sb{b}_{i}", [128, 8, 128], BF16) for i in range(2)]
                st2 = k4.sb(f"st2_{b}", [128, 2, 6], F32)
                mv2 = k4.sb(f"mv2_{b}", [128, 2], F32)
                rstd2 = k4.sb(f"rstd2_{b}", [128, 1], F32)
                s_wo = k4.sem(f"a2_wo{b}")
                g1_bc = k4.sb(f"g1_bc{b}", [128, D], F32)
                b1_bc = k4.sb(f"b1_bc{b}", [128, D], F32)
                s_g1 = k4.sem(f"a2_g1{b}")
                k4.inc(SP.dma_start(out=g1_bc[:], in_=ln1_g.partition_broadcast(128)), s_g1, 16)
                k4.wait(DVE, k4.inc(SP.dma_start(out=b1_bc[:], in_=ln1_b.partition_broadcast(128)), s_g1, 16))

                t_wo = k4.inc(POOL.dma_start(out=wo_sb[:], in_=w_out.rearrange("(k p) c -> p k c", p=128)), s_wo, 16)
                k4.wait(PE, t_wo)
                k4.wait(DVE, t_wo)

                def a2_load(n):
                    sx = ch_x.pslot(SP)
                    i = SP.dma_start(out=x_sb[sx][:], in_=xtm[b, n * 128:(n + 1) * 128, :])
                    ch_x.produced(ring_x.start(i, sx))

                def a2_pe_op(n):
                    so = ch_op.pslot(PE)
                    for hf in range(2):
                        for k in range(8):
                            i = PE.matmul(banks[2 * so + hf][:], catT[:, k, n * 128:(n + 1) * 128], wo_sb[:, k, hf * 512:(hf + 1) * 512],
                                          start=(k == 0), stop=(k == 7))
                    ch_op.produced(M(PE, i))

                def a2_dve_A(n):
                    so = ch_op.cslot(DVE)
                    sx = ch_x.cslot(DVE)
                    sh = n % 2
                    hf_ = hf_sb[sh]
                    if n >= 2:
                        kb.wait(DVE, tD[n - 2])
                    for hf in range(2):
                        i = DVE.scalar_tensor_tensor(out=hf_[:, hf * 512:(hf + 1) * 512], in0=x_sb[sx][:, hf * 512:(hf + 1) * 512],
                                                     scalar=ALPHA, in1=banks[2 * so + hf][:], op0=ALU.mult, op1=ALU.add)
                    t = M(DVE, i)
                    ch_op.consumed(t)
                    ch_x.consumed(t)
                    for hf in range(2):
                        DVE.bn_stats(out=st2[:, hf, :], in_=hf_[:, hf * 512:(hf + 1) * 512])
                    i = DVE.bn_aggr(out=mv2[:], in_=st2[:].rearrange("p a c -> p (a c)"))
                    kb.wait(ACT, M(DVE, i))
                    i = ACT.activation(out=rstd2[:], in_=mv2[:, 1:2], func=AF.Sqrt, bias=eps_t[:, 0:1], scale=1.0)
                    kb.wait(DVE, M(ACT, i))
                    DVE.reciprocal(out=rstd2[:], in_=rstd2[:])
                    DVE.tensor_scalar(out=hf_[:], in0=hf_[:], scalar1=mv2[:, 0:1], scalar2=rstd2[:, 0:1],
                                      op0=ALU.subtract, op1=ALU.mult)
                    DVE.tensor_tensor(out=hf_[:], in0=hf_[:], in1=g1_bc[:], op=ALU.mult)
                    t = M(DVE, DVE.tensor_tensor(out=hf_[:], in0=hf_[:], in1=b1_bc[:], op=ALU.add))
                    kb.wait(ACT, t)
                    ACT.activation(out=hb_all[:, n, :], in_=hf_[:], func=AF.Copy)
                    tC = M(ACT, ACT.activation(out=ybuf[:, n, :], in_=hf_[:], func=AF.Copy, scale=ALPHA))
                    pend_D.append((n, sh, tC))

                def a2_dve_D():
                    n, sh, tC = pend_D.pop(0)
                    kb.wait(POOL, tC)
                    sl_ = ch_hb.pslot(POOL)
                    t = M(POOL, POOL.tensor_tensor(out=lo_sb[sl_][:], in0=hf_sb[sh][:], in1=hb_all[:, n, :], op=ALU.subtract))
                    ch_hb.produced(t)
                    tD[n] = t

                def a2_pe_tp(n):
                    sh = ch_hb.cslot(PE)
                    st = ch_tp.pslot(PE)
                    tpv = banks[6 + st][:].bitcast(BF16)
                    for k in range(8):
                        i = PE.transpose(tpv[:, k * 128:(k + 1) * 128], hb_all[:, n, k * 128:(k + 1) * 128], ident[:])
                    ch_tp.produced(M(PE, i))
                    st = ch_tp.pslot(PE)
                    tpv = banks[6 + st][:].bitcast(BF16)
                    for k in range(8):
                        i = PE.transpose(tpv[:, k * 128:(k + 1) * 128], lo_sb[sh][:, k * 128:(k + 1) * 128], ident[:])
                    t = M(PE, i)
                    ch_hb.consumed(t)
                    ch_tp.produced(t)

                def a2_act_tp(n):
                    st = ch_tp.cslot(ACT)
                    tpv = banks[6 + st][:].bitcast(BF16)
                    sht = ch_ht.pslot(ACT)
                    i = ACT.activation(out=hT_sb[sht][:], in_=tpv.rearrange("p (k t) -> p k t", k=8), func=AF.Copy)
                    t_h = M(ACT, i)
                    ch_tp.consumed(t_h)
                    ch_ht.produced(t_h)
                    st = ch_tp.cslot(ACT)
                    tpv = banks[6 + st][:].bitcast(BF16)
                    sl = ch_lo.pslot(ACT)
                    i = ACT.activation(out=loT_sb[sl][:], in_=tpv.rearrange("p (k t) -> p k t", k=8), func=AF.Copy)
                    t = M(ACT, i)
                    ch_tp.consumed(t)
                    ch_lo.produced(t)
                    return t_h

                def a2_pe_route(n, t_h):
                    sl = ch_lo.cslot(PE)
                    sht = ch_ht.cslot(PE)
                    ch_r.pslot(PE)
                    rp = banks[4]
                    for k in range(8):
                        PE.matmul(rp[:, 0:72], hT_sb[sht][:, k, :], wr_hl[:, k, 0:72], start=(k == 0), stop=False,
                                  skip_group_check=True)
                    for k in range(8):
                        i = PE.matmul(rp[:, 0:36], loT_sb[sl][:, k, :], wr_hl[:, k, 0:36], start=False, stop=(k == 7),
                                      skip_group_check=True)
                    t = M(PE, i)
                    ch_lo.consumed(t)
                    ch_ht.consumed(t)
                    ch_r.produced(t)

                def a2_route_copy(n):
                    ch_r.cslot(ACT)
                    kb.wait(ACT, math_done[0])
                    t = M(ACT, ACT.activation(out=lg_all[:, n % NH, :], in_=banks[4][:, 0:72], func=AF.Copy))
                    ch_r.consumed(t)
                    return t

                def a2_route_math(n0, t_last):
                    kb.wait(DVE, t_last)
                    sl = slice(n0, n0 + NH)
                    L = lg_all[:, :, 0:36]
                    DVE.tensor_tensor(out=L, in0=L, in1=lg_all[:, :, 36:72], op=ALU.add)
                    DVE.tensor_tensor(out=L, in0=L, in1=br_bc[:].unsqueeze(1).broadcast_to([128, NH, 36]), op=ALU.add)
                    gmax, oh, ge, sume = rq[:, :, 0:1], rq[:, :, 1:5], rq[:, :, 5:9], rq[:, :, 9:10]
                    esel, m1, eq1, e2 = rq[:, :, 11:19], rq[:, :, 19:20], rq[:, :, 20:28], rq[:, :, 28:36]
                    m2, eq2, dd, w1, w2, tmp8 = rq[:, :, 36:37], rq[:, :, 37:45], rq[:, :, 45:46], rq[:, :, 46:47], rq[:, :, 47:48], rq[:, :, 48:56]
                    bc = lambda ap, k: ap.broadcast_to([128, NH, k])
                    DVE.tensor_reduce(out=gmax, in_=lg_all[:, :, 0:4], axis=AX.X, op=ALU.max)
                    DVE.tensor_tensor(out=oh, in0=lg_all[:, :, 0:4], in1=bc(gmax, 4), op=ALU.is_equal)
                    DVE.tensor_tensor(out=ge, in0=lg_all[:, :, 0:4], in1=bc(gmax, 4), op=ALU.subtract)
                    DVE.tensor_tensor(out=esel, in0=lg_all[:, :, 4:12], in1=bc(oh[:, :, 0:1], 8), op=ALU.mult)
                    for g in range(1, 4):
                        DVE.tensor_tensor(out=tmp8, in0=lg_all[:, :, 4 + 8 * g:12 + 8 * g], in1=bc(oh[:, :, g:g + 1], 8), op=ALU.mult)
                        DVE.tensor_tensor(out=esel, in0=esel, in1=tmp8, op=ALU.add)
                    DVE.tensor_reduce(out=m1, in_=esel, axis=AX.X, op=ALU.max)
                    DVE.tensor_tensor(out=eq1, in0=esel, in1=bc(m1, 8), op=ALU.is_equal)
                    DVE.scalar_tensor_tensor(out=e2, in0=eq1, scalar=-1e30, in1=esel, op0=ALU.mult, op1=ALU.add)
                    DVE.tensor_reduce(out=m2, in_=e2, axis=AX.X, op=ALU.max)
                    DVE.tensor_tensor(out=eq2, in0=e2, in1=bc(m2, 8), op=ALU.is_equal)
                    i = DVE.tensor_tensor(out=dd, in0=m2, in1=m1, op=ALU.subtract)
                    kb.wait(ACT, M(DVE, i))
                    ACT.activation(out=ge, in_=ge, func=AF.Exp)
                    i = ACT.activation(out=dd, in_=dd, func=AF.Exp)
                    kb.wait(DVE, M(ACT, i))
                    DVE.tensor_reduce(out=sume, in_=ge, axis=AX.X, op=ALU.add)
                    DVE.reciprocal(out=sume, in_=sume)
                    DVE.tensor_scalar(out=w1, in0=dd, scalar1=1.0, scalar2=None, op0=ALU.add)
                    DVE.reciprocal(out=w1, in_=w1)
                    DVE.tensor_tensor(out=w2, in0=dd, in1=w1, op=ALU.mult)
                    DVE.tensor_tensor(out=g12[:, 0, sl].unsqueeze(2), in0=w1, in1=sume, op=ALU.mult)
                    DVE.tensor_tensor(out=g12[:, 1, sl].unsqueeze(2), in0=w2, in1=sume, op=ALU.mult)
                    for g in range(4):
                        DVE.tensor_tensor(out=M1a[:, sl, g * 8:(g + 1) * 8], in0=eq1, in1=bc(oh[:, :, g:g + 1], 8), op=ALU.mult)
                        tm = DVE.tensor_tensor(out=M2a[:, sl, g * 8:(g + 1) * 8], in0=eq2, in1=bc(oh[:, :, g:g + 1], 8), op=ALU.mult)
                    math_done[0] = M(DVE, tm)

                a2_load(0)
                t_hs = {}
                for n in range(NT + 3):
                    if n + 1 < NT:
                        a2_load(n + 1)
                    if n < NT:
                        a2_pe_op(n)
                        a2_dve_A(n)
                    if 1 <= n <= NT:
                        a2_dve_D()
                    if 2 <= n <= NT + 1:
                        a2_pe_tp(n - 2)
                        t_hs[n - 2] = a2_act_tp(n - 2)
                    if 3 <= n:
                        a2_pe_route(n - 3, t_hs[n - 3])
                        t_c = a2_route_copy(n - 3)
                        if (n - 3) % NH == NH - 1:
                            a2_route_math(n - 3 - (NH - 1), t_c)
                barrier()

            if DEBUG and b == DEBUG_B:
                with contextlib.ExitStack() as esd:
                    kd = KB(nc, esd, kb.waited, kb.prog)
                    sd = kd.sem("dbg2")
                    kd.inc(SP.dma_start(out=dbg_y, in_=ybuf[:]), sd, 16)
                    kd.wait(SP, kd.inc(SP.dma_start(out=dbg_gate, in_=M1a[:]), sd, 16))
                    barrier()
            if not DEBUG or b == DEBUG_B:
                stop_if("A2")

            with contextlib.ExitStack() as es5:
                k5 = KB(nc, es5, kb.waited, kb.prog)
                NWS = 3
                wgu_sb = [catT[:, 2 * i:2 * i + 2, :].rearrange("p a (k f) -> p (a k) f", f=512) for i in range(3)]
                wdn_t = [k5.sb(f"wdn_t{b}_{i}", [128, 2, D], BF16) for i in range(2)]
                wdn_sb = [t_[:] for t_ in wdn_t] + [catT[:, 6 + i, :].rearrange("p (k f) -> p k f", f=1024) for i in range(2)]
                thr = k5.sb(f"thr{b}", [128, NJ, 32], F32)
                Mb = k5.sb(f"Mb{b}", [128, NT * 32], BF16)
                Ms = k5.sb(f"Ms{b}", [128, NT, 32], F32)
                cs = k5.sb(f"cs{b}", [128, NT, 32], F32)
                off = k5.sb(f"off{b}", [128, NT, 32], F32)
                Sf = k5.sb(f"Sf{b}", [128, NT, 32], F32)
                tS = Ms
                sc_a = k5.sb(f"sc_a{b}", [128, 32], F32)
                sc_b = k5.sb(f"sc_b{b}", [128, 32], F32)
                pt = k5.sb(f"pt{b}", [128, 32], F32)
                base = k5.sb(f"base{b}", [128, 32], F32)
                qi = k5.sb(f"qi{b}", [128, 32], I32)
                sl_f = k5.sb(f"sl_f{b}", [128, 2, NT], F32)
                ej_f = k5.sb(f"ej_f{b}", [128, NJ], F32)
                wi_f = thr[:].rearrange("p j e -> p (j e)")[:, 0:NJ * 3].rearrange("p (j c) -> p j c", c=3)
                wi_i = k5.sb(f"wi_i{b}", [128, NJ, 3], I32)
                pidx = k5.sb(f"pidx{b}", [128, 1], F32)
                ko = k5.sb(f"ko{b}", [128, 8], F32)
                Lst = k5.sb(f"Lst{b}", [128, 128], BF16)
                xt_sb = [k5.sb(f"xt_sb{b}_{i}", [128, 2, D], BF16) for i in range(2)]
                xsT = [k5.sb(f"xsT{b}_{i}", [128, 8, 256], BF16) for i in range(2)]
                sl_sb = [k5.sb(f"sl_sb{b}_{i}", [128, 512], F32) for i in range(2)]
                at_sb = [k5.sb(f"at_sb{b}_{i}", [128, 2, 256], BF16) for i in range(3)]
                yo_sb = [k5.sb(f"yo_sb{b}_{i}", [128, D], F32) for i in range(2)]

                POOL.affine_select(out=Lst[:], in_=ones_bf[:], pattern=[[1, 128]], compare_op=ALU.is_gt,
                                   fill=0.0, base=0, channel_multiplier=-1)
                t_thr = M(POOL, POOL.iota(thr[:], pattern=[[256, NJ], [0, 32]], base=0, channel_multiplier=0,
                                          allow_small_or_imprecise_dtypes=True))
                POOL.iota(pidx[:], pattern=[[0, 1]], base=0, channel_multiplier=1, allow_small_or_imprecise_dtypes=True)
                t_io = M(POOL, POOL.iota(ko[:], pattern=[[128, 8]], base=0, channel_multiplier=0, allow_small_or_imprecise_dtypes=True))
                DVE.tensor_tensor(out=Ms[:], in0=M1a[:], in1=M2a[:], op=ALU.add)
                t = M(DVE, DVE.tensor_copy(out=Mb[:], in_=Ms[:].rearrange("p n e -> p (n e)")))
                kb.wait(PE, t)
                kb.wait(PE, t_thr)
                PE.matmul(banks[0][:], Lst[:], Mb[:], start=True, stop=True)
                t = M(PE, PE.matmul(banks[1][:], ones_bf[:], Mb[:], start=True, stop=True))
                kb.wait(DVE, t)
                DVE.tensor_copy(out=cs[:], in_=banks[1][:].rearrange("p (n e) -> p n e", e=32))
                DVE.memset(off[:, 0, :], 0.0)
                for n in range(1, NT):
                    DVE.tensor_tensor(out=off[:, n, :], in0=off[:, n - 1, :], in1=cs[:, n - 1, :], op=ALU.add)
                DVE.tensor_tensor(out=sc_a[:], in0=off[:, NT - 1, :], in1=cs[:, NT - 1, :], op=ALU.add)
                DVE.tensor_scalar(out=sc_b[:], in0=sc_a[:], scalar1=127.5, scalar2=1.0 / 256.0, op0=ALU.add, op1=ALU.mult)
                DVE.tensor_copy(out=qi[:], in_=sc_b[:])
                DVE.tensor_scalar(out=pt[:], in0=qi[:], scalar1=256.0, scalar2=None, op0=ALU.mult)
                DVE.tensor_copy(out=sc_a[:], in_=pt[:])
                pa, pb = sc_a, sc_b
                for sh in (1, 2, 4, 8, 16):
                    DVE.tensor_copy(out=pb[:, 0:sh], in_=pa[:, 0:sh])
                    DVE.tensor_tensor(out=pb[:, sh:32], in0=pa[:, sh:32], in1=pa[:, 0:32 - sh], op=ALU.add)
                    pa, pb = pb, pa
                incl = pa
                DVE.tensor_tensor(out=base[:], in0=incl[:], in1=pt[:], op=ALU.subtract)
                DVE.tensor_tensor(out=Sf[:], in0=banks[0][:].rearrange("p (n e) -> p n e", e=32), in1=off[:], op=ALU.add)
                DVE.tensor_tensor(out=Sf[:], in0=Sf[:], in1=base[:].unsqueeze(1).broadcast_to([128, NT, 32]), op=ALU.add)
                DVE.tensor_tensor(out=tS[:], in0=Sf[:], in1=M1a[:], op=ALU.mult)
                DVE.tensor_reduce(out=sl_f[:, 0, :], in_=tS[:], axis=AX.X, op=ALU.add)
                DVE.tensor_tensor(out=tS[:], in0=Sf[:], in1=M2a[:], op=ALU.mult)
                DVE.tensor_reduce(out=sl_f[:, 1, :], in_=tS[:], axis=AX.X, op=ALU.add)
                DVE.tensor_copy(out=sl_i[:], in_=sl_f[:])
                kb.wait(DVE, t_thr)
                DVE.tensor_tensor(out=thr[:], in0=thr[:], in1=incl[:].unsqueeze(1).broadcast_to([128, NJ, 32]), op=ALU.is_ge)
                DVE.tensor_reduce(out=ej_f[:], in_=thr[:], axis=AX.X, op=ALU.add)
                kb.wait(DVE, t_io)
                DVE.tensor_scalar(out=ej_f[:], in0=ej_f[:], scalar1=256.0, scalar2=pidx[:, 0:1], op0=ALU.mult, op1=ALU.add)
                DVE.tensor_tensor(out=wi_f[:, :, 0:2], in0=ej_f[:].unsqueeze(2).broadcast_to([128, NJ, 2]),
                                  in1=ko[:, 0:2].unsqueeze(1).broadcast_to([128, NJ, 2]), op=ALU.add)
                DVE.tensor_scalar(out=ej_f[:], in0=ej_f[:], scalar1=pidx[:, 0:1], scalar2=0.5, op0=ALU.subtract, op1=ALU.mult)
                DVE.tensor_scalar(out=wi_f[:, :, 2:3], in0=ej_f[:].unsqueeze(2), scalar1=pidx[:, 0:1], scalar2=None, op0=ALU.add)
                t_disp = M(DVE, DVE.tensor_copy(out=wi_i[:], in_=wi_f))

                kb.wait(POOL, t_disp)
                kb.wait(POOL, zf_state["tok"])
                for n in range(NT):
                    for kk in range(2):
                        i = POOL.indirect_dma_start(out=xs_d, out_offset=bass.IndirectOffsetOnAxis(ap=sl_i[:, kk, n:n + 1], axis=0),
                                                    in_=hb_all[:, n, :], in_offset=None)
                        kb.inc(i, s_sc, 16)
                tok_sc = (s_sc, s_sc.n)
                kb.wait(SP, tok_sc)
                kb.wait(POOL, tok_sc)

                wgu_rows = w_gu.rearrange("e (d4 i) f -> (e d4) (i f)", i=4)
                wdn_rows = w_dn.rearrange("e (f2 i) d -> (e f2) (i d)", i=2)

                def m_load_w(j):
                    sg_ = ch_wg.pslot(POOL)
                    gv = wgu_sb[sg_].rearrange("p (k4 i) f -> p k4 (i f)", i=4)
                    for k4 in range(2):
                        i = POOL.indirect_dma_start(out=gv[:, k4, :], out_offset=None, in_=wgu_rows,
                                                    in_offset=bass.IndirectOffsetOnAxis(ap=wi_i[:, j, k4:k4 + 1], axis=0),
                                                    bounds_check=rb_gu, oob_is_err=False)
                        t1 = ring_wg.start(i, sg_)
                    ch_wg.produced(t1)
                    sd_ = ch_wd.pslot(POOL)
                    i = POOL.indirect_dma_start(out=wdn_sb[sd_].rearrange("p i d -> p (i d)"), out_offset=None, in_=wdn_rows,
                                                in_offset=bass.IndirectOffsetOnAxis(ap=wi_i[:, j, 2:3], axis=0),
                                                bounds_check=rb_dn, oob_is_err=False)
                    ch_wd.produced(ring_wd.start(i, sd_))

                def m_load_x(j):
                    sx = ch_xs.pslot(SP)
                    i = SP.dma_start(out=xt_sb[sx][:], in_=xs_d[256 * j:256 * (j + 1), :].rearrange("(a p) d -> p a d", p=128))
                    ch_xs.produced(ring_xs.start(i, sx))

                def m_tp(j):
                    sx = ch_xs.cslot(PE)
                    ch_tp.pslot(PE)
                    ch_tp.pslot(PE)
                    for half in range(2):
                        tpv = banks[6 + half][:].bitcast(BF16).rearrange("p (k a t) -> p k a t", k=4, a=2)
                        for k4 in range(4):
                            for a_ in range(2):
                                k = half * 4 + k4
                                c0 = 512 * (k // 4) + (k % 4)
                                i = PE.transpose(tpv[:, k4, a_, :], xt_sb[sx][:, a_, c0:c0 + 509:4], ident[:])
                    t = M(PE, i)
                    ch_xs.consumed(t)
                    ch_tp.produced(t)
                    ch_tp.produced(t)
                    sT = ch_xT.pslot(ACT, DVE)
                    ch_tp.cslot(ACT)
                    ta = M(ACT, ACT.activation(out=xsT[sT][:, 0:4, :], in_=banks[6][:].bitcast(BF16).rearrange("p (k t) -> p k t", k=4), func=AF.Copy))
                    ch_tp.consumed(ta)
                    ch_tp.cslot(DVE)
                    td = M(DVE, DVE.tensor_copy(out=xsT[sT][:, 4:8, :], in_=banks[7][:].bitcast(BF16).rearrange("p (k t) -> p k t", k=4)))
                    ch_tp.consumed(td)
                    ch_xT.produced(ta, td)

                def m_gu(j):
                    sw = j % 3
                    sT = ch_xT.cslot(PE)
                    for tok in ch_wg.ready[j]:
                        kb.wait(PE, tok)
                    sg = ch_g2.pslot(PE)
                    for part in range(2):
                        for fcp in range(2):
                            fc = part * 256 + fcp * 128
                            for k in range(8):
                                i = PE.matmul(banks[2 * sg + part][:, fcp * 256:(fcp + 1) * 256], wgu_sb[sw][:, k, fc:fc + 128], xsT[sT][:, k, :],
                                              start=(fcp == 0 and k == 0), stop=(k == 7), skip_group_check=True)
                    t = M(PE, i)
                    ch_xT.consumed(t)
                    ch_wg.consumed(t)
                    ch_g2.produced(t)
                    sg = ch_g2.cslot(ACT, 0)
                    ss = ch_sl.pslot(ACT)
                    t = M(ACT, ACT.activation(out=sl_sb[ss][:], in_=banks[2 * sg][:], func=AF.Silu))
                    ch_g2.consumed(t, 0)
                    ch_sl.produced(t)
                    sg = ch_g2.cslot(DVE, 1)
                    ss = ch_sl.cslot(DVE)
                    sa = ch_at.pslot(DVE)
                    t = M(DVE, DVE.tensor_tensor(out=at_sb[sa][:].rearrange("p c t -> p (c t)"), in0=banks[2 * sg + 1][:], in1=sl_sb[ss][:], op=ALU.mult))
                    ch_g2.consumed(t, 1)
                    ch_sl.consumed(t)
                    ch_at.produced(t)

                def m_dn(j):
                    sw = j % 4
                    sa = ch_at.cslot(PE)
                    for tok in ch_wd.ready[j]:
                        kb.wait(PE, tok)
                    for a_ in range(2):
                        ch_dn.pslot(PE)
                        for hf in range(2):
                            for fc in range(2):
                                i = PE.matmul(banks[4 + hf][:], at_sb[sa][:, fc, a_ * 128:(a_ + 1) * 128],
                                              wdn_sb[sw][:, fc, hf * 512:(hf + 1) * 512], start=(fc == 0), stop=(fc == 1))
                        t = M(PE, i)
                        ch_dn.produced(t)
                        so = ch_yo.pslot(ACT, DVE)
                        ch_dn.cslot(ACT, 0)
                        ta = M(ACT, ACT.activation(out=yo_sb[so][:, 0:512], in_=banks[4][:], func=AF.Copy))
                        ch_dn.consumed(ta, 0)
                        ch_dn.cslot(DVE, 1)
                        td = M(DVE, DVE.tensor_copy(out=yo_sb[so][:, 512:1024], in_=banks[5][:]))
                        ch_dn.consumed(td, 1)
                        ch_yo.produced(ta, td)
                        so = ch_yo.cslot(SP)
                        i = SP.dma_start(out=ys_d[256 * j + 128 * a_:256 * j + 128 * (a_ + 1), :], in_=yo_sb[so][:])
                        ch_yo.consumed(ring_ys.start(i, so))
                        last_ys[so] = (ring_ys.sems[so], ring_ys.sems[so].n)
                    ch_at.consumed(t)
                    ch_wd.consumed(t)

                last_ys = {}
                m_load_w(0)
                m_load_w(1)
                m_load_x(0)
                m_load_x(1)
                m_tp(0)
                for j in range(NJ):
                    if j + 2 < NJ:
                        m_load_x(j + 2)
                    if j + 1 < NJ:
                        m_tp(j + 1)
                    if j >= 2:
                        m_dn(j - 2)
                    if j + 2 < NJ:
                        m_load_w(j + 2)
                    m_gu(j)
                m_dn(NJ - 2)
                m_dn(NJ - 1)
                barrier()
                stop_if("M")

            with contextlib.ExitStack() as es6:
                k6 = KB(nc, es6, kb.waited, kb.prog)
                o_sb = [k6.sb(f"o_sb{b}_{i}", [128, D], F32) for i in range(2)]
                rg_sb = [k6.sb(f"rg_sb{b}_{i}", [128, D], F32) for i in range(4)]
                st3 = k6.sb(f"st3_{b}", [128, 2, 6], F32)
                mv3 = k6.sb(f"mv3_{b}", [128, 2], F32)
                rstd3 = k6.sb(f"rstd3_{b}", [128, 1], F32)
                eps3 = k6.sb(f"eps3_{b}", [128, 1], F32)
                g2_bc = k6.sb(f"g2_bc{b}", [128, D], F32)
                b2_bc = k6.sb(f"b2_bc{b}", [128, D], F32)
                s_g2 = k6.sem(f"f_g2{b}")
                k6.inc(SP.dma_start(out=g2_bc[:], in_=ln2_g.partition_broadcast(128)), s_g2, 16)
                k6.wait(DVE, k6.inc(SP.dma_start(out=b2_bc[:], in_=ln2_b.partition_broadcast(128)), s_g2, 16))
                k6.wait(ACT, M(POOL, POOL.memset(eps3[:], EPS)))
                for so, tk in last_ys.items():
                    kb.wait(POOL, tk)

                def f_gather(n):
                    for kk in range(2):
                        sr = ch_rg.pslot(POOL)
                        i = POOL.indirect_dma_start(out=rg_sb[sr][:], out_offset=None, in_=ys_d,
                                                    in_offset=bass.IndirectOffsetOnAxis(ap=sl_i[:, kk, n:n + 1], axis=0))
                        ch_rg.produced(ring_g4.start(i, sr))

                last2 = []
                f_gather(0)
                for n in range(NT):
                    if n + 1 < NT:
                        f_gather(n + 1)
                    for kk in range(2):
                        sr = ch_rg.cslot(DVE)
                        t = M(DVE, DVE.scalar_tensor_tensor(out=ybuf[:, n, :], in0=rg_sb[sr][:], scalar=g12[:, kk, n:n + 1], in1=ybuf[:, n, :],
                                                            op0=ALU.mult, op1=ALU.add))
                        ch_rg.consumed(t)
                    for hf in range(2):
                        DVE.bn_stats(out=st3[:, hf, :], in_=ybuf[:, n, hf * 512:(hf + 1) * 512])
                    i = DVE.bn_aggr(out=mv3[:], in_=st3[:].rearrange("p a c -> p (a c)"))
                    kb.wait(ACT, M(DVE, i))
                    i = ACT.activation(out=rstd3[:], in_=mv3[:, 1:2], func=AF.Sqrt, bias=eps3[:, 0:1], scale=1.0)
                    kb.wait(DVE, M(ACT, i))
                    DVE.reciprocal(out=rstd3[:], in_=rstd3[:])
                    so = ch_ot.pslot(DVE)
                    DVE.tensor_scalar(out=o_sb[so][:], in0=ybuf[:, n, :], scalar1=mv3[:, 0:1], scalar2=rstd3[:, 0:1],
                                      op0=ALU.subtract, op1=ALU.mult)
                    DVE.tensor_tensor(out=o_sb[so][:], in0=o_sb[so][:], in1=g2_bc[:], op=ALU.mult)
                    i = DVE.tensor_tensor(out=o_sb[so][:], in0=o_sb[so][:], in1=b2_bc[:], op=ALU.add)
                    ch_ot.produced(M(DVE, i))
                    so = ch_ot.cslot(SP)
                    i = SP.dma_start(out=out[b, n * 128:(n + 1) * 128, :], in_=o_sb[so][:])
                    t = ring_o.start(i, so)
                    ch_ot.consumed(t)
                    last2 = (last2 + [t])[-2:]
                for t in last2:
                    kb.wait(SP, t)
                barrier()
            esp.close()
    return nc


def _prep_inputs(inputs):
    x = np.ascontiguousarray(inputs["x"], dtype=np.float32)
    pos = np.ascontiguousarray(inputs["positions"], dtype=np.int32)
    w_r = np.concatenate([inputs["w_group"][0], np.transpose(inputs["w_expert"][0], (1, 0, 2)).reshape(D, 32)], axis=1)
    b_r = np.concatenate([inputs["b_group"][0], inputs["b_expert"][0].reshape(32)])[None, :]
    ws = inputs["w_spatial"][0]
    shared = {
        "w_in": np.ascontiguousarray(inputs["w_in"][0]),
        "w_out": np.ascontiguousarray(inputs["w_out"][0]),
        "ws_tgs": np.ascontiguousarray(np.transpose(ws, (1, 0, 2))),
        "ws_sgt": np.ascontiguousarray(np.transpose(ws, (2, 0, 1))),
        "bspT": np.ascontiguousarray(inputs["b_spatial"][0].T),
        "sgu_g": np.ascontiguousarray(inputs["sgu_ln_g"]),
        "sgu_b": np.ascontiguousarray(inputs["sgu_ln_b"]),
        "ln1_g": np.ascontiguousarray(inputs["ln1_g"]),
        "ln1_b": np.ascontiguousarray(inputs["ln1_b"]),
        "ln2_g": np.ascontiguousarray(inputs["ln2_g"]),
        "ln2_b": np.ascontiguousarray(inputs["ln2_b"]),
        "w_r": np.ascontiguousarray(w_r, dtype=np.float32),
        "b_r": np.ascontiguousarray(b_r, dtype=np.float32),
        "w_gu": np.ascontiguousarray(np.transpose(inputs["w_gate_up"][0].reshape(NE, D, 2, 128, 2), (0, 1, 2, 4, 3)).reshape(NE, D, 512)),
        "w_dn": np.ascontiguousarray(inputs["w_down"][0].reshape(NE, 256, D)),
    }
    in_maps = []
    for c in range(NCORES):
        xs = x[c * NB:(c + 1) * NB]
        m = dict(shared)
        m["xT"] = np.ascontiguousarray(np.transpose(xs, (0, 2, 1)))
        m["xtm"] = xs
        m["posT"] = np.ascontiguousarray(np.transpose(pos[c * NB:(c + 1) * NB].reshape(NB, NT, 128), (0, 2, 1)))
        in_maps.append(m)
    return in_maps


def kernel(**inputs):
    in_maps = _prep_inputs(inputs)
    nc = build_nc()
    res = run_bass_kernel_spmd(nc, in_maps, core_ids=list(range(NCORES)))
    return np.concatenate([r["out"] for r in res.results], axis=0).astype(np.float32)
```
